# Optimizing a Trainium2 kernel written in Bass

```python
import math
import jax, jax.numpy as jnp
from jax import lax
import numpy as np

D_MODEL = 1024
BATCH = 8
SEQ = 8192
DEPTH = 2

GRID_W = 64
CTX_LEN = 256
N_EVEN = (DEPTH + 1) // 2
N_ODD = DEPTH // 2
EPS = 1e-6
CHUNK = 64
HA = 8
DKA = 64
DVA = 64
WA = HA * DVA
HB = 8
PB = 64
D_INNER = HB * PB
N_GROUPS = 2
D_STATE = 128
CONV_W = 5
XBC_DIM = D_INNER + 2 * N_GROUPS * D_STATE
REC_SPLITS = (HA * DKA, HA * DKA, HA * DKA, WA, WA, D_INNER, D_INNER, N_GROUPS * D_STATE, N_GROUPS * D_STATE, HB, HB)
IN_COLS = 3 * HA * DKA + 2 * WA + 2 * D_INNER + 2 * N_GROUPS * D_STATE + 2 * HB
H_C = 8
NOPE = 128
ROPE = 64
VH = 128
Q_LORA = 384
KV_LORA = 256
ROPE_THETA = 10000.0
Q_BLOCK = 128
ATTN_SCALE = 1.0 / math.sqrt(NOPE + ROPE)
N_EXPERTS = 16
EXPERT_FF = 1024
CAP_FACTOR = 2

kernel_name = 'hybrid_hgrn2_ssd_mla_ecmoe_diffusion'


def rmsnorm(x, gain):
    xf = x.astype(jnp.float32)
    y = xf * lax.rsqrt(jnp.mean(xf * xf, axis=-1, keepdims=True) + EPS)
    return (y * gain.astype(jnp.float32)).astype(x.dtype)


def modulate(x, gain, shift, scale):
    return rmsnorm(x, gain) * (1 + scale) + shift


def split_cols(p, sizes):
    out, off = [], 0
    for s in sizes:
        out.append(p[..., off:off + s])
        off += s
    return out


def gla_chunked(q, k, v, log_a, s0):
    B, T, H, K = q.shape
    V = v.shape[-1]
    n = T // CHUNK
    r = lambda a: a.reshape(B, n, CHUNK, H, a.shape[-1])
    q, k, v, log_a = r(q), r(k), r(v), r(log_a)
    b = jnp.cumsum(log_a, axis=2)
    b_ref = b[:, :, CHUNK // 2:CHUNK // 2 + 1]
    b_last = b[:, :, -1:]
    att = jnp.einsum('bnthk,bnshk->bnhts', q * jnp.exp(b - b_ref), k * jnp.exp(b_ref - b))
    mask = jnp.tril(jnp.ones((CHUNK, CHUNK), dtype=bool))
    att = jnp.where(mask, att, 0.0)
    o = jnp.einsum('bnhts,bnshv->bnthv', att, v)
    u = jnp.einsum('bnshk,bnshv->nbhkv', k * jnp.exp(b_last - b), v)
    dec = jnp.exp(jnp.moveaxis(b_last[:, :, 0], 1, 0))

    def step(s, xs):
        d, uu = xs
        return d[..., None] * s + uu, s

    s_fin, s_in = lax.scan(step, s0, (dec, u))
    o = o + jnp.einsum('bnthk,nbhkv->bnthv', q * jnp.exp(b), s_in)
    return o.reshape(B, T, H, V), s_fin


def ssd_chunked(x, dt, la, bm, cm, s0):
    Bsz, T, H, P = x.shape
    G, N = bm.shape[-2], bm.shape[-1]
    hpg = H // G
    n = T // CHUNK
    xdt = (x * dt[..., None]).reshape(Bsz, n, CHUNK, G, hpg, P)
    la = la.reshape(Bsz, n, CHUNK, G, hpg)
    bm = bm.reshape(Bsz, n, CHUNK, G, N)
    cm = cm.reshape(Bsz, n, CHUNK, G, N)
    cum = jnp.cumsum(la, axis=2)
    cum_h = jnp.moveaxis(cum, 2, -1)
    diff = cum_h[..., :, None] - cum_h[..., None, :]
    mask = jnp.tril(jnp.ones((CHUNK, CHUNK), dtype=bool))
    lmat = jnp.exp(jnp.where(mask, diff, -jnp.inf))
    cb = jnp.einsum('bntgd,bnsgd->bngts', cm, bm)
    y = jnp.einsum('bnghts,bnsghp->bntghp', cb[:, :, :, None] * lmat, xdt)
    cum_last = cum[:, :, -1]
    w_end = jnp.exp(cum_last[:, :, None] - cum)
    u = jnp.einsum('bnsgd,bnsgh,bnsghp->nbghdp', bm, w_end, xdt)
    dec = jnp.exp(jnp.moveaxis(cum_last, 1, 0))

    def step(s, xs):
        d, uu = xs
        return d[..., None, None] * s + uu, s

    s_fin, s_in = lax.scan(step, s0, (dec, u))
    y = y + jnp.einsum('bntgd,bntgh,nbghdp->bntghp', cm, jnp.exp(cum), s_in)
    return y.reshape(Bsz, T, H, P), s_fin


def bidir_two_stream(scan, ctx_f, ctx_b, lat_f, lat_b, s0):
    flip = lambda args: tuple(jnp.flip(a, axis=1) for a in args)
    oc_f, sc_f = scan(*ctx_f, s0)
    ol_f, _ = scan(*lat_f, sc_f)
    oc_b, sc_b = scan(*flip(ctx_b), s0)
    ol_b, _ = scan(*flip(lat_b), sc_b)
    return oc_f + jnp.flip(oc_b, axis=1), ol_f + jnp.flip(ol_b, axis=1)


def centred_dwconv_silu(u, w, b):
    C = u.shape[-1]
    y = lax.conv_general_dilated(u, w[:, None, :].astype(u.dtype), window_strides=(1,),
                                 padding=[(CONV_W // 2, CONV_W // 2)],
                                 dimension_numbers=('NWC', 'WIO', 'NWC'), feature_group_count=C)
    return jax.nn.silu(y + b)


def recurrent_mixer(h_ctx, h_lat, w_in, w_out, conv_w, conv_b, lb, dt_bias, a_log, d_skip,
                    hgrn_norm, mamba_norm, need_ctx):
    f32 = jnp.float32

    def prepare(h):
        B, T, _ = h.shape
        q, ff, fb, i, g, z, xs, bs, cs, dtf, dtb = split_cols(h @ w_in, REC_SPLITS)
        heads_k = lambda a: a.reshape(B, T, HA, DKA)
        q = heads_k(jax.nn.silu(q.astype(f32)))
        v = i.astype(f32).reshape(B, T, HA, DVA)

        def forget(fr, lbd):
            f = lbd + (1.0 - lbd) * jax.nn.sigmoid(fr.astype(f32))
            return heads_k(1.0 - f), heads_k(jnp.log(f))

        kf, laf = forget(ff, lb[0])
        kb, lab = forget(fb, lb[1])
        xbc = centred_dwconv_silu(jnp.concatenate([xs, bs, cs], axis=-1), conv_w, conv_b).astype(f32)
        xm, bm, cm = split_cols(xbc, (D_INNER, N_GROUPS * D_STATE, N_GROUPS * D_STATE))
        xm = xm.reshape(B, T, HB, PB)
        bm = bm.reshape(B, T, N_GROUPS, D_STATE)
        cm = cm.reshape(B, T, N_GROUPS, D_STATE)

        def step_size(dr, bias, alog):
            dt = jax.nn.softplus(dr.astype(f32) + bias)
            return dt, -dt * jnp.exp(alog.astype(f32))

        dt_f, la_f = step_size(dtf, dt_bias[0], a_log[0])
        dt_b, la_b = step_size(dtb, dt_bias[1], a_log[1])
        return ((q, kf, v, laf), (q, kb, v, lab),
                (xm, dt_f, la_f, bm, cm), (xm, dt_b, la_b, bm, cm), (g, z, xm))

    cp = prepare(h_ctx)
    lp = prepare(h_lat)
    B = h_lat.shape[0]
    s0_h = jnp.zeros((B, HA, DKA, DVA), f32)
    s0_m = jnp.zeros((B, N_GROUPS, HB // N_GROUPS, D_STATE, PB), f32)
    oh_c, oh_l = bidir_two_stream(gla_chunked, cp[0], cp[1], lp[0], lp[1], s0_h)
    om_c, om_l = bidir_two_stream(ssd_chunked, cp[2], cp[3], lp[2], lp[3], s0_m)

    def merge(oh, om, extras, h):
        g, z, xm = extras
        B, T, _ = h.shape
        oh = rmsnorm(oh, hgrn_norm.reshape(HA, DVA)).reshape(B, T, WA) * jax.nn.sigmoid(g.astype(f32))
        y = (om + d_skip.astype(f32)[:, None] * xm).reshape(B, T, D_INNER)
        y = rmsnorm(y * jax.nn.silu(z.astype(f32)), mamba_norm)
        return jnp.concatenate([oh, y], axis=-1).astype(h.dtype) @ w_out

    o_lat = merge(oh_l, om_l, lp[4], h_lat)
    o_ctx = merge(oh_c, om_c, cp[4], h_ctx) if need_ctx else None
    return o_ctx, o_lat


def axial_rope_tables(T):
    rows = T // GRID_W
    row = jnp.repeat(jnp.arange(rows, dtype=jnp.int32), GRID_W)
    col = jnp.tile(jnp.arange(GRID_W, dtype=jnp.int32), rows)
    nf = ROPE // 4
    inv_freq = ROPE_THETA ** (-jnp.arange(nf, dtype=jnp.float32) / nf)
    pos = jnp.stack([row, col], axis=-1).astype(jnp.float32)
    ang = pos[..., None] * inv_freq
    return jnp.cos(ang), jnp.sin(ang)


def rope2d(x, cos, sin):
    nf = ROPE // 4
    xr = x.reshape(x.shape[:-1] + (2, 2, nf))
    x1, x2 = xr[..., 0, :], xr[..., 1, :]
    cos, sin = cos.astype(x.dtype), sin.astype(x.dtype)
    return jnp.stack([x1 * cos - x2 * sin, x2 * cos + x1 * sin], axis=-2).reshape(x.shape)


def attend(qn, qr, kn, kr, v):
    s = jnp.einsum('bqhd,bkhd->bhqk', qn, kn) + jnp.einsum('bqhr,bkr->bhqk', qr, kr)
    p = jax.nn.softmax(s.astype(jnp.float32) * ATTN_SCALE, axis=-1).astype(v.dtype)
    return jnp.einsum('bhqk,bkhv->bqhv', p, v)


def mla_mixer(h_ctx, h_lat, w_dq, q_norm, w_uq, w_dkv, kv_norm, w_ukv, w_kr, w_o, need_ctx):
    def q_proj(h):
        B, T, _ = h.shape
        q = (rmsnorm(h @ w_dq, q_norm) @ w_uq).reshape(B, T, H_C, NOPE + ROPE)
        return q[..., :NOPE], q[..., NOPE:]

    def kv_proj(h):
        B, T, _ = h.shape
        kv = (rmsnorm(h @ w_dkv, kv_norm) @ w_ukv).reshape(B, T, H_C, NOPE + VH)
        return kv[..., :NOPE], h @ w_kr, kv[..., NOPE:]

    B, T, _ = h_lat.shape
    cos, sin = axial_rope_tables(T)
    kn_c, kr_c, v_c = kv_proj(h_ctx)
    kn_l, kr_l, v_l = kv_proj(h_lat)
    qn_l, qr_l = q_proj(h_lat)
    qr_l = rope2d(qr_l, cos[:, None], sin[:, None])
    kr_l = rope2d(kr_l, cos, sin)
    kn = jnp.concatenate([kn_c, kn_l], axis=1)
    kr = jnp.concatenate([kr_c, kr_l], axis=1)
    v = jnp.concatenate([v_c, v_l], axis=1)
    nb = T // Q_BLOCK

    def to_blocks(a):
        return jnp.moveaxis(a.reshape((B, nb, Q_BLOCK) + a.shape[2:]), 1, 0)

    o_l = lax.map(lambda qb: attend(qb[0], qb[1], kn, kr, v), (to_blocks(qn_l), to_blocks(qr_l)))
    o_l = jnp.moveaxis(o_l, 0, 1).reshape(B, T, H_C * VH) @ w_o
    o_c = None
    if need_ctx:
        qn_c, qr_c = q_proj(h_ctx)
        o_c = attend(qn_c, qr_c, kn_c, kr_c, v_c).reshape(B, h_ctx.shape[1], H_C * VH) @ w_o
    return o_c, o_l


def expert_choice_ffn(h, w_router, w_gate, w_up, w_down):
    B, T, _ = h.shape
    cap = CAP_FACTOR * T // N_EXPERTS
    aff = jax.nn.softmax((h @ w_router).astype(jnp.float32), axis=-1)
    g, idx = lax.top_k(jnp.swapaxes(aff, 1, 2), cap)
    bidx = jnp.arange(B)[:, None, None]
    xg = h[bidx, idx]
    hid = jax.nn.silu(jnp.einsum('becd,edf->becf', xg, w_gate)) * jnp.einsum('becd,edf->becf', xg, w_up)
    y = jnp.einsum('becf,efd->becd', hid, w_down) * g[..., None].astype(h.dtype)
    return jnp.zeros_like(h).at[bidx, idx].add(y)


def setup_inputs(seed: int = 0) -> dict:
    key = jax.random.key(seed)
    ks = jax.random.split(key, 32)
    f32 = jnp.float32
    nrm = lambda k, shape, fan_in: jax.random.normal(k, shape, f32) * fan_in ** -0.5
    gain = lambda k, shape: 1.0 + 0.02 * jax.random.normal(k, shape, f32)
    D = D_MODEL
    dt0 = jnp.exp(jax.random.uniform(ks[14], (N_EVEN, 2, HB), f32, math.log(1e-3), math.log(1e-1)))
    return {
        'x': jax.random.normal(ks[0], (BATCH, SEQ, D), f32),
        'c': jax.random.normal(ks[1], (BATCH, D), f32),
        'ctx': jax.random.normal(ks[2], (BATCH, CTX_LEN, D), f32),
        'c_ctx': jax.random.normal(ks[3], (D,), f32),
        'w_mod': 0.5 * nrm(ks[4], (DEPTH, D, 6 * D), D),
        'b_mod': 0.02 * jax.random.normal(ks[5], (DEPTH, 6 * D), f32),
        'norm_mix': gain(ks[6], (DEPTH, D)),
        'norm_ffn': gain(ks[7], (DEPTH, D)),
        'norm_out': gain(ks[8], (D,)),
        'w_in': nrm(ks[9], (N_EVEN, D, IN_COLS), D),
        'w_out_rec': nrm(ks[10], (N_EVEN, WA + D_INNER, D), WA + D_INNER),
        'conv_w': nrm(ks[11], (N_EVEN, CONV_W, XBC_DIM), CONV_W),
        'conv_b': 0.02 * jax.random.normal(ks[12], (N_EVEN, XBC_DIM), f32),
        'lb_gamma': 0.1 * jax.random.normal(ks[13], (N_EVEN + 1, 2, HA * DKA), f32),
        'dt_bias': dt0 + jnp.log(-jnp.expm1(-dt0)),
        'a_log': jnp.log(jax.random.uniform(ks[15], (N_EVEN, 2, HB), f32, 1.0, 16.0)),
        'd_skip': 1.0 + 0.1 * jax.random.normal(ks[16], (N_EVEN, HB), f32),
        'hgrn_norm': gain(ks[17], (N_EVEN, WA)),
        'mamba_norm': gain(ks[18], (N_EVEN, D_INNER)),
        'w_dq': nrm(ks[19], (N_ODD, D, Q_LORA), D),
        'q_norm': gain(ks[20], (N_ODD, Q_LORA)),
        'w_uq': nrm(ks[21], (N_ODD, Q_LORA, H_C * (NOPE + ROPE)), Q_LORA),
        'w_dkv': nrm(ks[22], (N_ODD, D, KV_LORA), D),
        'kv_norm': gain(ks[23], (N_ODD, KV_LORA)),
        'w_ukv': nrm(ks[24], (N_ODD, KV_LORA, H_C * (NOPE + VH)), KV_LORA),
        'w_kr': nrm(ks[25], (N_ODD, D, ROPE), D),
        'w_o': nrm(ks[26], (N_ODD, H_C * VH, D), H_C * VH),
        'w_router': nrm(ks[27], (DEPTH, D, N_EXPERTS), D),
        'w_gate': nrm(ks[28], (DEPTH, N_EXPERTS, D, EXPERT_FF), D),
        'w_up': nrm(ks[29], (DEPTH, N_EXPERTS, D, EXPERT_FF), D),
        'w_down': nrm(ks[30], (DEPTH, N_EXPERTS, EXPERT_FF, D), EXPERT_FF),
    }


def reference(x, c, ctx, c_ctx, w_mod, b_mod, norm_mix, norm_ffn, norm_out, w_in, w_out_rec,
              conv_w, conv_b, lb_gamma, dt_bias, a_log, d_skip, hgrn_norm, mamba_norm,
              w_dq, q_norm, w_uq, w_dkv, kv_norm, w_ukv, w_kr, w_o,
              w_router, w_gate, w_up, w_down):
    lb = jnp.cumsum(jax.nn.softmax(lb_gamma.astype(jnp.float32), axis=0), axis=0)
    s_lat = jax.nn.silu(c)
    s_ctx = jax.nn.silu(c_ctx)
    x_lat, x_ctx = x, ctx
    for l in range(DEPTH):
        need_ctx = l < DEPTH - 1
        m_lat = jnp.split((s_lat @ w_mod[l] + b_mod[l])[:, None, :], 6, axis=-1)
        m_ctx = jnp.split(s_ctx @ w_mod[l] + b_mod[l], 6, axis=-1)
        a_lat = modulate(x_lat, norm_mix[l], m_lat[0], m_lat[1])
        a_ctx = modulate(x_ctx, norm_mix[l], m_ctx[0], m_ctx[1])
        if l % 2 == 0:
            e = l // 2
            o_ctx, o_lat = recurrent_mixer(a_ctx, a_lat, w_in[e], w_out_rec[e], conv_w[e], conv_b[e],
                                           lb[e], dt_bias[e], a_log[e], d_skip[e], hgrn_norm[e],
                                           mamba_norm[e], need_ctx)
        else:
            j = l // 2
            o_ctx, o_lat = mla_mixer(a_ctx, a_lat, w_dq[j], q_norm[j], w_uq[j], w_dkv[j], kv_norm[j],
                                     w_ukv[j], w_kr[j], w_o[j], need_ctx)
        x_lat = x_lat + m_lat[2] * o_lat
        x_lat = x_lat + m_lat[5] * expert_choice_ffn(modulate(x_lat, norm_ffn[l], m_lat[3], m_lat[4]),
                                                     w_router[l], w_gate[l], w_up[l], w_down[l])
        if need_ctx:
            x_ctx = x_ctx + m_ctx[2] * o_ctx
            x_ctx = x_ctx + m_ctx[5] * expert_choice_ffn(modulate(x_ctx, norm_ffn[l], m_ctx[3], m_ctx[4]),
                                                         w_router[l], w_gate[l], w_up[l], w_down[l])
    return rmsnorm(x_lat, norm_out)
```

```python
import numpy as np
import concourse.bass as bass
import concourse.mybir as mybir

F32 = mybir.dt.float32
BF16 = mybir.dt.bfloat16
I32 = mybir.dt.int32
U32 = mybir.dt.uint32
AF = mybir.ActivationFunctionType
ALU = mybir.AluOpType
AX = mybir.AxisListType


class KF:
    NDS = 8

    def __init__(self, nc):
        self.nc = nc
        self.eng = {"pe": nc.tensor, "dve": nc.vector, "act": nc.scalar, "pool": nc.gpsimd, "sp": nc.sync}
        self.csem = {}
        self.ccnt = {}
        for e in self.eng:
            self.csem[e] = nc.alloc_semaphore("cs_" + e)
            self.ccnt[e] = 0
        self.dsem = {}
        self.dval = {}
        self.drot = {}
        for q in ("sp", "pool", "act"):
            self.dsem[q] = [nc.alloc_semaphore("ds_%s%d" % (q, i)) for i in range(self.NDS)]
            self.dval[q] = [0] * self.NDS
            self.drot[q] = 0
        self.waited = {e: {} for e in self.eng}
        self.lastw = {}
        self.readers = {}
        self.same_eng_sync = {"pe": False, "dve": True, "act": True, "pool": True, "sp": True}
        self.nins = 0

    def _wait(self, e, tok):
        sem, val, src = tok
        if src == e and not self.same_eng_sync[e]:
            return
        key = id(sem)
        if self.waited[e].get(key, 0) >= val:
            return
        self.eng[e].wait_ge(sem, val)
        self.waited[e][key] = val
        self.nins += 1

    def _deps(self, e, r, w):
        for k in r:
            t = self.lastw.get(k)
            if t is not None:
                self._wait(e, t)
        for k in w:
            t = self.lastw.get(k)
            if t is not None:
                self._wait(e, t)
            for t in self.readers.get(k, ()):
                self._wait(e, t)

    def _commit(self, tok, r, w):
        for k in w:
            self.lastw[k] = tok
            self.readers[k] = []
        for k in r:
            self.readers.setdefault(k, []).append(tok)

    def op(self, e, fn, r=(), w=()):
        r = [x for x in r if x is not None]
        w = [x for x in w if x is not None]
        self._deps(e, r, w)
        ins = fn(self.eng[e])
        self.ccnt[e] += 1
        ins.then_inc(self.csem[e], 1)
        tok = (self.csem[e], self.ccnt[e], e)
        self._commit(tok, r, w)
        self.nins += 1
        return tok

    def dma(self, q, fn, r=(), w=()):
        r = [x for x in r if x is not None]
        w = [x for x in w if x is not None]
        i = self.drot[q]
        self.drot[q] = (i + 1) % self.NDS
        sem = self.dsem[q][i]
        self._wait(q, (sem, self.dval[q][i], None))
        self._deps(q, r, w)
        ins = fn(self.eng[q])
        self.dval[q][i] += 16
        ins.then_inc(sem, 16)
        tok = (sem, self.dval[q][i], None)
        self._commit(tok, r, w)
        self.nins += 1
        return tok

    def barrier(self):
        toks = []
        for e in self.eng:
            if self.ccnt[e] > 0:
                toks.append((self.csem[e], self.ccnt[e], None))
        for q in self.dsem:
            for i in range(self.NDS):
                if self.dval[q][i] > 0:
                    toks.append((self.dsem[q][i], self.dval[q][i], None))
        for e in self.eng:
            for t in toks:
                if t[0] is self.csem[e]:
                    continue
                self._wait(e, t)
        self.lastw = {}
        self.readers = {}

    def finish(self):
        for e in self.eng:
            if e != "sp" and self.ccnt[e] > 0:
                self._wait("sp", (self.csem[e], self.ccnt[e], None))
        for q in self.dsem:
            for i in range(self.NDS):
                if self.dval[q][i] > 0:
                    self._wait("sp", (self.dsem[q][i], self.dval[q][i], None))

from contextlib import ExitStack

T_LAT = 8192
T_CTX = 256
NTOK = T_LAT + T_CTX
NROWS = NTOK + 128
DM = 1024
EPS = 1e-6
NE = 16


def host_consts():
    c = {}
    c["ident"] = np.eye(128, dtype=np.float32)
    p = np.arange(128)
    c["triu_incl"] = (p[:, None] <= p[None, :]).astype(np.float32)
    c["triu_strict"] = (p[:, None] < p[None, :]).astype(np.float32)
    c["tril_incl"] = (p[:, None] >= p[None, :]).astype(np.float32)
    c["tril_strict"] = (p[:, None] > p[None, :]).astype(np.float32)
    c["iota_q"] = np.tile(np.arange(1024, dtype=np.float32)[None, :], (128, 1))
    q = np.arange(128, dtype=np.float32)
    c["ctxadd"] = np.tile((T_LAT + np.maximum(q - 32, 0))[None, :], (128, 1)).astype(np.float32)
    return c


class PB:
    def __init__(self, nc):
        self.nc = nc
        self.k = KF(nc)
        self.D = {}
        self.uid = 0

    def din(self, name, shape, dt=F32):
        self.D[name] = self.nc.dram_tensor(name, list(shape), dt, kind="ExternalInput").ap()
        return self.D[name]

    def dout(self, name, shape, dt=F32):
        self.D[name] = self.nc.dram_tensor(name, list(shape), dt, kind="ExternalOutput").ap()
        return self.D[name]

    def dscr(self, name, shape, dt=F32):
        self.D[name] = self.nc.dram_tensor(name, list(shape), dt, kind="Internal").ap()
        return self.D[name]

    def sb(self, es, name, shape, dt):
        self.uid += 1
        return es.enter_context(self.nc.sbuf_tensor("%s_%d" % (name, self.uid), list(shape), dt))

    def ps(self, es, name, shape, dt):
        self.uid += 1
        return es.enter_context(self.nc.psum_tensor("%s_%d" % (name, self.uid), list(shape), dt))


def load_consts(pb, es):
    k, D = pb.k, pb.D
    C = {}
    for nm in ("ident", "triu_incl", "triu_strict", "tril_incl", "tril_strict"):
        f = pb.sb(es, nm + "_f", [128, 128], F32)
        b = pb.sb(es, nm + "_b", [128, 128], BF16)
        k.dma("sp", lambda e, f=f, nm=nm: e.dma_start(out=f[:], in_=D["c_" + nm]), w=[nm + "_f"])
        k.op("dve", lambda e, f=f, b=b: e.tensor_copy(out=b[:], in_=f[:]), r=[nm + "_f"], w=[nm + "_b"])
        C[nm + "_f"] = f
        C[nm + "_b"] = b
    ones_f = pb.sb(es, "ones_f", [128, 128], F32)
    ones_b = pb.sb(es, "ones_b", [128, 128], BF16)
    k.op("dve", lambda e: e.memset(ones_f[:], 1.0), w=["ones_f"])
    k.op("dve", lambda e: e.memset(ones_b[:], 1.0), w=["ones_b"])
    C["ones_f"] = ones_f
    C["ones_b"] = ones_b
    return C


def phase_init(pb, copy_x=True):
    k, D = pb.k, pb.D
    with ExitStack() as es:
        z = pb.sb(es, "zt", [128, 1024], F32)
        zb = pb.sb(es, "zb", [128, 1024], BF16)
        k.op("dve", lambda e: e.memset(z[:], 0.0), w=["zt"])
        k.op("dve", lambda e: e.memset(zb[:], 0.0), w=["zb"])
        if copy_x:
            for i in range(8):
                k.dma("sp", lambda e, i=i: e.dma_start(out=D["xres"][i * 1024:(i + 1) * 1024, :],
                                                       in_=D["x"][i * 1024:(i + 1) * 1024, :]), w=["xres"])
            k.dma("sp", lambda e: e.dma_start(out=D["xres"][T_LAT:NTOK, :], in_=D["ctx"]), w=["xres"])
        k.dma("sp", lambda e: e.dma_start(out=D["xres"][NTOK:NROWS, :], in_=z[:]), r=["zt"], w=["xres"])
        k.dma("sp", lambda e: e.dma_start(out=D["hn"][NTOK:NROWS, :], in_=zb[:]), r=["zb"], w=["hn"])
        k.dma("sp", lambda e: e.dma_start(out=D["aff"][NTOK:NROWS, :], in_=z[:, 0:16]), r=["zt"], w=["aff"])
        k.barrier()


def phase_mod(pb, C):
    k, D, nc = pb.k, pb.D, pb.nc
    with ExitStack() as es:
        s8 = pb.sb(es, "s8", [8, 2, 128], F32)
        scol = pb.sb(es, "scol", [128, 2, 8], F32)
        srep = pb.sb(es, "srep", [128, 2, 8, 128], F32)
        ps_t = pb.ps(es, "ps_t", [128, 512], F32)
        k.dma("sp", lambda e: e.dma_start(out=s8[:, 0, :], in_=D["c"].rearrange("o (a b) -> (o a) b", a=8)), w=["s8"])
        k.dma("sp", lambda e: e.dma_start(out=s8[:, 1, :], in_=D["c_ctx"].rearrange("o (a b) -> (o a) b", a=8)), w=["s8"])
        k.op("act", lambda e: e.activation(out=s8[:], in_=s8[:], func=AF.Silu), r=["s8"], w=["s8"])
        for s in range(2):
            k.op("pe", lambda e, s=s: e.transpose(out=ps_t[:, s * 8:(s + 1) * 8], in_=s8[:, s, :],
                                                  identity=C["ident_f"][0:8, 0:8]), r=["s8", "ident_f"], w=["ps_t"])
        k.op("dve", lambda e: e.tensor_copy(out=scol[:].rearrange("p s c -> p (s c)"), in_=ps_t[:, 0:16]),
             r=["ps_t"], w=["scol"])
        for s in range(2):
            k.op("dve", lambda e, s=s: e.tensor_copy(out=srep[:, s, :, :],
                                                     in_=scol[:, s, :].unsqueeze(2).to_broadcast([128, 8, 128])),
                 r=["scol"], w=["srep"])
        wm = [pb.sb(es, "wm%d" % i, [128, 8, 512], F32) for i in range(2)]
        bm = [pb.sb(es, "bm%d" % i, [128, 512], F32) for i in range(2)]
        gn = [pb.sb(es, "gn%d" % i, [128, 512], F32) for i in range(2)]
        ob = [pb.sb(es, "ob%d" % i, [128, 512], F32) for i in range(4)]
        psm = [pb.ps(es, "psm%d" % i, [128, 512], F32) for i in range(2)]
        it = 0
        oi = 0
        for l in range(2):
            for ncn in range(12):
                b = it % 2
                it += 1
                n0 = ncn * 512
                slot = ncn // 2
                half = ncn % 2
                k.dma("sp", lambda e, b=b, l=l, n0=n0: e.dma_start(
                    out=wm[b][:], in_=D["w_mod"][l, :, n0:n0 + 512].rearrange("(kc p) n -> p kc n", p=128)),
                    w=["wm%d" % b])
                k.dma("sp", lambda e, b=b, l=l, n0=n0: e.dma_start(
                    out=bm[b][:], in_=D["b_mod"][l:l + 1, n0:n0 + 512].to_broadcast([128, 512])), w=["bm%d" % b])
                if slot in (1, 4):
                    gsrc = D["norm_mix"] if slot == 1 else D["norm_ffn"]
                    k.dma("sp", lambda e, b=b, l=l, half=half, gsrc=gsrc: e.dma_start(
                        out=gn[b][:], in_=gsrc[l:l + 1, half * 512:(half + 1) * 512].to_broadcast([128, 512])),
                        w=["gn%d" % b])
                for s in range(2):
                    pm = psm[s]
                    for kc in range(8):
                        k.op("pe", lambda e, s=s, kc=kc, b=b, pm=pm: e.matmul(
                            pm[:], srep[:, s, kc, :], wm[b][:, kc, :], start=(kc == 0), stop=(kc == 7)),
                            r=["srep", "wm%d" % b], w=["psm%d" % s])
                    o = ob[oi % 4]
                    okey = "ob%d" % (oi % 4)
                    oi += 1
                    k.op("dve", lambda e, o=o, pm=pm, b=b: e.tensor_tensor(out=o[:], in0=pm[:], in1=bm[b][:], op=ALU.add),
                         r=["psm%d" % s, "bm%d" % b], w=[okey])
                    if slot in (1, 4):
                        k.op("dve", lambda e, o=o, b=b: e.scalar_tensor_tensor(
                            out=o[:], in0=o[:], scalar=1.0, in1=gn[b][:], op0=ALU.add, op1=ALU.mult),
                            r=[okey, "gn%d" % b], w=[okey])
                    k.dma("sp", lambda e, o=o, l=l, s=s, slot=slot, half=half: e.dma_start(
                        out=D["modrep"][l, s, slot, :, half * 512:(half + 1) * 512], in_=o[:]),
                        r=[okey], w=["modrep"])
        k.barrier()


def norm_mod_tile(pb, xt, xkey, rstd, rkey, junk, t1, G, SH, hn, hnkey):
    k = pb.k
    k.op("act", lambda e: e.activation(out=junk[:], in_=xt[:], func=AF.Square, accum_out=rstd[:, 0:1]),
         r=[xkey], w=["junk", rkey])
    k.op("act", lambda e: e.activation(out=rstd[:, 1:2], in_=rstd[:, 0:1], func=AF.Sqrt, bias=EPS, scale=1.0 / DM),
         r=[rkey], w=[rkey])
    k.op("dve", lambda e: e.reciprocal(out=rstd[:, 2:3], in_=rstd[:, 1:2]), r=[rkey], w=[rkey])
    k.op("dve", lambda e: e.scalar_tensor_tensor(out=t1[:], in0=xt[:], scalar=rstd[:, 2:3], in1=G[:],
                                                 op0=ALU.mult, op1=ALU.mult),
         r=[xkey, rkey, "modv"], w=["t1"])
    k.op("pool", lambda e: e.tensor_tensor(out=hn[:], in0=t1[:], in1=SH[:], op=ALU.add),
         r=["t1", "modv"], w=[hnkey])


def load_modv(pb, es, l, slots):
    k, D = pb.k, pb.D
    out = {}
    for s in range(2):
        for slot in slots:
            t = pb.sb(es, "mv%d%d" % (s, slot), [128, 1024], F32)
            k.dma("sp", lambda e, t=t, s=s, slot=slot: e.dma_start(out=t[:], in_=D["modrep"][l, s, slot, :, :]),
                  r=["modrep"], w=["modv"])
            out[(s, slot)] = t
    return out


def topk_threshold(pb, es, C, aff, J, cap, tag, psum):
    k = pb.k
    lo = pb.sb(es, "lo" + tag, [128, 16], F32)
    hi = pb.sb(es, "hi" + tag, [128, 16], F32)
    mid = pb.sb(es, "mid" + tag, [128, 16], F32)
    cnt = pb.sb(es, "cnt" + tag, [128, 16], F32)
    ge = pb.sb(es, "ge" + tag, [128, 16], U32)
    lt = pb.sb(es, "lt" + tag, [128, 16], U32)
    cmp = pb.sb(es, "cmp" + tag, [128, J, 16], BF16)
    K = "tk" + tag
    k.op("dve", lambda e: e.memset(lo[:], 0.0), w=[K])
    k.op("dve", lambda e: e.memset(hi[:], 1.0), w=[K])
    ncol = J * 16
    for it in range(30):
        k.op("dve", lambda e: e.tensor_tensor(out=mid[:], in0=lo[:], in1=hi[:], op=ALU.add), r=[K], w=[K + "m"])
        k.op("dve", lambda e: e.tensor_scalar(out=mid[:], in0=mid[:], scalar1=0.5, scalar2=None, op0=ALU.mult),
             r=[K + "m"], w=[K + "m"])
        k.op("dve", lambda e: e.tensor_tensor(out=cmp[:], in0=aff, in1=mid[:].unsqueeze(1).to_broadcast([128, J, 16]),
                                              op=ALU.is_ge), r=[K + "m", "aff_all"], w=[K + "c"])
        cf = cmp[:].rearrange("p j e -> p (j e)")
        for c0 in range(0, ncol, 512):
            c1 = min(ncol, c0 + 512)
            k.op("pe", lambda e, c0=c0, c1=c1: e.matmul(psum[:, c0:c1], C["ones_b"][:], cf[:, c0:c1], start=True, stop=True),
                 r=[K + "c", "ones_b"], w=[K + "p"])
        k.op("dve", lambda e: e.tensor_reduce(out=cnt[:], in_=psum[:, 0:ncol].rearrange("p (j e) -> p e j", e=16),
                                              axis=AX.X, op=ALU.add), r=[K + "p"], w=[K + "n"])
        k.op("dve", lambda e: e.tensor_scalar(out=ge[:], in0=cnt[:], scalar1=float(cap), scalar2=None, op0=ALU.is_ge),
             r=[K + "n"], w=[K + "g"])
        k.op("dve", lambda e: e.tensor_scalar(out=lt[:], in0=cnt[:], scalar1=float(cap), scalar2=None, op0=ALU.is_lt),
             r=[K + "n"], w=[K + "g"])
        k.op("dve", lambda e: e.copy_predicated(out=lo[:], mask=ge[:], data=mid[:]), r=[K + "g", K + "m"], w=[K])
        k.op("dve", lambda e: e.copy_predicated(out=hi[:], mask=lt[:], data=mid[:]), r=[K + "g", K + "m"], w=[K])
    return lo, K


def phase_moe(pb, C, l):
    k, D, nc = pb.k, pb.D, pb.nc
    has_ctx = (l == 0)
    NT = 66 if has_ctx else 64
    NPT = 9 if has_ctx else 8
    NP = NPT * 128
    with ExitStack() as es:
        mv = load_modv(pb, es, l, (3, 4, 5))
        iota_q = pb.sb(es, "iota_q", [128, 1024], F32)
        ctxadd = pb.sb(es, "ctxadd", [128, 128], F32)
        k.dma("sp", lambda e: e.dma_start(out=iota_q[:], in_=D["c_iota_q"]), w=["iota_q"])
        k.dma("sp", lambda e: e.dma_start(out=ctxadd[:], in_=D["c_ctxadd"]), w=["ctxadd"])
        aff_all = pb.sb(es, "aff_all", [128, NT, 16], F32)
        wr_f = pb.sb(es, "wr_f", [128, 8, 16], F32)
        wr = pb.sb(es, "wr", [128, 8, 16], BF16)
        k.dma("sp", lambda e: e.dma_start(out=wr_f[:], in_=D["w_router"][l].rearrange("(kc p) n -> p kc n", p=128)),
              w=["wr_f"])
        k.op("dve", lambda e: e.tensor_copy(out=wr[:], in_=wr_f[:]), r=["wr_f"], w=["wr"])
        with ExitStack() as es2:
            xt = [pb.sb(es2, "xt%d" % i, [128, 1024], F32) for i in range(2)]
            hn = [pb.sb(es2, "hn%d" % i, [128, 1024], BF16) for i in range(2)]
            hnT = [pb.sb(es2, "hnT%d" % i, [128, 8, 128], BF16) for i in range(2)]
            junk = pb.sb(es2, "junk", [128, 1024], BF16)
            t1 = pb.sb(es2, "t1", [128, 1024], F32)
            rstd = [pb.sb(es2, "rstd%d" % i, [128, 4], F32) for i in range(2)]
            psT = [pb.ps(es2, "psT%d" % i, [128, 1024], BF16) for i in range(2)]
            psL = [pb.ps(es2, "psL%d" % i, [128, 16], F32) for i in range(2)]
            for j in range(NT):
                b = j % 2
                s = 0 if j < 64 else 1
                r0 = j * 128
                k.dma("sp", lambda e, b=b, r0=r0: e.dma_start(out=xt[b][:], in_=D["xres"][r0:r0 + 128, :]),
                      r=["xres"], w=["xt%d" % b])
                norm_mod_tile(pb, xt[b], "xt%d" % b, rstd[b], "rstd%d" % b, junk, t1, mv[(s, 4)], mv[(s, 3)], hn[b], "hn%d" % b)
                k.dma("sp", lambda e, b=b, r0=r0: e.dma_start(out=D["hn"][r0:r0 + 128, :], in_=hn[b][:]),
                      r=["hn%d" % b], w=["hn"])
                for kc in range(8):
                    k.op("pe", lambda e, b=b, kc=kc: e.transpose(out=psT[b][:, kc * 128:(kc + 1) * 128],
                                                                 in_=hn[b][:, kc * 128:(kc + 1) * 128],
                                                                 identity=C["ident_b"][:]),
                         r=["hn%d" % b, "ident_b"], w=["psT%d" % b])
                k.op("act", lambda e, b=b: e.activation(out=hnT[b][:].rearrange("p a t -> p (a t)"), in_=psT[b][:], func=AF.Copy),
                     r=["psT%d" % b], w=["hnT%d" % b])
                for kc in range(8):
                    k.op("pe", lambda e, b=b, kc=kc: e.matmul(psL[b][:], hnT[b][:, kc, :], wr[:, kc, :],
                                                              start=(kc == 0), stop=(kc == 7)),
                         r=["hnT%d" % b, "wr"], w=["psL%d" % b])
                k.op("dve", lambda e, b=b, j=j: e.tensor_copy(out=aff_all[:, j, :], in_=psL[b][:]),
                     r=["psL%d" % b], w=["aff_all"])
            mx = pb.sb(es2, "mx", [128, NT], F32)
            k.op("dve", lambda e: e.tensor_reduce(out=mx[:], in_=aff_all[:], axis=AX.X, op=ALU.max), r=["aff_all"], w=["mx"])
            k.op("dve", lambda e: e.tensor_tensor(out=aff_all[:], in0=aff_all[:],
                                                  in1=mx[:].unsqueeze(2).to_broadcast([128, NT, 16]), op=ALU.subtract),
                 r=["aff_all", "mx"], w=["aff_all"])
            k.op("act", lambda e: e.activation(out=aff_all[:], in_=aff_all[:], func=AF.Exp), r=["aff_all"], w=["aff_all"])
            k.op("dve", lambda e: e.tensor_reduce(out=mx[:], in_=aff_all[:], axis=AX.X, op=ALU.add), r=["aff_all"], w=["mx"])
            k.op("dve", lambda e: e.reciprocal(out=mx[:], in_=mx[:]), r=["mx"], w=["mx"])
            k.op("dve", lambda e: e.tensor_tensor(out=aff_all[:], in0=aff_all[:],
                                                  in1=mx[:].unsqueeze(2).to_broadcast([128, NT, 16]), op=ALU.mult),
                 r=["aff_all", "mx"], w=["aff_all"])
            k.dma("sp", lambda e: e.dma_start(out=D["aff"][0:NT * 128, :].rearrange("(j p) e -> p j e", p=128), in_=aff_all[:]),
                  r=["aff_all"], w=["aff"])
            k.barrier()
        m_all = pb.sb(es, "m_all", [128, NT, 16], BF16)
        cnt_incl = pb.sb(es, "cnt_incl", [128, NT, 16], F32)
        with ExitStack() as es2:
            pst = [pb.ps(es2, "pst%d" % i, [128, 512], F32) for i in range(3)]
            psbig = pb.ps(es2, "psbig", [128, 1024], F32)
            thr_l, Kl = topk_threshold(pb, es2, C, aff_all[:, 0:64, :], 64, 1024, "L", psbig)
            k.op("dve", lambda e: e.tensor_tensor(out=m_all[:, 0:64, :], in0=aff_all[:, 0:64, :],
                                                  in1=thr_l[:].unsqueeze(1).to_broadcast([128, 64, 16]), op=ALU.is_ge),
                 r=["aff_all", Kl], w=["m_all"])
            if has_ctx:
                thr_c, Kc = topk_threshold(pb, es2, C, aff_all[:, 64:66, :], 2, 32, "C", psbig)
                k.op("dve", lambda e: e.tensor_tensor(out=m_all[:, 64:66, :], in0=aff_all[:, 64:66, :],
                                                      in1=thr_c[:].unsqueeze(1).to_broadcast([128, 2, 16]), op=ALU.is_ge),
                     r=["aff_all", Kc], w=["m_all"])
            tot = pb.sb(es2, "tot", [128, NT, 16], F32)
            binc = pb.sb(es2, "binc", [128, NT, 16], F32)
            mf = m_all[:].rearrange("p j e -> p (j e)")
            tf = tot[:].rearrange("p j e -> p (j e)")
            cf = cnt_incl[:].rearrange("p j e -> p (j e)")
            ncol = NT * 16
            for ci, c0 in enumerate(range(0, ncol, 512)):
                c1 = min(ncol, c0 + 512)
                w_ = c1 - c0
                k.op("pe", lambda e, c0=c0, c1=c1, ci=ci, w_=w_: e.matmul(pst[ci][:, 0:w_], C["ones_b"][:], mf[:, c0:c1],
                                                                          start=True, stop=True),
                     r=["m_all", "ones_b"], w=["pst%d" % ci])
                k.op("dve", lambda e, c0=c0, c1=c1, ci=ci, w_=w_: e.tensor_copy(out=tf[:, c0:c1], in_=pst[ci][:, 0:w_]),
                     r=["pst%d" % ci], w=["tot"])
            for e_ in range(16):
                k.op("dve", lambda e, e_=e_: e.tensor_tensor_scan(out=binc[:, 0:64, e_], data0=C["ones_f"][:, 0:64],
                                                                  data1=tot[:, 0:64, e_], initial=0.0,
                                                                  op0=ALU.mult, op1=ALU.add),
                     r=["tot", "ones_f"], w=["binc"])
            if has_ctx:
                k.op("dve", lambda e: e.tensor_copy(out=binc[:, 64, :], in_=tot[:, 64, :]), r=["tot"], w=["binc"])
                k.op("dve", lambda e: e.tensor_tensor(out=binc[:, 65, :], in0=tot[:, 64, :], in1=tot[:, 65, :], op=ALU.add),
                     r=["tot"], w=["binc"])
            k.op("dve", lambda e: e.tensor_tensor(out=binc[:], in0=binc[:], in1=tot[:], op=ALU.subtract),
                 r=["binc", "tot"], w=["binc"])
            bf_ = binc[:].rearrange("p j e -> p (j e)")
            for ci, c0 in enumerate(range(0, ncol, 512)):
                c1 = min(ncol, c0 + 512)
                w_ = c1 - c0
                k.op("pe", lambda e, c0=c0, c1=c1, ci=ci, w_=w_: e.matmul(pst[ci][:, 0:w_], C["triu_incl_b"][:], mf[:, c0:c1],
                                                                          start=True, stop=True),
                     r=["m_all", "triu_incl_b", "tot"], w=["pst%d" % ci])
                k.op("dve", lambda e, c0=c0, c1=c1, ci=ci, w_=w_: e.tensor_tensor(out=cf[:, c0:c1], in0=pst[ci][:, 0:w_],
                                                                                  in1=bf_[:, c0:c1], op=ALU.add),
                     r=["pst%d" % ci, "binc"], w=["cnt_incl"])
            k.barrier()
        with ExitStack() as es2:
            Bt = [pb.sb(es2, "Bt%d" % i, [128, 1024], BF16) for i in range(2)]
            row_sb = pb.sb(es2, "row_sb", [1, NP], F32)
            idx = pb.sb(es2, "idx", [128, NPT], I32)
            xg = pb.sb(es2, "xg", [128, NPT, 1024], BF16)
            ga = pb.sb(es2, "ga", [128, NPT, 16], F32)
            xgT = pb.sb(es2, "xgT", [128, 8, NP], BF16)
            hidT = pb.sb(es2, "hidT", [128, 8, NP], BF16)
            sg = [pb.sb(es2, "sg%d" % i, [128, 512], F32) for i in range(2)]
            yt = [pb.sb(es2, "yt%d" % i, [128, 1024], F32) for i in range(2)]
            Wb = {nm: pb.sb(es2, "W" + nm, [128, 8, 1024], BF16) for nm in ("g", "u", "d")}
            stg = [pb.sb(es2, "stg%d" % i, [128, 4, 1024], F32) for i in range(2)]
            rowA = pb.ps(es2, "rowA", [1, 512], F32)
            rowB = pb.ps(es2, "rowB", [1, 512], F32)
            rowC = pb.ps(es2, "rowC", [128, 512], F32)
            psX = pb.ps(es2, "psX", [128, 1024], BF16)
            psG = pb.ps(es2, "psG", [128, 512], F32)
            psU = pb.ps(es2, "psU", [128, 512], F32)
            psY = [pb.ps(es2, "psY%d" % i, [128, 512], F32) for i in range(2)]
            sti = 0
            bi = 0
            yi = 0
            pchunks = [(0, 512), (512, 1024)] + ([(1024, 1152)] if has_ctx else [])
            for ex in range(NE):
                for nm, src in (("g", D["w_gate"]), ("u", D["w_up"]), ("d", D["w_down"])):
                    for hf in range(2):
                        sbuf_ = stg[sti % 2]
                        skey = "stg%d" % (sti % 2)
                        sti += 1
                        k.dma("sp", lambda e, sbuf_=sbuf_, src=src, hf=hf, ex=ex: e.dma_start(
                            out=sbuf_[:], in_=src[l, ex, hf * 512:(hf + 1) * 512, :].rearrange("(kc p) n -> p kc n", p=128)),
                            w=[skey])
                        k.op("act", lambda e, sbuf_=sbuf_, nm=nm, hf=hf: e.activation(
                            out=Wb[nm][:, hf * 4:(hf + 1) * 4, :], in_=sbuf_[:], func=AF.Copy),
                            r=[skey], w=["W" + nm])
                for j in range(64):
                    B_ = Bt[bi % 2]
                    bkey = "Bt%d" % (bi % 2)
                    eng = "dve"
                    bi += 1
                    k.op(eng, lambda e, B_=B_, j=j, ex=ex: e.tensor_scalar(
                        out=B_[:], in0=iota_q[:], scalar1=cnt_incl[:, j, ex:ex + 1], scalar2=None, op0=ALU.is_ge),
                        r=["iota_q", "cnt_incl"], w=[bkey])
                    k.op("pe", lambda e, B_=B_, j=j: e.matmul(rowA[:], C["ones_b"][:, 0:1], B_[:, 0:512],
                                                              start=(j == 0), stop=(j == 63)),
                         r=[bkey, "ones_b"], w=["rowA"])
                    k.op("pe", lambda e, B_=B_, j=j: e.matmul(rowB[:], C["ones_b"][:, 0:1], B_[:, 512:1024],
                                                              start=(j == 0), stop=(j == 63)),
                         r=[bkey, "ones_b"], w=["rowB"])
                k.op("dve", lambda e: e.tensor_copy(out=row_sb[0:1, 0:512], in_=rowA[:]), r=["rowA"], w=["row_sb"])
                k.op("dve", lambda e: e.tensor_copy(out=row_sb[0:1, 512:1024], in_=rowB[:]), r=["rowB"], w=["row_sb"])
                if has_ctx:
                    for j in (64, 65):
                        B_ = Bt[bi % 2]
                        bkey = "Bt%d" % (bi % 2)
                        bi += 1
                        k.op("dve", lambda e, B_=B_, j=j, ex=ex: e.tensor_scalar(
                            out=B_[:, 0:128], in0=iota_q[:, 0:128], scalar1=cnt_incl[:, j, ex:ex + 1], scalar2=None,
                            op0=ALU.is_ge), r=["iota_q", "cnt_incl"], w=[bkey])
                        k.op("pe", lambda e, B_=B_, j=j: e.matmul(rowC[0:1, 0:128], C["ones_b"][:, 0:1], B_[:, 0:128],
                                                                  start=(j == 64), stop=(j == 65)),
                             r=[bkey, "ones_b"], w=["rowC"])
                    k.op("dve", lambda e: e.tensor_tensor(out=row_sb[0:1, 1024:1152], in0=rowC[0:1, 0:128],
                                                          in1=ctxadd[0:1, :], op=ALU.add),
                         r=["rowC", "ctxadd"], w=["row_sb"])
                for i in range(NPT):
                    k.op("pe", lambda e, i=i: e.transpose(out=rowC[:, 256 + i:257 + i], in_=row_sb[0:1, i * 128:(i + 1) * 128],
                                                          identity=C["ident_f"][0:1, 0:1]),
                         r=["row_sb", "ident_f"], w=["rowC"])
                k.op("dve", lambda e: e.tensor_copy(out=idx[:], in_=rowC[:, 256:256 + NPT]), r=["rowC"], w=["idx"])
                for i in range(NPT):
                    k.dma("pool", lambda e, i=i: e.indirect_dma_start(
                        out=xg[:, i, :], out_offset=None, in_=D["hn"],
                        in_offset=bass.IndirectOffsetOnAxis(ap=idx[:, i:i + 1], axis=0)),
                        r=["idx", "hn"], w=["xg"])
                    k.dma("pool", lambda e, i=i: e.indirect_dma_start(
                        out=ga[:, i, :], out_offset=None, in_=D["aff"],
                        in_offset=bass.IndirectOffsetOnAxis(ap=idx[:, i:i + 1], axis=0)),
                        r=["idx", "aff"], w=["ga"])
                for i in range(NPT):
                    for kc in range(8):
                        k.op("pe", lambda e, i=i, kc=kc: e.transpose(out=psX[:, kc * 128:(kc + 1) * 128],
                                                                     in_=xg[:, i, kc * 128:(kc + 1) * 128],
                                                                     identity=C["ident_b"][:]),
                             r=["xg", "ident_b"], w=["psX"])
                    k.op("dve", lambda e, i=i: e.tensor_copy(out=xgT[:, :, i * 128:(i + 1) * 128],
                                                             in_=psX[:].rearrange("p (a t) -> p a t", a=8)),
                         r=["psX"], w=["xgT"])
                for fc in range(8):
                    for (p0, p1) in pchunks:
                        n = p1 - p0
                        for kc in range(8):
                            k.op("pe", lambda e, fc=fc, kc=kc, p0=p0, p1=p1, n=n: e.matmul(
                                psG[:, 0:n], Wb["g"][:, kc, fc * 128:(fc + 1) * 128], xgT[:, kc, p0:p1],
                                start=(kc == 0), stop=(kc == 7)), r=["Wg", "xgT"], w=["psG"])
                        for kc in range(8):
                            k.op("pe", lambda e, fc=fc, kc=kc, p0=p0, p1=p1, n=n: e.matmul(
                                psU[:, 0:n], Wb["u"][:, kc, fc * 128:(fc + 1) * 128], xgT[:, kc, p0:p1],
                                start=(kc == 0), stop=(kc == 7)), r=["Wu", "xgT"], w=["psU"])
                        s_ = sg[yi % 2]
                        skey = "sg%d" % (yi % 2)
                        yi += 1
                        k.op("act", lambda e, s_=s_, n=n: e.activation(out=s_[:, 0:n], in_=psG[:, 0:n], func=AF.Silu),
                             r=["psG"], w=[skey])
                        k.op("dve", lambda e, s_=s_, n=n, fc=fc, p0=p0, p1=p1: e.tensor_tensor(
                            out=hidT[:, fc, p0:p1], in0=psU[:, 0:n], in1=s_[:, 0:n], op=ALU.mult),
                            r=["psU", skey], w=["hidT"])
                for i in range(NPT):
                    s = 0 if i < 8 else 1
                    y_ = yt[i % 2]
                    ykey = "yt%d" % (i % 2)
                    for dh in range(2):
                        py = psY[dh]
                        for fc in range(8):
                            k.op("pe", lambda e, i=i, fc=fc, dh=dh, py=py: e.matmul(
                                py[:], hidT[:, fc, i * 128:(i + 1) * 128], Wb["d"][:, fc, dh * 512:(dh + 1) * 512],
                                start=(fc == 0), stop=(fc == 7)), r=["hidT", "Wd"], w=["psY%d" % dh])
                        k.op("dve", lambda e, i=i, dh=dh, py=py, y_=y_, s=s, ex=ex: e.scalar_tensor_tensor(
                            out=y_[:, dh * 512:(dh + 1) * 512], in0=py[:], scalar=ga[:, i, ex:ex + 1],
                            in1=mv[(s, 5)][:, dh * 512:(dh + 1) * 512], op0=ALU.mult, op1=ALU.mult),
                            r=["psY%d" % dh, "ga", "modv"], w=[ykey])
                    k.dma("pool", lambda e, i=i, y_=y_: e.indirect_dma_start(
                        out=D["xres"], out_offset=bass.IndirectOffsetOnAxis(ap=idx[:, i:i + 1], axis=0),
                        in_=y_[:], in_offset=None, compute_op=ALU.add),
                        r=[ykey, "idx"], w=["xres"])
            k.barrier()


def phase_final(pb, C):
    k, D = pb.k, pb.D
    with ExitStack() as es:
        g = pb.sb(es, "gfin", [128, 1024], F32)
        k.dma("sp", lambda e: e.dma_start(out=g[:], in_=D["norm_out"].to_broadcast([128, 1024])), w=["gfin"])
        xt = [pb.sb(es, "fx%d" % i, [128, 1024], F32) for i in range(2)]
        ot = [pb.sb(es, "fo%d" % i, [128, 1024], F32) for i in range(2)]
        junk = pb.sb(es, "fjunk", [128, 1024], BF16)
        rstd = [pb.sb(es, "frs%d" % i, [128, 4], F32) for i in range(2)]
        for j in range(64):
            b = j % 2
            r0 = j * 128
            k.dma("sp", lambda e, b=b, r0=r0: e.dma_start(out=xt[b][:], in_=D["xres"][r0:r0 + 128, :]),
                  r=["xres"], w=["fx%d" % b])
            k.op("act", lambda e, b=b: e.activation(out=junk[:], in_=xt[b][:], func=AF.Square, accum_out=rstd[b][:, 0:1]),
                 r=["fx%d" % b], w=["fjunk", "frs%d" % b])
            k.op("act", lambda e, b=b: e.activation(out=rstd[b][:, 1:2], in_=rstd[b][:, 0:1], func=AF.Sqrt, bias=EPS,
                                                    scale=1.0 / DM), r=["frs%d" % b], w=["frs%d" % b])
            k.op("dve", lambda e, b=b: e.reciprocal(out=rstd[b][:, 2:3], in_=rstd[b][:, 1:2]), r=["frs%d" % b], w=["frs%d" % b])
            k.op("dve", lambda e, b=b: e.scalar_tensor_tensor(out=ot[b][:], in0=xt[b][:], scalar=rstd[b][:, 2:3], in1=g[:],
                                                              op0=ALU.mult, op1=ALU.mult),
                 r=["fx%d" % b, "frs%d" % b, "gfin"], w=["fo%d" % b])
            k.dma("sp", lambda e, b=b, r0=r0: e.dma_start(out=D["out"][r0:r0 + 128, :], in_=ot[b][:]),
                  r=["fo%d" % b], w=["out"])
        k.barrier()


NH = 8
ATT_SCALE = 1.0 / float(np.sqrt(192.0))


def attn_host_consts():
    c = {}
    t = np.arange(T_LAT)
    row = (t // 64).astype(np.float32)
    col = (t % 64).astype(np.float32)
    nf = 16
    inv = (10000.0 ** (-np.arange(nf, dtype=np.float32) / nf)).astype(np.float32)
    cosT = np.zeros((64, T_LAT), np.float32)
    sinT = np.zeros((64, T_LAT), np.float32)
    for a, pos in ((0, row), (1, col)):
        ang = (pos[None, :] * inv[:, None]).astype(np.float32)
        for b in range(2):
            d0 = a * 32 + b * 16
            cosT[d0:d0 + 16] = np.cos(ang)
            sinT[d0:d0 + 16] = np.sin(ang) * (-1.0 if b == 0 else 1.0)
    c["cos2"] = np.concatenate([cosT, cosT], 0)
    c["sin2"] = np.concatenate([sinT, sinT], 0)
    return c


def rope_swap_perm():
    d = np.arange(64)
    a, b, f = d // 32, (d // 16) % 2, d % 16
    return a * 32 + (1 - b) * 16 + f


def attn_layout_weights(w_uq, w_ukv, w_kr):
    sp = rope_swap_perm()
    nope = np.concatenate([np.arange(h * 192, h * 192 + 128) for h in range(NH)])
    rope = np.concatenate([np.arange(h * 192 + 128, h * 192 + 192) for h in range(NH)])
    ropes = np.concatenate([h * 192 + 128 + sp for h in range(NH)])
    w_uq_r = np.ascontiguousarray(w_uq[:, np.concatenate([nope, rope, ropes])])
    kn = np.concatenate([np.arange(h * 256, h * 256 + 128) for h in range(NH)])
    vv = np.concatenate([np.arange(h * 256 + 128, h * 256 + 256) for h in range(NH)])
    w_ukv_r = np.ascontiguousarray(w_ukv[:, np.concatenate([kn, vv])])
    w_kr_r = np.ascontiguousarray(np.concatenate([w_kr, w_kr[:, sp]], 1))
    return w_uq_r, w_ukv_r, w_kr_r


def load_w_bf16(pb, es, name, src_ap, kc, n, stg, stgkey):
    k = pb.k
    t = pb.sb(es, name, [128, kc, n], BF16)
    for c0 in range(0, n, 512):
        c1 = min(n, c0 + 512)
        for q in range(kc):
            k.dma("sp", lambda e, q=q, c0=c0, c1=c1: e.dma_start(out=stg[:, 0:c1 - c0], in_=src_ap[q * 128:(q + 1) * 128, c0:c1]),
                  w=[stgkey])
            k.op("dve", lambda e, q=q, c0=c0, c1=c1: e.tensor_copy(out=t[:, q, c0:c1], in_=stg[:, 0:c1 - c0]),
                 r=[stgkey], w=[name])
    return t


def rms_small(pb, ps_ap, n, rstd, rkey, junk, gain_rep, outbf, outkey, pskey):
    k = pb.k
    k.op("act", lambda e: e.activation(out=junk[:, 0:n], in_=ps_ap, func=AF.Square, accum_out=rstd[:, 0:1]),
         r=[pskey], w=["junk", rkey])
    k.op("act", lambda e: e.activation(out=rstd[:, 1:2], in_=rstd[:, 0:1], func=AF.Sqrt, bias=EPS, scale=1.0 / n),
         r=[rkey], w=[rkey])
    k.op("dve", lambda e: e.reciprocal(out=rstd[:, 2:3], in_=rstd[:, 1:2]), r=[rkey], w=[rkey])
    k.op("dve", lambda e: e.scalar_tensor_tensor(out=outbf, in0=ps_ap, scalar=rstd[:, 2:3], in1=gain_rep,
                                                 op0=ALU.mult, op1=ALU.mult), r=[pskey, rkey, "gains"], w=[outkey])


def phase_attn_pre(pb, C):
    k, D, nc = pb.k, pb.D, pb.nc
    l = 1
    with ExitStack() as es:
        mv = load_modv(pb, es, l, (0, 1))
        stg = pb.sb(es, "wstg", [128, 512], F32)
        w_dq = load_w_bf16(pb, es, "w_dq", D["w_dq"], 8, 384, stg, "wstg")
        w_dkv = load_w_bf16(pb, es, "w_dkv", D["w_dkv"], 8, 256, stg, "wstg")
        w_kr = load_w_bf16(pb, es, "w_kr", D["w_kr_r"], 8, 128, stg, "wstg")
        w_uq = load_w_bf16(pb, es, "w_uq", D["w_uq_r"], 3, 2048, stg, "wstg")
        w_ukv = load_w_bf16(pb, es, "w_ukv", D["w_ukv_r"], 2, 2048, stg, "wstg")
        qn_rep = pb.sb(es, "qn_rep", [128, 384], F32)
        kvn_rep = pb.sb(es, "kvn_rep", [128, 256], F32)
        k.dma("sp", lambda e: e.dma_start(out=qn_rep[:], in_=D["q_norm"].to_broadcast([128, 384])), w=["gains"])
        k.dma("sp", lambda e: e.dma_start(out=kvn_rep[:], in_=D["kv_norm"].to_broadcast([128, 256])), w=["gains"])
        xt = [pb.sb(es, "xt%d" % i, [128, 1024], F32) for i in range(2)]
        an = [pb.sb(es, "an%d" % i, [128, 1024], BF16) for i in range(2)]
        junk = pb.sb(es, "junk", [128, 1024], BF16)
        t1 = pb.sb(es, "t1", [128, 1024], F32)
        rstd = [pb.sb(es, "rstd%d" % i, [128, 4], F32) for i in range(2)]
        rs2 = [pb.sb(es, "rs2%d" % i, [128, 4], F32) for i in range(2)]
        aT = pb.sb(es, "aT", [128, 8, 512], BF16)
        cqT = pb.sb(es, "cqT", [128, 3, 512], BF16)
        ckvT = pb.sb(es, "ckvT", [128, 2, 512], BF16)
        cqn = pb.sb(es, "cqn", [128, 384], BF16)
        ckvn = pb.sb(es, "ckvn", [128, 256], BF16)
        cosb = pb.sb(es, "cosb", [128, 512], F32)
        sinb = pb.sb(es, "sinb", [128, 512], F32)
        ostg = [pb.sb(es, "ostg%d" % i, [128, 512], BF16) for i in range(3)]
        vstg = [pb.sb(es, "vstg%d" % i, [128, 1024], BF16) for i in range(2)]
        ra = pb.sb(es, "ra", [128, 512], F32)
        rb = pb.sb(es, "rb", [128, 512], F32)
        psT = pb.ps(es, "psT", [128, 1024], BF16)
        psS = pb.ps(es, "psS", [128, 512], F32)
        psA = pb.ps(es, "psA", [128, 512], F32)
        psB = pb.ps(es, "psB", [128, 512], F32)
        psV = [pb.ps(es, "psV%d" % i, [128, 512], F32) for i in range(2)]
        oi = 0
        vi = 0
        xi = 0
        supers = [(i * 512, 4, 0) for i in range(16)] + [(T_LAT, 2, 1)]
        for (t0, ntile, s) in supers:
            nt = ntile * 128
            is_lat = (s == 0)
            for tl in range(ntile):
                b = xi % 2
                xi += 1
                r0 = t0 + tl * 128
                k.dma("sp", lambda e, b=b, r0=r0: e.dma_start(out=xt[b][:], in_=D["xres"][r0:r0 + 128, :]),
                      r=["xres"], w=["xt%d" % b])
                norm_mod_tile(pb, xt[b], "xt%d" % b, rstd[b], "rstd%d" % b, junk, t1, mv[(s, 1)], mv[(s, 0)], an[b], "an%d" % b)
                for kc in range(8):
                    k.op("pe", lambda e, b=b, kc=kc: e.transpose(out=psT[:, kc * 128:(kc + 1) * 128],
                                                                 in_=an[b][:, kc * 128:(kc + 1) * 128], identity=C["ident_b"][:]),
                         r=["an%d" % b, "ident_b"], w=["psT"])
                k.op("act", lambda e, tl=tl: e.activation(out=aT[:, :, tl * 128:(tl + 1) * 128],
                                                          in_=psT[:].rearrange("p (a t) -> p a t", a=8), func=AF.Copy),
                     r=["psT"], w=["aT"])
                if is_lat:
                    for kc in range(8):
                        k.op("pe", lambda e, kc=kc, tl=tl: e.matmul(psS[:, 0:384], aT[:, kc, tl * 128:(tl + 1) * 128], w_dq[:, kc, :],
                                                                    start=(kc == 0), stop=(kc == 7)), r=["aT", "w_dq"], w=["psS"])
                    rms_small(pb, psS[:, 0:384], 384, rs2[b], "rs2%d" % b, junk, qn_rep[:], cqn[:], "cqn", "psS")
                    for c in range(3):
                        k.op("pe", lambda e, c=c: e.transpose(out=psT[:, c * 128:(c + 1) * 128], in_=cqn[:, c * 128:(c + 1) * 128],
                                                              identity=C["ident_b"][:]), r=["cqn", "ident_b"], w=["psT"])
                    k.op("act", lambda e, tl=tl: e.activation(out=cqT[:, :, tl * 128:(tl + 1) * 128],
                                                              in_=psT[:, 0:384].rearrange("p (a t) -> p a t", a=3), func=AF.Copy),
                         r=["psT"], w=["cqT"])
                for kc in range(8):
                    k.op("pe", lambda e, kc=kc, tl=tl: e.matmul(psS[:, 0:256], aT[:, kc, tl * 128:(tl + 1) * 128], w_dkv[:, kc, :],
                                                                start=(kc == 0), stop=(kc == 7)), r=["aT", "w_dkv"], w=["psS"])
                rms_small(pb, psS[:, 0:256], 256, rs2[b], "rs2%d" % b, junk, kvn_rep[:], ckvn[:], "ckvn", "psS")
                for c in range(2):
                    k.op("pe", lambda e, c=c: e.transpose(out=psT[:, c * 128:(c + 1) * 128], in_=ckvn[:, c * 128:(c + 1) * 128],
                                                          identity=C["ident_b"][:]), r=["ckvn", "ident_b"], w=["psT"])
                k.op("act", lambda e, tl=tl: e.activation(out=ckvT[:, :, tl * 128:(tl + 1) * 128],
                                                          in_=psT[:, 0:256].rearrange("p (a t) -> p a t", a=2), func=AF.Copy),
                     r=["psT"], w=["ckvT"])
                v_ = vstg[vi % 2]
                vkey = "vstg%d" % (vi % 2)
                vi += 1
                for dh in range(2):
                    for c in range(2):
                        k.op("pe", lambda e, c=c, dh=dh, tl=tl: e.matmul(
                            psV[dh][:], ckvT[:, c, tl * 128:(tl + 1) * 128], w_ukv[:, c, 1024 + dh * 512:1024 + (dh + 1) * 512],
                            start=(c == 0), stop=(c == 1)), r=["ckvT", "w_ukv"], w=["psV%d" % dh])
                    k.op("act", lambda e, dh=dh, v_=v_: e.activation(out=v_[:, dh * 512:(dh + 1) * 512], in_=psV[dh][:], func=AF.Copy),
                         r=["psV%d" % dh], w=[vkey])
                k.dma("sp", lambda e, v_=v_, r0=r0: e.dma_start(out=D["Vd"][r0:r0 + 128, :], in_=v_[:]), r=[vkey], w=["Vd"])
            if is_lat:
                k.dma("sp", lambda e, t0=t0: e.dma_start(out=cosb[:], in_=D["c_cos2"][:, t0:t0 + 512]), w=["cosb"])
                k.dma("sp", lambda e, t0=t0: e.dma_start(out=sinb[:], in_=D["c_sin2"][:, t0:t0 + 512]), w=["sinb"])
            for h in range(NH):
                for c in range(2):
                    k.op("pe", lambda e, c=c, h=h: e.matmul(psA[:, 0:nt], w_ukv[:, c, h * 128:(h + 1) * 128], ckvT[:, c, 0:nt],
                                                            start=(c == 0), stop=(c == 1)), r=["ckvT", "w_ukv"], w=["psA"])
                o_ = ostg[oi % 3]
                okey = "ostg%d" % (oi % 3)
                oi += 1
                k.op("act", lambda e, o_=o_: e.activation(out=o_[:, 0:nt], in_=psA[:, 0:nt], func=AF.Copy), r=["psA"], w=[okey])
                k.dma("sp", lambda e, o_=o_, h=h, t0=t0: e.dma_start(out=D["KT"][h, :, t0:t0 + nt], in_=o_[:, 0:nt]),
                      r=[okey], w=["KT"])
            for kc in range(8):
                k.op("pe", lambda e, kc=kc: e.matmul(psA[0:64, 0:nt], w_kr[:, kc, 0:64], aT[:, kc, 0:nt],
                                                     start=(kc == 0), stop=(kc == 7)), r=["aT", "w_kr"], w=["psA"])
            o_ = ostg[oi % 3]
            okey = "ostg%d" % (oi % 3)
            oi += 1
            if is_lat:
                for kc in range(8):
                    k.op("pe", lambda e, kc=kc: e.matmul(psB[0:64, 0:nt], w_kr[:, kc, 64:128], aT[:, kc, 0:nt],
                                                         start=(kc == 0), stop=(kc == 7)), r=["aT", "w_kr"], w=["psB"])
                k.op("dve", lambda e: e.tensor_tensor(out=ra[0:64, :], in0=psA[0:64, :], in1=cosb[0:64, :], op=ALU.mult),
                     r=["psA", "cosb"], w=["ra"])
                k.op("dve", lambda e: e.tensor_tensor(out=rb[0:64, :], in0=psB[0:64, :], in1=sinb[0:64, :], op=ALU.mult),
                     r=["psB", "sinb"], w=["rb"])
                k.op("dve", lambda e, o_=o_: e.tensor_tensor(out=o_[0:64, :], in0=ra[0:64, :], in1=rb[0:64, :], op=ALU.add),
                     r=["ra", "rb"], w=[okey])
            else:
                k.op("act", lambda e, o_=o_: e.activation(out=o_[0:64, 0:nt], in_=psA[0:64, 0:nt], func=AF.Copy), r=["psA"], w=[okey])
            k.dma("sp", lambda e, o_=o_, t0=t0: e.dma_start(out=D["KRT"][:, t0:t0 + nt], in_=o_[0:64, 0:nt]), r=[okey], w=["KRT"])
            if not is_lat:
                continue
            for h in range(NH):
                for c in range(3):
                    k.op("pe", lambda e, c=c, h=h: e.matmul(psA[:], w_uq[:, c, h * 128:(h + 1) * 128], cqT[:, c, :],
                                                            start=(c == 0), stop=(c == 2)), r=["cqT", "w_uq"], w=["psA"])
                o_ = ostg[oi % 3]
                okey = "ostg%d" % (oi % 3)
                oi += 1
                k.op("act", lambda e, o_=o_: e.activation(out=o_[:], in_=psA[:], func=AF.Copy), r=["psA"], w=[okey])
                k.dma("sp", lambda e, o_=o_, h=h, t0=t0: e.dma_start(out=D["QT"][h, :, t0:t0 + 512], in_=o_[:]), r=[okey], w=["QT"])
            for hp in range(4):
                for c in range(3):
                    k.op("pe", lambda e, c=c, hp=hp: e.matmul(psA[:], w_uq[:, c, 1024 + hp * 128:1024 + (hp + 1) * 128], cqT[:, c, :],
                                                              start=(c == 0), stop=(c == 2)), r=["cqT", "w_uq"], w=["psA"])
                for c in range(3):
                    k.op("pe", lambda e, c=c, hp=hp: e.matmul(psB[:], w_uq[:, c, 1536 + hp * 128:1536 + (hp + 1) * 128], cqT[:, c, :],
                                                              start=(c == 0), stop=(c == 2)), r=["cqT", "w_uq"], w=["psB"])
                o_ = ostg[oi % 3]
                okey = "ostg%d" % (oi % 3)
                oi += 1
                k.op("dve", lambda e: e.tensor_tensor(out=ra[:], in0=psA[:], in1=cosb[:], op=ALU.mult), r=["psA", "cosb"], w=["ra"])
                k.op("dve", lambda e: e.tensor_tensor(out=rb[:], in0=psB[:], in1=sinb[:], op=ALU.mult), r=["psB", "sinb"], w=["rb"])
                k.op("dve", lambda e, o_=o_: e.tensor_tensor(out=o_[:], in0=ra[:], in1=rb[:], op=ALU.add), r=["ra", "rb"], w=[okey])
                k.dma("sp", lambda e, o_=o_, hp=hp, t0=t0: e.dma_start(
                    out=D["QRT"][2 * hp:2 * hp + 2, :, t0:t0 + 512].rearrange("h d t -> (h d) t"), in_=o_[:]), r=[okey], w=["QRT"])
        k.barrier()


def phase_attn_main(pb, C, heads=range(NH), qblocks=range(16)):
    k, D, nc = pb.k, pb.D, pb.nc
    NKB = NTOK // 128
    with ExitStack() as es:
        KRT = pb.sb(es, "KRT", [64, NTOK], BF16)
        k.dma("sp", lambda e: e.dma_start(out=KRT[:], in_=D["KRT"]), r=["KRT"], w=["sKRT"])
        KT = [pb.sb(es, "KT%d" % i, [128, NTOK], BF16) for i in range(2)]
        Vh = [pb.sb(es, "Vh%d" % i, [128, NKB, 130], BF16) for i in range(2)]
        for i in range(2):
            k.op("dve", lambda e, i=i: e.memset(Vh[i][:, :, 128:130], 1.0), w=["Vh%d" % i])
        Qb = [pb.sb(es, "Qb%d" % i, [128, 512], BF16) for i in range(2)]
        QRb = [pb.sb(es, "QRb%d" % i, [64, 512], BF16) for i in range(2)]
        PT = [pb.sb(es, "PT%d" % i, [128, 512], BF16) for i in range(3)]
        Ost = [pb.sb(es, "Ost%d" % i, [128, 4, 128], BF16) for i in range(2)]
        rinv = pb.sb(es, "rinv", [128, 8], F32)
        psS = [pb.ps(es, "psS%d" % i, [128, 512], F32) for i in range(2)]
        psO = [pb.ps(es, "psO%d" % i, [128, 512], F32) for i in range(4)]
        qi = 0
        pi = 0
        si = 0
        for hi, h in enumerate(heads):
            hb = hi % 2
            k.dma("sp", lambda e, hb=hb, h=h: e.dma_start(out=KT[hb][:], in_=D["KT"][h]), r=["KT"], w=["KT%d" % hb])
            k.dma("sp", lambda e, hb=hb, h=h: e.dma_start(
                out=Vh[hb][:, :, 0:128], in_=D["Vd"][:, h * 128:(h + 1) * 128].rearrange("(kb p) v -> p kb v", p=128)),
                r=["Vd"], w=["Vh%d" % hb])
            for qb in qblocks:
                b = qi % 2
                qi += 1
                q0 = qb * 512
                k.dma("sp", lambda e, b=b, h=h, q0=q0: e.dma_start(out=Qb[b][:], in_=D["QT"][h, :, q0:q0 + 512]),
                      r=["QT"], w=["Qb%d" % b])
                k.dma("sp", lambda e, b=b, h=h, q0=q0: e.dma_start(out=QRb[b][:], in_=D["QRT"][h, :, q0:q0 + 512]),
                      r=["QRT"], w=["QRb%d" % b])

                def qk(kb, sb_):
                    k.op("pe", lambda e: e.matmul(psS[sb_][:], KT[hb][:, kb * 128:(kb + 1) * 128], Qb[b][:], start=True, stop=False),
                         r=["KT%d" % hb, "Qb%d" % b], w=["psS%d" % sb_])
                    k.op("pe", lambda e: e.matmul(psS[sb_][:], KRT[:, kb * 128:(kb + 1) * 128], QRb[b][:], start=False, stop=True),
                         r=["sKRT", "QRb%d" % b], w=["psS%d" % sb_])

                sbs = []
                sbs.append(si % 2)
                qk(0, si % 2)
                si += 1
                for kb in range(NKB):
                    if kb + 1 < NKB:
                        sbs.append(si % 2)
                        qk(kb + 1, si % 2)
                        si += 1
                    sb_ = sbs[kb]
                    p_ = PT[pi % 3]
                    pkey = "PT%d" % (pi % 3)
                    pi += 1
                    k.op("act", lambda e, sb_=sb_, p_=p_: e.activation(out=p_[:], in_=psS[sb_][:], func=AF.Exp, scale=ATT_SCALE),
                         r=["psS%d" % sb_], w=[pkey])
                    for qt in range(4):
                        k.op("pe", lambda e, qt=qt, p_=p_, kb=kb: e.matmul(
                            psO[qt][:, 0:130], p_[:, qt * 128:(qt + 1) * 128], Vh[hb][:, kb, :],
                            start=(kb == 0), stop=(kb == NKB - 1)), r=[pkey, "Vh%d" % hb], w=["psO%d" % qt])
                o_ = Ost[b]
                okey = "Ost%d" % b
                for qt in range(4):
                    k.op("dve", lambda e, qt=qt: e.reciprocal(out=rinv[:, qt:qt + 1], in_=psO[qt][:, 128:129]),
                         r=["psO%d" % qt], w=["rinv"])
                    k.op("dve", lambda e, qt=qt, o_=o_: e.tensor_scalar(out=o_[:, qt, :], in0=psO[qt][:, 0:128],
                                                                        scalar1=rinv[:, qt:qt + 1], scalar2=None, op0=ALU.mult),
                         r=["psO%d" % qt, "rinv"], w=[okey])
                k.dma("sp", lambda e, o_=o_, h=h, q0=q0: e.dma_start(
                    out=D["Od"][q0:q0 + 512, h * 128:(h + 1) * 128].rearrange("(qt p) v -> p qt v", p=128), in_=o_[:]),
                    r=[okey], w=["Od"])
        k.barrier()


def phase_attn_post(pb, C):
    k, D, nc = pb.k, pb.D, pb.nc
    with ExitStack() as es:
        mv = load_modv(pb, es, 1, (2,))
        stg = pb.sb(es, "wstg", [128, 512], F32)
        w_o = load_w_bf16(pb, es, "w_o", D["w_o"], 8, 1024, stg, "wstg")
        ot = [pb.sb(es, "ot%d" % i, [128, 1024], BF16) for i in range(2)]
        oT = [pb.sb(es, "oT%d" % i, [128, 8, 128], BF16) for i in range(2)]
        xt = [pb.sb(es, "xt%d" % i, [128, 1024], F32) for i in range(2)]
        tm = [pb.sb(es, "tm%d" % i, [128, 1024], F32) for i in range(2)]
        psT = pb.ps(es, "psT", [128, 1024], BF16)
        psY = [pb.ps(es, "psY%d" % i, [128, 512], F32) for i in range(2)]
        for j in range(64):
            b = j % 2
            r0 = j * 128
            k.dma("sp", lambda e, b=b, r0=r0: e.dma_start(out=ot[b][:], in_=D["Od"][r0:r0 + 128, :]), r=["Od"], w=["ot%d" % b])
            k.dma("sp", lambda e, b=b, r0=r0: e.dma_start(out=xt[b][:], in_=D["xres"][r0:r0 + 128, :]), r=["xres"], w=["xt%d" % b])
            for c in range(8):
                k.op("pe", lambda e, b=b, c=c: e.transpose(out=psT[:, c * 128:(c + 1) * 128], in_=ot[b][:, c * 128:(c + 1) * 128],
                                                           identity=C["ident_b"][:]), r=["ot%d" % b, "ident_b"], w=["psT"])
            k.op("act", lambda e, b=b: e.activation(out=oT[b][:].rearrange("p a t -> p (a t)"), in_=psT[:], func=AF.Copy),
                 r=["psT"], w=["oT%d" % b])
            for dh in range(2):
                for c in range(8):
                    k.op("pe", lambda e, b=b, c=c, dh=dh: e.matmul(psY[dh][:], oT[b][:, c, :], w_o[:, c, dh * 512:(dh + 1) * 512],
                                                                   start=(c == 0), stop=(c == 7)), r=["oT%d" % b, "w_o"], w=["psY%d" % dh])
                k.op("dve", lambda e, b=b, dh=dh: e.tensor_tensor(out=tm[b][:, dh * 512:(dh + 1) * 512], in0=psY[dh][:],
                                                                  in1=mv[(0, 2)][:, dh * 512:(dh + 1) * 512], op=ALU.mult),
                     r=["psY%d" % dh, "modv"], w=["tm%d" % b])
            k.op("pool", lambda e, b=b: e.tensor_tensor(out=tm[b][:], in0=tm[b][:], in1=xt[b][:], op=ALU.add),
                 r=["tm%d" % b, "xt%d" % b], w=["tm%d" % b])
            k.dma("sp", lambda e, b=b, r0=r0: e.dma_start(out=D["xres"][r0:r0 + 128, :], in_=tm[b][:]), r=["tm%d" % b], w=["xres"])
        k.barrier()


def load_cols(pb, es, C, name, src_rows_ap, n, psbank, pskey):
    k = pb.k
    rows = pb.sb(es, name + "_r", [n, 128], F32)
    cols = pb.sb(es, name, [128, n], F32)
    k.dma("sp", lambda e: e.dma_start(out=rows[:], in_=src_rows_ap), w=[name + "_r"])
    k.op("pe", lambda e: e.transpose(out=psbank[:, 0:n], in_=rows[:], identity=C["ident_f"][0:n, 0:n]),
         r=[name + "_r", "ident_f"], w=[pskey])
    k.op("dve", lambda e: e.tensor_copy(out=cols[:], in_=psbank[:, 0:n]), r=[pskey], w=[name])
    return cols


def phase_mix_pass(pb, C, dirn, nsup_lat=32, do_ctx=True):
    k, D, nc = pb.k, pb.D, pb.nc
    W = 256
    with ExitStack() as es:
        P = [pb.ps(es, "P%d" % i, [128, 512], F32) for i in range(8)]
        PK = ["P%d" % i for i in range(8)]
        P0b = P[0][:].bitcast(BF16)
        mv = load_modv(pb, es, 0, (0, 1))
        stg = pb.sb(es, "wstg", [128, 512], F32)
        win = D["w_in"]
        wq = load_w_bf16(pb, es, "wq", win[:, 0:512], 8, 512, stg, "wstg")
        wf = load_w_bf16(pb, es, "wf", win[:, 512 + 512 * dirn:1024 + 512 * dirn], 8, 512, stg, "wstg")
        wi = load_w_bf16(pb, es, "wi", win[:, 1536:2048], 8, 512, stg, "wstg")
        wx = load_w_bf16(pb, es, "wx", win[:, 3072:4096], 8, 1024, stg, "wstg")
        wdt = load_w_bf16(pb, es, "wdt", win[:, 4096 + 8 * dirn:4104 + 8 * dirn], 8, 8, stg, "wstg")
        lg0 = load_cols(pb, es, C, "lg0", D["lb_gamma"][0, dirn, :].rearrange("(c p) -> c p", p=128), 4, P[7], PK[7])
        lg1 = load_cols(pb, es, C, "lg1", D["lb_gamma"][1, dirn, :].rearrange("(c p) -> c p", p=128), 4, P[7], PK[7])
        lbc = pb.sb(es, "lbc", [128, 4], F32)
        oml = pb.sb(es, "oml", [128, 4], F32)
        k.op("dve", lambda e: e.tensor_tensor(out=lbc[:], in0=lg0[:], in1=lg1[:], op=ALU.subtract), r=["lg0", "lg1"], w=["lbc"])
        k.op("act", lambda e: e.activation(out=lbc[:], in_=lbc[:], func=AF.Sigmoid), r=["lbc"], w=["lbc"])
        k.op("dve", lambda e: e.tensor_scalar(out=oml[:], in0=lbc[:], scalar1=-1.0, scalar2=1.0, op0=ALU.mult, op1=ALU.add),
             r=["lbc"], w=["oml"])
        cw = [load_cols(pb, es, C, "cw%d" % j, D["conv_w"][j, :].rearrange("(c p) -> c p", p=128), 8, P[7], PK[7]) for j in range(5)]
        cbc = load_cols(pb, es, C, "cbc", D["conv_b"].rearrange("o (c p) -> (o c) p", p=128), 8, P[7], PK[7])
        dtb = pb.sb(es, "dtb", [128, 8], F32)
        negA = pb.sb(es, "negA", [128, 8], F32)
        k.dma("sp", lambda e: e.dma_start(out=dtb[:], in_=D["dt_bias"][dirn:dirn + 1, :].to_broadcast([128, 8])), w=["dtb"])
        k.dma("sp", lambda e: e.dma_start(out=negA[:], in_=D["a_log"][dirn:dirn + 1, :].to_broadcast([128, 8])), w=["negA"])
        k.op("act", lambda e: e.activation(out=negA[:], in_=negA[:], func=AF.Exp), r=["negA"], w=["negA"])
        k.op("dve", lambda e: e.tensor_scalar(out=negA[:], in0=negA[:], scalar1=-1.0, scalar2=None, op0=ALU.mult), r=["negA"], w=["negA"])
        dsk = pb.sb(es, "dsk", [128, 8], F32)
        k.dma("sp", lambda e: e.dma_start(out=dsk[:], in_=D["d_skip"].to_broadcast([128, 8])), w=["dsk"])
        if getattr(pb, 'stop', 99) == 0:
            k.barrier()
            return
        if dirn == 0:
            m_att_b, m_att_f = C["triu_incl_b"], C["triu_incl_f"]
            m_cum, m_wend, m_lh = C["triu_incl_f"], C["tril_strict_f"], C["tril_strict_f"]
            m_rhs = C["triu_incl_f"]
        else:
            m_att_b, m_att_f = C["tril_incl_b"], C["tril_incl_f"]
            m_cum, m_wend, m_lh = C["tril_incl_f"], C["triu_strict_f"], C["triu_strict_f"]
            m_rhs = C["tril_incl_f"]
        xt = [pb.sb(es, "xt%d" % i, [128, 1024], F32) for i in range(2)]
        an = [pb.sb(es, "an%d" % i, [128, 1024], BF16) for i in range(2)]
        hx = pb.sb(es, "hx", [4, 1024], F32)
        ah = pb.sb(es, "ah", [4, 1024], BF16)
        junk = pb.sb(es, "junk", [128, 1024], BF16)
        t1 = pb.sb(es, "t1", [128, 1024], F32)
        rstd = [pb.sb(es, "rstd%d" % i, [128, 4], F32) for i in range(3)]
        aT = pb.sb(es, "aT", [128, 8, W + 4], BF16)
        Qsil = pb.sb(es, "Qsil", [128, 4, W], F32)
        fg = pb.sb(es, "fg", [128, 4, W], F32)
        la = pb.sb(es, "la", [128, 4, W], F32)
        kk = pb.sb(es, "kk", [128, 4, W], F32)
        u = pb.sb(es, "u", [128, 8, W + 4], F32)
        acc = pb.sb(es, "acc", [128, 8, W], F32)
        tmp = pb.sb(es, "tmp", [128, 8, W], F32)
        xbc = pb.sb(es, "xbc", [128, 8, W], BF16)
        vtok = pb.sb(es, "vtok", [128, 2, 512], BF16)
        dtr = pb.sb(es, "dtr", [128, 2, 8], F32)
        bb = pb.sb(es, "bb", [128, 4, 128], F32)
        b2 = pb.sb(es, "b2", [128, 4, 128], F32)
        sc = pb.sb(es, "sc", [128, 3, 4], F32)
        esc = pb.sb(es, "esc", [128, 3, 4], F32)
        nb = pb.sb(es, "nb", [128, 4], F32)
        Eq = pb.sb(es, "Eq", [128, 4, 128], F32)
        Ek = pb.sb(es, "Ek", [128, 4, 128], F32)
        Qt = pb.sb(es, "Qt", [128, 4, 128], BF16)
        Kt = pb.sb(es, "Kt", [128, 4, 128], BF16)
        Ktok = pb.sb(es, "Ktok", [128, 4, 128], BF16)
        attm = pb.sb(es, "attm", [128, 8, 128], BF16)
        Sh = pb.sb(es, "Sh", [128, 4, 64], F32)
        ShpA = pb.sb(es, "ShpA", [128, 4, 64], BF16)
        ShpB = pb.sb(es, "ShpB", [128, 4, 64], BF16)
        KtA = pb.sb(es, "KtA", [128, 4, 128], BF16)
        KtB = pb.sb(es, "KtB", [128, 4, 128], BF16)
        for nm_, t_ in (("ShpA", ShpA), ("ShpB", ShpB), ("KtA", KtA), ("KtB", KtB)):
            k.op("dve", lambda e, t_=t_: e.memset(t_[:], 0.0), w=[nm_])
        tU = pb.sb(es, "tU", [128, 4, 64], F32)
        osb = pb.sb(es, "osb", [128, 512], F32)
        xmt = pb.sb(es, "xmt", [128, 8, 64], BF16)
        Btok = pb.sb(es, "Btok", [128, 2, 128], BF16)
        dts = pb.sb(es, "dts", [128, 4, 8], F32)
        ec = pb.sb(es, "ec", [128, 24], F32)
        LH = pb.sb(es, "LH", [128, 8, 128], F32)
        Em = pb.sb(es, "Em", [128, 8, 128], F32)
        cbm = pb.sb(es, "cbm", [128, 2, 128], F32)
        Mm = pb.sb(es, "Mm", [128, 8, 128], BF16)
        xdt = pb.sb(es, "xdt", [128, 8, 64], BF16)
        xdtw = pb.sb(es, "xdtw", [128, 8, 64], BF16)
        yi = pb.sb(es, "yi", [128, 8, 64], F32)
        ysb = pb.sb(es, "ysb", [128, 8, 64], F32)
        Ss = pb.sb(es, "Ss", [128, 8, 64], F32)
        Ssb = pb.sb(es, "Ssb", [128, 8, 64], BF16)
        k.op("dve", lambda e: e.memset(Sh[:], 0.0), w=["Sh"])
        k.op("dve", lambda e: e.memset(Ss[:], 0.0), w=["Ss"])
        k.op("dve", lambda e: e.memset(Ssb[:], 0.0), w=["Ssb"])

        supers = []
        if do_ctx:
            supers.append((1, 0))
        lat_order = list(range(nsup_lat)) if dirn == 0 else list(range(nsup_lat - 1, -1, -1))
        supers += [(0, i) for i in lat_order]
        bank_rot = [1, 2, 3]
        bri = 0
        xi = 0
        for (s, si) in supers:
            src = D["x"] if s == 0 else D["ctx"]
            Ts = T_LAT if s == 0 else T_CTX
            r0 = si * W
            g0 = r0 if s == 0 else T_LAT + r0
            for tl in range(2):
                b = xi % 2
                xi += 1
                rr = r0 + tl * 128
                k.dma("sp", lambda e, b=b, rr=rr: e.dma_start(out=xt[b][:], in_=src[rr:rr + 128, :]), w=["xt%d" % b])
                norm_mod_tile(pb, xt[b], "xt%d" % b, rstd[b], "rstd%d" % b, junk, t1, mv[(s, 1)], mv[(s, 0)], an[b], "an%d" % b)
                for kc in range(8):
                    k.op("pe", lambda e, b=b, kc=kc: e.transpose(out=P0b[:, kc * 128:(kc + 1) * 128],
                                                                 in_=an[b][:, kc * 128:(kc + 1) * 128], identity=C["ident_b"][:]),
                         r=["an%d" % b, "ident_b"], w=[PK[0]])
                k.op("act", lambda e, tl=tl: e.activation(out=aT[:, :, 2 + tl * 128:2 + (tl + 1) * 128],
                                                          in_=P0b.rearrange("p (a t) -> p a t", a=8), func=AF.Copy),
                     r=[PK[0]], w=["aT"])
            if getattr(pb, 'stop', 99) == 1:
                k.barrier()
                return
            has_l = r0 >= 2
            has_r = r0 + W + 2 <= Ts
            lrow = r0 - 2 if has_l else 0
            rrow = r0 + W if has_r else Ts - 2
            k.dma("sp", lambda e: e.dma_start(out=hx[0:2, :], in_=src[lrow:lrow + 2, :]), w=["hx"])
            k.dma("sp", lambda e: e.dma_start(out=hx[2:4, :], in_=src[rrow:rrow + 2, :]), w=["hx"])
            k.op("act", lambda e: e.activation(out=junk[0:4, :], in_=hx[:], func=AF.Square, accum_out=rstd[2][0:4, 0:1]),
                 r=["hx"], w=["junk", "rstd2"])
            k.op("act", lambda e: e.activation(out=rstd[2][0:4, 1:2], in_=rstd[2][0:4, 0:1], func=AF.Sqrt, bias=EPS, scale=1.0 / DM),
                 r=["rstd2"], w=["rstd2"])
            k.op("dve", lambda e: e.reciprocal(out=rstd[2][0:4, 2:3], in_=rstd[2][0:4, 1:2]), r=["rstd2"], w=["rstd2"])
            k.op("dve", lambda e: e.scalar_tensor_tensor(out=t1[0:4, :], in0=hx[:], scalar=rstd[2][0:4, 2:3], in1=mv[(s, 1)][0:4, :],
                                                         op0=ALU.mult, op1=ALU.mult), r=["hx", "rstd2", "modv"], w=["t1"])
            k.op("pool", lambda e: e.tensor_tensor(out=ah[:], in0=t1[0:4, :], in1=mv[(s, 0)][0:4, :], op=ALU.add),
                 r=["t1", "modv"], w=["ah"])
            for kc in range(8):
                k.op("pe", lambda e, kc=kc: e.transpose(out=P0b[:, kc * 4:(kc + 1) * 4], in_=ah[:, kc * 128:(kc + 1) * 128],
                                                        identity=C["ident_b"][0:4, 0:4]), r=["ah", "ident_b"], w=[PK[0]])
            hv = P0b[:, 0:32].rearrange("p (a t) -> p a t", a=8)
            if has_l:
                k.op("dve", lambda e: e.tensor_copy(out=aT[:, :, 0:2], in_=hv[:, :, 0:2]), r=[PK[0]], w=["aT"])
            else:
                k.op("dve", lambda e: e.memset(aT[:, :, 0:2], 0.0), r=[PK[0]], w=["aT"])
            if has_r:
                k.op("dve", lambda e: e.tensor_copy(out=aT[:, :, W + 2:W + 4], in_=hv[:, :, 2:4]), r=[PK[0]], w=["aT"])
            else:
                k.op("dve", lambda e: e.memset(aT[:, :, W + 2:W + 4], 0.0), r=[PK[0]], w=["aT"])
            if getattr(pb, 'stop', 99) == 2:
                k.barrier()
                return
            for c in range(4):
                bk = bank_rot[bri % 3]
                bri += 1
                for kc in range(8):
                    k.op("pe", lambda e, c=c, kc=kc, bk=bk: e.matmul(P[bk][:, 0:W], wq[:, kc, c * 128:(c + 1) * 128], aT[:, kc, 2:W + 2],
                                                                     start=(kc == 0), stop=(kc == 7)), r=["wq", "aT"], w=[PK[bk]])
                k.op("act", lambda e, c=c, bk=bk: e.activation(out=Qsil[:, c, :], in_=P[bk][:, 0:W], func=AF.Silu), r=[PK[bk]], w=["Qsil"])
            for c in range(4):
                bk = bank_rot[bri % 3]
                bri += 1
                for kc in range(8):
                    k.op("pe", lambda e, c=c, kc=kc, bk=bk: e.matmul(P[bk][:, 0:W], wf[:, kc, c * 128:(c + 1) * 128], aT[:, kc, 2:W + 2],
                                                                     start=(kc == 0), stop=(kc == 7)), r=["wf", "aT"], w=[PK[bk]])
                k.op("act", lambda e, c=c, bk=bk: e.activation(out=fg[:, c, :], in_=P[bk][:, 0:W], func=AF.Sigmoid), r=[PK[bk]], w=["fg"])
                k.op("dve", lambda e, c=c: e.tensor_scalar(out=fg[:, c, :], in0=fg[:, c, :], scalar1=oml[:, c:c + 1], scalar2=lbc[:, c:c + 1],
                                                           op0=ALU.mult, op1=ALU.add), r=["fg", "oml", "lbc"], w=["fg"])
            k.op("act", lambda e: e.activation(out=la[:], in_=fg[:], func=AF.Ln), r=["fg"], w=["la"])
            k.op("dve", lambda e: e.tensor_scalar(out=kk[:], in0=fg[:], scalar1=-1.0, scalar2=1.0, op0=ALU.mult, op1=ALU.add),
                 r=["fg"], w=["kk"])
            for c in range(8):
                bk = bank_rot[bri % 3]
                bri += 1
                for kc in range(8):
                    k.op("pe", lambda e, c=c, kc=kc, bk=bk: e.matmul(P[bk][:, 0:W + 4], wx[:, kc, c * 128:(c + 1) * 128], aT[:, kc, :],
                                                                     start=(kc == 0), stop=(kc == 7)), r=["wx", "aT"], w=[PK[bk]])
                k.op("act", lambda e, c=c, bk=bk: e.activation(out=u[:, c, :], in_=P[bk][:, 0:W + 4], func=AF.Copy), r=[PK[bk]], w=["u"])
            if getattr(pb, 'stop', 99) == 3:
                k.barrier()
                return
            for j in range(5):
                if j == 0:
                    k.op("dve", lambda e, j=j: e.tensor_tensor(out=acc[:], in0=u[:, :, j:j + W],
                                                               in1=cw[j][:].unsqueeze(2).to_broadcast([128, 8, W]), op=ALU.mult),
                         r=["u", "cw0"], w=["acc"])
                else:
                    k.op("pool", lambda e, j=j: e.tensor_tensor(out=tmp[:], in0=u[:, :, j:j + W],
                                                                in1=cw[j][:].unsqueeze(2).to_broadcast([128, 8, W]), op=ALU.mult),
                         r=["u", "cw%d" % j], w=["tmp"])
                    k.op("dve", lambda e: e.tensor_tensor(out=acc[:], in0=acc[:], in1=tmp[:], op=ALU.add), r=["acc", "tmp"], w=["acc"])
            k.op("dve", lambda e: e.tensor_tensor(out=acc[:], in0=acc[:], in1=cbc[:].unsqueeze(2).to_broadcast([128, 8, W]), op=ALU.add),
                 r=["acc", "cbc"], w=["acc"])
            k.op("act", lambda e: e.activation(out=xbc[:], in_=acc[:], func=AF.Silu), r=["acc"], w=["xbc"])
            if getattr(pb, 'stop', 99) == 4:
                k.barrier()
                return
            for tl in range(2):
                sl0 = 2 + tl * 128
                for kc in range(8):
                    k.op("pe", lambda e, kc=kc, sl0=sl0: e.matmul(P[6][:], aT[:, kc, sl0:sl0 + 128], wi[:, kc, :],
                                                                  start=(kc == 0), stop=(kc == 7)), r=["aT", "wi"], w=[PK[6]])
                k.op("act", lambda e, tl=tl: e.activation(out=vtok[:, tl, :], in_=P[6][:], func=AF.Copy), r=[PK[6]], w=["vtok"])
                for kc in range(8):
                    k.op("pe", lambda e, kc=kc, sl0=sl0: e.matmul(P[7][:, 0:8], aT[:, kc, sl0:sl0 + 128], wdt[:, kc, :],
                                                                  start=(kc == 0), stop=(kc == 7)), r=["aT", "wdt"], w=[PK[7]])
                k.op("dve", lambda e, tl=tl: e.tensor_tensor(out=dtr[:, tl, :], in0=P[7][:, 0:8], in1=dtb[:], op=ALU.add),
                     r=[PK[7], "dtb"], w=["dtr"])
            if getattr(pb, 'stop', 99) == 5:
                k.barrier()
                return
            tls = [0, 1] if dirn == 0 else [1, 0]
            for tl in tls:
                sl = slice(tl * 128, (tl + 1) * 128)
                grow = g0 + tl * 128
                for c in range(4):
                    k.op("dve", lambda e, c=c: e.tensor_tensor_scan(out=b2[:, c, :], data0=C["ones_f"][:, 0:128], data1=la[:, c, sl],
                                                                    initial=0.0, op0=ALU.mult, op1=ALU.add),
                         r=["la", "ones_f"], w=["b2"])
                if dirn == 0:
                    bsrc, bkey, last = b2, "b2", 127
                else:
                    k.op("dve", lambda e: e.tensor_tensor(out=bb[:], in0=la[:, :, sl], in1=b2[:], op=ALU.subtract), r=["la", "b2"], w=["bb"])
                    for c in range(4):
                        k.op("dve", lambda e, c=c: e.tensor_scalar(out=bb[:, c, :], in0=bb[:, c, :], scalar1=b2[:, c, 127:128], scalar2=None,
                                                                   op0=ALU.add), r=["bb", "b2"], w=["bb"])
                    bsrc, bkey, last = bb, "bb", 0
                k.op("dve", lambda e: e.tensor_copy(out=sc[:, 0, :], in_=bsrc[:, :, 64]), r=[bkey], w=["sc"])
                k.op("dve", lambda e: e.tensor_copy(out=sc[:, 2, :], in_=bsrc[:, :, last]), r=[bkey], w=["sc"])
                k.op("dve", lambda e: e.tensor_tensor(out=sc[:, 1, :], in0=sc[:, 2, :], in1=sc[:, 0, :], op=ALU.subtract), r=["sc"], w=["sc"])
                k.op("dve", lambda e: e.tensor_scalar(out=nb[:], in0=sc[:, 0, :], scalar1=-1.0, scalar2=None, op0=ALU.mult), r=["sc"], w=["nb"])
                k.op("act", lambda e: e.activation(out=esc[:], in_=sc[:], func=AF.Exp), r=["sc"], w=["esc"])
                for c in range(4):
                    k.op("act", lambda e, c=c: e.activation(out=Eq[:, c, :], in_=bsrc[:, c, :], func=AF.Exp, bias=nb[:, c:c + 1], scale=1.0),
                         r=[bkey, "nb"], w=["Eq"])
                    k.op("act", lambda e, c=c: e.activation(out=Ek[:, c, :], in_=bsrc[:, c, :], func=AF.Exp, bias=sc[:, 0, c:c + 1], scale=-1.0),
                         r=[bkey, "sc"], w=["Ek"])
                k.op("dve", lambda e: e.tensor_tensor(out=Qt[:], in0=Qsil[:, :, sl], in1=Eq[:], op=ALU.mult), r=["Qsil", "Eq"], w=["Qt"])
                k.op("dve", lambda e: e.tensor_tensor(out=Kt[:], in0=kk[:, :, sl], in1=Ek[:], op=ALU.mult), r=["kk", "Ek"], w=["Kt"])
                k.op("pool", lambda e: e.tensor_copy(out=KtA[0:64, :, :], in_=Kt[0:64, :, :]), r=["Kt"], w=["KtA"])
                k.op("pool", lambda e: e.tensor_copy(out=KtB[64:128, :, :], in_=Kt[64:128, :, :]), r=["Kt"], w=["KtB"])
                k.op("dve", lambda e: e.tensor_tensor(out=ShpA[0:64, :, :], in0=Sh[0:64, :, :],
                                                      in1=esc[0:64, 0, :].unsqueeze(2).to_broadcast([64, 4, 64]), op=ALU.mult),
                     r=["Sh", "esc"], w=["ShpA"])
                k.op("dve", lambda e: e.tensor_tensor(out=ShpB[64:128, :, :], in0=Sh[64:128, :, :],
                                                      in1=esc[64:128, 0, :].unsqueeze(2).to_broadcast([64, 4, 64]), op=ALU.mult),
                     r=["Sh", "esc"], w=["ShpB"])
                if getattr(pb, 'stop', 99) == 51:
                    k.barrier()
                    return
                for c in range(4):
                    k.op("pe", lambda e, c=c: e.transpose(out=P0b[:, c * 128:(c + 1) * 128], in_=Kt[:, c, :], identity=C["ident_b"][:]),
                         r=["Kt", "ident_b"], w=[PK[0]])
                k.op("act", lambda e: e.activation(out=Ktok[:].rearrange("p a t -> p (a t)"), in_=P0b[:, 0:512], func=AF.Copy),
                     r=[PK[0]], w=["Ktok"])
                if getattr(pb, 'stop', 99) == 52:
                    k.barrier()
                    return
                for h in range(8):
                    c, base = h // 2, (h % 2) * 64
                    bk = 1 + h // 4
                    Kz = KtA if h % 2 == 0 else KtB
                    k.op("pe", lambda e, h=h, c=c, bk=bk, Kz=Kz: e.matmul(
                        P[bk][:, (h % 4) * 128:(h % 4 + 1) * 128], Kz[:, c, :], Qt[:, c, :], start=True, stop=True),
                        r=["KtA", "KtB", "Qt"], w=[PK[bk]])
                for hh in range(2):
                    k.op("dve", lambda e, hh=hh: e.tensor_tensor(
                        out=attm[:, hh * 4:(hh + 1) * 4, :], in0=P[1 + hh][:].rearrange("p (a t) -> p a t", a=4),
                        in1=m_att_f[:].unsqueeze(1).to_broadcast([128, 4, 128]), op=ALU.mult), r=[PK[1 + hh], "masks"], w=["attm"])
                if getattr(pb, 'stop', 99) == 53:
                    k.barrier()
                    return
                for h in range(8):
                    c, base = h // 2, (h % 2) * 64
                    k.op("pe", lambda e, h=h: e.matmul(P[3][:, h * 64:(h + 1) * 64], attm[:, h, :], vtok[:, tl, h * 64:(h + 1) * 64],
                                                       start=True, stop=False), r=["attm", "vtok"], w=[PK[3]])
                    Sz = ShpA if h % 2 == 0 else ShpB
                    k.op("pe", lambda e, h=h, c=c, Sz=Sz: e.matmul(P[3][:, h * 64:(h + 1) * 64], Qt[:, c, :],
                                                                   Sz[:, c, :], start=False, stop=True),
                         r=["Qt", "ShpA", "ShpB"], w=[PK[3]])
                k.op("act", lambda e: e.activation(out=osb[:], in_=P[3][:], func=AF.Copy), r=[PK[3]], w=["osb"])
                k.dma("sp", lambda e, grow=grow: e.dma_start(out=D["oh"][dirn, grow:grow + 128, :], in_=osb[:]), r=["osb"], w=["oh"])
                if getattr(pb, 'stop', 99) == 54:
                    k.barrier()
                    return
                for c in range(4):
                    k.op("pe", lambda e, c=c: e.matmul(P[4][:, c * 128:(c + 1) * 128], Ktok[:, c, :], vtok[:, tl, c * 128:(c + 1) * 128],
                                                       start=True, stop=True), r=["Ktok", "vtok"], w=[PK[4]])
                p4v = P[4][:].rearrange("p (c x) -> p c x", c=4)
                k.op("dve", lambda e: e.tensor_tensor(out=tU[0:64, :, :], in0=p4v[0:64, :, 0:64],
                                                      in1=esc[0:64, 1, :].unsqueeze(2).to_broadcast([64, 4, 64]), op=ALU.mult),
                     r=[PK[4], "esc"], w=["tU"])
                k.op("dve", lambda e: e.tensor_tensor(out=tU[64:128, :, :], in0=p4v[64:128, :, 64:128],
                                                      in1=esc[64:128, 1, :].unsqueeze(2).to_broadcast([64, 4, 64]), op=ALU.mult),
                     r=[PK[4], "esc"], w=["tU"])
                k.op("dve", lambda e: e.tensor_tensor(out=Sh[:], in0=Sh[:], in1=esc[:, 2, :].unsqueeze(2).to_broadcast([128, 4, 64]), op=ALU.mult),
                     r=["Sh", "esc"], w=["Sh"])
                k.op("dve", lambda e: e.tensor_tensor(out=Sh[:], in0=Sh[:], in1=tU[:], op=ALU.add), r=["Sh", "tU"], w=["Sh"])
                if getattr(pb, 'stop', 99) == 6:
                    k.barrier()
                    return
                for c in range(4):
                    k.op("pe", lambda e, c=c: e.transpose(out=P0b[:, c * 128:(c + 1) * 128], in_=xbc[:, c, sl], identity=C["ident_b"][:]),
                         r=["xbc", "ident_b"], w=[PK[0]])
                for g in range(2):
                    k.op("pe", lambda e, g=g: e.transpose(out=P0b[:, 512 + g * 128:512 + (g + 1) * 128], in_=xbc[:, 4 + g, sl],
                                                          identity=C["ident_b"][:]), r=["xbc", "ident_b"], w=[PK[0]])
                k.op("act", lambda e: e.activation(out=xmt[:].rearrange("p a v -> p (a v)"), in_=P0b[:, 0:512], func=AF.Copy),
                     r=[PK[0]], w=["xmt"])
                k.op("act", lambda e: e.activation(out=Btok[:].rearrange("p a v -> p (a v)"), in_=P0b[:, 512:768], func=AF.Copy),
                     r=[PK[0]], w=["Btok"])
                k.op("act", lambda e: e.activation(out=dts[:, 3, :], in_=dtr[:, tl, :], func=AF.Exp), r=["dtr"], w=["dts3"])
                k.op("act", lambda e: e.activation(out=dts[:, 0, :], in_=dts[:, 3, :], func=AF.Ln, bias=1.0, scale=1.0), r=["dts3"], w=["dts0"])
                k.op("dve", lambda e: e.tensor_tensor(out=dts[:, 1, :], in0=dts[:, 0, :], in1=negA[:], op=ALU.mult), r=["dts0", "negA"], w=["dts1"])
                if getattr(pb, 'stop', 99) == 7:
                    k.barrier()
                    return
                k.op("pe", lambda e: e.matmul(P[7][:, 0:8], m_cum[:], dts[:, 1, :], start=True, stop=True), r=["dts1", "masks"], w=[PK[7]])
                k.op("pe", lambda e: e.matmul(P[7][:, 8:16], m_wend[:], dts[:, 1, :], start=True, stop=True), r=["dts1", "masks"], w=[PK[7]])
                k.op("pe", lambda e: e.matmul(P[7][:, 16:24], C["ones_f"][:], dts[:, 1, :], start=True, stop=True), r=["dts1", "ones_f"], w=[PK[7]])
                k.op("act", lambda e: e.activation(out=ec[:], in_=P[7][:, 0:24], func=AF.Exp), r=[PK[7]], w=["ec"])
                k.op("dve", lambda e: e.tensor_tensor(out=LH[:], in0=m_lh[:].unsqueeze(1).to_broadcast([128, 8, 128]),
                                                      in1=dts[:, 1, :].unsqueeze(2).to_broadcast([128, 8, 128]), op=ALU.mult),
                     r=["dts1", "masks"], w=["LH"])
                for h in range(8):
                    bk = 1 + h // 4
                    k.op("pe", lambda e, h=h, bk=bk: e.matmul(P[bk][:, (h % 4) * 128:(h % 4 + 1) * 128], LH[:, h, :], m_rhs[:],
                                                              start=True, stop=True), r=["LH", "masks"], w=[PK[bk]])
                for hh in range(2):
                    k.op("act", lambda e, hh=hh: e.activation(out=Em[:, hh * 4:(hh + 1) * 4, :].rearrange("p a t -> p (a t)"), in_=P[1 + hh][:],
                                                              func=AF.Exp), r=[PK[1 + hh]], w=["Em"])
                if getattr(pb, 'stop', 99) == 8:
                    k.barrier()
                    return
                for g in range(2):
                    k.op("pe", lambda e, g=g: e.matmul(P[3][:, g * 128:(g + 1) * 128], xbc[:, 4 + g, sl], xbc[:, 6 + g, sl], start=True, stop=True),
                         r=["xbc"], w=[PK[3]])
                k.op("dve", lambda e: e.tensor_tensor(out=cbm[:], in0=P[3][:, 0:256].rearrange("p (a t) -> p a t", a=2),
                                                      in1=m_att_f[:].unsqueeze(1).to_broadcast([128, 2, 128]), op=ALU.mult),
                     r=[PK[3], "masks"], w=["cbm"])
                for g in range(2):
                    k.op("dve", lambda e, g=g: e.tensor_tensor(out=Mm[:, g * 4:(g + 1) * 4, :], in0=Em[:, g * 4:(g + 1) * 4, :],
                                                               in1=cbm[:, g, :].unsqueeze(1).to_broadcast([128, 4, 128]), op=ALU.mult),
                         r=["Em", "cbm"], w=["Mm"])
                k.op("dve", lambda e: e.tensor_tensor(out=dts[:, 2, :], in0=dts[:, 0, :], in1=ec[:, 8:16], op=ALU.mult), r=["dts0", "ec"], w=["dts2"])
                k.op("pool", lambda e: e.tensor_tensor(out=xdt[:], in0=xmt[:], in1=dts[:, 0, :].unsqueeze(2).to_broadcast([128, 8, 64]), op=ALU.mult),
                     r=["xmt", "dts0"], w=["xdt"])
                k.op("pool", lambda e: e.tensor_tensor(out=xdtw[:], in0=xmt[:], in1=dts[:, 2, :].unsqueeze(2).to_broadcast([128, 8, 64]), op=ALU.mult),
                     r=["xmt", "dts2"], w=["xdtw"])
                if getattr(pb, 'stop', 99) == 9:
                    k.barrier()
                    return
                for h in range(8):
                    k.op("pe", lambda e, h=h: e.matmul(P[4][:, h * 64:(h + 1) * 64], Mm[:, h, :], xdt[:, h, :], start=True, stop=True),
                         r=["Mm", "xdt"], w=[PK[4]])
                for g in range(2):
                    k.op("pe", lambda e, g=g: e.matmul(P[5][:, g * 256:(g + 1) * 256], xbc[:, 6 + g, sl],
                                                       Ssb[:, g * 4:(g + 1) * 4, :].rearrange("p a v -> p (a v)"), start=True, stop=True),
                         r=["xbc", "Ssb"], w=[PK[5]])
                k.op("dve", lambda e: e.tensor_tensor(out=yi[:], in0=P[5][:].rearrange("p (a v) -> p a v", a=8),
                                                      in1=ec[:, 0:8].unsqueeze(2).to_broadcast([128, 8, 64]), op=ALU.mult),
                     r=[PK[5], "ec"], w=["yi"])
                k.op("dve", lambda e: e.tensor_tensor(out=ysb[:], in0=P[4][:].rearrange("p (a v) -> p a v", a=8), in1=yi[:], op=ALU.add),
                     r=[PK[4], "yi"], w=["ysb"])
                if dirn == 0:
                    k.op("pool", lambda e: e.tensor_tensor(out=yi[:], in0=xmt[:], in1=dsk[:].unsqueeze(2).to_broadcast([128, 8, 64]), op=ALU.mult),
                         r=["xmt", "dsk", "ysb"], w=["yi"])
                    k.op("dve", lambda e: e.tensor_tensor(out=ysb[:], in0=ysb[:], in1=yi[:], op=ALU.add), r=["ysb", "yi"], w=["ysb"])
                k.dma("sp", lambda e, grow=grow: e.dma_start(out=D["ys"][dirn, grow:grow + 128, :], in_=ysb[:].rearrange("p a v -> p (a v)")),
                      r=["ysb"], w=["ys"])
                for g in range(2):
                    k.op("pe", lambda e, g=g: e.matmul(P[6][:, g * 256:(g + 1) * 256], Btok[:, g, :],
                                                       xdtw[:, g * 4:(g + 1) * 4, :].rearrange("p a v -> p (a v)"), start=True, stop=True),
                         r=["Btok", "xdtw"], w=[PK[6]])
                k.op("dve", lambda e: e.tensor_tensor(out=Ss[:], in0=Ss[:], in1=ec[:, 16:24].unsqueeze(2).to_broadcast([128, 8, 64]), op=ALU.mult),
                     r=["Ss", "ec"], w=["Ss"])
                k.op("dve", lambda e: e.tensor_tensor(out=Ss[:], in0=Ss[:], in1=P[6][:].rearrange("p (a v) -> p a v", a=8), op=ALU.add),
                     r=["Ss", PK[6]], w=["Ss"])
                k.op("act", lambda e: e.activation(out=Ssb[:], in_=Ss[:], func=AF.Copy), r=["Ss"], w=["Ssb"])
        k.barrier()


def phase_mix_merge(pb, C, ntiles=66):
    k, D, nc = pb.k, pb.D, pb.nc
    with ExitStack() as es:
        P = [pb.ps(es, "P%d" % i, [128, 512], F32) for i in range(8)]
        PK = ["P%d" % i for i in range(8)]
        P0b = P[0][:].bitcast(BF16)
        mv = load_modv(pb, es, 0, (0, 1, 2))
        stg = pb.sb(es, "wstg", [128, 512], F32)
        wg = load_w_bf16(pb, es, "wg", D["w_in"][:, 2048:2560], 8, 512, stg, "wstg")
        wz = load_w_bf16(pb, es, "wz", D["w_in"][:, 2560:3072], 8, 512, stg, "wstg")
        wo = load_w_bf16(pb, es, "wo", D["w_out_rec"], 8, 1024, stg, "wstg")
        hg = pb.sb(es, "hg", [128, 512], F32)
        mn = pb.sb(es, "mn", [128, 512], F32)
        k.dma("sp", lambda e: e.dma_start(out=hg[:], in_=D["hgrn_norm"].to_broadcast([128, 512])), w=["gains"])
        k.dma("sp", lambda e: e.dma_start(out=mn[:], in_=D["mamba_norm"].to_broadcast([128, 512])), w=["gains"])
        xt = [pb.sb(es, "xt%d" % i, [128, 1024], F32) for i in range(2)]
        an = [pb.sb(es, "an%d" % i, [128, 1024], BF16) for i in range(2)]
        junk = pb.sb(es, "junk", [128, 1024], BF16)
        t1 = pb.sb(es, "t1", [128, 1024], F32)
        rstd = [pb.sb(es, "rstd%d" % i, [128, 4], F32) for i in range(2)]
        aT = pb.sb(es, "aT", [128, 8, 128], BF16)
        sg = pb.sb(es, "sg", [128, 512], F32)
        sz = pb.sb(es, "sz", [128, 512], F32)
        o0 = [pb.sb(es, "o0%d" % i, [128, 512], F32) for i in range(2)]
        o1 = [pb.sb(es, "o1%d" % i, [128, 512], F32) for i in range(2)]
        y0 = [pb.sb(es, "y0%d" % i, [128, 512], F32) for i in range(2)]
        y1 = [pb.sb(es, "y1%d" % i, [128, 512], F32) for i in range(2)]
        sq = pb.sb(es, "sq", [128, 512], F32)
        st8 = pb.sb(es, "st8", [128, 3, 8], F32)
        rs2 = pb.sb(es, "rs2", [128, 4], F32)
        cat = pb.sb(es, "cat", [128, 1024], BF16)
        catT = pb.sb(es, "catT", [128, 8, 128], BF16)
        xo = [pb.sb(es, "xo%d" % i, [128, 1024], F32) for i in range(2)]
        for j in range(ntiles):
            b = j % 2
            if j < 2:
                s, src, rr, grow = 1, D["ctx"], j * 128, T_LAT + j * 128
            else:
                s, src, rr, grow = 0, D["x"], (j - 2) * 128, (j - 2) * 128
            k.dma("sp", lambda e: e.dma_start(out=xt[b][:], in_=src[rr:rr + 128, :]), w=["xt%d" % b])
            k.dma("sp", lambda e: e.dma_start(out=o0[b][:], in_=D["oh"][0, grow:grow + 128, :]), r=["oh"], w=["o0%d" % b])
            k.dma("sp", lambda e: e.dma_start(out=o1[b][:], in_=D["oh"][1, grow:grow + 128, :]), r=["oh"], w=["o1%d" % b])
            k.dma("sp", lambda e: e.dma_start(out=y0[b][:], in_=D["ys"][0, grow:grow + 128, :]), r=["ys"], w=["y0%d" % b])
            k.dma("sp", lambda e: e.dma_start(out=y1[b][:], in_=D["ys"][1, grow:grow + 128, :]), r=["ys"], w=["y1%d" % b])
            norm_mod_tile(pb, xt[b], "xt%d" % b, rstd[b], "rstd%d" % b, junk, t1, mv[(s, 1)], mv[(s, 0)], an[b], "an%d" % b)
            for kc in range(8):
                k.op("pe", lambda e, kc=kc: e.transpose(out=P0b[:, kc * 128:(kc + 1) * 128], in_=an[b][:, kc * 128:(kc + 1) * 128],
                                                        identity=C["ident_b"][:]), r=["an%d" % b, "ident_b"], w=[PK[0]])
            k.op("act", lambda e: e.activation(out=aT[:].rearrange("p a t -> p (a t)"), in_=P0b, func=AF.Copy), r=[PK[0]], w=["aT"])
            for kc in range(8):
                k.op("pe", lambda e, kc=kc: e.matmul(P[1][:], aT[:, kc, :], wg[:, kc, :], start=(kc == 0), stop=(kc == 7)),
                     r=["aT", "wg"], w=[PK[1]])
            k.op("act", lambda e: e.activation(out=sg[:], in_=P[1][:], func=AF.Sigmoid), r=[PK[1]], w=["sg"])
            for kc in range(8):
                k.op("pe", lambda e, kc=kc: e.matmul(P[2][:], aT[:, kc, :], wz[:, kc, :], start=(kc == 0), stop=(kc == 7)),
                     r=["aT", "wz"], w=[PK[2]])
            k.op("act", lambda e: e.activation(out=sz[:], in_=P[2][:], func=AF.Silu), r=[PK[2]], w=["sz"])
            ok0, ok1, yk0, yk1 = "o0%d" % b, "o1%d" % b, "y0%d" % b, "y1%d" % b
            k.op("dve", lambda e: e.tensor_tensor(out=o0[b][:], in0=o0[b][:], in1=o1[b][:], op=ALU.add), r=[ok0, ok1], w=[ok0])
            k.op("pool", lambda e: e.tensor_tensor(out=sq[:], in0=o0[b][:], in1=o0[b][:], op=ALU.mult), r=[ok0], w=["sq"])
            k.op("dve", lambda e: e.tensor_reduce(out=st8[:, 0, :], in_=sq[:].rearrange("p (a v) -> p a v", a=8), axis=AX.X, op=ALU.add),
                 r=["sq"], w=["st8"])
            k.op("act", lambda e: e.activation(out=st8[:, 1, :], in_=st8[:, 0, :], func=AF.Sqrt, bias=EPS, scale=1.0 / 64), r=["st8"], w=["st8"])
            k.op("dve", lambda e: e.reciprocal(out=st8[:, 2, :], in_=st8[:, 1, :]), r=["st8"], w=["st8"])
            k.op("dve", lambda e: e.tensor_tensor(out=o0[b][:].rearrange("p (a v) -> p a v", a=8), in0=o0[b][:].rearrange("p (a v) -> p a v", a=8),
                                                  in1=st8[:, 2, :].unsqueeze(2).to_broadcast([128, 8, 64]), op=ALU.mult), r=[ok0, "st8"], w=[ok0])
            k.op("pool", lambda e: e.tensor_tensor(out=o0[b][:], in0=o0[b][:], in1=hg[:], op=ALU.mult), r=[ok0, "gains"], w=[ok0])
            k.op("dve", lambda e: e.tensor_tensor(out=cat[:, 0:512], in0=o0[b][:], in1=sg[:], op=ALU.mult), r=[ok0, "sg"], w=["cat"])
            k.op("pool", lambda e: e.tensor_tensor(out=y0[b][:], in0=y0[b][:], in1=y1[b][:], op=ALU.add), r=[yk0, yk1], w=[yk0])
            k.op("dve", lambda e: e.tensor_tensor(out=y0[b][:], in0=y0[b][:], in1=sz[:], op=ALU.mult), r=[yk0, "sz"], w=[yk0])
            k.op("act", lambda e: e.activation(out=junk[:, 0:512], in_=y0[b][:], func=AF.Square, accum_out=rs2[:, 0:1]), r=[yk0], w=["junk", "rs2"])
            k.op("act", lambda e: e.activation(out=rs2[:, 1:2], in_=rs2[:, 0:1], func=AF.Sqrt, bias=EPS, scale=1.0 / 512), r=["rs2"], w=["rs2"])
            k.op("dve", lambda e: e.reciprocal(out=rs2[:, 2:3], in_=rs2[:, 1:2]), r=["rs2"], w=["rs2"])
            k.op("dve", lambda e: e.scalar_tensor_tensor(out=cat[:, 512:1024], in0=y0[b][:], scalar=rs2[:, 2:3], in1=mn[:],
                                                         op0=ALU.mult, op1=ALU.mult), r=[yk0, "rs2", "gains"], w=["cat"])
            for c in range(8):
                k.op("pe", lambda e, c=c: e.transpose(out=P0b[:, c * 128:(c + 1) * 128], in_=cat[:, c * 128:(c + 1) * 128],
                                                      identity=C["ident_b"][:]), r=["cat", "ident_b"], w=[PK[0]])
            k.op("act", lambda e: e.activation(out=catT[:].rearrange("p a t -> p (a t)"), in_=P0b, func=AF.Copy), r=[PK[0]], w=["catT"])
            for dh in range(2):
                for c in range(8):
                    k.op("pe", lambda e, c=c, dh=dh: e.matmul(P[3 + dh][:], catT[:, c, :], wo[:, c, dh * 512:(dh + 1) * 512],
                                                              start=(c == 0), stop=(c == 7)), r=["catT", "wo"], w=[PK[3 + dh]])
                k.op("dve", lambda e, dh=dh: e.tensor_tensor(out=xo[b][:, dh * 512:(dh + 1) * 512], in0=P[3 + dh][:],
                                                             in1=mv[(s, 2)][:, dh * 512:(dh + 1) * 512], op=ALU.mult),
                     r=[PK[3 + dh], "modv"], w=["xo%d" % b])
            k.op("pool", lambda e: e.tensor_tensor(out=xo[b][:], in0=xo[b][:], in1=xt[b][:], op=ALU.add), r=["xo%d" % b, "xt%d" % b], w=["xo%d" % b])
            k.dma("sp", lambda e: e.dma_start(out=D["xres"][grow:grow + 128, :], in_=xo[b][:]), r=["xo%d" % b], w=["xres"])
        k.barrier()


def declare_all(pb):
    pb.din("x", [T_LAT, DM]); pb.din("ctx", [T_CTX, DM]); pb.din("c", [1, DM]); pb.din("c_ctx", [1, DM])
    pb.din("w_mod", [2, DM, 6 * DM]); pb.din("b_mod", [2, 6 * DM]); pb.din("norm_mix", [2, DM]); pb.din("norm_ffn", [2, DM])
    pb.din("norm_out", [1, DM])
    pb.din("w_in", [DM, 4112]); pb.din("w_out_rec", [DM, DM]); pb.din("conv_w", [5, DM]); pb.din("conv_b", [1, DM])
    pb.din("lb_gamma", [2, 2, 512]); pb.din("dt_bias", [2, 8]); pb.din("a_log", [2, 8]); pb.din("d_skip", [1, 8])
    pb.din("hgrn_norm", [1, 512]); pb.din("mamba_norm", [1, 512])
    pb.din("w_dq", [DM, 384]); pb.din("q_norm", [1, 384]); pb.din("w_uq_r", [384, 2048]); pb.din("w_dkv", [DM, 256])
    pb.din("kv_norm", [1, 256]); pb.din("w_ukv_r", [256, 2048]); pb.din("w_kr_r", [DM, 128]); pb.din("w_o", [DM, DM])
    pb.din("w_router", [2, DM, NE]); pb.din("w_gate", [2, NE, DM, DM]); pb.din("w_up", [2, NE, DM, DM]); pb.din("w_down", [2, NE, DM, DM])
    for nm, v in host_consts().items():
        pb.din("c_" + nm, v.shape)
    for nm, v in attn_host_consts().items():
        pb.din("c_" + nm, v.shape)
    pb.dscr("xres", [NROWS, DM]); pb.dscr("hn", [NROWS, DM], BF16); pb.dscr("aff", [NROWS, 16]); pb.dscr("modrep", [2, 2, 6, 128, DM])
    pb.dscr("oh", [2, NTOK, 512]); pb.dscr("ys", [2, NTOK, 512])
    pb.dscr("KT", [NH, 128, NTOK], BF16); pb.dscr("KRT", [64, NTOK], BF16); pb.dscr("Vd", [NTOK, 1024], BF16)
    pb.dscr("QT", [NH, 128, T_LAT], BF16); pb.dscr("QRT", [NH, 64, T_LAT], BF16); pb.dscr("Od", [T_LAT, 1024], BF16)


def make_inputs(inp, b):
    f = lambda a: np.ascontiguousarray(a, dtype=np.float32)
    w_uq_r, w_ukv_r, w_kr_r = attn_layout_weights(inp["w_uq"][0], inp["w_ukv"][0], inp["w_kr"][0])
    im = {"x": f(inp["x"][b]), "ctx": f(inp["ctx"][b]), "c": f(inp["c"][b:b + 1]), "c_ctx": f(inp["c_ctx"][None, :]),
          "w_mod": f(inp["w_mod"]), "b_mod": f(inp["b_mod"]), "norm_mix": f(inp["norm_mix"]), "norm_ffn": f(inp["norm_ffn"]),
          "norm_out": f(inp["norm_out"][None, :]),
          "w_in": f(inp["w_in"][0]), "w_out_rec": f(inp["w_out_rec"][0]), "conv_w": f(inp["conv_w"][0]), "conv_b": f(inp["conv_b"]),
          "lb_gamma": f(inp["lb_gamma"]), "dt_bias": f(inp["dt_bias"][0]), "a_log": f(inp["a_log"][0]), "d_skip": f(inp["d_skip"]),
          "hgrn_norm": f(inp["hgrn_norm"]), "mamba_norm": f(inp["mamba_norm"]),
          "w_dq": f(inp["w_dq"][0]), "q_norm": f(inp["q_norm"]), "w_uq_r": f(w_uq_r), "w_dkv": f(inp["w_dkv"][0]),
          "kv_norm": f(inp["kv_norm"]), "w_ukv_r": f(w_ukv_r), "w_kr_r": f(w_kr_r), "w_o": f(inp["w_o"][0]),
          "w_router": f(inp["w_router"]), "w_gate": f(inp["w_gate"]), "w_up": f(inp["w_up"]), "w_down": f(inp["w_down"])}
    for nm, v in host_consts().items():
        im["c_" + nm] = v
    for nm, v in attn_host_consts().items():
        im["c_" + nm] = v
    return im


def build_program(phases, dbg=False, opts=None):
    opts = opts or {}
    nc = bass.Bass("TRN2", target_bir_lowering=False)
    pb = PB(nc)
    declare_all(pb)
    pb.dout("out", [T_LAT, DM])
    if dbg:
        pb.dout("dbg", [NTOK, DM])
    with ExitStack() as es:
        C = load_consts(pb, es)
        phase_init(pb, copy_x=("init" in phases))
        phase_mod(pb, C)
        if "mixf" in phases:
            phase_mix_pass(pb, C, 0, **opts.get("mix", {}))
        if "mixb" in phases:
            phase_mix_pass(pb, C, 1, **opts.get("mix", {}))
        if "mixm" in phases:
            phase_mix_merge(pb, C, **opts.get("merge", {}))
        if "moe0" in phases:
            phase_moe(pb, C, 0)
        if "attn_pre" in phases:
            phase_attn_pre(pb, C)
        if "attn_main" in phases:
            phase_attn_main(pb, C, **opts.get("attn", {}))
        if "attn_post" in phases:
            phase_attn_post(pb, C)
        if "moe1" in phases:
            phase_moe(pb, C, 1)
        if "final" in phases:
            phase_final(pb, C)
        k = pb.k
        if dbg:
            for i in range(8):
                k.dma("sp", lambda e, i=i: e.dma_start(out=pb.D["dbg"][i * 1056:(i + 1) * 1056, :], in_=pb.D["xres"][i * 1056:(i + 1) * 1056, :]),
                      r=["xres"], w=["dbg"])
        k.finish()
    return nc, pb


ALL_PHASES = ["mixf", "mixb", "mixm", "moe0", "attn_pre", "attn_main", "attn_post", "moe1", "final"]


def kernel(**inputs):
    from concourse.bass_utils import run_bass_kernel_spmd
    inp = {k: np.asarray(v) for k, v in inputs.items()}
    nb = inp["x"].shape[0]
    nc, pb = build_program(ALL_PHASES)
    in_maps = [make_inputs(inp, b) for b in range(nb)]
    res = run_bass_kernel_spmd(nc, in_maps, core_ids=list(range(nb)))
    out = np.stack([np.asarray(res.results[b]["out"]) for b in range(nb)], axis=0)
    return out.astype(np.float32)
```

```python
import numpy as np
import concourse.bass as bass
import concourse.mybir as mybir

F32 = mybir.dt.float32
BF16 = mybir.dt.bfloat16
I32 = mybir.dt.int32
U32 = mybir.dt.uint32
AF = mybir.ActivationFunctionType
ALU = mybir.AluOpType
AX = mybir.AxisListType


class KF:
    NDS = 8

    def __init__(self, nc):
        self.nc = nc
        self.eng = {"pe": nc.tensor, "dve": nc.vector, "act": nc.scalar, "pool": nc.gpsimd, "sp": nc.sync}
        self.csem = {}
        self.ccnt = {}
        for e in self.eng:
            self.csem[e] = nc.alloc_semaphore("cs_" + e)
            self.ccnt[e] = 0
        self.dsem = {}
        self.dval = {}
        self.drot = {}
        for q in ("sp", "pool", "act"):
            self.dsem[q] = [nc.alloc_semaphore("ds_%s%d" % (q, i)) for i in range(self.NDS)]
            self.dval[q] = [0] * self.NDS
            self.drot[q] = 0
        self.waited = {e: {} for e in self.eng}
        self.lastw = {}
        self.readers = {}
        self.same_eng_sync = {"pe": False, "dve": True, "act": True, "pool": True, "sp": True}
        self.nins = 0

    def _wait(self, e, tok):
        sem, val, src = tok
        if src == e and not self.same_eng_sync[e]:
            return
        key = id(sem)
        if self.waited[e].get(key, 0) >= val:
            return
        self.eng[e].wait_ge(sem, val)
        self.waited[e][key] = val
        self.nins += 1

    def _deps(self, e, r, w):
        for k in r:
            t = self.lastw.get(k)
            if t is not None:
                self._wait(e, t)
        for k in w:
            t = self.lastw.get(k)
            if t is not None:
                self._wait(e, t)
            for t in self.readers.get(k, ()):
                self._wait(e, t)

    def _commit(self, tok, r, w):
        for k in w:
            self.lastw[k] = tok
            self.readers[k] = []
        for k in r:
            self.readers.setdefault(k, []).append(tok)

    def op(self, e, fn, r=(), w=()):
        r = [x for x in r if x is not None]
        w = [x for x in w if x is not None]
        self._deps(e, r, w)
        ins = fn(self.eng[e])
        self.ccnt[e] += 1
        ins.then_inc(self.csem[e], 1)
        tok = (self.csem[e], self.ccnt[e], e)
        self._commit(tok, r, w)
        self.nins += 1
        return tok

    def dma(self, q, fn, r=(), w=()):
        r = [x for x in r if x is not None]
        w = [x for x in w if x is not None]
        i = self.drot[q]
        self.drot[q] = (i + 1) % self.NDS
        sem = self.dsem[q][i]
        self._wait(q, (sem, self.dval[q][i], None))
        self._deps(q, r, w)
        ins = fn(self.eng[q])
        self.dval[q][i] += 16
        ins.then_inc(sem, 16)
        tok = (sem, self.dval[q][i], None)
        self._commit(tok, r, w)
        self.nins += 1
        return tok

    def barrier(self):
        toks = []
        for e in self.eng:
            if self.ccnt[e] > 0:
                toks.append((self.csem[e], self.ccnt[e], None))
        for q in self.dsem:
            for i in range(self.NDS):
                if self.dval[q][i] > 0:
                    toks.append((self.dsem[q][i], self.dval[q][i], None))
        for e in self.eng:
            for t in toks:
                if t[0] is self.csem[e]:
                    continue
                self._wait(e, t)
        self.lastw = {}
        self.readers = {}

    def finish(self):
        for e in self.eng:
            if e != "sp" and self.ccnt[e] > 0:
                self._wait("sp", (self.csem[e], self.ccnt[e], None))
        for q in self.dsem:
            for i in range(self.NDS):
                if self.dval[q][i] > 0:
                    self._wait("sp", (self.dsem[q][i], self.dval[q][i], None))

from contextlib import ExitStack

T_LAT = 8192
T_CTX = 256
NTOK = T_LAT + T_CTX
NROWS = NTOK + 128
DM = 1024
EPS = 1e-6
NE = 16


def host_consts():
    c = {}
    c["ident"] = np.eye(128, dtype=np.float32)
    p = np.arange(128)
    c["triu_incl"] = (p[:, None] <= p[None, :]).astype(np.float32)
    c["triu_strict"] = (p[:, None] < p[None, :]).astype(np.float32)
    c["tril_incl"] = (p[:, None] >= p[None, :]).astype(np.float32)
    c["tril_strict"] = (p[:, None] > p[None, :]).astype(np.float32)
    c["iota_q"] = np.tile(np.arange(1024, dtype=np.float32)[None, :], (128, 1))
    q = np.arange(128, dtype=np.float32)
    c["ctxadd"] = np.tile((T_LAT + np.maximum(q - 32, 0))[None, :], (128, 1)).astype(np.float32)
    return c


class PB:
    def __init__(self, nc):
        self.nc = nc
        self.k = KF(nc)
        self.D = {}
        self.uid = 0

    def din(self, name, shape, dt=F32):
        self.D[name] = self.nc.dram_tensor(name, list(shape), dt, kind="ExternalInput").ap()
        return self.D[name]

    def dout(self, name, shape, dt=F32):
        self.D[name] = self.nc.dram_tensor(name, list(shape), dt, kind="ExternalOutput").ap()
        return self.D[name]

    def dscr(self, name, shape, dt=F32):
        self.D[name] = self.nc.dram_tensor(name, list(shape), dt, kind="Internal").ap()
        return self.D[name]

    def sb(self, es, name, shape, dt):
        self.uid += 1
        return es.enter_context(self.nc.sbuf_tensor("%s_%d" % (name, self.uid), list(shape), dt))

    def ps(self, es, name, shape, dt):
        self.uid += 1
        return es.enter_context(self.nc.psum_tensor("%s_%d" % (name, self.uid), list(shape), dt))


def load_consts(pb, es):
    k, D = pb.k, pb.D
    C = {}
    for nm in ("ident", "triu_incl", "triu_strict", "tril_incl", "tril_strict"):
        f = pb.sb(es, nm + "_f", [128, 128], F32)
        b = pb.sb(es, nm + "_b", [128, 128], BF16)
        k.dma("sp", lambda e, f=f, nm=nm: e.dma_start(out=f[:], in_=D["c_" + nm]), w=[nm + "_f"])
        k.op("dve", lambda e, f=f, b=b: e.tensor_copy(out=b[:], in_=f[:]), r=[nm + "_f"], w=[nm + "_b"])
        C[nm + "_f"] = f
        C[nm + "_b"] = b
    ones_f = pb.sb(es, "ones_f", [128, 128], F32)
    ones_b = pb.sb(es, "ones_b", [128, 128], BF16)
    k.op("dve", lambda e: e.memset(ones_f[:], 1.0), w=["ones_f"])
    k.op("dve", lambda e: e.memset(ones_b[:], 1.0), w=["ones_b"])
    C["ones_f"] = ones_f
    C["ones_b"] = ones_b
    return C


def phase_init(pb, copy_x=True):
    k, D = pb.k, pb.D
    with ExitStack() as es:
        z = pb.sb(es, "zt", [128, 1024], F32)
        zb = pb.sb(es, "zb", [128, 1024], BF16)
        k.op("dve", lambda e: e.memset(z[:], 0.0), w=["zt"])
        k.op("dve", lambda e: e.memset(zb[:], 0.0), w=["zb"])
        if copy_x:
            for i in range(8):
                k.dma("sp", lambda e, i=i: e.dma_start(out=D["xres"][i * 1024:(i + 1) * 1024, :],
                                                       in_=D["x"][i * 1024:(i + 1) * 1024, :]), w=["xres"])
            k.dma("sp", lambda e: e.dma_start(out=D["xres"][T_LAT:NTOK, :], in_=D["ctx"]), w=["xres"])
        k.dma("sp", lambda e: e.dma_start(out=D["xres"][NTOK:NROWS, :], in_=z[:]), r=["zt"], w=["xres"])
        k.dma("sp", lambda e: e.dma_start(out=D["hn"][NTOK:NROWS, :], in_=zb[:]), r=["zb"], w=["hn"])
        k.dma("sp", lambda e: e.dma_start(out=D["aff"][NTOK:NROWS, :], in_=z[:, 0:16]), r=["zt"], w=["aff"])
        k.barrier()


def phase_mod(pb, C):
    k, D, nc = pb.k, pb.D, pb.nc
    with ExitStack() as es:
        s8 = pb.sb(es, "s8", [8, 2, 128], F32)
        scol = pb.sb(es, "scol", [128, 2, 8], F32)
        srep = pb.sb(es, "srep", [128, 2, 8, 128], F32)
        ps_t = pb.ps(es, "ps_t", [128, 512], F32)
        k.dma("sp", lambda e: e.dma_start(out=s8[:, 0, :], in_=D["c"].rearrange("o (a b) -> (o a) b", a=8)), w=["s8"])
        k.dma("sp", lambda e: e.dma_start(out=s8[:, 1, :], in_=D["c_ctx"].rearrange("o (a b) -> (o a) b", a=8)), w=["s8"])
        k.op("act", lambda e: e.activation(out=s8[:], in_=s8[:], func=AF.Silu), r=["s8"], w=["s8"])
        for s in range(2):
            k.op("pe", lambda e, s=s: e.transpose(out=ps_t[:, s * 8:(s + 1) * 8], in_=s8[:, s, :],
                                                  identity=C["ident_f"][0:8, 0:8]), r=["s8", "ident_f"], w=["ps_t"])
        k.op("dve", lambda e: e.tensor_copy(out=scol[:].rearrange("p s c -> p (s c)"), in_=ps_t[:, 0:16]),
             r=["ps_t"], w=["scol"])
        for s in range(2):
            k.op("dve", lambda e, s=s: e.tensor_copy(out=srep[:, s, :, :],
                                                     in_=scol[:, s, :].unsqueeze(2).to_broadcast([128, 8, 128])),
                 r=["scol"], w=["srep"])
        wm = [pb.sb(es, "wm%d" % i, [128, 8, 512], F32) for i in range(2)]
        bm = [pb.sb(es, "bm%d" % i, [128, 512], F32) for i in range(2)]
        gn = [pb.sb(es, "gn%d" % i, [128, 512], F32) for i in range(2)]
        ob = [pb.sb(es, "ob%d" % i, [128, 512], F32) for i in range(4)]
        psm = [pb.ps(es, "psm%d" % i, [128, 512], F32) for i in range(2)]
        it = 0
        oi = 0
        for l in range(2):
            for ncn in range(12):
                b = it % 2
                it += 1
                n0 = ncn * 512
                slot = ncn // 2
                half = ncn % 2
                k.dma("sp", lambda e, b=b, l=l, n0=n0: e.dma_start(
                    out=wm[b][:], in_=D["w_mod"][l, :, n0:n0 + 512].rearrange("(kc p) n -> p kc n", p=128)),
                    w=["wm%d" % b])
                k.dma("sp", lambda e, b=b, l=l, n0=n0: e.dma_start(
                    out=bm[b][:], in_=D["b_mod"][l:l + 1, n0:n0 + 512].to_broadcast([128, 512])), w=["bm%d" % b])
                if slot in (1, 4):
                    gsrc = D["norm_mix"] if slot == 1 else D["norm_ffn"]
                    k.dma("sp", lambda e, b=b, l=l, half=half, gsrc=gsrc: e.dma_start(
                        out=gn[b][:], in_=gsrc[l:l + 1, half * 512:(half + 1) * 512].to_broadcast([128, 512])),
                        w=["gn%d" % b])
                for s in range(2):
                    pm = psm[s]
                    for kc in range(8):
                        k.op("pe", lambda e, s=s, kc=kc, b=b, pm=pm: e.matmul(
                            pm[:], srep[:, s, kc, :], wm[b][:, kc, :], start=(kc == 0), stop=(kc == 7)),
                            r=["srep", "wm%d" % b], w=["psm%d" % s])
                    o = ob[oi % 4]
                    okey = "ob%d" % (oi % 4)
                    oi += 1
                    k.op("dve", lambda e, o=o, pm=pm, b=b: e.tensor_tensor(out=o[:], in0=pm[:], in1=bm[b][:], op=ALU.add),
                         r=["psm%d" % s, "bm%d" % b], w=[okey])
                    if slot in (1, 4):
                        k.op("dve", lambda e, o=o, b=b: e.scalar_tensor_tensor(
                            out=o[:], in0=o[:], scalar=1.0, in1=gn[b][:], op0=ALU.add, op1=ALU.mult),
                            r=[okey, "gn%d" % b], w=[okey])
                    k.dma("sp", lambda e, o=o, l=l, s=s, slot=slot, half=half: e.dma_start(
                        out=D["modrep"][l, s, slot, :, half * 512:(half + 1) * 512], in_=o[:]),
                        r=[okey], w=["modrep"])
        k.barrier()


def norm_mod_tile(pb, xt, xkey, rstd, rkey, junk, t1, G, SH, hn, hnkey):
    k = pb.k
    k.op("act", lambda e: e.activation(out=junk[:], in_=xt[:], func=AF.Square, accum_out=rstd[:, 0:1]),
         r=[xkey], w=["junk", rkey])
    k.op("act", lambda e: e.activation(out=rstd[:, 1:2], in_=rstd[:, 0:1], func=AF.Sqrt, bias=EPS, scale=1.0 / DM),
         r=[rkey], w=[rkey])
    k.op("dve", lambda e: e.reciprocal(out=rstd[:, 2:3], in_=rstd[:, 1:2]), r=[rkey], w=[rkey])
    k.op("dve", lambda e: e.scalar_tensor_tensor(out=t1[:], in0=xt[:], scalar=rstd[:, 2:3], in1=G[:],
                                                 op0=ALU.mult, op1=ALU.mult),
         r=[xkey, rkey, "modv"], w=["t1"])
    k.op("pool", lambda e: e.tensor_tensor(out=hn[:], in0=t1[:], in1=SH[:], op=ALU.add),
         r=["t1", "modv"], w=[hnkey])


def load_modv(pb, es, l, slots):
    k, D = pb.k, pb.D
    out = {}
    for s in range(2):
        for slot in slots:
            t = pb.sb(es, "mv%d%d" % (s, slot), [128, 1024], F32)
            k.dma("sp", lambda e, t=t, s=s, slot=slot: e.dma_start(out=t[:], in_=D["modrep"][l, s, slot, :, :]),
                  r=["modrep"], w=["modv"])
            out[(s, slot)] = t
    return out


def topk_threshold(pb, es, C, aff, J, cap, tag, psum):
    k = pb.k
    lo = pb.sb(es, "lo" + tag, [128, 16], F32)
    hi = pb.sb(es, "hi" + tag, [128, 16], F32)
    mid = pb.sb(es, "mid" + tag, [128, 16], F32)
    cnt = pb.sb(es, "cnt" + tag, [128, 16], F32)
    ge = pb.sb(es, "ge" + tag, [128, 16], U32)
    lt = pb.sb(es, "lt" + tag, [128, 16], U32)
    cmp = pb.sb(es, "cmp" + tag, [128, J, 16], BF16)
    K = "tk" + tag
    k.op("dve", lambda e: e.memset(lo[:], 0.0), w=[K])
    k.op("dve", lambda e: e.memset(hi[:], 1.0), w=[K])
    ncol = J * 16
    for it in range(30):
        k.op("dve", lambda e: e.tensor_tensor(out=mid[:], in0=lo[:], in1=hi[:], op=ALU.add), r=[K], w=[K + "m"])
        k.op("dve", lambda e: e.tensor_scalar(out=mid[:], in0=mid[:], scalar1=0.5, scalar2=None, op0=ALU.mult),
             r=[K + "m"], w=[K + "m"])
        k.op("dve", lambda e: e.tensor_tensor(out=cmp[:], in0=aff, in1=mid[:].unsqueeze(1).to_broadcast([128, J, 16]),
                                              op=ALU.is_ge), r=[K + "m", "aff_all"], w=[K + "c"])
        cf = cmp[:].rearrange("p j e -> p (j e)")
        for c0 in range(0, ncol, 512):
            c1 = min(ncol, c0 + 512)
            k.op("pe", lambda e, c0=c0, c1=c1: e.matmul(psum[:, c0:c1], C["ones_b"][:], cf[:, c0:c1], start=True, stop=True),
                 r=[K + "c", "ones_b"], w=[K + "p"])
        k.op("dve", lambda e: e.tensor_reduce(out=cnt[:], in_=psum[:, 0:ncol].rearrange("p (j e) -> p e j", e=16),
                                              axis=AX.X, op=ALU.add), r=[K + "p"], w=[K + "n"])
        k.op("dve", lambda e: e.tensor_scalar(out=ge[:], in0=cnt[:], scalar1=float(cap), scalar2=None, op0=ALU.is_ge),
             r=[K + "n"], w=[K + "g"])
        k.op("dve", lambda e: e.tensor_scalar(out=lt[:], in0=cnt[:], scalar1=float(cap), scalar2=None, op0=ALU.is_lt),
             r=[K + "n"], w=[K + "g"])
        k.op("dve", lambda e: e.copy_predicated(out=lo[:], mask=ge[:], data=mid[:]), r=[K + "g", K + "m"], w=[K])
        k.op("dve", lambda e: e.copy_predicated(out=hi[:], mask=lt[:], data=mid[:]), r=[K + "g", K + "m"], w=[K])
    return lo, K


def phase_moe(pb, C, l):
    k, D, nc = pb.k, pb.D, pb.nc
    has_ctx = (l == 0)
    NT = 66 if has_ctx else 64
    NPT = 9 if has_ctx else 8
    NP = NPT * 128
    with ExitStack() as es:
        mv = load_modv(pb, es, l, (3, 4, 5))
        iota_q = pb.sb(es, "iota_q", [128, 1024], F32)
        ctxadd = pb.sb(es, "ctxadd", [128, 128], F32)
        k.dma("sp", lambda e: e.dma_start(out=iota_q[:], in_=D["c_iota_q"]), w=["iota_q"])
        k.dma("sp", lambda e: e.dma_start(out=ctxadd[:], in_=D["c_ctxadd"]), w=["ctxadd"])
        aff_all = pb.sb(es, "aff_all", [128, NT, 16], F32)
        wr_f = pb.sb(es, "wr_f", [128, 8, 16], F32)
        wr = pb.sb(es, "wr", [128, 8, 16], BF16)
        k.dma("sp", lambda e: e.dma_start(out=wr_f[:], in_=D["w_router"][l].rearrange("(kc p) n -> p kc n", p=128)),
              w=["wr_f"])
        k.op("dve", lambda e: e.tensor_copy(out=wr[:], in_=wr_f[:]), r=["wr_f"], w=["wr"])
        with ExitStack() as es2:
            xt = [pb.sb(es2, "xt%d" % i, [128, 1024], F32) for i in range(2)]
            hn = [pb.sb(es2, "hn%d" % i, [128, 1024], BF16) for i in range(2)]
            hnT = [pb.sb(es2, "hnT%d" % i, [128, 8, 128], BF16) for i in range(2)]
            junk = pb.sb(es2, "junk", [128, 1024], BF16)
            t1 = pb.sb(es2, "t1", [128, 1024], F32)
            rstd = [pb.sb(es2, "rstd%d" % i, [128, 4], F32) for i in range(2)]
            psT = [pb.ps(es2, "psT%d" % i, [128, 1024], BF16) for i in range(2)]
            psL = [pb.ps(es2, "psL%d" % i, [128, 16], F32) for i in range(2)]
            for j in range(NT):
                b = j % 2
                s = 0 if j < 64 else 1
                r0 = j * 128
                k.dma("sp", lambda e, b=b, r0=r0: e.dma_start(out=xt[b][:], in_=D["xres"][r0:r0 + 128, :]),
                      r=["xres"], w=["xt%d" % b])
                norm_mod_tile(pb, xt[b], "xt%d" % b, rstd[b], "rstd%d" % b, junk, t1, mv[(s, 4)], mv[(s, 3)], hn[b], "hn%d" % b)
                k.dma("sp", lambda e, b=b, r0=r0: e.dma_start(out=D["hn"][r0:r0 + 128, :], in_=hn[b][:]),
                      r=["hn%d" % b], w=["hn"])
                for kc in range(8):
                    k.op("pe", lambda e, b=b, kc=kc: e.transpose(out=psT[b][:, kc * 128:(kc + 1) * 128],
                                                                 in_=hn[b][:, kc * 128:(kc + 1) * 128],
                                                                 identity=C["ident_b"][:]),
                         r=["hn%d" % b, "ident_b"], w=["psT%d" % b])
                k.op("act", lambda e, b=b: e.activation(out=hnT[b][:].rearrange("p a t -> p (a t)"), in_=psT[b][:], func=AF.Copy),
                     r=["psT%d" % b], w=["hnT%d" % b])
                for kc in range(8):
                    k.op("pe", lambda e, b=b, kc=kc: e.matmul(psL[b][:], hnT[b][:, kc, :], wr[:, kc, :],
                                                              start=(kc == 0), stop=(kc == 7)),
                         r=["hnT%d" % b, "wr"], w=["psL%d" % b])
                k.op("dve", lambda e, b=b, j=j: e.tensor_copy(out=aff_all[:, j, :], in_=psL[b][:]),
                     r=["psL%d" % b], w=["aff_all"])
            mx = pb.sb(es2, "mx", [128, NT], F32)
            k.op("dve", lambda e: e.tensor_reduce(out=mx[:], in_=aff_all[:], axis=AX.X, op=ALU.max), r=["aff_all"], w=["mx"])
            k.op("dve", lambda e: e.tensor_tensor(out=aff_all[:], in0=aff_all[:],
                                                  in1=mx[:].unsqueeze(2).to_broadcast([128, NT, 16]), op=ALU.subtract),
                 r=["aff_all", "mx"], w=["aff_all"])
            k.op("act", lambda e: e.activation(out=aff_all[:], in_=aff_all[:], func=AF.Exp), r=["aff_all"], w=["aff_all"])
            k.op("dve", lambda e: e.tensor_reduce(out=mx[:], in_=aff_all[:], axis=AX.X, op=ALU.add), r=["aff_all"], w=["mx"])
            k.op("dve", lambda e: e.reciprocal(out=mx[:], in_=mx[:]), r=["mx"], w=["mx"])
            k.op("dve", lambda e: e.tensor_tensor(out=aff_all[:], in0=aff_all[:],
                                                  in1=mx[:].unsqueeze(2).to_broadcast([128, NT, 16]), op=ALU.mult),
                 r=["aff_all", "mx"], w=["aff_all"])
            k.dma("sp", lambda e: e.dma_start(out=D["aff"][0:NT * 128, :].rearrange("(j p) e -> p j e", p=128), in_=aff_all[:]),
                  r=["aff_all"], w=["aff"])
            k.barrier()
        m_all = pb.sb(es, "m_all", [128, NT, 16], BF16)
        cnt_incl = pb.sb(es, "cnt_incl", [128, NT, 16], F32)
        with ExitStack() as es2:
            pst = [pb.ps(es2, "pst%d" % i, [128, 512], F32) for i in range(3)]
            psbig = pb.ps(es2, "psbig", [128, 1024], F32)
            thr_l, Kl = topk_threshold(pb, es2, C, aff_all[:, 0:64, :], 64, 1024, "L", psbig)
            k.op("dve", lambda e: e.tensor_tensor(out=m_all[:, 0:64, :], in0=aff_all[:, 0:64, :],
                                                  in1=thr_l[:].unsqueeze(1).to_broadcast([128, 64, 16]), op=ALU.is_ge),
                 r=["aff_all", Kl], w=["m_all"])
            if has_ctx:
                thr_c, Kc = topk_threshold(pb, es2, C, aff_all[:, 64:66, :], 2, 32, "C", psbig)
                k.op("dve", lambda e: e.tensor_tensor(out=m_all[:, 64:66, :], in0=aff_all[:, 64:66, :],
                                                      in1=thr_c[:].unsqueeze(1).to_broadcast([128, 2, 16]), op=ALU.is_ge),
                     r=["aff_all", Kc], w=["m_all"])
            tot = pb.sb(es2, "tot", [128, NT, 16], F32)
            binc = pb.sb(es2, "binc", [128, NT, 16], F32)
            mf = m_all[:].rearrange("p j e -> p (j e)")
            tf = tot[:].rearrange("p j e -> p (j e)")
            cf = cnt_incl[:].rearrange("p j e -> p (j e)")
            ncol = NT * 16
            for ci, c0 in enumerate(range(0, ncol, 512)):
                c1 = min(ncol, c0 + 512)
                w_ = c1 - c0
                k.op("pe", lambda e, c0=c0, c1=c1, ci=ci, w_=w_: e.matmul(pst[ci][:, 0:w_], C["ones_b"][:], mf[:, c0:c1],
                                                                          start=True, stop=True),
                     r=["m_all", "ones_b"], w=["pst%d" % ci])
                k.op("dve", lambda e, c0=c0, c1=c1, ci=ci, w_=w_: e.tensor_copy(out=tf[:, c0:c1], in_=pst[ci][:, 0:w_]),
                     r=["pst%d" % ci], w=["tot"])
            for e_ in range(16):
                k.op("dve", lambda e, e_=e_: e.tensor_tensor_scan(out=binc[:, 0:64, e_], data0=C["ones_f"][:, 0:64],
                                                                  data1=tot[:, 0:64, e_], initial=0.0,
                                                                  op0=ALU.mult, op1=ALU.add),
                     r=["tot", "ones_f"], w=["binc"])
            if has_ctx:
                k.op("dve", lambda e: e.tensor_copy(out=binc[:, 64, :], in_=tot[:, 64, :]), r=["tot"], w=["binc"])
                k.op("dve", lambda e: e.tensor_tensor(out=binc[:, 65, :], in0=tot[:, 64, :], in1=tot[:, 65, :], op=ALU.add),
                     r=["tot"], w=["binc"])
            k.op("dve", lambda e: e.tensor_tensor(out=binc[:], in0=binc[:], in1=tot[:], op=ALU.subtract),
                 r=["binc", "tot"], w=["binc"])
            bf_ = binc[:].rearrange("p j e -> p (j e)")
            for ci, c0 in enumerate(range(0, ncol, 512)):
                c1 = min(ncol, c0 + 512)
                w_ = c1 - c0
                k.op("pe", lambda e, c0=c0, c1=c1, ci=ci, w_=w_: e.matmul(pst[ci][:, 0:w_], C["triu_incl_b"][:], mf[:, c0:c1],
                                                                          start=True, stop=True),
                     r=["m_all", "triu_incl_b", "tot"], w=["pst%d" % ci])
                k.op("dve", lambda e, c0=c0, c1=c1, ci=ci, w_=w_: e.tensor_tensor(out=cf[:, c0:c1], in0=pst[ci][:, 0:w_],
                                                                                  in1=bf_[:, c0:c1], op=ALU.add),
                     r=["pst%d" % ci, "binc"], w=["cnt_incl"])
            k.barrier()
        with ExitStack() as es2:
            Bt = [pb.sb(es2, "Bt%d" % i, [128, 1024], BF16) for i in range(2)]
            row_sb = pb.sb(es2, "row_sb", [1, NP], F32)
            idx = pb.sb(es2, "idx", [128, NPT], I32)
            xg = pb.sb(es2, "xg", [128, NPT, 1024], BF16)
            ga = pb.sb(es2, "ga", [128, NPT, 16], F32)
            xgT = pb.sb(es2, "xgT", [128, 8, NP], BF16)
            hidT = pb.sb(es2, "hidT", [128, 8, NP], BF16)
            sg = [pb.sb(es2, "sg%d" % i, [128, 512], F32) for i in range(2)]
            yt = [pb.sb(es2, "yt%d" % i, [128, 1024], F32) for i in range(2)]
            Wb = {nm: pb.sb(es2, "W" + nm, [128, 8, 1024], BF16) for nm in ("g", "u", "d")}
            stg = [pb.sb(es2, "stg%d" % i, [128, 4, 1024], F32) for i in range(2)]
            rowA = pb.ps(es2, "rowA", [1, 512], F32)
            rowB = pb.ps(es2, "rowB", [1, 512], F32)
            rowC = pb.ps(es2, "rowC", [128, 512], F32)
            psX = pb.ps(es2, "psX", [128, 1024], BF16)
            psG = pb.ps(es2, "psG", [128, 512], F32)
            psU = pb.ps(es2, "psU", [128, 512], F32)
            psY = [pb.ps(es2, "psY%d" % i, [128, 512], F32) for i in range(2)]
            sti = 0
            bi = 0
            yi = 0
            pchunks = [(0, 512), (512, 1024)] + ([(1024, 1152)] if has_ctx else [])
            prev_sc, cur_sc = [], []
            for ex in range(NE):
                for nm, src in (("g", D["w_gate"]), ("u", D["w_up"]), ("d", D["w_down"])):
                    for hf in range(2):
                        sbuf_ = stg[sti % 2]
                        skey = "stg%d" % (sti % 2)
                        sti += 1
                        k.dma("sp", lambda e, sbuf_=sbuf_, src=src, hf=hf, ex=ex: e.dma_start(
                            out=sbuf_[:], in_=src[l, ex, hf * 512:(hf + 1) * 512, :].rearrange("(kc p) n -> p kc n", p=128)),
                            w=[skey])
                        k.op("act", lambda e, sbuf_=sbuf_, nm=nm, hf=hf: e.activation(
                            out=Wb[nm][:, hf * 4:(hf + 1) * 4, :], in_=sbuf_[:], func=AF.Copy),
                            r=[skey], w=["W" + nm])
                for j in range(64):
                    B_ = Bt[bi % 2]
                    bkey = "Bt%d" % (bi % 2)
                    eng = "dve"
                    bi += 1
                    k.op(eng, lambda e, B_=B_, j=j, ex=ex: e.tensor_scalar(
                        out=B_[:], in0=iota_q[:], scalar1=cnt_incl[:, j, ex:ex + 1], scalar2=None, op0=ALU.is_ge),
                        r=["iota_q", "cnt_incl"], w=[bkey])
                    k.op("pe", lambda e, B_=B_, j=j: e.matmul(rowA[:], C["ones_b"][:, 0:1], B_[:, 0:512],
                                                              start=(j == 0), stop=(j == 63)),
                         r=[bkey, "ones_b"], w=["rowA"])
                    k.op("pe", lambda e, B_=B_, j=j: e.matmul(rowB[:], C["ones_b"][:, 0:1], B_[:, 512:1024],
                                                              start=(j == 0), stop=(j == 63)),
                         r=[bkey, "ones_b"], w=["rowB"])
                k.op("dve", lambda e: e.tensor_copy(out=row_sb[0:1, 0:512], in_=rowA[:]), r=["rowA"], w=["row_sb"])
                k.op("dve", lambda e: e.tensor_copy(out=row_sb[0:1, 512:1024], in_=rowB[:]), r=["rowB"], w=["row_sb"])
                if has_ctx:
                    for j in (64, 65):
                        B_ = Bt[bi % 2]
                        bkey = "Bt%d" % (bi % 2)
                        bi += 1
                        k.op("dve", lambda e, B_=B_, j=j, ex=ex: e.tensor_scalar(
                            out=B_[:, 0:128], in0=iota_q[:, 0:128], scalar1=cnt_incl[:, j, ex:ex + 1], scalar2=None,
                            op0=ALU.is_ge), r=["iota_q", "cnt_incl"], w=[bkey])
                        k.op("pe", lambda e, B_=B_, j=j: e.matmul(rowC[0:1, 0:128], C["ones_b"][:, 0:1], B_[:, 0:128],
                                                                  start=(j == 64), stop=(j == 65)),
                             r=[bkey, "ones_b"], w=["rowC"])
                    k.op("dve", lambda e: e.tensor_tensor(out=row_sb[0:1, 1024:1152], in0=rowC[0:1, 0:128],
                                                          in1=ctxadd[0:1, :], op=ALU.add),
                         r=["rowC", "ctxadd"], w=["row_sb"])
                for i in range(NPT):
                    k.op("pe", lambda e, i=i: e.transpose(out=rowC[:, 256 + i:257 + i], in_=row_sb[0:1, i * 128:(i + 1) * 128],
                                                          identity=C["ident_f"][0:1, 0:1]),
                         r=["row_sb", "ident_f"], w=["rowC"])
                k.op("dve", lambda e: e.tensor_copy(out=idx[:], in_=rowC[:, 256:256 + NPT]), r=["rowC"], w=["idx"])
                for i in range(NPT):
                    k.dma("pool", lambda e, i=i: e.indirect_dma_start(
                        out=xg[:, i, :], out_offset=None, in_=D["hn"],
                        in_offset=bass.IndirectOffsetOnAxis(ap=idx[:, i:i + 1], axis=0)),
                        r=["idx", "hn"], w=["xg%d" % i])
                    k.dma("pool", lambda e, i=i: e.indirect_dma_start(
                        out=ga[:, i, :], out_offset=None, in_=D["aff"],
                        in_offset=bass.IndirectOffsetOnAxis(ap=idx[:, i:i + 1], axis=0)),
                        r=["idx", "aff"], w=["ga%d" % i])
                for i in range(NPT):
                    for kc in range(8):
                        k.op("pe", lambda e, i=i, kc=kc: e.transpose(out=psX[:, kc * 128:(kc + 1) * 128],
                                                                     in_=xg[:, i, kc * 128:(kc + 1) * 128],
                                                                     identity=C["ident_b"][:]),
                             r=["xg%d" % i, "ident_b"], w=["psX"])
                    k.op("dve", lambda e, i=i: e.tensor_copy(out=xgT[:, :, i * 128:(i + 1) * 128],
                                                             in_=psX[:].rearrange("p (a t) -> p a t", a=8)),
                         r=["psX"], w=["xgT"])
                for fc in range(8):
                    for (p0, p1) in pchunks:
                        n = p1 - p0
                        for kc in range(8):
                            k.op("pe", lambda e, fc=fc, kc=kc, p0=p0, p1=p1, n=n: e.matmul(
                                psG[:, 0:n], Wb["g"][:, kc, fc * 128:(fc + 1) * 128], xgT[:, kc, p0:p1],
                                start=(kc == 0), stop=(kc == 7)), r=["Wg", "xgT"], w=["psG"])
                        for kc in range(8):
                            k.op("pe", lambda e, fc=fc, kc=kc, p0=p0, p1=p1, n=n: e.matmul(
                                psU[:, 0:n], Wb["u"][:, kc, fc * 128:(fc + 1) * 128], xgT[:, kc, p0:p1],
                                start=(kc == 0), stop=(kc == 7)), r=["Wu", "xgT"], w=["psU"])
                        s_ = sg[yi % 2]
                        skey = "sg%d" % (yi % 2)
                        yi += 1
                        k.op("act", lambda e, s_=s_, n=n: e.activation(out=s_[:, 0:n], in_=psG[:, 0:n], func=AF.Silu),
                             r=["psG"], w=[skey])
                        k.op("dve", lambda e, s_=s_, n=n, fc=fc, p0=p0, p1=p1: e.tensor_tensor(
                            out=hidT[:, fc, p0:p1], in0=psU[:, 0:n], in1=s_[:, 0:n], op=ALU.mult),
                            r=["psU", skey], w=["hidT"])
                for i in range(NPT):
                    s = 0 if i < 8 else 1
                    y_ = yt[i % 2]
                    ykey = "yt%d" % (i % 2)
                    for dh in range(2):
                        py = psY[dh]
                        for fc in range(8):
                            k.op("pe", lambda e, i=i, fc=fc, dh=dh, py=py: e.matmul(
                                py[:], hidT[:, fc, i * 128:(i + 1) * 128], Wb["d"][:, fc, dh * 512:(dh + 1) * 512],
                                start=(fc == 0), stop=(fc == 7)), r=["hidT", "Wd"], w=["psY%d" % dh])
                        k.op("dve", lambda e, i=i, dh=dh, py=py, y_=y_, s=s, ex=ex: e.scalar_tensor_tensor(
                            out=y_[:, dh * 512:(dh + 1) * 512], in0=py[:], scalar=ga[:, i, ex:ex + 1],
                            in1=mv[(s, 5)][:, dh * 512:(dh + 1) * 512], op0=ALU.mult, op1=ALU.mult),
                            r=["psY%d" % dh, "ga%d" % i, "modv"], w=[ykey])
                    for t_ in prev_sc:
                        k._wait("pool", t_)
                    cur_sc.append(k.dma("pool", lambda e, i=i, y_=y_: e.indirect_dma_start(
                        out=D["xres"], out_offset=bass.IndirectOffsetOnAxis(ap=idx[:, i:i + 1], axis=0),
                        in_=y_[:], in_offset=None, compute_op=ALU.add),
                        r=[ykey, "idx"], w=[]))
                prev_sc, cur_sc = cur_sc, []
            k.barrier()


def phase_final(pb, C):
    k, D = pb.k, pb.D
    with ExitStack() as es:
        g = pb.sb(es, "gfin", [128, 1024], F32)
        k.dma("sp", lambda e: e.dma_start(out=g[:], in_=D["norm_out"].to_broadcast([128, 1024])), w=["gfin"])
        xt = [pb.sb(es, "fx%d" % i, [128, 1024], F32) for i in range(2)]
        ot = [pb.sb(es, "fo%d" % i, [128, 1024], F32) for i in range(2)]
        junk = pb.sb(es, "fjunk", [128, 1024], BF16)
        rstd = [pb.sb(es, "frs%d" % i, [128, 4], F32) for i in range(2)]
        for j in range(64):
            b = j % 2
            r0 = j * 128
            k.dma("sp", lambda e, b=b, r0=r0: e.dma_start(out=xt[b][:], in_=D["xres"][r0:r0 + 128, :]),
                  r=["xres"], w=["fx%d" % b])
            k.op("act", lambda e, b=b: e.activation(out=junk[:], in_=xt[b][:], func=AF.Square, accum_out=rstd[b][:, 0:1]),
                 r=["fx%d" % b], w=["fjunk", "frs%d" % b])
            k.op("act", lambda e, b=b: e.activation(out=rstd[b][:, 1:2], in_=rstd[b][:, 0:1], func=AF.Sqrt, bias=EPS,
                                                    scale=1.0 / DM), r=["frs%d" % b], w=["frs%d" % b])
            k.op("dve", lambda e, b=b: e.reciprocal(out=rstd[b][:, 2:3], in_=rstd[b][:, 1:2]), r=["frs%d" % b], w=["frs%d" % b])
            k.op("dve", lambda e, b=b: e.scalar_tensor_tensor(out=ot[b][:], in0=xt[b][:], scalar=rstd[b][:, 2:3], in1=g[:],
                                                              op0=ALU.mult, op1=ALU.mult),
                 r=["fx%d" % b, "frs%d" % b, "gfin"], w=["fo%d" % b])
            k.dma("sp", lambda e, b=b, r0=r0: e.dma_start(out=D["out"][r0:r0 + 128, :], in_=ot[b][:]),
                  r=["fo%d" % b], w=["out"])
        k.barrier()


NH = 8
ATT_SCALE = 1.0 / float(np.sqrt(192.0))


def attn_host_consts():
    c = {}
    t = np.arange(T_LAT)
    row = (t // 64).astype(np.float32)
    col = (t % 64).astype(np.float32)
    nf = 16
    inv = (10000.0 ** (-np.arange(nf, dtype=np.float32) / nf)).astype(np.float32)
    cosT = np.zeros((64, T_LAT), np.float32)
    sinT = np.zeros((64, T_LAT), np.float32)
    for a, pos in ((0, row), (1, col)):
        ang = (pos[None, :] * inv[:, None]).astype(np.float32)
        for b in range(2):
            d0 = a * 32 + b * 16
            cosT[d0:d0 + 16] = np.cos(ang)
            sinT[d0:d0 + 16] = np.sin(ang) * (-1.0 if b == 0 else 1.0)
    c["cos2"] = np.concatenate([cosT, cosT], 0)
    c["sin2"] = np.concatenate([sinT, sinT], 0)
    return c


def rope_swap_perm():
    d = np.arange(64)
    a, b, f = d // 32, (d // 16) % 2, d % 16
    return a * 32 + (1 - b) * 16 + f


def attn_layout_weights(w_uq, w_ukv, w_kr):
    sp = rope_swap_perm()
    nope = np.concatenate([np.arange(h * 192, h * 192 + 128) for h in range(NH)])
    rope = np.concatenate([np.arange(h * 192 + 128, h * 192 + 192) for h in range(NH)])
    ropes = np.concatenate([h * 192 + 128 + sp for h in range(NH)])
    w_uq_r = np.ascontiguousarray(w_uq[:, np.concatenate([nope, rope, ropes])])
    kn = np.concatenate([np.arange(h * 256, h * 256 + 128) for h in range(NH)])
    vv = np.concatenate([np.arange(h * 256 + 128, h * 256 + 256) for h in range(NH)])
    w_ukv_r = np.ascontiguousarray(w_ukv[:, np.concatenate([kn, vv])])
    w_kr_r = np.ascontiguousarray(np.concatenate([w_kr, w_kr[:, sp]], 1))
    return w_uq_r, w_ukv_r, w_kr_r


def load_w_bf16(pb, es, name, src_ap, kc, n, stg, stgkey):
    k = pb.k
    t = pb.sb(es, name, [128, kc, n], BF16)
    for c0 in range(0, n, 512):
        c1 = min(n, c0 + 512)
        for q in range(kc):
            k.dma("sp", lambda e, q=q, c0=c0, c1=c1: e.dma_start(out=stg[:, 0:c1 - c0], in_=src_ap[q * 128:(q + 1) * 128, c0:c1]),
                  w=[stgkey])
            k.op("dve", lambda e, q=q, c0=c0, c1=c1: e.tensor_copy(out=t[:, q, c0:c1], in_=stg[:, 0:c1 - c0]),
                 r=[stgkey], w=[name])
    return t


def rms_small(pb, ps_ap, n, rstd, rkey, junk, gain_rep, outbf, outkey, pskey):
    k = pb.k
    k.op("act", lambda e: e.activation(out=junk[:, 0:n], in_=ps_ap, func=AF.Square, accum_out=rstd[:, 0:1]),
         r=[pskey], w=["junk", rkey])
    k.op("act", lambda e: e.activation(out=rstd[:, 1:2], in_=rstd[:, 0:1], func=AF.Sqrt, bias=EPS, scale=1.0 / n),
         r=[rkey], w=[rkey])
    k.op("dve", lambda e: e.reciprocal(out=rstd[:, 2:3], in_=rstd[:, 1:2]), r=[rkey], w=[rkey])
    k.op("dve", lambda e: e.scalar_tensor_tensor(out=outbf, in0=ps_ap, scalar=rstd[:, 2:3], in1=gain_rep,
                                                 op0=ALU.mult, op1=ALU.mult), r=[pskey, rkey, "gains"], w=[outkey])


def phase_attn_pre(pb, C):
    k, D, nc = pb.k, pb.D, pb.nc
    l = 1
    with ExitStack() as es:
        mv = load_modv(pb, es, l, (0, 1))
        stg = pb.sb(es, "wstg", [128, 512], F32)
        w_dq = load_w_bf16(pb, es, "w_dq", D["w_dq"], 8, 384, stg, "wstg")
        w_dkv = load_w_bf16(pb, es, "w_dkv", D["w_dkv"], 8, 256, stg, "wstg")
        w_kr = load_w_bf16(pb, es, "w_kr", D["w_kr_r"], 8, 128, stg, "wstg")
        w_uq = load_w_bf16(pb, es, "w_uq", D["w_uq_r"], 3, 2048, stg, "wstg")
        w_ukv = load_w_bf16(pb, es, "w_ukv", D["w_ukv_r"], 2, 2048, stg, "wstg")
        qn_rep = pb.sb(es, "qn_rep", [128, 384], F32)
        kvn_rep = pb.sb(es, "kvn_rep", [128, 256], F32)
        k.dma("sp", lambda e: e.dma_start(out=qn_rep[:], in_=D["q_norm"].to_broadcast([128, 384])), w=["gains"])
        k.dma("sp", lambda e: e.dma_start(out=kvn_rep[:], in_=D["kv_norm"].to_broadcast([128, 256])), w=["gains"])
        xt = [pb.sb(es, "xt%d" % i, [128, 1024], F32) for i in range(2)]
        an = [pb.sb(es, "an%d" % i, [128, 1024], BF16) for i in range(2)]
        junk = pb.sb(es, "junk", [128, 1024], BF16)
        t1 = pb.sb(es, "t1", [128, 1024], F32)
        rstd = [pb.sb(es, "rstd%d" % i, [128, 4], F32) for i in range(2)]
        rs2 = [pb.sb(es, "rs2%d" % i, [128, 4], F32) for i in range(2)]
        aT = pb.sb(es, "aT", [128, 8, 512], BF16)
        cqT = pb.sb(es, "cqT", [128, 3, 512], BF16)
        ckvT = pb.sb(es, "ckvT", [128, 2, 512], BF16)
        cqn = pb.sb(es, "cqn", [128, 384], BF16)
        ckvn = pb.sb(es, "ckvn", [128, 256], BF16)
        cosb = pb.sb(es, "cosb", [128, 512], F32)
        sinb = pb.sb(es, "sinb", [128, 512], F32)
        ostg = [pb.sb(es, "ostg%d" % i, [128, 512], BF16) for i in range(3)]
        vstg = [pb.sb(es, "vstg%d" % i, [128, 1024], BF16) for i in range(2)]
        ra = pb.sb(es, "ra", [128, 512], F32)
        rb = pb.sb(es, "rb", [128, 512], F32)
        psT = pb.ps(es, "psT", [128, 1024], BF16)
        psS = pb.ps(es, "psS", [128, 512], F32)
        psA = pb.ps(es, "psA", [128, 512], F32)
        psB = pb.ps(es, "psB", [128, 512], F32)
        psV = [pb.ps(es, "psV%d" % i, [128, 512], F32) for i in range(2)]
        oi = 0
        vi = 0
        xi = 0
        supers = [(i * 512, 4, 0) for i in range(16)] + [(T_LAT, 2, 1)]
        for (t0, ntile, s) in supers:
            nt = ntile * 128
            is_lat = (s == 0)
            for tl in range(ntile):
                b = xi % 2
                xi += 1
                r0 = t0 + tl * 128
                k.dma("sp", lambda e, b=b, r0=r0: e.dma_start(out=xt[b][:], in_=D["xres"][r0:r0 + 128, :]),
                      r=["xres"], w=["xt%d" % b])
                norm_mod_tile(pb, xt[b], "xt%d" % b, rstd[b], "rstd%d" % b, junk, t1, mv[(s, 1)], mv[(s, 0)], an[b], "an%d" % b)
                for kc in range(8):
                    k.op("pe", lambda e, b=b, kc=kc: e.transpose(out=psT[:, kc * 128:(kc + 1) * 128],
                                                                 in_=an[b][:, kc * 128:(kc + 1) * 128], identity=C["ident_b"][:]),
                         r=["an%d" % b, "ident_b"], w=["psT"])
                k.op("act", lambda e, tl=tl: e.activation(out=aT[:, :, tl * 128:(tl + 1) * 128],
                                                          in_=psT[:].rearrange("p (a t) -> p a t", a=8), func=AF.Copy),
                     r=["psT"], w=["aT"])
                if is_lat:
                    for kc in range(8):
                        k.op("pe", lambda e, kc=kc, tl=tl: e.matmul(psS[:, 0:384], aT[:, kc, tl * 128:(tl + 1) * 128], w_dq[:, kc, :],
                                                                    start=(kc == 0), stop=(kc == 7)), r=["aT", "w_dq"], w=["psS"])
                    rms_small(pb, psS[:, 0:384], 384, rs2[b], "rs2%d" % b, junk, qn_rep[:], cqn[:], "cqn", "psS")
                    for c in range(3):
                        k.op("pe", lambda e, c=c: e.transpose(out=psT[:, c * 128:(c + 1) * 128], in_=cqn[:, c * 128:(c + 1) * 128],
                                                              identity=C["ident_b"][:]), r=["cqn", "ident_b"], w=["psT"])
                    k.op("act", lambda e, tl=tl: e.activation(out=cqT[:, :, tl * 128:(tl + 1) * 128],
                                                              in_=psT[:, 0:384].rearrange("p (a t) -> p a t", a=3), func=AF.Copy),
                         r=["psT"], w=["cqT"])
                for kc in range(8):
                    k.op("pe", lambda e, kc=kc, tl=tl: e.matmul(psS[:, 0:256], aT[:, kc, tl * 128:(tl + 1) * 128], w_dkv[:, kc, :],
                                                                start=(kc == 0), stop=(kc == 7)), r=["aT", "w_dkv"], w=["psS"])
                rms_small(pb, psS[:, 0:256], 256, rs2[b], "rs2%d" % b, junk, kvn_rep[:], ckvn[:], "ckvn", "psS")
                for c in range(2):
                    k.op("pe", lambda e, c=c: e.transpose(out=psT[:, c * 128:(c + 1) * 128], in_=ckvn[:, c * 128:(c + 1) * 128],
                                                          identity=C["ident_b"][:]), r=["ckvn", "ident_b"], w=["psT"])
                k.op("act", lambda e, tl=tl: e.activation(out=ckvT[:, :, tl * 128:(tl + 1) * 128],
                                                          in_=psT[:, 0:256].rearrange("p (a t) -> p a t", a=2), func=AF.Copy),
                     r=["psT"], w=["ckvT"])
                v_ = vstg[vi % 2]
                vkey = "vstg%d" % (vi % 2)
                vi += 1
                for dh in range(2):
                    for c in range(2):
                        k.op("pe", lambda e, c=c, dh=dh, tl=tl: e.matmul(
                            psV[dh][:], ckvT[:, c, tl * 128:(tl + 1) * 128], w_ukv[:, c, 1024 + dh * 512:1024 + (dh + 1) * 512],
                            start=(c == 0), stop=(c == 1)), r=["ckvT", "w_ukv"], w=["psV%d" % dh])
                    k.op("act", lambda e, dh=dh, v_=v_: e.activation(out=v_[:, dh * 512:(dh + 1) * 512], in_=psV[dh][:], func=AF.Copy),
                         r=["psV%d" % dh], w=[vkey])
                k.dma("sp", lambda e, v_=v_, r0=r0: e.dma_start(out=D["Vd"][r0:r0 + 128, :], in_=v_[:]), r=[vkey], w=["Vd"])
            if is_lat:
                k.dma("sp", lambda e, t0=t0: e.dma_start(out=cosb[:], in_=D["c_cos2"][:, t0:t0 + 512]), w=["cosb"])
                k.dma("sp", lambda e, t0=t0: e.dma_start(out=sinb[:], in_=D["c_sin2"][:, t0:t0 + 512]), w=["sinb"])
            for h in range(NH):
                for c in range(2):
                    k.op("pe", lambda e, c=c, h=h: e.matmul(psA[:, 0:nt], w_ukv[:, c, h * 128:(h + 1) * 128], ckvT[:, c, 0:nt],
                                                            start=(c == 0), stop=(c == 1)), r=["ckvT", "w_ukv"], w=["psA"])
                o_ = ostg[oi % 3]
                okey = "ostg%d" % (oi % 3)
                oi += 1
                k.op("act", lambda e, o_=o_: e.activation(out=o_[:, 0:nt], in_=psA[:, 0:nt], func=AF.Copy), r=["psA"], w=[okey])
                k.dma("sp", lambda e, o_=o_, h=h, t0=t0: e.dma_start(out=D["KT"][h, :, t0:t0 + nt], in_=o_[:, 0:nt]),
                      r=[okey], w=["KT"])
            for kc in range(8):
                k.op("pe", lambda e, kc=kc: e.matmul(psA[0:64, 0:nt], w_kr[:, kc, 0:64], aT[:, kc, 0:nt],
                                                     start=(kc == 0), stop=(kc == 7)), r=["aT", "w_kr"], w=["psA"])
            o_ = ostg[oi % 3]
            okey = "ostg%d" % (oi % 3)
            oi += 1
            if is_lat:
                for kc in range(8):
                    k.op("pe", lambda e, kc=kc: e.matmul(psB[0:64, 0:nt], w_kr[:, kc, 64:128], aT[:, kc, 0:nt],
                                                         start=(kc == 0), stop=(kc == 7)), r=["aT", "w_kr"], w=["psB"])
                k.op("dve", lambda e: e.tensor_tensor(out=ra[0:64, :], in0=psA[0:64, :], in1=cosb[0:64, :], op=ALU.mult),
                     r=["psA", "cosb"], w=["ra"])
                k.op("dve", lambda e: e.tensor_tensor(out=rb[0:64, :], in0=psB[0:64, :], in1=sinb[0:64, :], op=ALU.mult),
                     r=["psB", "sinb"], w=["rb"])
                k.op("dve", lambda e, o_=o_: e.tensor_tensor(out=o_[0:64, :], in0=ra[0:64, :], in1=rb[0:64, :], op=ALU.add),
                     r=["ra", "rb"], w=[okey])
            else:
                k.op("act", lambda e, o_=o_: e.activation(out=o_[0:64, 0:nt], in_=psA[0:64, 0:nt], func=AF.Copy), r=["psA"], w=[okey])
            k.dma("sp", lambda e, o_=o_, t0=t0: e.dma_start(out=D["KRT"][:, t0:t0 + nt], in_=o_[0:64, 0:nt]), r=[okey], w=["KRT"])
            if not is_lat:
                continue
            for h in range(NH):
                for c in range(3):
                    k.op("pe", lambda e, c=c, h=h: e.matmul(psA[:], w_uq[:, c, h * 128:(h + 1) * 128], cqT[:, c, :],
                                                            start=(c == 0), stop=(c == 2)), r=["cqT", "w_uq"], w=["psA"])
                o_ = ostg[oi % 3]
                okey = "ostg%d" % (oi % 3)
                oi += 1
                k.op("act", lambda e, o_=o_: e.activation(out=o_[:], in_=psA[:], func=AF.Copy), r=["psA"], w=[okey])
                k.dma("sp", lambda e, o_=o_, h=h, t0=t0: e.dma_start(out=D["QT"][h, :, t0:t0 + 512], in_=o_[:]), r=[okey], w=["QT"])
            for hp in range(4):
                for c in range(3):
                    k.op("pe", lambda e, c=c, hp=hp: e.matmul(psA[:], w_uq[:, c, 1024 + hp * 128:1024 + (hp + 1) * 128], cqT[:, c, :],
                                                              start=(c == 0), stop=(c == 2)), r=["cqT", "w_uq"], w=["psA"])
                for c in range(3):
                    k.op("pe", lambda e, c=c, hp=hp: e.matmul(psB[:], w_uq[:, c, 1536 + hp * 128:1536 + (hp + 1) * 128], cqT[:, c, :],
                                                              start=(c == 0), stop=(c == 2)), r=["cqT", "w_uq"], w=["psB"])
                o_ = ostg[oi % 3]
                okey = "ostg%d" % (oi % 3)
                oi += 1
                k.op("dve", lambda e: e.tensor_tensor(out=ra[:], in0=psA[:], in1=cosb[:], op=ALU.mult), r=["psA", "cosb"], w=["ra"])
                k.op("dve", lambda e: e.tensor_tensor(out=rb[:], in0=psB[:], in1=sinb[:], op=ALU.mult), r=["psB", "sinb"], w=["rb"])
                k.op("dve", lambda e, o_=o_: e.tensor_tensor(out=o_[:], in0=ra[:], in1=rb[:], op=ALU.add), r=["ra", "rb"], w=[okey])
                k.dma("sp", lambda e, o_=o_, hp=hp, t0=t0: e.dma_start(
                    out=D["QRT"][2 * hp:2 * hp + 2, :, t0:t0 + 512].rearrange("h d t -> (h d) t"), in_=o_[:]), r=[okey], w=["QRT"])
        k.barrier()


def phase_attn_main(pb, C, heads=range(NH), qblocks=range(16)):
    k, D, nc = pb.k, pb.D, pb.nc
    NKB = NTOK // 128
    with ExitStack() as es:
        KRT = pb.sb(es, "KRT", [128, NTOK], BF16)
        k.op("pool", lambda e: e.memset(KRT[64:128, :], 0.0), w=["sKRT"])
        k.dma("sp", lambda e: e.dma_start(out=KRT[0:64, :], in_=D["KRT"]), r=["KRT"], w=["sKRT"])
        KT = [pb.sb(es, "KT%d" % i, [128, NTOK], BF16) for i in range(2)]
        Vh = [pb.sb(es, "Vh%d" % i, [128, NKB, 130], BF16) for i in range(2)]
        for i in range(2):
            k.op("dve", lambda e, i=i: e.memset(Vh[i][:, :, 128:130], 1.0), w=["Vh%d" % i])
        Qb = [pb.sb(es, "Qb%d" % i, [128, 512], BF16) for i in range(2)]
        QRb = [pb.sb(es, "QRb%d" % i, [128, 512], BF16) for i in range(2)]
        for i in range(2):
            k.op("pool", lambda e, i=i: e.memset(QRb[i][64:128, :], 0.0), w=["QRb%d" % i])
        PT = [pb.sb(es, "PT%d" % i, [128, 512], BF16) for i in range(3)]
        Ost = [pb.sb(es, "Ost%d" % i, [128, 4, 128], BF16) for i in range(2)]
        rinv = pb.sb(es, "rinv", [128, 8], F32)
        psS = [pb.ps(es, "psS%d" % i, [128, 512], F32) for i in range(2)]
        psO = [pb.ps(es, "psO%d" % i, [128, 512], F32) for i in range(4)]
        qi = 0
        pi = 0
        si = 0
        for hi, h in enumerate(heads):
            hb = hi % 2
            k.dma("sp", lambda e, hb=hb, h=h: e.dma_start(out=KT[hb][:], in_=D["KT"][h]), r=["KT"], w=["KT%d" % hb])
            k.dma("sp", lambda e, hb=hb, h=h: e.dma_start(
                out=Vh[hb][:, :, 0:128], in_=D["Vd"][:, h * 128:(h + 1) * 128].rearrange("(kb p) v -> p kb v", p=128)),
                r=["Vd"], w=["Vh%d" % hb])
            for qb in qblocks:
                b = qi % 2
                qi += 1
                q0 = qb * 512
                k.dma("sp", lambda e, b=b, h=h, q0=q0: e.dma_start(out=Qb[b][:], in_=D["QT"][h, :, q0:q0 + 512]),
                      r=["QT"], w=["Qb%d" % b])
                k.dma("sp", lambda e, b=b, h=h, q0=q0: e.dma_start(out=QRb[b][0:64, :], in_=D["QRT"][h, :, q0:q0 + 512]),
                      r=["QRT"], w=["QRb%d" % b])

                def qk(kb, sb_):
                    k.op("pe", lambda e: e.matmul(psS[sb_][:], KT[hb][:, kb * 128:(kb + 1) * 128], Qb[b][:], start=True, stop=False),
                         r=["KT%d" % hb, "Qb%d" % b], w=["psS%d" % sb_])
                    k.op("pe", lambda e: e.matmul(psS[sb_][:], KRT[:, kb * 128:(kb + 1) * 128], QRb[b][:], start=False, stop=True),
                         r=["sKRT", "QRb%d" % b], w=["psS%d" % sb_])

                sbs = []
                sbs.append(si % 2)
                qk(0, si % 2)
                si += 1
                for kb in range(NKB):
                    if kb + 1 < NKB:
                        sbs.append(si % 2)
                        qk(kb + 1, si % 2)
                        si += 1
                    sb_ = sbs[kb]
                    p_ = PT[pi % 3]
                    pkey = "PT%d" % (pi % 3)
                    pi += 1
                    k.op("act", lambda e, sb_=sb_, p_=p_: e.activation(out=p_[:], in_=psS[sb_][:], func=AF.Exp, scale=ATT_SCALE),
                         r=["psS%d" % sb_], w=[pkey])
                    for qt in range(4):
                        k.op("pe", lambda e, qt=qt, p_=p_, kb=kb: e.matmul(
                            psO[qt][:, 0:130], p_[:, qt * 128:(qt + 1) * 128], Vh[hb][:, kb, :],
                            start=(kb == 0), stop=(kb == NKB - 1)), r=[pkey, "Vh%d" % hb], w=["psO%d" % qt])
                o_ = Ost[b]
                okey = "Ost%d" % b
                for qt in range(4):
                    k.op("dve", lambda e, qt=qt: e.reciprocal(out=rinv[:, qt:qt + 1], in_=psO[qt][:, 128:129]),
                         r=["psO%d" % qt], w=["rinv"])
                    k.op("dve", lambda e, qt=qt, o_=o_: e.tensor_scalar(out=o_[:, qt, :], in0=psO[qt][:, 0:128],
                                                                        scalar1=rinv[:, qt:qt + 1], scalar2=None, op0=ALU.mult),
                         r=["psO%d" % qt, "rinv"], w=[okey])
                k.dma("sp", lambda e, o_=o_, h=h, q0=q0: e.dma_start(
                    out=D["Od"][q0:q0 + 512, h * 128:(h + 1) * 128].rearrange("(qt p) v -> p qt v", p=128), in_=o_[:]),
                    r=[okey], w=["Od"])
        k.barrier()


def phase_attn_post(pb, C):
    k, D, nc = pb.k, pb.D, pb.nc
    with ExitStack() as es:
        mv = load_modv(pb, es, 1, (2,))
        stg = pb.sb(es, "wstg", [128, 512], F32)
        w_o = load_w_bf16(pb, es, "w_o", D["w_o"], 8, 1024, stg, "wstg")
        ot = [pb.sb(es, "ot%d" % i, [128, 1024], BF16) for i in range(2)]
        oT = [pb.sb(es, "oT%d" % i, [128, 8, 128], BF16) for i in range(2)]
        xt = [pb.sb(es, "xt%d" % i, [128, 1024], F32) for i in range(2)]
        tm = [pb.sb(es, "tm%d" % i, [128, 1024], F32) for i in range(2)]
        psT = pb.ps(es, "psT", [128, 1024], BF16)
        psY = [pb.ps(es, "psY%d" % i, [128, 512], F32) for i in range(2)]
        for j in range(64):
            b = j % 2
            r0 = j * 128
            k.dma("sp", lambda e, b=b, r0=r0: e.dma_start(out=ot[b][:], in_=D["Od"][r0:r0 + 128, :]), r=["Od"], w=["ot%d" % b])
            k.dma("sp", lambda e, b=b, r0=r0: e.dma_start(out=xt[b][:], in_=D["xres"][r0:r0 + 128, :]), r=["xres"], w=["xt%d" % b])
            for c in range(8):
                k.op("pe", lambda e, b=b, c=c: e.transpose(out=psT[:, c * 128:(c + 1) * 128], in_=ot[b][:, c * 128:(c + 1) * 128],
                                                           identity=C["ident_b"][:]), r=["ot%d" % b, "ident_b"], w=["psT"])
            k.op("act", lambda e, b=b: e.activation(out=oT[b][:].rearrange("p a t -> p (a t)"), in_=psT[:], func=AF.Copy),
                 r=["psT"], w=["oT%d" % b])
            for dh in range(2):
                for c in range(8):
                    k.op("pe", lambda e, b=b, c=c, dh=dh: e.matmul(psY[dh][:], oT[b][:, c, :], w_o[:, c, dh * 512:(dh + 1) * 512],
                                                                   start=(c == 0), stop=(c == 7)), r=["oT%d" % b, "w_o"], w=["psY%d" % dh])
                k.op("dve", lambda e, b=b, dh=dh: e.tensor_tensor(out=tm[b][:, dh * 512:(dh + 1) * 512], in0=psY[dh][:],
                                                                  in1=mv[(0, 2)][:, dh * 512:(dh + 1) * 512], op=ALU.mult),
                     r=["psY%d" % dh, "modv"], w=["tm%d" % b])
            k.op("pool", lambda e, b=b: e.tensor_tensor(out=tm[b][:], in0=tm[b][:], in1=xt[b][:], op=ALU.add),
                 r=["tm%d" % b, "xt%d" % b], w=["tm%d" % b])
            k.dma("sp", lambda e, b=b, r0=r0: e.dma_start(out=D["xres"][r0:r0 + 128, :], in_=tm[b][:]), r=["tm%d" % b], w=["xres"])
        k.barrier()


def load_cols(pb, es, C, name, src_rows_ap, n, psbank, pskey):
    k = pb.k
    rows = pb.sb(es, name + "_r", [n, 128], F32)
    cols = pb.sb(es, name, [128, n], F32)
    k.dma("sp", lambda e: e.dma_start(out=rows[:], in_=src_rows_ap), w=[name + "_r"])
    k.op("pe", lambda e: e.transpose(out=psbank[:, 0:n], in_=rows[:], identity=C["ident_f"][0:n, 0:n]),
         r=[name + "_r", "ident_f"], w=[pskey])
    k.op("dve", lambda e: e.tensor_copy(out=cols[:], in_=psbank[:, 0:n]), r=[pskey], w=[name])
    return cols


def phase_mix_pass(pb, C, dirn, nsup_lat=32, do_ctx=True):
    k, D, nc = pb.k, pb.D, pb.nc
    W = 256
    with ExitStack() as es:
        P = [pb.ps(es, "P%d" % i, [128, 512], F32) for i in range(8)]
        PK = ["P%d" % i for i in range(8)]
        P0b = P[0][:].bitcast(BF16)
        mv = load_modv(pb, es, 0, (0, 1))
        stg = pb.sb(es, "wstg", [128, 512], F32)
        win = D["w_in"]
        wq = load_w_bf16(pb, es, "wq", win[:, 0:512], 8, 512, stg, "wstg")
        wf = load_w_bf16(pb, es, "wf", win[:, 512 + 512 * dirn:1024 + 512 * dirn], 8, 512, stg, "wstg")
        wi = load_w_bf16(pb, es, "wi", win[:, 1536:2048], 8, 512, stg, "wstg")
        wx = load_w_bf16(pb, es, "wx", win[:, 3072:4096], 8, 1024, stg, "wstg")
        wdt = load_w_bf16(pb, es, "wdt", win[:, 4096 + 8 * dirn:4104 + 8 * dirn], 8, 8, stg, "wstg")
        lg0 = load_cols(pb, es, C, "lg0", D["lb_gamma"][0, dirn, :].rearrange("(c p) -> c p", p=128), 4, P[7], PK[7])
        lg1 = load_cols(pb, es, C, "lg1", D["lb_gamma"][1, dirn, :].rearrange("(c p) -> c p", p=128), 4, P[7], PK[7])
        lbc = pb.sb(es, "lbc", [128, 4], F32)
        oml = pb.sb(es, "oml", [128, 4], F32)
        k.op("dve", lambda e: e.tensor_tensor(out=lbc[:], in0=lg0[:], in1=lg1[:], op=ALU.subtract), r=["lg0", "lg1"], w=["lbc"])
        k.op("act", lambda e: e.activation(out=lbc[:], in_=lbc[:], func=AF.Sigmoid), r=["lbc"], w=["lbc"])
        k.op("dve", lambda e: e.tensor_scalar(out=oml[:], in0=lbc[:], scalar1=-1.0, scalar2=1.0, op0=ALU.mult, op1=ALU.add),
             r=["lbc"], w=["oml"])
        cw = [load_cols(pb, es, C, "cw%d" % j, D["conv_w"][j, :].rearrange("(c p) -> c p", p=128), 8, P[7], PK[7]) for j in range(5)]
        cbc = load_cols(pb, es, C, "cbc", D["conv_b"].rearrange("o (c p) -> (o c) p", p=128), 8, P[7], PK[7])
        dtb = pb.sb(es, "dtb", [128, 8], F32)
        negA = pb.sb(es, "negA", [128, 8], F32)
        k.dma("sp", lambda e: e.dma_start(out=dtb[:], in_=D["dt_bias"][dirn:dirn + 1, :].to_broadcast([128, 8])), w=["dtb"])
        k.dma("sp", lambda e: e.dma_start(out=negA[:], in_=D["a_log"][dirn:dirn + 1, :].to_broadcast([128, 8])), w=["negA"])
        k.op("act", lambda e: e.activation(out=negA[:], in_=negA[:], func=AF.Exp), r=["negA"], w=["negA"])
        k.op("dve", lambda e: e.tensor_scalar(out=negA[:], in0=negA[:], scalar1=-1.0, scalar2=None, op0=ALU.mult), r=["negA"], w=["negA"])
        dsk = pb.sb(es, "dsk", [128, 8], F32)
        k.dma("sp", lambda e: e.dma_start(out=dsk[:], in_=D["d_skip"].to_broadcast([128, 8])), w=["dsk"])
        if getattr(pb, 'stop', 99) == 0:
            k.barrier()
            return
        if dirn == 0:
            m_att_b, m_att_f = C["triu_incl_b"], C["triu_incl_f"]
            m_cum, m_wend, m_lh = C["triu_incl_f"], C["tril_strict_f"], C["tril_strict_f"]
            m_rhs = C["triu_incl_f"]
        else:
            m_att_b, m_att_f = C["tril_incl_b"], C["tril_incl_f"]
            m_cum, m_wend, m_lh = C["tril_incl_f"], C["triu_strict_f"], C["triu_strict_f"]
            m_rhs = C["tril_incl_f"]
        xt = [pb.sb(es, "xt%d" % i, [128, 1024], F32) for i in range(2)]
        an = [pb.sb(es, "an%d" % i, [128, 1024], BF16) for i in range(2)]
        hx = pb.sb(es, "hx", [4, 1024], F32)
        ah = pb.sb(es, "ah", [4, 1024], BF16)
        junk = pb.sb(es, "junk", [128, 1024], BF16)
        t1 = pb.sb(es, "t1", [128, 1024], F32)
        rstd = [pb.sb(es, "rstd%d" % i, [128, 4], F32) for i in range(3)]
        aT = pb.sb(es, "aT", [128, 8, W + 4], BF16)
        Qsil = pb.sb(es, "Qsil", [128, 4, W], F32)
        fg = pb.sb(es, "fg", [128, 4, W], F32)
        la = pb.sb(es, "la", [128, 4, W], F32)
        kk = pb.sb(es, "kk", [128, 4, W], F32)
        u = pb.sb(es, "u", [128, 8, W + 4], F32)
        acc = pb.sb(es, "acc", [128, 8, W], F32)
        tmp = pb.sb(es, "tmp", [128, 8, W], F32)
        xbc = pb.sb(es, "xbc", [128, 8, W], BF16)
        vtok = pb.sb(es, "vtok", [128, 2, 512], BF16)
        dtr = pb.sb(es, "dtr", [128, 2, 8], F32)
        bb = pb.sb(es, "bb", [128, 4, 128], F32)
        b2 = pb.sb(es, "b2", [128, 4, 128], F32)
        sc = pb.sb(es, "sc", [128, 3, 4], F32)
        esc = pb.sb(es, "esc", [128, 3, 4], F32)
        nb = pb.sb(es, "nb", [128, 4], F32)
        Eq = pb.sb(es, "Eq", [128, 4, 128], F32)
        Ek = pb.sb(es, "Ek", [128, 4, 128], F32)
        Qt = pb.sb(es, "Qt", [128, 4, 128], BF16)
        Kt = pb.sb(es, "Kt", [128, 4, 128], BF16)
        Ktok = pb.sb(es, "Ktok", [128, 4, 128], BF16)
        attm = pb.sb(es, "attm", [128, 8, 128], BF16)
        Sh = pb.sb(es, "Sh", [128, 4, 64], F32)
        ShpA = pb.sb(es, "ShpA", [128, 4, 64], BF16)
        ShpB = pb.sb(es, "ShpB", [128, 4, 64], BF16)
        KtA = pb.sb(es, "KtA", [128, 4, 128], BF16)
        KtB = pb.sb(es, "KtB", [128, 4, 128], BF16)
        for nm_, t_ in (("ShpA", ShpA), ("ShpB", ShpB), ("KtA", KtA), ("KtB", KtB)):
            k.op("dve", lambda e, t_=t_: e.memset(t_[:], 0.0), w=[nm_])
        tU = pb.sb(es, "tU", [128, 4, 64], F32)
        osb = pb.sb(es, "osb", [128, 512], F32)
        xmt = pb.sb(es, "xmt", [128, 8, 64], BF16)
        Btok = pb.sb(es, "Btok", [128, 2, 128], BF16)
        dts = pb.sb(es, "dts", [128, 4, 8], F32)
        ec = pb.sb(es, "ec", [128, 24], F32)
        LH = pb.sb(es, "LH", [128, 8, 128], F32)
        Em = pb.sb(es, "Em", [128, 8, 128], F32)
        cbm = pb.sb(es, "cbm", [128, 2, 128], F32)
        Mm = pb.sb(es, "Mm", [128, 8, 128], BF16)
        xdt = pb.sb(es, "xdt", [128, 8, 64], BF16)
        xdtw = pb.sb(es, "xdtw", [128, 8, 64], BF16)
        yi = pb.sb(es, "yi", [128, 8, 64], F32)
        ysb = pb.sb(es, "ysb", [128, 8, 64], F32)
        Ss = pb.sb(es, "Ss", [128, 8, 64], F32)
        Ssb = pb.sb(es, "Ssb", [128, 8, 64], BF16)
        k.op("dve", lambda e: e.memset(Sh[:], 0.0), w=["Sh"])
        k.op("dve", lambda e: e.memset(Ss[:], 0.0), w=["Ss"])
        k.op("dve", lambda e: e.memset(Ssb[:], 0.0), w=["Ssb"])

        supers = []
        if do_ctx:
            supers.append((1, 0))
        lat_order = list(range(nsup_lat)) if dirn == 0 else list(range(nsup_lat - 1, -1, -1))
        supers += [(0, i) for i in lat_order]
        bank_rot = [1, 2, 3]
        bri = 0
        xi = 0
        for (s, si) in supers:
            src = D["x"] if s == 0 else D["ctx"]
            Ts = T_LAT if s == 0 else T_CTX
            r0 = si * W
            g0 = r0 if s == 0 else T_LAT + r0
            for tl in range(2):
                b = xi % 2
                xi += 1
                rr = r0 + tl * 128
                k.dma("sp", lambda e, b=b, rr=rr: e.dma_start(out=xt[b][:], in_=src[rr:rr + 128, :]), w=["xt%d" % b])
                norm_mod_tile(pb, xt[b], "xt%d" % b, rstd[b], "rstd%d" % b, junk, t1, mv[(s, 1)], mv[(s, 0)], an[b], "an%d" % b)
                for kc in range(8):
                    k.op("pe", lambda e, b=b, kc=kc: e.transpose(out=P0b[:, kc * 128:(kc + 1) * 128],
                                                                 in_=an[b][:, kc * 128:(kc + 1) * 128], identity=C["ident_b"][:]),
                         r=["an%d" % b, "ident_b"], w=[PK[0]])
                k.op("act", lambda e, tl=tl: e.activation(out=aT[:, :, 2 + tl * 128:2 + (tl + 1) * 128],
                                                          in_=P0b.rearrange("p (a t) -> p a t", a=8), func=AF.Copy),
                     r=[PK[0]], w=["aT"])
            if getattr(pb, 'stop', 99) == 1:
                k.barrier()
                return
            has_l = r0 >= 2
            has_r = r0 + W + 2 <= Ts
            lrow = r0 - 2 if has_l else 0
            rrow = r0 + W if has_r else Ts - 2
            k.dma("sp", lambda e: e.dma_start(out=hx[0:2, :], in_=src[lrow:lrow + 2, :]), w=["hx"])
            k.dma("sp", lambda e: e.dma_start(out=hx[2:4, :], in_=src[rrow:rrow + 2, :]), w=["hx"])
            k.op("act", lambda e: e.activation(out=junk[0:4, :], in_=hx[:], func=AF.Square, accum_out=rstd[2][0:4, 0:1]),
                 r=["hx"], w=["junk", "rstd2"])
            k.op("act", lambda e: e.activation(out=rstd[2][0:4, 1:2], in_=rstd[2][0:4, 0:1], func=AF.Sqrt, bias=EPS, scale=1.0 / DM),
                 r=["rstd2"], w=["rstd2"])
            k.op("dve", lambda e: e.reciprocal(out=rstd[2][0:4, 2:3], in_=rstd[2][0:4, 1:2]), r=["rstd2"], w=["rstd2"])
            k.op("dve", lambda e: e.scalar_tensor_tensor(out=t1[0:4, :], in0=hx[:], scalar=rstd[2][0:4, 2:3], in1=mv[(s, 1)][0:4, :],
                                                         op0=ALU.mult, op1=ALU.mult), r=["hx", "rstd2", "modv"], w=["t1"])
            k.op("pool", lambda e: e.tensor_tensor(out=ah[:], in0=t1[0:4, :], in1=mv[(s, 0)][0:4, :], op=ALU.add),
                 r=["t1", "modv"], w=["ah"])
            for kc in range(8):
                k.op("pe", lambda e, kc=kc: e.transpose(out=P0b[:, kc * 4:(kc + 1) * 4], in_=ah[:, kc * 128:(kc + 1) * 128],
                                                        identity=C["ident_b"][0:4, 0:4]), r=["ah", "ident_b"], w=[PK[0]])
            hv = P0b[:, 0:32].rearrange("p (a t) -> p a t", a=8)
            if has_l:
                k.op("dve", lambda e: e.tensor_copy(out=aT[:, :, 0:2], in_=hv[:, :, 0:2]), r=[PK[0]], w=["aT"])
            else:
                k.op("dve", lambda e: e.memset(aT[:, :, 0:2], 0.0), r=[PK[0]], w=["aT"])
            if has_r:
                k.op("dve", lambda e: e.tensor_copy(out=aT[:, :, W + 2:W + 4], in_=hv[:, :, 2:4]), r=[PK[0]], w=["aT"])
            else:
                k.op("dve", lambda e: e.memset(aT[:, :, W + 2:W + 4], 0.0), r=[PK[0]], w=["aT"])
            if getattr(pb, 'stop', 99) == 2:
                k.barrier()
                return
            for c in range(4):
                bk = bank_rot[bri % 3]
                bri += 1
                for kc in range(8):
                    k.op("pe", lambda e, c=c, kc=kc, bk=bk: e.matmul(P[bk][:, 0:W], wq[:, kc, c * 128:(c + 1) * 128], aT[:, kc, 2:W + 2],
                                                                     start=(kc == 0), stop=(kc == 7)), r=["wq", "aT"], w=[PK[bk]])
                k.op("act", lambda e, c=c, bk=bk: e.activation(out=Qsil[:, c, :], in_=P[bk][:, 0:W], func=AF.Silu), r=[PK[bk]], w=["Qsil"])
            for c in range(4):
                bk = bank_rot[bri % 3]
                bri += 1
                for kc in range(8):
                    k.op("pe", lambda e, c=c, kc=kc, bk=bk: e.matmul(P[bk][:, 0:W], wf[:, kc, c * 128:(c + 1) * 128], aT[:, kc, 2:W + 2],
                                                                     start=(kc == 0), stop=(kc == 7)), r=["wf", "aT"], w=[PK[bk]])
                k.op("act", lambda e, c=c, bk=bk: e.activation(out=fg[:, c, :], in_=P[bk][:, 0:W], func=AF.Sigmoid), r=[PK[bk]], w=["fg"])
                k.op("dve", lambda e, c=c: e.tensor_scalar(out=fg[:, c, :], in0=fg[:, c, :], scalar1=oml[:, c:c + 1], scalar2=lbc[:, c:c + 1],
                                                           op0=ALU.mult, op1=ALU.add), r=["fg", "oml", "lbc"], w=["fg"])
            k.op("act", lambda e: e.activation(out=la[:], in_=fg[:], func=AF.Ln), r=["fg"], w=["la"])
            k.op("dve", lambda e: e.tensor_scalar(out=kk[:], in0=fg[:], scalar1=-1.0, scalar2=1.0, op0=ALU.mult, op1=ALU.add),
                 r=["fg"], w=["kk"])
            for c in range(8):
                bk = bank_rot[bri % 3]
                bri += 1
                for kc in range(8):
                    k.op("pe", lambda e, c=c, kc=kc, bk=bk: e.matmul(P[bk][:, 0:W + 4], wx[:, kc, c * 128:(c + 1) * 128], aT[:, kc, :],
                                                                     start=(kc == 0), stop=(kc == 7)), r=["wx", "aT"], w=[PK[bk]])
                k.op("act", lambda e, c=c, bk=bk: e.activation(out=u[:, c, :], in_=P[bk][:, 0:W + 4], func=AF.Copy), r=[PK[bk]], w=["u"])
            if getattr(pb, 'stop', 99) == 3:
                k.barrier()
                return
            for j in range(5):
                if j == 0:
                    k.op("dve", lambda e, j=j: e.tensor_tensor(out=acc[:], in0=u[:, :, j:j + W],
                                                               in1=cw[j][:].unsqueeze(2).to_broadcast([128, 8, W]), op=ALU.mult),
                         r=["u", "cw0"], w=["acc"])
                else:
                    k.op("pool", lambda e, j=j: e.tensor_tensor(out=tmp[:], in0=u[:, :, j:j + W],
                                                                in1=cw[j][:].unsqueeze(2).to_broadcast([128, 8, W]), op=ALU.mult),
                         r=["u", "cw%d" % j], w=["tmp"])
                    k.op("dve", lambda e: e.tensor_tensor(out=acc[:], in0=acc[:], in1=tmp[:], op=ALU.add), r=["acc", "tmp"], w=["acc"])
            k.op("dve", lambda e: e.tensor_tensor(out=acc[:], in0=acc[:], in1=cbc[:].unsqueeze(2).to_broadcast([128, 8, W]), op=ALU.add),
                 r=["acc", "cbc"], w=["acc"])
            k.op("act", lambda e: e.activation(out=xbc[:], in_=acc[:], func=AF.Silu), r=["acc"], w=["xbc"])
            if getattr(pb, 'stop', 99) == 4:
                k.barrier()
                return
            for tl in range(2):
                sl0 = 2 + tl * 128
                for kc in range(8):
                    k.op("pe", lambda e, kc=kc, sl0=sl0: e.matmul(P[6][:], aT[:, kc, sl0:sl0 + 128], wi[:, kc, :],
                                                                  start=(kc == 0), stop=(kc == 7)), r=["aT", "wi"], w=[PK[6]])
                k.op("act", lambda e, tl=tl: e.activation(out=vtok[:, tl, :], in_=P[6][:], func=AF.Copy), r=[PK[6]], w=["vtok"])
                for kc in range(8):
                    k.op("pe", lambda e, kc=kc, sl0=sl0: e.matmul(P[7][:, 0:8], aT[:, kc, sl0:sl0 + 128], wdt[:, kc, :],
                                                                  start=(kc == 0), stop=(kc == 7)), r=["aT", "wdt"], w=[PK[7]])
                k.op("dve", lambda e, tl=tl: e.tensor_tensor(out=dtr[:, tl, :], in0=P[7][:, 0:8], in1=dtb[:], op=ALU.add),
                     r=[PK[7], "dtb"], w=["dtr"])
            if getattr(pb, 'stop', 99) == 5:
                k.barrier()
                return
            tls = [0, 1] if dirn == 0 else [1, 0]
            for tl in tls:
                sl = slice(tl * 128, (tl + 1) * 128)
                grow = g0 + tl * 128
                for c in range(4):
                    k.op("dve", lambda e, c=c: e.tensor_tensor_scan(out=b2[:, c, :], data0=C["ones_f"][:, 0:128], data1=la[:, c, sl],
                                                                    initial=0.0, op0=ALU.mult, op1=ALU.add),
                         r=["la", "ones_f"], w=["b2"])
                if dirn == 0:
                    bsrc, bkey, last = b2, "b2", 127
                else:
                    k.op("dve", lambda e: e.tensor_tensor(out=bb[:], in0=la[:, :, sl], in1=b2[:], op=ALU.subtract), r=["la", "b2"], w=["bb"])
                    for c in range(4):
                        k.op("dve", lambda e, c=c: e.tensor_scalar(out=bb[:, c, :], in0=bb[:, c, :], scalar1=b2[:, c, 127:128], scalar2=None,
                                                                   op0=ALU.add), r=["bb", "b2"], w=["bb"])
                    bsrc, bkey, last = bb, "bb", 0
                k.op("dve", lambda e: e.tensor_copy(out=sc[:, 0, :], in_=bsrc[:, :, 64]), r=[bkey], w=["sc"])
                k.op("dve", lambda e: e.tensor_copy(out=sc[:, 2, :], in_=bsrc[:, :, last]), r=[bkey], w=["sc"])
                k.op("dve", lambda e: e.tensor_tensor(out=sc[:, 1, :], in0=sc[:, 2, :], in1=sc[:, 0, :], op=ALU.subtract), r=["sc"], w=["sc"])
                k.op("dve", lambda e: e.tensor_scalar(out=nb[:], in0=sc[:, 0, :], scalar1=-1.0, scalar2=None, op0=ALU.mult), r=["sc"], w=["nb"])
                k.op("act", lambda e: e.activation(out=esc[:], in_=sc[:], func=AF.Exp), r=["sc"], w=["esc"])
                for c in range(4):
                    k.op("act", lambda e, c=c: e.activation(out=Eq[:, c, :], in_=bsrc[:, c, :], func=AF.Exp, bias=nb[:, c:c + 1], scale=1.0),
                         r=[bkey, "nb"], w=["Eq"])
                    k.op("act", lambda e, c=c: e.activation(out=Ek[:, c, :], in_=bsrc[:, c, :], func=AF.Exp, bias=sc[:, 0, c:c + 1], scale=-1.0),
                         r=[bkey, "sc"], w=["Ek"])
                k.op("dve", lambda e: e.tensor_tensor(out=Qt[:], in0=Qsil[:, :, sl], in1=Eq[:], op=ALU.mult), r=["Qsil", "Eq"], w=["Qt"])
                k.op("dve", lambda e: e.tensor_tensor(out=Kt[:], in0=kk[:, :, sl], in1=Ek[:], op=ALU.mult), r=["kk", "Ek"], w=["Kt"])
                k.op("pool", lambda e: e.tensor_copy(out=KtA[0:64, :, :], in_=Kt[0:64, :, :]), r=["Kt"], w=["KtA"])
                k.op("pool", lambda e: e.tensor_copy(out=KtB[64:128, :, :], in_=Kt[64:128, :, :]), r=["Kt"], w=["KtB"])
                k.op("dve", lambda e: e.tensor_tensor(out=ShpA[0:64, :, :], in0=Sh[0:64, :, :],
                                                      in1=esc[0:64, 0, :].unsqueeze(2).to_broadcast([64, 4, 64]), op=ALU.mult),
                     r=["Sh", "esc"], w=["ShpA"])
                k.op("dve", lambda e: e.tensor_tensor(out=ShpB[64:128, :, :], in0=Sh[64:128, :, :],
                                                      in1=esc[64:128, 0, :].unsqueeze(2).to_broadcast([64, 4, 64]), op=ALU.mult),
                     r=["Sh", "esc"], w=["ShpB"])
                if getattr(pb, 'stop', 99) == 51:
                    k.barrier()
                    return
                for c in range(4):
                    k.op("pe", lambda e, c=c: e.transpose(out=P0b[:, c * 128:(c + 1) * 128], in_=Kt[:, c, :], identity=C["ident_b"][:]),
                         r=["Kt", "ident_b"], w=[PK[0]])
                k.op("act", lambda e: e.activation(out=Ktok[:].rearrange("p a t -> p (a t)"), in_=P0b[:, 0:512], func=AF.Copy),
                     r=[PK[0]], w=["Ktok"])
                if getattr(pb, 'stop', 99) == 52:
                    k.barrier()
                    return
                for h in range(8):
                    c, base = h // 2, (h % 2) * 64
                    bk = 1 + h // 4
                    Kz = KtA if h % 2 == 0 else KtB
                    k.op("pe", lambda e, h=h, c=c, bk=bk, Kz=Kz: e.matmul(
                        P[bk][:, (h % 4) * 128:(h % 4 + 1) * 128], Kz[:, c, :], Qt[:, c, :], start=True, stop=True),
                        r=["KtA", "KtB", "Qt"], w=[PK[bk]])
                for hh in range(2):
                    k.op("dve", lambda e, hh=hh: e.tensor_tensor(
                        out=attm[:, hh * 4:(hh + 1) * 4, :], in0=P[1 + hh][:].rearrange("p (a t) -> p a t", a=4),
                        in1=m_att_f[:].unsqueeze(1).to_broadcast([128, 4, 128]), op=ALU.mult), r=[PK[1 + hh], "masks"], w=["attm"])
                if getattr(pb, 'stop', 99) == 53:
                    k.barrier()
                    return
                for h in range(8):
                    c, base = h // 2, (h % 2) * 64
                    k.op("pe", lambda e, h=h: e.matmul(P[3][:, h * 64:(h + 1) * 64], attm[:, h, :], vtok[:, tl, h * 64:(h + 1) * 64],
                                                       start=True, stop=False), r=["attm", "vtok"], w=[PK[3]])
                    Sz = ShpA if h % 2 == 0 else ShpB
                    k.op("pe", lambda e, h=h, c=c, Sz=Sz: e.matmul(P[3][:, h * 64:(h + 1) * 64], Qt[:, c, :],
                                                                   Sz[:, c, :], start=False, stop=True),
                         r=["Qt", "ShpA", "ShpB"], w=[PK[3]])
                k.op("act", lambda e: e.activation(out=osb[:], in_=P[3][:], func=AF.Copy), r=[PK[3]], w=["osb"])
                k.dma("sp", lambda e, grow=grow: e.dma_start(out=D["oh"][dirn, grow:grow + 128, :], in_=osb[:]), r=["osb"], w=["oh"])
                if getattr(pb, 'stop', 99) == 54:
                    k.barrier()
                    return
                for c in range(4):
                    k.op("pe", lambda e, c=c: e.matmul(P[4][:, c * 128:(c + 1) * 128], Ktok[:, c, :], vtok[:, tl, c * 128:(c + 1) * 128],
                                                       start=True, stop=True), r=["Ktok", "vtok"], w=[PK[4]])
                p4v = P[4][:].rearrange("p (c x) -> p c x", c=4)
                k.op("dve", lambda e: e.tensor_tensor(out=tU[0:64, :, :], in0=p4v[0:64, :, 0:64],
                                                      in1=esc[0:64, 1, :].unsqueeze(2).to_broadcast([64, 4, 64]), op=ALU.mult),
                     r=[PK[4], "esc"], w=["tU"])
                k.op("dve", lambda e: e.tensor_tensor(out=tU[64:128, :, :], in0=p4v[64:128, :, 64:128],
                                                      in1=esc[64:128, 1, :].unsqueeze(2).to_broadcast([64, 4, 64]), op=ALU.mult),
                     r=[PK[4], "esc"], w=["tU"])
                k.op("dve", lambda e: e.tensor_tensor(out=Sh[:], in0=Sh[:], in1=esc[:, 2, :].unsqueeze(2).to_broadcast([128, 4, 64]), op=ALU.mult),
                     r=["Sh", "esc"], w=["Sh"])
                k.op("dve", lambda e: e.tensor_tensor(out=Sh[:], in0=Sh[:], in1=tU[:], op=ALU.add), r=["Sh", "tU"], w=["Sh"])
                if getattr(pb, 'stop', 99) == 6:
                    k.barrier()
                    return
                for c in range(4):
                    k.op("pe", lambda e, c=c: e.transpose(out=P0b[:, c * 128:(c + 1) * 128], in_=xbc[:, c, sl], identity=C["ident_b"][:]),
                         r=["xbc", "ident_b"], w=[PK[0]])
                for g in range(2):
                    k.op("pe", lambda e, g=g: e.transpose(out=P0b[:, 512 + g * 128:512 + (g + 1) * 128], in_=xbc[:, 4 + g, sl],
                                                          identity=C["ident_b"][:]), r=["xbc", "ident_b"], w=[PK[0]])
                k.op("act", lambda e: e.activation(out=xmt[:].rearrange("p a v -> p (a v)"), in_=P0b[:, 0:512], func=AF.Copy),
                     r=[PK[0]], w=["xmt"])
                k.op("act", lambda e: e.activation(out=Btok[:].rearrange("p a v -> p (a v)"), in_=P0b[:, 512:768], func=AF.Copy),
                     r=[PK[0]], w=["Btok"])
                k.op("act", lambda e: e.activation(out=dts[:, 3, :], in_=dtr[:, tl, :], func=AF.Exp), r=["dtr"], w=["dts3"])
                k.op("act", lambda e: e.activation(out=dts[:, 0, :], in_=dts[:, 3, :], func=AF.Ln, bias=1.0, scale=1.0), r=["dts3"], w=["dts0"])
                k.op("dve", lambda e: e.tensor_tensor(out=dts[:, 1, :], in0=dts[:, 0, :], in1=negA[:], op=ALU.mult), r=["dts0", "negA"], w=["dts1"])
                if getattr(pb, 'stop', 99) == 7:
                    k.barrier()
                    return
                k.op("pe", lambda e: e.matmul(P[7][:, 0:8], m_cum[:], dts[:, 1, :], start=True, stop=True), r=["dts1", "masks"], w=[PK[7]])
                k.op("pe", lambda e: e.matmul(P[7][:, 8:16], m_wend[:], dts[:, 1, :], start=True, stop=True), r=["dts1", "masks"], w=[PK[7]])
                k.op("pe", lambda e: e.matmul(P[7][:, 16:24], C["ones_f"][:], dts[:, 1, :], start=True, stop=True), r=["dts1", "ones_f"], w=[PK[7]])
                k.op("act", lambda e: e.activation(out=ec[:], in_=P[7][:, 0:24], func=AF.Exp), r=[PK[7]], w=["ec"])
                k.op("dve", lambda e: e.tensor_tensor(out=LH[:], in0=m_lh[:].unsqueeze(1).to_broadcast([128, 8, 128]),
                                                      in1=dts[:, 1, :].unsqueeze(2).to_broadcast([128, 8, 128]), op=ALU.mult),
                     r=["dts1", "masks"], w=["LH"])
                for h in range(8):
                    bk = 1 + h // 4
                    k.op("pe", lambda e, h=h, bk=bk: e.matmul(P[bk][:, (h % 4) * 128:(h % 4 + 1) * 128], LH[:, h, :], m_rhs[:],
                                                              start=True, stop=True), r=["LH", "masks"], w=[PK[bk]])
                for hh in range(2):
                    k.op("act", lambda e, hh=hh: e.activation(out=Em[:, hh * 4:(hh + 1) * 4, :].rearrange("p a t -> p (a t)"), in_=P[1 + hh][:],
                                                              func=AF.Exp), r=[PK[1 + hh]], w=["Em"])
                if getattr(pb, 'stop', 99) == 8:
                    k.barrier()
                    return
                for g in range(2):
                    k.op("pe", lambda e, g=g: e.matmul(P[3][:, g * 128:(g + 1) * 128], xbc[:, 4 + g, sl], xbc[:, 6 + g, sl], start=True, stop=True),
                         r=["xbc"], w=[PK[3]])
                k.op("dve", lambda e: e.tensor_tensor(out=cbm[:], in0=P[3][:, 0:256].rearrange("p (a t) -> p a t", a=2),
                                                      in1=m_att_f[:].unsqueeze(1).to_broadcast([128, 2, 128]), op=ALU.mult),
                     r=[PK[3], "masks"], w=["cbm"])
                for g in range(2):
                    k.op("dve", lambda e, g=g: e.tensor_tensor(out=Mm[:, g * 4:(g + 1) * 4, :], in0=Em[:, g * 4:(g + 1) * 4, :],
                                                               in1=cbm[:, g, :].unsqueeze(1).to_broadcast([128, 4, 128]), op=ALU.mult),
                         r=["Em", "cbm"], w=["Mm"])
                k.op("dve", lambda e: e.tensor_tensor(out=dts[:, 2, :], in0=dts[:, 0, :], in1=ec[:, 8:16], op=ALU.mult), r=["dts0", "ec"], w=["dts2"])
                k.op("pool", lambda e: e.tensor_tensor(out=xdt[:], in0=xmt[:], in1=dts[:, 0, :].unsqueeze(2).to_broadcast([128, 8, 64]), op=ALU.mult),
                     r=["xmt", "dts0"], w=["xdt"])
                k.op("pool", lambda e: e.tensor_tensor(out=xdtw[:], in0=xmt[:], in1=dts[:, 2, :].unsqueeze(2).to_broadcast([128, 8, 64]), op=ALU.mult),
                     r=["xmt", "dts2"], w=["xdtw"])
                if getattr(pb, 'stop', 99) == 9:
                    k.barrier()
                    return
                for h in range(8):
                    k.op("pe", lambda e, h=h: e.matmul(P[4][:, h * 64:(h + 1) * 64], Mm[:, h, :], xdt[:, h, :], start=True, stop=True),
                         r=["Mm", "xdt"], w=[PK[4]])
                for g in range(2):
                    k.op("pe", lambda e, g=g: e.matmul(P[5][:, g * 256:(g + 1) * 256], xbc[:, 6 + g, sl],
                                                       Ssb[:, g * 4:(g + 1) * 4, :].rearrange("p a v -> p (a v)"), start=True, stop=True),
                         r=["xbc", "Ssb"], w=[PK[5]])
                k.op("dve", lambda e: e.tensor_tensor(out=yi[:], in0=P[5][:].rearrange("p (a v) -> p a v", a=8),
                                                      in1=ec[:, 0:8].unsqueeze(2).to_broadcast([128, 8, 64]), op=ALU.mult),
                     r=[PK[5], "ec"], w=["yi"])
                k.op("dve", lambda e: e.tensor_tensor(out=ysb[:], in0=P[4][:].rearrange("p (a v) -> p a v", a=8), in1=yi[:], op=ALU.add),
                     r=[PK[4], "yi"], w=["ysb"])
                if dirn == 0:
                    k.op("pool", lambda e: e.tensor_tensor(out=yi[:], in0=xmt[:], in1=dsk[:].unsqueeze(2).to_broadcast([128, 8, 64]), op=ALU.mult),
                         r=["xmt", "dsk", "ysb"], w=["yi"])
                    k.op("dve", lambda e: e.tensor_tensor(out=ysb[:], in0=ysb[:], in1=yi[:], op=ALU.add), r=["ysb", "yi"], w=["ysb"])
                k.dma("sp", lambda e, grow=grow: e.dma_start(out=D["ys"][dirn, grow:grow + 128, :], in_=ysb[:].rearrange("p a v -> p (a v)")),
                      r=["ysb"], w=["ys"])
                for g in range(2):
                    k.op("pe", lambda e, g=g: e.matmul(P[6][:, g * 256:(g + 1) * 256], Btok[:, g, :],
                                                       xdtw[:, g * 4:(g + 1) * 4, :].rearrange("p a v -> p (a v)"), start=True, stop=True),
                         r=["Btok", "xdtw"], w=[PK[6]])
                k.op("dve", lambda e: e.tensor_tensor(out=Ss[:], in0=Ss[:], in1=ec[:, 16:24].unsqueeze(2).to_broadcast([128, 8, 64]), op=ALU.mult),
                     r=["Ss", "ec"], w=["Ss"])
                k.op("dve", lambda e: e.tensor_tensor(out=Ss[:], in0=Ss[:], in1=P[6][:].rearrange("p (a v) -> p a v", a=8), op=ALU.add),
                     r=["Ss", PK[6]], w=["Ss"])
                k.op("act", lambda e: e.activation(out=Ssb[:], in_=Ss[:], func=AF.Copy), r=["Ss"], w=["Ssb"])
        k.barrier()


def phase_mix_merge(pb, C, ntiles=66):
    k, D, nc = pb.k, pb.D, pb.nc
    with ExitStack() as es:
        P = [pb.ps(es, "P%d" % i, [128, 512], F32) for i in range(8)]
        PK = ["P%d" % i for i in range(8)]
        P0b = P[0][:].bitcast(BF16)
        mv = load_modv(pb, es, 0, (0, 1, 2))
        stg = pb.sb(es, "wstg", [128, 512], F32)
        wg = load_w_bf16(pb, es, "wg", D["w_in"][:, 2048:2560], 8, 512, stg, "wstg")
        wz = load_w_bf16(pb, es, "wz", D["w_in"][:, 2560:3072], 8, 512, stg, "wstg")
        wo = load_w_bf16(pb, es, "wo", D["w_out_rec"], 8, 1024, stg, "wstg")
        hg = pb.sb(es, "hg", [128, 512], F32)
        mn = pb.sb(es, "mn", [128, 512], F32)
        k.dma("sp", lambda e: e.dma_start(out=hg[:], in_=D["hgrn_norm"].to_broadcast([128, 512])), w=["gains"])
        k.dma("sp", lambda e: e.dma_start(out=mn[:], in_=D["mamba_norm"].to_broadcast([128, 512])), w=["gains"])
        xt = [pb.sb(es, "xt%d" % i, [128, 1024], F32) for i in range(2)]
        an = [pb.sb(es, "an%d" % i, [128, 1024], BF16) for i in range(2)]
        junk = pb.sb(es, "junk", [128, 1024], BF16)
        t1 = pb.sb(es, "t1", [128, 1024], F32)
        rstd = [pb.sb(es, "rstd%d" % i, [128, 4], F32) for i in range(2)]
        aT = pb.sb(es, "aT", [128, 8, 128], BF16)
        sg = pb.sb(es, "sg", [128, 512], F32)
        sz = pb.sb(es, "sz", [128, 512], F32)
        o0 = [pb.sb(es, "o0%d" % i, [128, 512], F32) for i in range(2)]
        o1 = [pb.sb(es, "o1%d" % i, [128, 512], F32) for i in range(2)]
        y0 = [pb.sb(es, "y0%d" % i, [128, 512], F32) for i in range(2)]
        y1 = [pb.sb(es, "y1%d" % i, [128, 512], F32) for i in range(2)]
        sq = pb.sb(es, "sq", [128, 512], F32)
        st8 = pb.sb(es, "st8", [128, 3, 8], F32)
        rs2 = pb.sb(es, "rs2", [128, 4], F32)
        cat = pb.sb(es, "cat", [128, 1024], BF16)
        catT = pb.sb(es, "catT", [128, 8, 128], BF16)
        xo = [pb.sb(es, "xo%d" % i, [128, 1024], F32) for i in range(2)]
        for j in range(ntiles):
            b = j % 2
            if j < 2:
                s, src, rr, grow = 1, D["ctx"], j * 128, T_LAT + j * 128
            else:
                s, src, rr, grow = 0, D["x"], (j - 2) * 128, (j - 2) * 128
            k.dma("sp", lambda e: e.dma_start(out=xt[b][:], in_=src[rr:rr + 128, :]), w=["xt%d" % b])
            k.dma("sp", lambda e: e.dma_start(out=o0[b][:], in_=D["oh"][0, grow:grow + 128, :]), r=["oh"], w=["o0%d" % b])
            k.dma("sp", lambda e: e.dma_start(out=o1[b][:], in_=D["oh"][1, grow:grow + 128, :]), r=["oh"], w=["o1%d" % b])
            k.dma("sp", lambda e: e.dma_start(out=y0[b][:], in_=D["ys"][0, grow:grow + 128, :]), r=["ys"], w=["y0%d" % b])
            k.dma("sp", lambda e: e.dma_start(out=y1[b][:], in_=D["ys"][1, grow:grow + 128, :]), r=["ys"], w=["y1%d" % b])
            norm_mod_tile(pb, xt[b], "xt%d" % b, rstd[b], "rstd%d" % b, junk, t1, mv[(s, 1)], mv[(s, 0)], an[b], "an%d" % b)
            for kc in range(8):
                k.op("pe", lambda e, kc=kc: e.transpose(out=P0b[:, kc * 128:(kc + 1) * 128], in_=an[b][:, kc * 128:(kc + 1) * 128],
                                                        identity=C["ident_b"][:]), r=["an%d" % b, "ident_b"], w=[PK[0]])
            k.op("act", lambda e: e.activation(out=aT[:].rearrange("p a t -> p (a t)"), in_=P0b, func=AF.Copy), r=[PK[0]], w=["aT"])
            for kc in range(8):
                k.op("pe", lambda e, kc=kc: e.matmul(P[1][:], aT[:, kc, :], wg[:, kc, :], start=(kc == 0), stop=(kc == 7)),
                     r=["aT", "wg"], w=[PK[1]])
            k.op("act", lambda e: e.activation(out=sg[:], in_=P[1][:], func=AF.Sigmoid), r=[PK[1]], w=["sg"])
            for kc in range(8):
                k.op("pe", lambda e, kc=kc: e.matmul(P[2][:], aT[:, kc, :], wz[:, kc, :], start=(kc == 0), stop=(kc == 7)),
                     r=["aT", "wz"], w=[PK[2]])
            k.op("act", lambda e: e.activation(out=sz[:], in_=P[2][:], func=AF.Silu), r=[PK[2]], w=["sz"])
            ok0, ok1, yk0, yk1 = "o0%d" % b, "o1%d" % b, "y0%d" % b, "y1%d" % b
            k.op("dve", lambda e: e.tensor_tensor(out=o0[b][:], in0=o0[b][:], in1=o1[b][:], op=ALU.add), r=[ok0, ok1], w=[ok0])
            k.op("pool", lambda e: e.tensor_tensor(out=sq[:], in0=o0[b][:], in1=o0[b][:], op=ALU.mult), r=[ok0], w=["sq"])
            k.op("dve", lambda e: e.tensor_reduce(out=st8[:, 0, :], in_=sq[:].rearrange("p (a v) -> p a v", a=8), axis=AX.X, op=ALU.add),
                 r=["sq"], w=["st8"])
            k.op("act", lambda e: e.activation(out=st8[:, 1, :], in_=st8[:, 0, :], func=AF.Sqrt, bias=EPS, scale=1.0 / 64), r=["st8"], w=["st8"])
            k.op("dve", lambda e: e.reciprocal(out=st8[:, 2, :], in_=st8[:, 1, :]), r=["st8"], w=["st8"])
            k.op("dve", lambda e: e.tensor_tensor(out=o0[b][:].rearrange("p (a v) -> p a v", a=8), in0=o0[b][:].rearrange("p (a v) -> p a v", a=8),
                                                  in1=st8[:, 2, :].unsqueeze(2).to_broadcast([128, 8, 64]), op=ALU.mult), r=[ok0, "st8"], w=[ok0])
            k.op("pool", lambda e: e.tensor_tensor(out=o0[b][:], in0=o0[b][:], in1=hg[:], op=ALU.mult), r=[ok0, "gains"], w=[ok0])
            k.op("dve", lambda e: e.tensor_tensor(out=cat[:, 0:512], in0=o0[b][:], in1=sg[:], op=ALU.mult), r=[ok0, "sg"], w=["cat"])
            k.op("pool", lambda e: e.tensor_tensor(out=y0[b][:], in0=y0[b][:], in1=y1[b][:], op=ALU.add), r=[yk0, yk1], w=[yk0])
            k.op("dve", lambda e: e.tensor_tensor(out=y0[b][:], in0=y0[b][:], in1=sz[:], op=ALU.mult), r=[yk0, "sz"], w=[yk0])
            k.op("act", lambda e: e.activation(out=junk[:, 0:512], in_=y0[b][:], func=AF.Square, accum_out=rs2[:, 0:1]), r=[yk0], w=["junk", "rs2"])
            k.op("act", lambda e: e.activation(out=rs2[:, 1:2], in_=rs2[:, 0:1], func=AF.Sqrt, bias=EPS, scale=1.0 / 512), r=["rs2"], w=["rs2"])
            k.op("dve", lambda e: e.reciprocal(out=rs2[:, 2:3], in_=rs2[:, 1:2]), r=["rs2"], w=["rs2"])
            k.op("dve", lambda e: e.scalar_tensor_tensor(out=cat[:, 512:1024], in0=y0[b][:], scalar=rs2[:, 2:3], in1=mn[:],
                                                         op0=ALU.mult, op1=ALU.mult), r=[yk0, "rs2", "gains"], w=["cat"])
            for c in range(8):
                k.op("pe", lambda e, c=c: e.transpose(out=P0b[:, c * 128:(c + 1) * 128], in_=cat[:, c * 128:(c + 1) * 128],
                                                      identity=C["ident_b"][:]), r=["cat", "ident_b"], w=[PK[0]])
            k.op("act", lambda e: e.activation(out=catT[:].rearrange("p a t -> p (a t)"), in_=P0b, func=AF.Copy), r=[PK[0]], w=["catT"])
            for dh in range(2):
                for c in range(8):
                    k.op("pe", lambda e, c=c, dh=dh: e.matmul(P[3 + dh][:], catT[:, c, :], wo[:, c, dh * 512:(dh + 1) * 512],
                                                              start=(c == 0), stop=(c == 7)), r=["catT", "wo"], w=[PK[3 + dh]])
                k.op("dve", lambda e, dh=dh: e.tensor_tensor(out=xo[b][:, dh * 512:(dh + 1) * 512], in0=P[3 + dh][:],
                                                             in1=mv[(s, 2)][:, dh * 512:(dh + 1) * 512], op=ALU.mult),
                     r=[PK[3 + dh], "modv"], w=["xo%d" % b])
            k.op("pool", lambda e: e.tensor_tensor(out=xo[b][:], in0=xo[b][:], in1=xt[b][:], op=ALU.add), r=["xo%d" % b, "xt%d" % b], w=["xo%d" % b])
            k.dma("sp", lambda e: e.dma_start(out=D["xres"][grow:grow + 128, :], in_=xo[b][:]), r=["xo%d" % b], w=["xres"])
        k.barrier()


def declare_all(pb):
    pb.din("x", [T_LAT, DM]); pb.din("ctx", [T_CTX, DM]); pb.din("c", [1, DM]); pb.din("c_ctx", [1, DM])
    pb.din("w_mod", [2, DM, 6 * DM]); pb.din("b_mod", [2, 6 * DM]); pb.din("norm_mix", [2, DM]); pb.din("norm_ffn", [2, DM])
    pb.din("norm_out", [1, DM])
    pb.din("w_in", [DM, 4112]); pb.din("w_out_rec", [DM, DM]); pb.din("conv_w", [5, DM]); pb.din("conv_b", [1, DM])
    pb.din("lb_gamma", [2, 2, 512]); pb.din("dt_bias", [2, 8]); pb.din("a_log", [2, 8]); pb.din("d_skip", [1, 8])
    pb.din("hgrn_norm", [1, 512]); pb.din("mamba_norm", [1, 512])
    pb.din("w_dq", [DM, 384]); pb.din("q_norm", [1, 384]); pb.din("w_uq_r", [384, 2048]); pb.din("w_dkv", [DM, 256])
    pb.din("kv_norm", [1, 256]); pb.din("w_ukv_r", [256, 2048]); pb.din("w_kr_r", [DM, 128]); pb.din("w_o", [DM, DM])
    pb.din("w_router", [2, DM, NE]); pb.din("w_gate", [2, NE, DM, DM]); pb.din("w_up", [2, NE, DM, DM]); pb.din("w_down", [2, NE, DM, DM])
    for nm, v in host_consts().items():
        pb.din("c_" + nm, v.shape)
    for nm, v in attn_host_consts().items():
        pb.din("c_" + nm, v.shape)
    pb.dscr("xres", [NROWS, DM]); pb.dscr("hn", [NROWS, DM], BF16); pb.dscr("aff", [NROWS, 16]); pb.dscr("modrep", [2, 2, 6, 128, DM])
    pb.dscr("oh", [2, NTOK, 512]); pb.dscr("ys", [2, NTOK, 512])
    pb.dscr("KT", [NH, 128, NTOK], BF16); pb.dscr("KRT", [64, NTOK], BF16); pb.dscr("Vd", [NTOK, 1024], BF16)
    pb.dscr("QT", [NH, 128, T_LAT], BF16); pb.dscr("QRT", [NH, 64, T_LAT], BF16); pb.dscr("Od", [T_LAT, 1024], BF16)


def make_inputs(inp, b):
    f = lambda a: np.ascontiguousarray(a, dtype=np.float32)
    w_uq_r, w_ukv_r, w_kr_r = attn_layout_weights(inp["w_uq"][0], inp["w_ukv"][0], inp["w_kr"][0])
    im = {"x": f(inp["x"][b]), "ctx": f(inp["ctx"][b]), "c": f(inp["c"][b:b + 1]), "c_ctx": f(inp["c_ctx"][None, :]),
          "w_mod": f(inp["w_mod"]), "b_mod": f(inp["b_mod"]), "norm_mix": f(inp["norm_mix"]), "norm_ffn": f(inp["norm_ffn"]),
          "norm_out": f(inp["norm_out"][None, :]),
          "w_in": f(inp["w_in"][0]), "w_out_rec": f(inp["w_out_rec"][0]), "conv_w": f(inp["conv_w"][0]), "conv_b": f(inp["conv_b"]),
          "lb_gamma": f(inp["lb_gamma"]), "dt_bias": f(inp["dt_bias"][0]), "a_log": f(inp["a_log"][0]), "d_skip": f(inp["d_skip"]),
          "hgrn_norm": f(inp["hgrn_norm"]), "mamba_norm": f(inp["mamba_norm"]),
          "w_dq": f(inp["w_dq"][0]), "q_norm": f(inp["q_norm"]), "w_uq_r": f(w_uq_r), "w_dkv": f(inp["w_dkv"][0]),
          "kv_norm": f(inp["kv_norm"]), "w_ukv_r": f(w_ukv_r), "w_kr_r": f(w_kr_r), "w_o": f(inp["w_o"][0]),
          "w_router": f(inp["w_router"]), "w_gate": f(inp["w_gate"]), "w_up": f(inp["w_up"]), "w_down": f(inp["w_down"])}
    for nm, v in host_consts().items():
        im["c_" + nm] = v
    for nm, v in attn_host_consts().items():
        im["c_" + nm] = v
    return im


def build_program(phases, dbg=False, opts=None):
    opts = opts or {}
    nc = bass.Bass("TRN2", target_bir_lowering=False)
    pb = PB(nc)
    declare_all(pb)
    pb.dout("out", [T_LAT, DM])
    if dbg:
        pb.dout("dbg", [NTOK, DM])
    with ExitStack() as es:
        C = load_consts(pb, es)
        phase_init(pb, copy_x=("init" in phases))
        phase_mod(pb, C)
        if "mixf" in phases:
            phase_mix_pass(pb, C, 0, **opts.get("mix", {}))
        if "mixb" in phases:
            phase_mix_pass(pb, C, 1, **opts.get("mix", {}))
        if "mixm" in phases:
            phase_mix_merge(pb, C, **opts.get("merge", {}))
        if "moe0" in phases:
            phase_moe(pb, C, 0)
        if "attn_pre" in phases:
            phase_attn_pre(pb, C)
        if "attn_main" in phases:
            phase_attn_main(pb, C, **opts.get("attn", {}))
        if "attn_post" in phases:
            phase_attn_post(pb, C)
        if "moe1" in phases:
            phase_moe(pb, C, 1)
        if "final" in phases:
            phase_final(pb, C)
        k = pb.k
        if dbg:
            for i in range(8):
                k.dma("sp", lambda e, i=i: e.dma_start(out=pb.D["dbg"][i * 1056:(i + 1) * 1056, :], in_=pb.D["xres"][i * 1056:(i + 1) * 1056, :]),
                      r=["xres"], w=["dbg"])
        k.finish()
    return nc, pb


ALL_PHASES = ["mixf", "mixb", "mixm", "moe0", "attn_pre", "attn_main", "attn_post", "moe1", "final"]


def kernel(**inputs):
    from concourse.bass_utils import run_bass_kernel_spmd
    inp = {k: np.asarray(v) for k, v in inputs.items()}
    nb = inp["x"].shape[0]
    nc, pb = build_program(ALL_PHASES)
    in_maps = [make_inputs(inp, b) for b in range(nb)]
    res = run_bass_kernel_spmd(nc, in_maps, core_ids=list(range(nb)))
    out = np.stack([np.asarray(res.results[b]["out"]) for b in range(nb)], axis=0)
    return out.astype(np.float32)
```

```python
import numpy as np
import concourse.bass as bass
import concourse.mybir as mybir

F32 = mybir.dt.float32
BF16 = mybir.dt.bfloat16
I32 = mybir.dt.int32
U32 = mybir.dt.uint32
AF = mybir.ActivationFunctionType
ALU = mybir.AluOpType
AX = mybir.AxisListType


class KF:
    NDS = 8

    def __init__(self, nc):
        self.nc = nc
        self.eng = {"pe": nc.tensor, "dve": nc.vector, "act": nc.scalar, "pool": nc.gpsimd, "sp": nc.sync}
        self.csem = {}
        self.ccnt = {}
        for e in self.eng:
            self.csem[e] = nc.alloc_semaphore("cs_" + e)
            self.ccnt[e] = 0
        self.dsem = {}
        self.dval = {}
        self.drot = {}
        for q in ("sp", "pool", "act"):
            self.dsem[q] = [nc.alloc_semaphore("ds_%s%d" % (q, i)) for i in range(self.NDS)]
            self.dval[q] = [0] * self.NDS
            self.drot[q] = 0
        self.waited = {e: {} for e in self.eng}
        self.lastw = {}
        self.readers = {}
        self.same_eng_sync = {"pe": False, "dve": True, "act": True, "pool": True, "sp": True}
        self.nins = 0

    def _wait(self, e, tok):
        sem, val, src = tok
        if src == e and not self.same_eng_sync[e]:
            return
        key = id(sem)
        if self.waited[e].get(key, 0) >= val:
            return
        self.eng[e].wait_ge(sem, val)
        self.waited[e][key] = val
        self.nins += 1

    def _deps(self, e, r, w):
        for k in r:
            t = self.lastw.get(k)
            if t is not None:
                self._wait(e, t)
        for k in w:
            t = self.lastw.get(k)
            if t is not None:
                self._wait(e, t)
            for t in self.readers.get(k, ()):
                self._wait(e, t)

    def _commit(self, tok, r, w):
        for k in w:
            self.lastw[k] = tok
            self.readers[k] = []
        for k in r:
            self.readers.setdefault(k, []).append(tok)

    def op(self, e, fn, r=(), w=()):
        r = [x for x in r if x is not None]
        w = [x for x in w if x is not None]
        self._deps(e, r, w)
        ins = fn(self.eng[e])
        self.ccnt[e] += 1
        ins.then_inc(self.csem[e], 1)
        tok = (self.csem[e], self.ccnt[e], e)
        self._commit(tok, r, w)
        self.nins += 1
        return tok

    def dma(self, q, fn, r=(), w=()):
        r = [x for x in r if x is not None]
        w = [x for x in w if x is not None]
        i = self.drot[q]
        self.drot[q] = (i + 1) % self.NDS
        sem = self.dsem[q][i]
        self._wait(q, (sem, self.dval[q][i], None))
        self._deps(q, r, w)
        ins = fn(self.eng[q])
        self.dval[q][i] += 16
        ins.then_inc(sem, 16)
        tok = (sem, self.dval[q][i], None)
        self._commit(tok, r, w)
        self.nins += 1
        return tok

    def barrier(self):
        toks = []
        for e in self.eng:
            if self.ccnt[e] > 0:
                toks.append((self.csem[e], self.ccnt[e], None))
        for q in self.dsem:
            for i in range(self.NDS):
                if self.dval[q][i] > 0:
                    toks.append((self.dsem[q][i], self.dval[q][i], None))
        for e in self.eng:
            for t in toks:
                if t[0] is self.csem[e]:
                    continue
                self._wait(e, t)
        self.lastw = {}
        self.readers = {}

    def finish(self):
        for e in self.eng:
            if e != "sp" and self.ccnt[e] > 0:
                self._wait("sp", (self.csem[e], self.ccnt[e], None))
        for q in self.dsem:
            for i in range(self.NDS):
                if self.dval[q][i] > 0:
                    self._wait("sp", (self.dsem[q][i], self.dval[q][i], None))

from contextlib import ExitStack

T_LAT = 8192
T_CTX = 256
NTOK = T_LAT + T_CTX
NROWS = NTOK + 128
DM = 1024
EPS = 1e-6
NE = 16


def host_consts():
    c = {}
    c["ident"] = np.eye(128, dtype=np.float32)
    p = np.arange(128)
    c["triu_incl"] = (p[:, None] <= p[None, :]).astype(np.float32)
    c["triu_strict"] = (p[:, None] < p[None, :]).astype(np.float32)
    c["tril_incl"] = (p[:, None] >= p[None, :]).astype(np.float32)
    c["tril_strict"] = (p[:, None] > p[None, :]).astype(np.float32)
    c["iota_q"] = np.tile(np.arange(1024, dtype=np.float32)[None, :], (128, 1))
    q = np.arange(128, dtype=np.float32)
    c["ctxadd"] = np.tile((T_LAT + np.maximum(q - 32, 0))[None, :], (128, 1)).astype(np.float32)
    return c


class PB:
    def __init__(self, nc):
        self.nc = nc
        self.k = KF(nc)
        self.D = {}
        self.uid = 0

    def din(self, name, shape, dt=F32):
        self.D[name] = self.nc.dram_tensor(name, list(shape), dt, kind="ExternalInput").ap()
        return self.D[name]

    def dout(self, name, shape, dt=F32):
        self.D[name] = self.nc.dram_tensor(name, list(shape), dt, kind="ExternalOutput").ap()
        return self.D[name]

    def dscr(self, name, shape, dt=F32):
        self.D[name] = self.nc.dram_tensor(name, list(shape), dt, kind="Internal").ap()
        return self.D[name]

    def sb(self, es, name, shape, dt):
        self.uid += 1
        return es.enter_context(self.nc.sbuf_tensor("%s_%d" % (name, self.uid), list(shape), dt))

    def ps(self, es, name, shape, dt):
        self.uid += 1
        return es.enter_context(self.nc.psum_tensor("%s_%d" % (name, self.uid), list(shape), dt))


def load_consts(pb, es):
    k, D = pb.k, pb.D
    C = {}
    for nm in ("ident", "triu_incl", "triu_strict", "tril_incl", "tril_strict"):
        f = pb.sb(es, nm + "_f", [128, 128], F32)
        b = pb.sb(es, nm + "_b", [128, 128], BF16)
        k.dma("sp", lambda e, f=f, nm=nm: e.dma_start(out=f[:], in_=D["c_" + nm]), w=[nm + "_f"])
        k.op("dve", lambda e, f=f, b=b: e.tensor_copy(out=b[:], in_=f[:]), r=[nm + "_f"], w=[nm + "_b"])
        C[nm + "_f"] = f
        C[nm + "_b"] = b
    ones_f = pb.sb(es, "ones_f", [128, 128], F32)
    ones_b = pb.sb(es, "ones_b", [128, 128], BF16)
    k.op("dve", lambda e: e.memset(ones_f[:], 1.0), w=["ones_f"])
    k.op("dve", lambda e: e.memset(ones_b[:], 1.0), w=["ones_b"])
    C["ones_f"] = ones_f
    C["ones_b"] = ones_b
    return C


def phase_init(pb, copy_x=True):
    k, D = pb.k, pb.D
    with ExitStack() as es:
        z = pb.sb(es, "zt", [128, 1024], F32)
        zb = pb.sb(es, "zb", [128, 1024], BF16)
        k.op("dve", lambda e: e.memset(z[:], 0.0), w=["zt"])
        k.op("dve", lambda e: e.memset(zb[:], 0.0), w=["zb"])
        if copy_x:
            for i in range(8):
                k.dma("sp", lambda e, i=i: e.dma_start(out=D["xres"][i * 1024:(i + 1) * 1024, :],
                                                       in_=D["x"][i * 1024:(i + 1) * 1024, :]), w=["xres"])
            k.dma("sp", lambda e: e.dma_start(out=D["xres"][T_LAT:NTOK, :], in_=D["ctx"]), w=["xres"])
        k.dma("sp", lambda e: e.dma_start(out=D["xres"][NTOK:NROWS, :], in_=z[:]), r=["zt"], w=["xres"])
        k.dma("sp", lambda e: e.dma_start(out=D["hn"][NTOK:NROWS, :], in_=zb[:]), r=["zb"], w=["hn"])
        k.dma("sp", lambda e: e.dma_start(out=D["aff"][NTOK:NROWS, :], in_=z[:, 0:16]), r=["zt"], w=["aff"])
        k.barrier()


def phase_mod(pb, C):
    k, D, nc = pb.k, pb.D, pb.nc
    with ExitStack() as es:
        s8 = pb.sb(es, "s8", [8, 2, 128], F32)
        scol = pb.sb(es, "scol", [128, 2, 8], F32)
        srep = pb.sb(es, "srep", [128, 2, 8, 128], F32)
        ps_t = pb.ps(es, "ps_t", [128, 512], F32)
        k.dma("sp", lambda e: e.dma_start(out=s8[:, 0, :], in_=D["c"].rearrange("o (a b) -> (o a) b", a=8)), w=["s8"])
        k.dma("sp", lambda e: e.dma_start(out=s8[:, 1, :], in_=D["c_ctx"].rearrange("o (a b) -> (o a) b", a=8)), w=["s8"])
        k.op("act", lambda e: e.activation(out=s8[:], in_=s8[:], func=AF.Silu), r=["s8"], w=["s8"])
        for s in range(2):
            k.op("pe", lambda e, s=s: e.transpose(out=ps_t[:, s * 8:(s + 1) * 8], in_=s8[:, s, :],
                                                  identity=C["ident_f"][0:8, 0:8]), r=["s8", "ident_f"], w=["ps_t"])
        k.op("dve", lambda e: e.tensor_copy(out=scol[:].rearrange("p s c -> p (s c)"), in_=ps_t[:, 0:16]),
             r=["ps_t"], w=["scol"])
        for s in range(2):
            k.op("dve", lambda e, s=s: e.tensor_copy(out=srep[:, s, :, :],
                                                     in_=scol[:, s, :].unsqueeze(2).to_broadcast([128, 8, 128])),
                 r=["scol"], w=["srep"])
        wm = [pb.sb(es, "wm%d" % i, [128, 8, 512], F32) for i in range(2)]
        bm = [pb.sb(es, "bm%d" % i, [128, 512], F32) for i in range(2)]
        gn = [pb.sb(es, "gn%d" % i, [128, 512], F32) for i in range(2)]
        ob = [pb.sb(es, "ob%d" % i, [128, 512], F32) for i in range(4)]
        psm = [pb.ps(es, "psm%d" % i, [128, 512], F32) for i in range(2)]
        it = 0
        oi = 0
        for l in range(2):
            for ncn in range(12):
                b = it % 2
                it += 1
                n0 = ncn * 512
                slot = ncn // 2
                half = ncn % 2
                k.dma("sp", lambda e, b=b, l=l, n0=n0: e.dma_start(
                    out=wm[b][:], in_=D["w_mod"][l, :, n0:n0 + 512].rearrange("(kc p) n -> p kc n", p=128)),
                    w=["wm%d" % b])
                k.dma("sp", lambda e, b=b, l=l, n0=n0: e.dma_start(
                    out=bm[b][:], in_=D["b_mod"][l:l + 1, n0:n0 + 512].to_broadcast([128, 512])), w=["bm%d" % b])
                if slot in (1, 4):
                    gsrc = D["norm_mix"] if slot == 1 else D["norm_ffn"]
                    k.dma("sp", lambda e, b=b, l=l, half=half, gsrc=gsrc: e.dma_start(
                        out=gn[b][:], in_=gsrc[l:l + 1, half * 512:(half + 1) * 512].to_broadcast([128, 512])),
                        w=["gn%d" % b])
                for s in range(2):
                    pm = psm[s]
                    for kc in range(8):
                        k.op("pe", lambda e, s=s, kc=kc, b=b, pm=pm: e.matmul(
                            pm[:], srep[:, s, kc, :], wm[b][:, kc, :], start=(kc == 0), stop=(kc == 7)),
                            r=["srep", "wm%d" % b], w=["psm%d" % s])
                    o = ob[oi % 4]
                    okey = "ob%d" % (oi % 4)
                    oi += 1
                    k.op("dve", lambda e, o=o, pm=pm, b=b: e.tensor_tensor(out=o[:], in0=pm[:], in1=bm[b][:], op=ALU.add),
                         r=["psm%d" % s, "bm%d" % b], w=[okey])
                    if slot in (1, 4):
                        k.op("dve", lambda e, o=o, b=b: e.scalar_tensor_tensor(
                            out=o[:], in0=o[:], scalar=1.0, in1=gn[b][:], op0=ALU.add, op1=ALU.mult),
                            r=[okey, "gn%d" % b], w=[okey])
                    k.dma("sp", lambda e, o=o, l=l, s=s, slot=slot, half=half: e.dma_start(
                        out=D["modrep"][l, s, slot, :, half * 512:(half + 1) * 512], in_=o[:]),
                        r=[okey], w=["modrep"])
        k.barrier()


def norm_mod_tile(pb, xt, xkey, rstd, rkey, junk, t1, G, SH, hn, hnkey):
    k = pb.k
    k.op("act", lambda e: e.activation(out=junk[:], in_=xt[:], func=AF.Square, accum_out=rstd[:, 0:1]),
         r=[xkey], w=["junk", rkey])
    k.op("act", lambda e: e.activation(out=rstd[:, 1:2], in_=rstd[:, 0:1], func=AF.Sqrt, bias=EPS, scale=1.0 / DM),
         r=[rkey], w=[rkey])
    k.op("dve", lambda e: e.reciprocal(out=rstd[:, 2:3], in_=rstd[:, 1:2]), r=[rkey], w=[rkey])
    k.op("dve", lambda e: e.scalar_tensor_tensor(out=t1[:], in0=xt[:], scalar=rstd[:, 2:3], in1=G[:],
                                                 op0=ALU.mult, op1=ALU.mult),
         r=[xkey, rkey, "modv"], w=["t1"])
    k.op("pool", lambda e: e.tensor_tensor(out=hn[:], in0=t1[:], in1=SH[:], op=ALU.add),
         r=["t1", "modv"], w=[hnkey])


def load_modv(pb, es, l, slots):
    k, D = pb.k, pb.D
    out = {}
    for s in range(2):
        for slot in slots:
            t = pb.sb(es, "mv%d%d" % (s, slot), [128, 1024], F32)
            k.dma("sp", lambda e, t=t, s=s, slot=slot: e.dma_start(out=t[:], in_=D["modrep"][l, s, slot, :, :]),
                  r=["modrep"], w=["modv"])
            out[(s, slot)] = t
    return out


def topk_threshold(pb, es, C, aff, J, cap, tag, psum):
    k = pb.k
    lo = pb.sb(es, "lo" + tag, [128, 16], F32)
    hi = pb.sb(es, "hi" + tag, [128, 16], F32)
    mid = pb.sb(es, "mid" + tag, [128, 16], F32)
    cnt = pb.sb(es, "cnt" + tag, [128, 16], F32)
    ge = pb.sb(es, "ge" + tag, [128, 16], U32)
    lt = pb.sb(es, "lt" + tag, [128, 16], U32)
    cmp = pb.sb(es, "cmp" + tag, [128, J, 16], BF16)
    K = "tk" + tag
    k.op("dve", lambda e: e.memset(lo[:], 0.0), w=[K])
    k.op("dve", lambda e: e.memset(hi[:], 1.0), w=[K])
    ncol = J * 16
    for it in range(30):
        k.op("dve", lambda e: e.tensor_tensor(out=mid[:], in0=lo[:], in1=hi[:], op=ALU.add), r=[K], w=[K + "m"])
        k.op("dve", lambda e: e.tensor_scalar(out=mid[:], in0=mid[:], scalar1=0.5, scalar2=None, op0=ALU.mult),
             r=[K + "m"], w=[K + "m"])
        k.op("dve", lambda e: e.tensor_tensor(out=cmp[:], in0=aff, in1=mid[:].unsqueeze(1).to_broadcast([128, J, 16]),
                                              op=ALU.is_ge), r=[K + "m", "aff_all"], w=[K + "c"])
        cf = cmp[:].rearrange("p j e -> p (j e)")
        for c0 in range(0, ncol, 512):
            c1 = min(ncol, c0 + 512)
            k.op("pe", lambda e, c0=c0, c1=c1: e.matmul(psum[:, c0:c1], C["ones_b"][:], cf[:, c0:c1], start=True, stop=True),
                 r=[K + "c", "ones_b"], w=[K + "p"])
        k.op("dve", lambda e: e.tensor_reduce(out=cnt[:], in_=psum[:, 0:ncol].rearrange("p (j e) -> p e j", e=16),
                                              axis=AX.X, op=ALU.add), r=[K + "p"], w=[K + "n"])
        k.op("dve", lambda e: e.tensor_scalar(out=ge[:], in0=cnt[:], scalar1=float(cap), scalar2=None, op0=ALU.is_ge),
             r=[K + "n"], w=[K + "g"])
        k.op("dve", lambda e: e.tensor_scalar(out=lt[:], in0=cnt[:], scalar1=float(cap), scalar2=None, op0=ALU.is_lt),
             r=[K + "n"], w=[K + "g"])
        k.op("dve", lambda e: e.copy_predicated(out=lo[:], mask=ge[:], data=mid[:]), r=[K + "g", K + "m"], w=[K])
        k.op("dve", lambda e: e.copy_predicated(out=hi[:], mask=lt[:], data=mid[:]), r=[K + "g", K + "m"], w=[K])
    return lo, K


def phase_moe(pb, C, l):
    k, D, nc = pb.k, pb.D, pb.nc
    has_ctx = (l == 0)
    NT = 66 if has_ctx else 64
    NPT = 9 if has_ctx else 8
    NP = NPT * 128
    with ExitStack() as es:
        mv = load_modv(pb, es, l, (3, 4, 5))
        iota_q = pb.sb(es, "iota_q", [128, 1024], F32)
        ctxadd = pb.sb(es, "ctxadd", [128, 128], F32)
        k.dma("sp", lambda e: e.dma_start(out=iota_q[:], in_=D["c_iota_q"]), w=["iota_q"])
        k.dma("sp", lambda e: e.dma_start(out=ctxadd[:], in_=D["c_ctxadd"]), w=["ctxadd"])
        aff_all = pb.sb(es, "aff_all", [128, NT, 16], F32)
        wr_f = pb.sb(es, "wr_f", [128, 8, 16], F32)
        wr = pb.sb(es, "wr", [128, 8, 16], BF16)
        k.dma("sp", lambda e: e.dma_start(out=wr_f[:], in_=D["w_router"][l].rearrange("(kc p) n -> p kc n", p=128)),
              w=["wr_f"])
        k.op("dve", lambda e: e.tensor_copy(out=wr[:], in_=wr_f[:]), r=["wr_f"], w=["wr"])
        with ExitStack() as es2:
            xt = [pb.sb(es2, "xt%d" % i, [128, 1024], F32) for i in range(2)]
            hn = [pb.sb(es2, "hn%d" % i, [128, 1024], BF16) for i in range(2)]
            hnT = [pb.sb(es2, "hnT%d" % i, [128, 8, 128], BF16) for i in range(2)]
            junk = pb.sb(es2, "junk", [128, 1024], BF16)
            t1 = pb.sb(es2, "t1", [128, 1024], F32)
            rstd = [pb.sb(es2, "rstd%d" % i, [128, 4], F32) for i in range(2)]
            psT = [pb.ps(es2, "psT%d" % i, [128, 1024], BF16) for i in range(2)]
            psL = [pb.ps(es2, "psL%d" % i, [128, 16], F32) for i in range(2)]
            for j in range(NT):
                b = j % 2
                s = 0 if j < 64 else 1
                r0 = j * 128
                k.dma("sp", lambda e, b=b, r0=r0: e.dma_start(out=xt[b][:], in_=D["xres"][r0:r0 + 128, :]),
                      r=["xres"], w=["xt%d" % b])
                norm_mod_tile(pb, xt[b], "xt%d" % b, rstd[b], "rstd%d" % b, junk, t1, mv[(s, 4)], mv[(s, 3)], hn[b], "hn%d" % b)
                k.dma("sp", lambda e, b=b, r0=r0: e.dma_start(out=D["hn"][r0:r0 + 128, :], in_=hn[b][:]),
                      r=["hn%d" % b], w=["hn"])
                for kc in range(8):
                    k.op("pe", lambda e, b=b, kc=kc: e.transpose(out=psT[b][:, kc * 128:(kc + 1) * 128],
                                                                 in_=hn[b][:, kc * 128:(kc + 1) * 128],
                                                                 identity=C["ident_b"][:]),
                         r=["hn%d" % b, "ident_b"], w=["psT%d" % b])
                k.op("act", lambda e, b=b: e.activation(out=hnT[b][:].rearrange("p a t -> p (a t)"), in_=psT[b][:], func=AF.Copy),
                     r=["psT%d" % b], w=["hnT%d" % b])
                for kc in range(8):
                    k.op("pe", lambda e, b=b, kc=kc: e.matmul(psL[b][:], hnT[b][:, kc, :], wr[:, kc, :],
                                                              start=(kc == 0), stop=(kc == 7)),
                         r=["hnT%d" % b, "wr"], w=["psL%d" % b])
                k.op("dve", lambda e, b=b, j=j: e.tensor_copy(out=aff_all[:, j, :], in_=psL[b][:]),
                     r=["psL%d" % b], w=["aff_all"])
            mx = pb.sb(es2, "mx", [128, NT], F32)
            k.op("dve", lambda e: e.tensor_reduce(out=mx[:], in_=aff_all[:], axis=AX.X, op=ALU.max), r=["aff_all"], w=["mx"])
            k.op("dve", lambda e: e.tensor_tensor(out=aff_all[:], in0=aff_all[:],
                                                  in1=mx[:].unsqueeze(2).to_broadcast([128, NT, 16]), op=ALU.subtract),
                 r=["aff_all", "mx"], w=["aff_all"])
            k.op("act", lambda e: e.activation(out=aff_all[:], in_=aff_all[:], func=AF.Exp), r=["aff_all"], w=["aff_all"])
            k.op("dve", lambda e: e.tensor_reduce(out=mx[:], in_=aff_all[:], axis=AX.X, op=ALU.add), r=["aff_all"], w=["mx"])
            k.op("dve", lambda e: e.reciprocal(out=mx[:], in_=mx[:]), r=["mx"], w=["mx"])
            k.op("dve", lambda e: e.tensor_tensor(out=aff_all[:], in0=aff_all[:],
                                                  in1=mx[:].unsqueeze(2).to_broadcast([128, NT, 16]), op=ALU.mult),
                 r=["aff_all", "mx"], w=["aff_all"])
            k.dma("sp", lambda e: e.dma_start(out=D["aff"][0:NT * 128, :].rearrange("(j p) e -> p j e", p=128), in_=aff_all[:]),
                  r=["aff_all"], w=["aff"])
            k.barrier()
        m_all = pb.sb(es, "m_all", [128, NT, 16], BF16)
        cnt_incl = pb.sb(es, "cnt_incl", [128, NT, 16], F32)
        with ExitStack() as es2:
            pst = [pb.ps(es2, "pst%d" % i, [128, 512], F32) for i in range(3)]
            psbig = pb.ps(es2, "psbig", [128, 1024], F32)
            thr_l, Kl = topk_threshold(pb, es2, C, aff_all[:, 0:64, :], 64, 1024, "L", psbig)
            k.op("dve", lambda e: e.tensor_tensor(out=m_all[:, 0:64, :], in0=aff_all[:, 0:64, :],
                                                  in1=thr_l[:].unsqueeze(1).to_broadcast([128, 64, 16]), op=ALU.is_ge),
                 r=["aff_all", Kl], w=["m_all"])
            if has_ctx:
                thr_c, Kc = topk_threshold(pb, es2, C, aff_all[:, 64:66, :], 2, 32, "C", psbig)
                k.op("dve", lambda e: e.tensor_tensor(out=m_all[:, 64:66, :], in0=aff_all[:, 64:66, :],
                                                      in1=thr_c[:].unsqueeze(1).to_broadcast([128, 2, 16]), op=ALU.is_ge),
                     r=["aff_all", Kc], w=["m_all"])
            tot = pb.sb(es2, "tot", [128, NT, 16], F32)
            binc = pb.sb(es2, "binc", [128, NT, 16], F32)
            mf = m_all[:].rearrange("p j e -> p (j e)")
            tf = tot[:].rearrange("p j e -> p (j e)")
            cf = cnt_incl[:].rearrange("p j e -> p (j e)")
            ncol = NT * 16
            for ci, c0 in enumerate(range(0, ncol, 512)):
                c1 = min(ncol, c0 + 512)
                w_ = c1 - c0
                k.op("pe", lambda e, c0=c0, c1=c1, ci=ci, w_=w_: e.matmul(pst[ci][:, 0:w_], C["ones_b"][:], mf[:, c0:c1],
                                                                          start=True, stop=True),
                     r=["m_all", "ones_b"], w=["pst%d" % ci])
                k.op("dve", lambda e, c0=c0, c1=c1, ci=ci, w_=w_: e.tensor_copy(out=tf[:, c0:c1], in_=pst[ci][:, 0:w_]),
                     r=["pst%d" % ci], w=["tot"])
            for e_ in range(16):
                k.op("dve", lambda e, e_=e_: e.tensor_tensor_scan(out=binc[:, 0:64, e_], data0=C["ones_f"][:, 0:64],
                                                                  data1=tot[:, 0:64, e_], initial=0.0,
                                                                  op0=ALU.mult, op1=ALU.add),
                     r=["tot", "ones_f"], w=["binc"])
            if has_ctx:
                k.op("dve", lambda e: e.tensor_copy(out=binc[:, 64, :], in_=tot[:, 64, :]), r=["tot"], w=["binc"])
                k.op("dve", lambda e: e.tensor_tensor(out=binc[:, 65, :], in0=tot[:, 64, :], in1=tot[:, 65, :], op=ALU.add),
                     r=["tot"], w=["binc"])
            k.op("dve", lambda e: e.tensor_tensor(out=binc[:], in0=binc[:], in1=tot[:], op=ALU.subtract),
                 r=["binc", "tot"], w=["binc"])
            bf_ = binc[:].rearrange("p j e -> p (j e)")
            for ci, c0 in enumerate(range(0, ncol, 512)):
                c1 = min(ncol, c0 + 512)
                w_ = c1 - c0
                k.op("pe", lambda e, c0=c0, c1=c1, ci=ci, w_=w_: e.matmul(pst[ci][:, 0:w_], C["triu_incl_b"][:], mf[:, c0:c1],
                                                                          start=True, stop=True),
                     r=["m_all", "triu_incl_b", "tot"], w=["pst%d" % ci])
                k.op("dve", lambda e, c0=c0, c1=c1, ci=ci, w_=w_: e.tensor_tensor(out=cf[:, c0:c1], in0=pst[ci][:, 0:w_],
                                                                                  in1=bf_[:, c0:c1], op=ALU.add),
                     r=["pst%d" % ci, "binc"], w=["cnt_incl"])
            k.barrier()
        with ExitStack() as es2:
            Bt = [pb.sb(es2, "Bt%d" % i, [128, 1024], BF16) for i in range(2)]
            row_sb = pb.sb(es2, "row_sb", [1, NP], F32)
            idx_all = pb.sb(es2, "idx_all", [128, NE, NPT], I32)
            xg = pb.sb(es2, "xg", [128, NPT, 1024], BF16)
            ga = [pb.sb(es2, "ga%d" % i_, [128, NPT, 16], F32) for i_ in range(2)]
            xgT = pb.sb(es2, "xgT", [128, 8, NP], BF16)
            hidT = pb.sb(es2, "hidT", [128, 8, NP], BF16)
            sg = [pb.sb(es2, "sg%d" % i, [128, 512], F32) for i in range(2)]
            yt = [pb.sb(es2, "yt%d" % i, [128, 1024], F32) for i in range(2)]
            Wb = {nm: pb.sb(es2, "W" + nm, [128, 8, 1024], BF16) for nm in ("g", "u", "d")}
            stg = [pb.sb(es2, "stg%d" % i, [128, 4, 1024], F32) for i in range(2)]
            rowA = pb.ps(es2, "rowA", [1, 512], F32)
            rowB = pb.ps(es2, "rowB", [1, 512], F32)
            rowC = pb.ps(es2, "rowC", [128, 512], F32)
            psX = pb.ps(es2, "psX", [128, 1024], BF16)
            psG = pb.ps(es2, "psG", [128, 512], F32)
            psU = pb.ps(es2, "psU", [128, 512], F32)
            psY = [pb.ps(es2, "psY%d" % i, [128, 512], F32) for i in range(2)]
            sti = 0
            bi = 0
            yi = 0
            pchunks = [(0, 512), (512, 1024)] + ([(1024, 1152)] if has_ctx else [])
            for ex in range(NE):
                for j in range(64):
                    B_ = Bt[bi % 2]
                    bkey = "Bt%d" % (bi % 2)
                    eng = "dve"
                    bi += 1
                    k.op(eng, lambda e, B_=B_, j=j, ex=ex: e.tensor_scalar(
                        out=B_[:], in0=iota_q[:], scalar1=cnt_incl[:, j, ex:ex + 1], scalar2=None, op0=ALU.is_ge),
                        r=["iota_q", "cnt_incl"], w=[bkey])
                    k.op("pe", lambda e, B_=B_, j=j: e.matmul(rowA[:], C["ones_b"][:, 0:1], B_[:, 0:512],
                                                              start=(j == 0), stop=(j == 63)),
                         r=[bkey, "ones_b"], w=["rowA"])
                    k.op("pe", lambda e, B_=B_, j=j: e.matmul(rowB[:], C["ones_b"][:, 0:1], B_[:, 512:1024],
                                                              start=(j == 0), stop=(j == 63)),
                         r=[bkey, "ones_b"], w=["rowB"])
                k.op("dve", lambda e: e.tensor_copy(out=row_sb[0:1, 0:512], in_=rowA[:]), r=["rowA"], w=["row_sb"])
                k.op("dve", lambda e: e.tensor_copy(out=row_sb[0:1, 512:1024], in_=rowB[:]), r=["rowB"], w=["row_sb"])
                if has_ctx:
                    for j in (64, 65):
                        B_ = Bt[bi % 2]
                        bkey = "Bt%d" % (bi % 2)
                        bi += 1
                        k.op("dve", lambda e, B_=B_, j=j, ex=ex: e.tensor_scalar(
                            out=B_[:, 0:128], in0=iota_q[:, 0:128], scalar1=cnt_incl[:, j, ex:ex + 1], scalar2=None,
                            op0=ALU.is_ge), r=["iota_q", "cnt_incl"], w=[bkey])
                        k.op("pe", lambda e, B_=B_, j=j: e.matmul(rowC[0:1, 0:128], C["ones_b"][:, 0:1], B_[:, 0:128],
                                                                  start=(j == 64), stop=(j == 65)),
                             r=[bkey, "ones_b"], w=["rowC"])
                    k.op("dve", lambda e: e.tensor_tensor(out=row_sb[0:1, 1024:1152], in0=rowC[0:1, 0:128],
                                                          in1=ctxadd[0:1, :], op=ALU.add),
                         r=["rowC", "ctxadd"], w=["row_sb"])
                for i in range(NPT):
                    k.op("pe", lambda e, i=i: e.transpose(out=rowC[:, 256 + i:257 + i], in_=row_sb[0:1, i * 128:(i + 1) * 128],
                                                          identity=C["ident_f"][0:1, 0:1]),
                         r=["row_sb", "ident_f"], w=["rowC"])
                k.op("dve", lambda e, ex=ex: e.tensor_copy(out=idx_all[:, ex, :], in_=rowC[:, 256:256 + NPT]), r=["rowC"], w=["idx%d" % ex])
            def gathers(ex, gb):
                for i in range(NPT):
                    k.dma("pool", lambda e, i=i: e.indirect_dma_start(
                        out=xg[:, i, :], out_offset=None, in_=D["hn"],
                        in_offset=bass.IndirectOffsetOnAxis(ap=idx_all[:, ex, i:i + 1], axis=0)),
                        r=["idx%d" % ex, "hn"], w=["xg%d" % i])
                    k.dma("pool", lambda e, i=i: e.indirect_dma_start(
                        out=ga[gb][:, i, :], out_offset=None, in_=D["aff"],
                        in_offset=bass.IndirectOffsetOnAxis(ap=idx_all[:, ex, i:i + 1], axis=0)),
                        r=["idx%d" % ex, "aff"], w=["ga%d_%d" % (gb, i)])

            gathers(0, 0)
            prev_sc, cur_sc = [], []
            for ex in range(NE):
                gb = ex % 2
                for nm, src in (("g", D["w_gate"]), ("u", D["w_up"]), ("d", D["w_down"])):
                    for hf in range(2):
                        sbuf_ = stg[sti % 2]
                        skey = "stg%d" % (sti % 2)
                        sti += 1
                        k.dma("sp", lambda e, sbuf_=sbuf_, src=src, hf=hf, ex=ex: e.dma_start(
                            out=sbuf_[:], in_=src[l, ex, hf * 512:(hf + 1) * 512, :].rearrange("(kc p) n -> p kc n", p=128)),
                            w=[skey])
                        k.op("act", lambda e, sbuf_=sbuf_, nm=nm, hf=hf: e.activation(
                            out=Wb[nm][:, hf * 4:(hf + 1) * 4, :], in_=sbuf_[:], func=AF.Copy),
                            r=[skey], w=["W" + nm])
                for i in range(NPT):
                    for kc in range(8):
                        k.op("pe", lambda e, i=i, kc=kc: e.transpose(out=psX[:, kc * 128:(kc + 1) * 128],
                                                                     in_=xg[:, i, kc * 128:(kc + 1) * 128],
                                                                     identity=C["ident_b"][:]),
                             r=["xg%d" % i, "ident_b"], w=["psX"])
                    k.op("dve", lambda e, i=i: e.tensor_copy(out=xgT[:, :, i * 128:(i + 1) * 128],
                                                             in_=psX[:].rearrange("p (a t) -> p a t", a=8)),
                         r=["psX"], w=["xgT"])
                if ex + 1 < NE:
                    gathers(ex + 1, 1 - gb)
                for fc in range(8):
                    for (p0, p1) in pchunks:
                        n = p1 - p0
                        for kc in range(8):
                            k.op("pe", lambda e, fc=fc, kc=kc, p0=p0, p1=p1, n=n: e.matmul(
                                psG[:, 0:n], Wb["g"][:, kc, fc * 128:(fc + 1) * 128], xgT[:, kc, p0:p1],
                                start=(kc == 0), stop=(kc == 7)), r=["Wg", "xgT"], w=["psG"])
                        for kc in range(8):
                            k.op("pe", lambda e, fc=fc, kc=kc, p0=p0, p1=p1, n=n: e.matmul(
                                psU[:, 0:n], Wb["u"][:, kc, fc * 128:(fc + 1) * 128], xgT[:, kc, p0:p1],
                                start=(kc == 0), stop=(kc == 7)), r=["Wu", "xgT"], w=["psU"])
                        s_ = sg[yi % 2]
                        skey = "sg%d" % (yi % 2)
                        yi += 1
                        k.op("act", lambda e, s_=s_, n=n: e.activation(out=s_[:, 0:n], in_=psG[:, 0:n], func=AF.Silu),
                             r=["psG"], w=[skey])
                        k.op("dve", lambda e, s_=s_, n=n, fc=fc, p0=p0, p1=p1: e.tensor_tensor(
                            out=hidT[:, fc, p0:p1], in0=psU[:, 0:n], in1=s_[:, 0:n], op=ALU.mult),
                            r=["psU", skey], w=["hidT"])
                for i in range(NPT):
                    s = 0 if i < 8 else 1
                    y_ = yt[i % 2]
                    ykey = "yt%d" % (i % 2)
                    for dh in range(2):
                        py = psY[dh]
                        for fc in range(8):
                            k.op("pe", lambda e, i=i, fc=fc, dh=dh, py=py: e.matmul(
                                py[:], hidT[:, fc, i * 128:(i + 1) * 128], Wb["d"][:, fc, dh * 512:(dh + 1) * 512],
                                start=(fc == 0), stop=(fc == 7)), r=["hidT", "Wd"], w=["psY%d" % dh])
                        k.op("dve", lambda e, i=i, dh=dh, py=py, y_=y_, s=s, ex=ex: e.scalar_tensor_tensor(
                            out=y_[:, dh * 512:(dh + 1) * 512], in0=py[:], scalar=ga[gb][:, i, ex:ex + 1],
                            in1=mv[(s, 5)][:, dh * 512:(dh + 1) * 512], op0=ALU.mult, op1=ALU.mult),
                            r=["psY%d" % dh, "ga%d_%d" % (gb, i), "modv"], w=[ykey])
                    for t_ in prev_sc:
                        k._wait("pool", t_)
                    cur_sc.append(k.dma("pool", lambda e, i=i, y_=y_: e.indirect_dma_start(
                        out=D["xres"], out_offset=bass.IndirectOffsetOnAxis(ap=idx_all[:, ex, i:i + 1], axis=0),
                        in_=y_[:], in_offset=None, compute_op=ALU.add),
                        r=[ykey, "idx%d" % ex], w=[]))
                prev_sc, cur_sc = cur_sc, []
            k.barrier()


def phase_final(pb, C):
    k, D = pb.k, pb.D
    with ExitStack() as es:
        g = pb.sb(es, "gfin", [128, 1024], F32)
        k.dma("sp", lambda e: e.dma_start(out=g[:], in_=D["norm_out"].to_broadcast([128, 1024])), w=["gfin"])
        xt = [pb.sb(es, "fx%d" % i, [128, 1024], F32) for i in range(2)]
        ot = [pb.sb(es, "fo%d" % i, [128, 1024], F32) for i in range(2)]
        junk = pb.sb(es, "fjunk", [128, 1024], BF16)
        rstd = [pb.sb(es, "frs%d" % i, [128, 4], F32) for i in range(2)]
        for j in range(64):
            b = j % 2
            r0 = j * 128
            k.dma("sp", lambda e, b=b, r0=r0: e.dma_start(out=xt[b][:], in_=D["xres"][r0:r0 + 128, :]),
                  r=["xres"], w=["fx%d" % b])
            k.op("act", lambda e, b=b: e.activation(out=junk[:], in_=xt[b][:], func=AF.Square, accum_out=rstd[b][:, 0:1]),
                 r=["fx%d" % b], w=["fjunk", "frs%d" % b])
            k.op("act", lambda e, b=b: e.activation(out=rstd[b][:, 1:2], in_=rstd[b][:, 0:1], func=AF.Sqrt, bias=EPS,
                                                    scale=1.0 / DM), r=["frs%d" % b], w=["frs%d" % b])
            k.op("dve", lambda e, b=b: e.reciprocal(out=rstd[b][:, 2:3], in_=rstd[b][:, 1:2]), r=["frs%d" % b], w=["frs%d" % b])
            k.op("dve", lambda e, b=b: e.scalar_tensor_tensor(out=ot[b][:], in0=xt[b][:], scalar=rstd[b][:, 2:3], in1=g[:],
                                                              op0=ALU.mult, op1=ALU.mult),
                 r=["fx%d" % b, "frs%d" % b, "gfin"], w=["fo%d" % b])
            k.dma("sp", lambda e, b=b, r0=r0: e.dma_start(out=D["out"][r0:r0 + 128, :], in_=ot[b][:]),
                  r=["fo%d" % b], w=["out"])
        k.barrier()


NH = 8
ATT_SCALE = 1.0 / float(np.sqrt(192.0))


def attn_host_consts():
    c = {}
    t = np.arange(T_LAT)
    row = (t // 64).astype(np.float32)
    col = (t % 64).astype(np.float32)
    nf = 16
    inv = (10000.0 ** (-np.arange(nf, dtype=np.float32) / nf)).astype(np.float32)
    cosT = np.zeros((64, T_LAT), np.float32)
    sinT = np.zeros((64, T_LAT), np.float32)
    for a, pos in ((0, row), (1, col)):
        ang = (pos[None, :] * inv[:, None]).astype(np.float32)
        for b in range(2):
            d0 = a * 32 + b * 16
            cosT[d0:d0 + 16] = np.cos(ang)
            sinT[d0:d0 + 16] = np.sin(ang) * (-1.0 if b == 0 else 1.0)
    c["cos2"] = np.concatenate([cosT, cosT], 0)
    c["sin2"] = np.concatenate([sinT, sinT], 0)
    return c


def rope_swap_perm():
    d = np.arange(64)
    a, b, f = d // 32, (d // 16) % 2, d % 16
    return a * 32 + (1 - b) * 16 + f


def attn_layout_weights(w_uq, w_ukv, w_kr):
    sp = rope_swap_perm()
    nope = np.concatenate([np.arange(h * 192, h * 192 + 128) for h in range(NH)])
    rope = np.concatenate([np.arange(h * 192 + 128, h * 192 + 192) for h in range(NH)])
    ropes = np.concatenate([h * 192 + 128 + sp for h in range(NH)])
    w_uq_r = np.ascontiguousarray(w_uq[:, np.concatenate([nope, rope, ropes])])
    kn = np.concatenate([np.arange(h * 256, h * 256 + 128) for h in range(NH)])
    vv = np.concatenate([np.arange(h * 256 + 128, h * 256 + 256) for h in range(NH)])
    w_ukv_r = np.ascontiguousarray(w_ukv[:, np.concatenate([kn, vv])])
    w_kr_r = np.ascontiguousarray(np.concatenate([w_kr, w_kr[:, sp]], 1))
    return w_uq_r, w_ukv_r, w_kr_r


def load_w_bf16(pb, es, name, src_ap, kc, n, stg, stgkey):
    k = pb.k
    t = pb.sb(es, name, [128, kc, n], BF16)
    for c0 in range(0, n, 512):
        c1 = min(n, c0 + 512)
        for q in range(kc):
            k.dma("sp", lambda e, q=q, c0=c0, c1=c1: e.dma_start(out=stg[:, 0:c1 - c0], in_=src_ap[q * 128:(q + 1) * 128, c0:c1]),
                  w=[stgkey])
            k.op("dve", lambda e, q=q, c0=c0, c1=c1: e.tensor_copy(out=t[:, q, c0:c1], in_=stg[:, 0:c1 - c0]),
                 r=[stgkey], w=[name])
    return t


def rms_small(pb, ps_ap, n, rstd, rkey, junk, gain_rep, outbf, outkey, pskey):
    k = pb.k
    k.op("act", lambda e: e.activation(out=junk[:, 0:n], in_=ps_ap, func=AF.Square, accum_out=rstd[:, 0:1]),
         r=[pskey], w=["junk", rkey])
    k.op("act", lambda e: e.activation(out=rstd[:, 1:2], in_=rstd[:, 0:1], func=AF.Sqrt, bias=EPS, scale=1.0 / n),
         r=[rkey], w=[rkey])
    k.op("dve", lambda e: e.reciprocal(out=rstd[:, 2:3], in_=rstd[:, 1:2]), r=[rkey], w=[rkey])
    k.op("dve", lambda e: e.scalar_tensor_tensor(out=outbf, in0=ps_ap, scalar=rstd[:, 2:3], in1=gain_rep,
                                                 op0=ALU.mult, op1=ALU.mult), r=[pskey, rkey, "gains"], w=[outkey])


def phase_attn_pre(pb, C):
    k, D, nc = pb.k, pb.D, pb.nc
    l = 1
    with ExitStack() as es:
        mv = load_modv(pb, es, l, (0, 1))
        stg = pb.sb(es, "wstg", [128, 512], F32)
        w_dq = load_w_bf16(pb, es, "w_dq", D["w_dq"], 8, 384, stg, "wstg")
        w_dkv = load_w_bf16(pb, es, "w_dkv", D["w_dkv"], 8, 256, stg, "wstg")
        w_kr = load_w_bf16(pb, es, "w_kr", D["w_kr_r"], 8, 128, stg, "wstg")
        w_uq = load_w_bf16(pb, es, "w_uq", D["w_uq_r"], 3, 2048, stg, "wstg")
        w_ukv = load_w_bf16(pb, es, "w_ukv", D["w_ukv_r"], 2, 2048, stg, "wstg")
        qn_rep = pb.sb(es, "qn_rep", [128, 384], F32)
        kvn_rep = pb.sb(es, "kvn_rep", [128, 256], F32)
        k.dma("sp", lambda e: e.dma_start(out=qn_rep[:], in_=D["q_norm"].to_broadcast([128, 384])), w=["gains"])
        k.dma("sp", lambda e: e.dma_start(out=kvn_rep[:], in_=D["kv_norm"].to_broadcast([128, 256])), w=["gains"])
        xt = [pb.sb(es, "xt%d" % i, [128, 1024], F32) for i in range(2)]
        an = [pb.sb(es, "an%d" % i, [128, 1024], BF16) for i in range(2)]
        junk = pb.sb(es, "junk", [128, 1024], BF16)
        t1 = pb.sb(es, "t1", [128, 1024], F32)
        rstd = [pb.sb(es, "rstd%d" % i, [128, 4], F32) for i in range(2)]
        rs2 = [pb.sb(es, "rs2%d" % i, [128, 4], F32) for i in range(2)]
        aT = pb.sb(es, "aT", [128, 8, 512], BF16)
        cqT = pb.sb(es, "cqT", [128, 3, 512], BF16)
        ckvT = pb.sb(es, "ckvT", [128, 2, 512], BF16)
        cqn = pb.sb(es, "cqn", [128, 384], BF16)
        ckvn = pb.sb(es, "ckvn", [128, 256], BF16)
        cosb = pb.sb(es, "cosb", [128, 512], F32)
        sinb = pb.sb(es, "sinb", [128, 512], F32)
        ostg = [pb.sb(es, "ostg%d" % i, [128, 512], BF16) for i in range(3)]
        vstg = [pb.sb(es, "vstg%d" % i, [128, 1024], BF16) for i in range(2)]
        ra = pb.sb(es, "ra", [128, 512], F32)
        rb = pb.sb(es, "rb", [128, 512], F32)
        psT = pb.ps(es, "psT", [128, 1024], BF16)
        psS = pb.ps(es, "psS", [128, 512], F32)
        psA = pb.ps(es, "psA", [128, 512], F32)
        psB = pb.ps(es, "psB", [128, 512], F32)
        psV = [pb.ps(es, "psV%d" % i, [128, 512], F32) for i in range(2)]
        oi = 0
        vi = 0
        xi = 0
        supers = [(i * 512, 4, 0) for i in range(16)] + [(T_LAT, 2, 1)]
        for (t0, ntile, s) in supers:
            nt = ntile * 128
            is_lat = (s == 0)
            for tl in range(ntile):
                b = xi % 2
                xi += 1
                r0 = t0 + tl * 128
                k.dma("sp", lambda e, b=b, r0=r0: e.dma_start(out=xt[b][:], in_=D["xres"][r0:r0 + 128, :]),
                      r=["xres"], w=["xt%d" % b])
                norm_mod_tile(pb, xt[b], "xt%d" % b, rstd[b], "rstd%d" % b, junk, t1, mv[(s, 1)], mv[(s, 0)], an[b], "an%d" % b)
                for kc in range(8):
                    k.op("pe", lambda e, b=b, kc=kc: e.transpose(out=psT[:, kc * 128:(kc + 1) * 128],
                                                                 in_=an[b][:, kc * 128:(kc + 1) * 128], identity=C["ident_b"][:]),
                         r=["an%d" % b, "ident_b"], w=["psT"])
                k.op("act", lambda e, tl=tl: e.activation(out=aT[:, :, tl * 128:(tl + 1) * 128],
                                                          in_=psT[:].rearrange("p (a t) -> p a t", a=8), func=AF.Copy),
                     r=["psT"], w=["aT"])
                if is_lat:
                    for kc in range(8):
                        k.op("pe", lambda e, kc=kc, tl=tl: e.matmul(psS[:, 0:384], aT[:, kc, tl * 128:(tl + 1) * 128], w_dq[:, kc, :],
                                                                    start=(kc == 0), stop=(kc == 7)), r=["aT", "w_dq"], w=["psS"])
                    rms_small(pb, psS[:, 0:384], 384, rs2[b], "rs2%d" % b, junk, qn_rep[:], cqn[:], "cqn", "psS")
                    for c in range(3):
                        k.op("pe", lambda e, c=c: e.transpose(out=psT[:, c * 128:(c + 1) * 128], in_=cqn[:, c * 128:(c + 1) * 128],
                                                              identity=C["ident_b"][:]), r=["cqn", "ident_b"], w=["psT"])
                    k.op("act", lambda e, tl=tl: e.activation(out=cqT[:, :, tl * 128:(tl + 1) * 128],
                                                              in_=psT[:, 0:384].rearrange("p (a t) -> p a t", a=3), func=AF.Copy),
                         r=["psT"], w=["cqT"])
                for kc in range(8):
                    k.op("pe", lambda e, kc=kc, tl=tl: e.matmul(psS[:, 0:256], aT[:, kc, tl * 128:(tl + 1) * 128], w_dkv[:, kc, :],
                                                                start=(kc == 0), stop=(kc == 7)), r=["aT", "w_dkv"], w=["psS"])
                rms_small(pb, psS[:, 0:256], 256, rs2[b], "rs2%d" % b, junk, kvn_rep[:], ckvn[:], "ckvn", "psS")
                for c in range(2):
                    k.op("pe", lambda e, c=c: e.transpose(out=psT[:, c * 128:(c + 1) * 128], in_=ckvn[:, c * 128:(c + 1) * 128],
                                                          identity=C["ident_b"][:]), r=["ckvn", "ident_b"], w=["psT"])
                k.op("act", lambda e, tl=tl: e.activation(out=ckvT[:, :, tl * 128:(tl + 1) * 128],
                                                          in_=psT[:, 0:256].rearrange("p (a t) -> p a t", a=2), func=AF.Copy),
                     r=["psT"], w=["ckvT"])
                v_ = vstg[vi % 2]
                vkey = "vstg%d" % (vi % 2)
                vi += 1
                for dh in range(2):
                    for c in range(2):
                        k.op("pe", lambda e, c=c, dh=dh, tl=tl: e.matmul(
                            psV[dh][:], ckvT[:, c, tl * 128:(tl + 1) * 128], w_ukv[:, c, 1024 + dh * 512:1024 + (dh + 1) * 512],
                            start=(c == 0), stop=(c == 1)), r=["ckvT", "w_ukv"], w=["psV%d" % dh])
                    k.op("act", lambda e, dh=dh, v_=v_: e.activation(out=v_[:, dh * 512:(dh + 1) * 512], in_=psV[dh][:], func=AF.Copy),
                         r=["psV%d" % dh], w=[vkey])
                k.dma("sp", lambda e, v_=v_, r0=r0: e.dma_start(out=D["Vd"][r0:r0 + 128, :], in_=v_[:]), r=[vkey], w=["Vd"])
            if is_lat:
                k.dma("sp", lambda e, t0=t0: e.dma_start(out=cosb[:], in_=D["c_cos2"][:, t0:t0 + 512]), w=["cosb"])
                k.dma("sp", lambda e, t0=t0: e.dma_start(out=sinb[:], in_=D["c_sin2"][:, t0:t0 + 512]), w=["sinb"])
            for h in range(NH):
                for c in range(2):
                    k.op("pe", lambda e, c=c, h=h: e.matmul(psA[:, 0:nt], w_ukv[:, c, h * 128:(h + 1) * 128], ckvT[:, c, 0:nt],
                                                            start=(c == 0), stop=(c == 1)), r=["ckvT", "w_ukv"], w=["psA"])
                o_ = ostg[oi % 3]
                okey = "ostg%d" % (oi % 3)
                oi += 1
                k.op("act", lambda e, o_=o_: e.activation(out=o_[:, 0:nt], in_=psA[:, 0:nt], func=AF.Copy), r=["psA"], w=[okey])
                k.dma("sp", lambda e, o_=o_, h=h, t0=t0: e.dma_start(out=D["KT"][h, :, t0:t0 + nt], in_=o_[:, 0:nt]),
                      r=[okey], w=["KT"])
            for kc in range(8):
                k.op("pe", lambda e, kc=kc: e.matmul(psA[0:64, 0:nt], w_kr[:, kc, 0:64], aT[:, kc, 0:nt],
                                                     start=(kc == 0), stop=(kc == 7)), r=["aT", "w_kr"], w=["psA"])
            o_ = ostg[oi % 3]
            okey = "ostg%d" % (oi % 3)
            oi += 1
            if is_lat:
                for kc in range(8):
                    k.op("pe", lambda e, kc=kc: e.matmul(psB[0:64, 0:nt], w_kr[:, kc, 64:128], aT[:, kc, 0:nt],
                                                         start=(kc == 0), stop=(kc == 7)), r=["aT", "w_kr"], w=["psB"])
                k.op("dve", lambda e: e.tensor_tensor(out=ra[0:64, :], in0=psA[0:64, :], in1=cosb[0:64, :], op=ALU.mult),
                     r=["psA", "cosb"], w=["ra"])
                k.op("dve", lambda e: e.tensor_tensor(out=rb[0:64, :], in0=psB[0:64, :], in1=sinb[0:64, :], op=ALU.mult),
                     r=["psB", "sinb"], w=["rb"])
                k.op("dve", lambda e, o_=o_: e.tensor_tensor(out=o_[0:64, :], in0=ra[0:64, :], in1=rb[0:64, :], op=ALU.add),
                     r=["ra", "rb"], w=[okey])
            else:
                k.op("act", lambda e, o_=o_: e.activation(out=o_[0:64, 0:nt], in_=psA[0:64, 0:nt], func=AF.Copy), r=["psA"], w=[okey])
            k.dma("sp", lambda e, o_=o_, t0=t0: e.dma_start(out=D["KRT"][:, t0:t0 + nt], in_=o_[0:64, 0:nt]), r=[okey], w=["KRT"])
            if not is_lat:
                continue
            for h in range(NH):
                for c in range(3):
                    k.op("pe", lambda e, c=c, h=h: e.matmul(psA[:], w_uq[:, c, h * 128:(h + 1) * 128], cqT[:, c, :],
                                                            start=(c == 0), stop=(c == 2)), r=["cqT", "w_uq"], w=["psA"])
                o_ = ostg[oi % 3]
                okey = "ostg%d" % (oi % 3)
                oi += 1
                k.op("act", lambda e, o_=o_: e.activation(out=o_[:], in_=psA[:], func=AF.Copy), r=["psA"], w=[okey])
                k.dma("sp", lambda e, o_=o_, h=h, t0=t0: e.dma_start(out=D["QT"][h, :, t0:t0 + 512], in_=o_[:]), r=[okey], w=["QT"])
            for hp in range(4):
                for c in range(3):
                    k.op("pe", lambda e, c=c, hp=hp: e.matmul(psA[:], w_uq[:, c, 1024 + hp * 128:1024 + (hp + 1) * 128], cqT[:, c, :],
                                                              start=(c == 0), stop=(c == 2)), r=["cqT", "w_uq"], w=["psA"])
                for c in range(3):
                    k.op("pe", lambda e, c=c, hp=hp: e.matmul(psB[:], w_uq[:, c, 1536 + hp * 128:1536 + (hp + 1) * 128], cqT[:, c, :],
                                                              start=(c == 0), stop=(c == 2)), r=["cqT", "w_uq"], w=["psB"])
                o_ = ostg[oi % 3]
                okey = "ostg%d" % (oi % 3)
                oi += 1
                k.op("dve", lambda e: e.tensor_tensor(out=ra[:], in0=psA[:], in1=cosb[:], op=ALU.mult), r=["psA", "cosb"], w=["ra"])
                k.op("dve", lambda e: e.tensor_tensor(out=rb[:], in0=psB[:], in1=sinb[:], op=ALU.mult), r=["psB", "sinb"], w=["rb"])
                k.op("dve", lambda e, o_=o_: e.tensor_tensor(out=o_[:], in0=ra[:], in1=rb[:], op=ALU.add), r=["ra", "rb"], w=[okey])
                k.dma("sp", lambda e, o_=o_, hp=hp, t0=t0: e.dma_start(
                    out=D["QRT"][2 * hp:2 * hp + 2, :, t0:t0 + 512].rearrange("h d t -> (h d) t"), in_=o_[:]), r=[okey], w=["QRT"])
        k.barrier()


def phase_attn_main(pb, C, heads=range(NH), qblocks=range(16)):
    k, D, nc = pb.k, pb.D, pb.nc
    NKB = NTOK // 128
    with ExitStack() as es:
        KRT = pb.sb(es, "KRT", [128, NTOK], BF16)
        k.op("pool", lambda e: e.memset(KRT[64:128, :], 0.0), w=["sKRT"])
        k.dma("sp", lambda e: e.dma_start(out=KRT[0:64, :], in_=D["KRT"]), r=["KRT"], w=["sKRT"])
        KT = [pb.sb(es, "KT%d" % i, [128, NTOK], BF16) for i in range(2)]
        Vh = [pb.sb(es, "Vh%d" % i, [128, NKB, 130], BF16) for i in range(2)]
        for i in range(2):
            k.op("dve", lambda e, i=i: e.memset(Vh[i][:, :, 128:130], 1.0), w=["Vh%d" % i])
        Qb = [pb.sb(es, "Qb%d" % i, [128, 512], BF16) for i in range(2)]
        QRb = [pb.sb(es, "QRb%d" % i, [128, 512], BF16) for i in range(2)]
        for i in range(2):
            k.op("pool", lambda e, i=i: e.memset(QRb[i][64:128, :], 0.0), w=["QRb%d" % i])
        PT = [pb.sb(es, "PT%d" % i, [128, 512], BF16) for i in range(3)]
        Ost = [pb.sb(es, "Ost%d" % i, [128, 4, 128], BF16) for i in range(2)]
        rinv = pb.sb(es, "rinv", [128, 8], F32)
        psS = [pb.ps(es, "psS%d" % i, [128, 512], F32) for i in range(2)]
        psO = [pb.ps(es, "psO%d" % i, [128, 512], F32) for i in range(4)]
        qi = 0
        pi = 0
        si = 0
        for hi, h in enumerate(heads):
            hb = hi % 2
            k.dma("sp", lambda e, hb=hb, h=h: e.dma_start(out=KT[hb][:], in_=D["KT"][h]), r=["KT"], w=["KT%d" % hb])
            k.dma("sp", lambda e, hb=hb, h=h: e.dma_start(
                out=Vh[hb][:, :, 0:128], in_=D["Vd"][:, h * 128:(h + 1) * 128].rearrange("(kb p) v -> p kb v", p=128)),
                r=["Vd"], w=["Vh%d" % hb])
            for qb in qblocks:
                b = qi % 2
                qi += 1
                q0 = qb * 512
                k.dma("sp", lambda e, b=b, h=h, q0=q0: e.dma_start(out=Qb[b][:], in_=D["QT"][h, :, q0:q0 + 512]),
                      r=["QT"], w=["Qb%d" % b])
                k.dma("sp", lambda e, b=b, h=h, q0=q0: e.dma_start(out=QRb[b][0:64, :], in_=D["QRT"][h, :, q0:q0 + 512]),
                      r=["QRT"], w=["QRb%d" % b])

                def qk(kb, sb_):
                    k.op("pe", lambda e: e.matmul(psS[sb_][:], KT[hb][:, kb * 128:(kb + 1) * 128], Qb[b][:], start=True, stop=False),
                         r=["KT%d" % hb, "Qb%d" % b], w=["psS%d" % sb_])
                    k.op("pe", lambda e: e.matmul(psS[sb_][:], KRT[:, kb * 128:(kb + 1) * 128], QRb[b][:], start=False, stop=True),
                         r=["sKRT", "QRb%d" % b], w=["psS%d" % sb_])

                sbs = []
                sbs.append(si % 2)
                qk(0, si % 2)
                si += 1
                for kb in range(NKB):
                    if kb + 1 < NKB:
                        sbs.append(si % 2)
                        qk(kb + 1, si % 2)
                        si += 1
                    sb_ = sbs[kb]
                    p_ = PT[pi % 3]
                    pkey = "PT%d" % (pi % 3)
                    pi += 1
                    k.op("act", lambda e, sb_=sb_, p_=p_: e.activation(out=p_[:], in_=psS[sb_][:], func=AF.Exp, scale=ATT_SCALE),
                         r=["psS%d" % sb_], w=[pkey])
                    for qt in range(4):
                        k.op("pe", lambda e, qt=qt, p_=p_, kb=kb: e.matmul(
                            psO[qt][:, 0:130], p_[:, qt * 128:(qt + 1) * 128], Vh[hb][:, kb, :],
                            start=(kb == 0), stop=(kb == NKB - 1)), r=[pkey, "Vh%d" % hb], w=["psO%d" % qt])
                o_ = Ost[b]
                okey = "Ost%d" % b
                for qt in range(4):
                    k.op("dve", lambda e, qt=qt: e.reciprocal(out=rinv[:, qt:qt + 1], in_=psO[qt][:, 128:129]),
                         r=["psO%d" % qt], w=["rinv"])
                    k.op("dve", lambda e, qt=qt, o_=o_: e.tensor_scalar(out=o_[:, qt, :], in0=psO[qt][:, 0:128],
                                                                        scalar1=rinv[:, qt:qt + 1], scalar2=None, op0=ALU.mult),
                         r=["psO%d" % qt, "rinv"], w=[okey])
                k.dma("sp", lambda e, o_=o_, h=h, q0=q0: e.dma_start(
                    out=D["Od"][q0:q0 + 512, h * 128:(h + 1) * 128].rearrange("(qt p) v -> p qt v", p=128), in_=o_[:]),
                    r=[okey], w=["Od"])
        k.barrier()


def phase_attn_post(pb, C):
    k, D, nc = pb.k, pb.D, pb.nc
    with ExitStack() as es:
        mv = load_modv(pb, es, 1, (2,))
        stg = pb.sb(es, "wstg", [128, 512], F32)
        w_o = load_w_bf16(pb, es, "w_o", D["w_o"], 8, 1024, stg, "wstg")
        ot = [pb.sb(es, "ot%d" % i, [128, 1024], BF16) for i in range(2)]
        oT = [pb.sb(es, "oT%d" % i, [128, 8, 128], BF16) for i in range(2)]
        xt = [pb.sb(es, "xt%d" % i, [128, 1024], F32) for i in range(2)]
        tm = [pb.sb(es, "tm%d" % i, [128, 1024], F32) for i in range(2)]
        psT = pb.ps(es, "psT", [128, 1024], BF16)
        psY = [pb.ps(es, "psY%d" % i, [128, 512], F32) for i in range(2)]
        for j in range(64):
            b = j % 2
            r0 = j * 128
            k.dma("sp", lambda e, b=b, r0=r0: e.dma_start(out=ot[b][:], in_=D["Od"][r0:r0 + 128, :]), r=["Od"], w=["ot%d" % b])
            k.dma("sp", lambda e, b=b, r0=r0: e.dma_start(out=xt[b][:], in_=D["xres"][r0:r0 + 128, :]), r=["xres"], w=["xt%d" % b])
            for c in range(8):
                k.op("pe", lambda e, b=b, c=c: e.transpose(out=psT[:, c * 128:(c + 1) * 128], in_=ot[b][:, c * 128:(c + 1) * 128],
                                                           identity=C["ident_b"][:]), r=["ot%d" % b, "ident_b"], w=["psT"])
            k.op("act", lambda e, b=b: e.activation(out=oT[b][:].rearrange("p a t -> p (a t)"), in_=psT[:], func=AF.Copy),
                 r=["psT"], w=["oT%d" % b])
            for dh in range(2):
                for c in range(8):
                    k.op("pe", lambda e, b=b, c=c, dh=dh: e.matmul(psY[dh][:], oT[b][:, c, :], w_o[:, c, dh * 512:(dh + 1) * 512],
                                                                   start=(c == 0), stop=(c == 7)), r=["oT%d" % b, "w_o"], w=["psY%d" % dh])
                k.op("dve", lambda e, b=b, dh=dh: e.tensor_tensor(out=tm[b][:, dh * 512:(dh + 1) * 512], in0=psY[dh][:],
                                                                  in1=mv[(0, 2)][:, dh * 512:(dh + 1) * 512], op=ALU.mult),
                     r=["psY%d" % dh, "modv"], w=["tm%d" % b])
            k.op("pool", lambda e, b=b: e.tensor_tensor(out=tm[b][:], in0=tm[b][:], in1=xt[b][:], op=ALU.add),
                 r=["tm%d" % b, "xt%d" % b], w=["tm%d" % b])
            k.dma("sp", lambda e, b=b, r0=r0: e.dma_start(out=D["xres"][r0:r0 + 128, :], in_=tm[b][:]), r=["tm%d" % b], w=["xres"])
        k.barrier()


def load_cols(pb, es, C, name, src_rows_ap, n, psbank, pskey):
    k = pb.k
    rows = pb.sb(es, name + "_r", [n, 128], F32)
    cols = pb.sb(es, name, [128, n], F32)
    k.dma("sp", lambda e: e.dma_start(out=rows[:], in_=src_rows_ap), w=[name + "_r"])
    k.op("pe", lambda e: e.transpose(out=psbank[:, 0:n], in_=rows[:], identity=C["ident_f"][0:n, 0:n]),
         r=[name + "_r", "ident_f"], w=[pskey])
    k.op("dve", lambda e: e.tensor_copy(out=cols[:], in_=psbank[:, 0:n]), r=[pskey], w=[name])
    return cols


def phase_mix_pass(pb, C, dirn, nsup_lat=32, do_ctx=True):
    k, D, nc = pb.k, pb.D, pb.nc
    W = 256
    with ExitStack() as es:
        P = [pb.ps(es, "P%d" % i, [128, 512], F32) for i in range(8)]
        PK = ["P%d" % i for i in range(8)]
        P0b = P[0][:].bitcast(BF16)
        mv = load_modv(pb, es, 0, (0, 1))
        stg = pb.sb(es, "wstg", [128, 512], F32)
        win = D["w_in"]
        wq = load_w_bf16(pb, es, "wq", win[:, 0:512], 8, 512, stg, "wstg")
        wf = load_w_bf16(pb, es, "wf", win[:, 512 + 512 * dirn:1024 + 512 * dirn], 8, 512, stg, "wstg")
        wi = load_w_bf16(pb, es, "wi", win[:, 1536:2048], 8, 512, stg, "wstg")
        wx = load_w_bf16(pb, es, "wx", win[:, 3072:4096], 8, 1024, stg, "wstg")
        wdt = load_w_bf16(pb, es, "wdt", win[:, 4096 + 8 * dirn:4104 + 8 * dirn], 8, 8, stg, "wstg")
        lg0 = load_cols(pb, es, C, "lg0", D["lb_gamma"][0, dirn, :].rearrange("(c p) -> c p", p=128), 4, P[7], PK[7])
        lg1 = load_cols(pb, es, C, "lg1", D["lb_gamma"][1, dirn, :].rearrange("(c p) -> c p", p=128), 4, P[7], PK[7])
        lbc = pb.sb(es, "lbc", [128, 4], F32)
        oml = pb.sb(es, "oml", [128, 4], F32)
        k.op("dve", lambda e: e.tensor_tensor(out=lbc[:], in0=lg0[:], in1=lg1[:], op=ALU.subtract), r=["lg0", "lg1"], w=["lbc"])
        k.op("act", lambda e: e.activation(out=lbc[:], in_=lbc[:], func=AF.Sigmoid), r=["lbc"], w=["lbc"])
        k.op("dve", lambda e: e.tensor_scalar(out=oml[:], in0=lbc[:], scalar1=-1.0, scalar2=1.0, op0=ALU.mult, op1=ALU.add),
             r=["lbc"], w=["oml"])
        cw = [load_cols(pb, es, C, "cw%d" % j, D["conv_w"][j, :].rearrange("(c p) -> c p", p=128), 8, P[7], PK[7]) for j in range(5)]
        cbc = load_cols(pb, es, C, "cbc", D["conv_b"].rearrange("o (c p) -> (o c) p", p=128), 8, P[7], PK[7])
        dtb = pb.sb(es, "dtb", [128, 8], F32)
        negA = pb.sb(es, "negA", [128, 8], F32)
        k.dma("sp", lambda e: e.dma_start(out=dtb[:], in_=D["dt_bias"][dirn:dirn + 1, :].to_broadcast([128, 8])), w=["dtb"])
        k.dma("sp", lambda e: e.dma_start(out=negA[:], in_=D["a_log"][dirn:dirn + 1, :].to_broadcast([128, 8])), w=["negA"])
        k.op("act", lambda e: e.activation(out=negA[:], in_=negA[:], func=AF.Exp), r=["negA"], w=["negA"])
        k.op("dve", lambda e: e.tensor_scalar(out=negA[:], in0=negA[:], scalar1=-1.0, scalar2=None, op0=ALU.mult), r=["negA"], w=["negA"])
        dsk = pb.sb(es, "dsk", [128, 8], F32)
        k.dma("sp", lambda e: e.dma_start(out=dsk[:], in_=D["d_skip"].to_broadcast([128, 8])), w=["dsk"])
        if getattr(pb, 'stop', 99) == 0:
            k.barrier()
            return
        if dirn == 0:
            m_att_b, m_att_f = C["triu_incl_b"], C["triu_incl_f"]
            m_cum, m_wend, m_lh = C["triu_incl_f"], C["tril_strict_f"], C["tril_strict_f"]
            m_rhs = C["triu_incl_f"]
        else:
            m_att_b, m_att_f = C["tril_incl_b"], C["tril_incl_f"]
            m_cum, m_wend, m_lh = C["tril_incl_f"], C["triu_strict_f"], C["triu_strict_f"]
            m_rhs = C["tril_incl_f"]
        xt = [pb.sb(es, "xt%d" % i, [128, 1024], F32) for i in range(2)]
        an = [pb.sb(es, "an%d" % i, [128, 1024], BF16) for i in range(2)]
        hx = pb.sb(es, "hx", [4, 1024], F32)
        ah = pb.sb(es, "ah", [4, 1024], BF16)
        junk = pb.sb(es, "junk", [128, 1024], BF16)
        t1 = pb.sb(es, "t1", [128, 1024], F32)
        rstd = [pb.sb(es, "rstd%d" % i, [128, 4], F32) for i in range(3)]
        aT = pb.sb(es, "aT", [128, 8, W + 4], BF16)
        Qsil = pb.sb(es, "Qsil", [128, 4, W], F32)
        fg = pb.sb(es, "fg", [128, 4, W], F32)
        la = pb.sb(es, "la", [128, 4, W], F32)
        kk = pb.sb(es, "kk", [128, 4, W], F32)
        u = pb.sb(es, "u", [128, 8, W + 4], F32)
        acc = pb.sb(es, "acc", [128, 8, W], F32)
        tmp = pb.sb(es, "tmp", [128, 8, W], F32)
        xbc = pb.sb(es, "xbc", [128, 8, W], BF16)
        vtok = pb.sb(es, "vtok", [128, 2, 512], BF16)
        dtr = pb.sb(es, "dtr", [128, 2, 8], F32)
        bb = pb.sb(es, "bb", [128, 4, 128], F32)
        b2 = pb.sb(es, "b2", [128, 4, 128], F32)
        sc = pb.sb(es, "sc", [128, 3, 4], F32)
        esc = pb.sb(es, "esc", [128, 3, 4], F32)
        nb = pb.sb(es, "nb", [128, 4], F32)
        Eq = pb.sb(es, "Eq", [128, 4, 128], F32)
        Ek = pb.sb(es, "Ek", [128, 4, 128], F32)
        Qt = pb.sb(es, "Qt", [128, 4, 128], BF16)
        Kt = pb.sb(es, "Kt", [128, 4, 128], BF16)
        Ktok = pb.sb(es, "Ktok", [128, 4, 128], BF16)
        attm = pb.sb(es, "attm", [128, 8, 128], BF16)
        Sh = pb.sb(es, "Sh", [128, 4, 64], F32)
        ShpA = pb.sb(es, "ShpA", [128, 4, 64], BF16)
        ShpB = pb.sb(es, "ShpB", [128, 4, 64], BF16)
        KtA = pb.sb(es, "KtA", [128, 4, 128], BF16)
        KtB = pb.sb(es, "KtB", [128, 4, 128], BF16)
        for nm_, t_ in (("ShpA", ShpA), ("ShpB", ShpB), ("KtA", KtA), ("KtB", KtB)):
            k.op("dve", lambda e, t_=t_: e.memset(t_[:], 0.0), w=[nm_])
        tU = pb.sb(es, "tU", [128, 4, 64], F32)
        osb = pb.sb(es, "osb", [128, 512], F32)
        xmt = pb.sb(es, "xmt", [128, 8, 64], BF16)
        Btok = pb.sb(es, "Btok", [128, 2, 128], BF16)
        dts = pb.sb(es, "dts", [128, 4, 8], F32)
        ec = pb.sb(es, "ec", [128, 24], F32)
        LH = pb.sb(es, "LH", [128, 8, 128], F32)
        Em = pb.sb(es, "Em", [128, 8, 128], F32)
        cbm = pb.sb(es, "cbm", [128, 2, 128], F32)
        Mm = pb.sb(es, "Mm", [128, 8, 128], BF16)
        xdt = pb.sb(es, "xdt", [128, 8, 64], BF16)
        xdtw = pb.sb(es, "xdtw", [128, 8, 64], BF16)
        yi = pb.sb(es, "yi", [128, 8, 64], F32)
        ysb = pb.sb(es, "ysb", [128, 8, 64], F32)
        Ss = pb.sb(es, "Ss", [128, 8, 64], F32)
        Ssb = pb.sb(es, "Ssb", [128, 8, 64], BF16)
        k.op("dve", lambda e: e.memset(Sh[:], 0.0), w=["Sh"])
        k.op("dve", lambda e: e.memset(Ss[:], 0.0), w=["Ss"])
        k.op("dve", lambda e: e.memset(Ssb[:], 0.0), w=["Ssb"])

        supers = []
        if do_ctx:
            supers.append((1, 0))
        lat_order = list(range(nsup_lat)) if dirn == 0 else list(range(nsup_lat - 1, -1, -1))
        supers += [(0, i) for i in lat_order]
        bank_rot = [1, 2, 3]
        bri = 0
        xi = 0
        for (s, si) in supers:
            src = D["x"] if s == 0 else D["ctx"]
            Ts = T_LAT if s == 0 else T_CTX
            r0 = si * W
            g0 = r0 if s == 0 else T_LAT + r0
            for tl in range(2):
                b = xi % 2
                xi += 1
                rr = r0 + tl * 128
                k.dma("sp", lambda e, b=b, rr=rr: e.dma_start(out=xt[b][:], in_=src[rr:rr + 128, :]), w=["xt%d" % b])
                norm_mod_tile(pb, xt[b], "xt%d" % b, rstd[b], "rstd%d" % b, junk, t1, mv[(s, 1)], mv[(s, 0)], an[b], "an%d" % b)
                for kc in range(8):
                    k.op("pe", lambda e, b=b, kc=kc: e.transpose(out=P0b[:, kc * 128:(kc + 1) * 128],
                                                                 in_=an[b][:, kc * 128:(kc + 1) * 128], identity=C["ident_b"][:]),
                         r=["an%d" % b, "ident_b"], w=[PK[0]])
                k.op("act", lambda e, tl=tl: e.activation(out=aT[:, :, 2 + tl * 128:2 + (tl + 1) * 128],
                                                          in_=P0b.rearrange("p (a t) -> p a t", a=8), func=AF.Copy),
                     r=[PK[0]], w=["aT"])
            if getattr(pb, 'stop', 99) == 1:
                k.barrier()
                return
            has_l = r0 >= 2
            has_r = r0 + W + 2 <= Ts
            lrow = r0 - 2 if has_l else 0
            rrow = r0 + W if has_r else Ts - 2
            k.dma("sp", lambda e: e.dma_start(out=hx[0:2, :], in_=src[lrow:lrow + 2, :]), w=["hx"])
            k.dma("sp", lambda e: e.dma_start(out=hx[2:4, :], in_=src[rrow:rrow + 2, :]), w=["hx"])
            k.op("act", lambda e: e.activation(out=junk[0:4, :], in_=hx[:], func=AF.Square, accum_out=rstd[2][0:4, 0:1]),
                 r=["hx"], w=["junk", "rstd2"])
            k.op("act", lambda e: e.activation(out=rstd[2][0:4, 1:2], in_=rstd[2][0:4, 0:1], func=AF.Sqrt, bias=EPS, scale=1.0 / DM),
                 r=["rstd2"], w=["rstd2"])
            k.op("dve", lambda e: e.reciprocal(out=rstd[2][0:4, 2:3], in_=rstd[2][0:4, 1:2]), r=["rstd2"], w=["rstd2"])
            k.op("dve", lambda e: e.scalar_tensor_tensor(out=t1[0:4, :], in0=hx[:], scalar=rstd[2][0:4, 2:3], in1=mv[(s, 1)][0:4, :],
                                                         op0=ALU.mult, op1=ALU.mult), r=["hx", "rstd2", "modv"], w=["t1"])
            k.op("pool", lambda e: e.tensor_tensor(out=ah[:], in0=t1[0:4, :], in1=mv[(s, 0)][0:4, :], op=ALU.add),
                 r=["t1", "modv"], w=["ah"])
            for kc in range(8):
                k.op("pe", lambda e, kc=kc: e.transpose(out=P0b[:, kc * 4:(kc + 1) * 4], in_=ah[:, kc * 128:(kc + 1) * 128],
                                                        identity=C["ident_b"][0:4, 0:4]), r=["ah", "ident_b"], w=[PK[0]])
            hv = P0b[:, 0:32].rearrange("p (a t) -> p a t", a=8)
            if has_l:
                k.op("dve", lambda e: e.tensor_copy(out=aT[:, :, 0:2], in_=hv[:, :, 0:2]), r=[PK[0]], w=["aT"])
            else:
                k.op("dve", lambda e: e.memset(aT[:, :, 0:2], 0.0), r=[PK[0]], w=["aT"])
            if has_r:
                k.op("dve", lambda e: e.tensor_copy(out=aT[:, :, W + 2:W + 4], in_=hv[:, :, 2:4]), r=[PK[0]], w=["aT"])
            else:
                k.op("dve", lambda e: e.memset(aT[:, :, W + 2:W + 4], 0.0), r=[PK[0]], w=["aT"])
            if getattr(pb, 'stop', 99) == 2:
                k.barrier()
                return
            for c in range(4):
                bk = bank_rot[bri % 3]
                bri += 1
                for kc in range(8):
                    k.op("pe", lambda e, c=c, kc=kc, bk=bk: e.matmul(P[bk][:, 0:W], wq[:, kc, c * 128:(c + 1) * 128], aT[:, kc, 2:W + 2],
                                                                     start=(kc == 0), stop=(kc == 7)), r=["wq", "aT"], w=[PK[bk]])
                k.op("act", lambda e, c=c, bk=bk: e.activation(out=Qsil[:, c, :], in_=P[bk][:, 0:W], func=AF.Silu), r=[PK[bk]], w=["Qsil"])
            for c in range(4):
                bk = bank_rot[bri % 3]
                bri += 1
                for kc in range(8):
                    k.op("pe", lambda e, c=c, kc=kc, bk=bk: e.matmul(P[bk][:, 0:W], wf[:, kc, c * 128:(c + 1) * 128], aT[:, kc, 2:W + 2],
                                                                     start=(kc == 0), stop=(kc == 7)), r=["wf", "aT"], w=[PK[bk]])
                k.op("act", lambda e, c=c, bk=bk: e.activation(out=fg[:, c, :], in_=P[bk][:, 0:W], func=AF.Sigmoid), r=[PK[bk]], w=["fg"])
                k.op("dve", lambda e, c=c: e.tensor_scalar(out=fg[:, c, :], in0=fg[:, c, :], scalar1=oml[:, c:c + 1], scalar2=lbc[:, c:c + 1],
                                                           op0=ALU.mult, op1=ALU.add), r=["fg", "oml", "lbc"], w=["fg"])
            k.op("act", lambda e: e.activation(out=la[:], in_=fg[:], func=AF.Ln), r=["fg"], w=["la"])
            k.op("dve", lambda e: e.tensor_scalar(out=kk[:], in0=fg[:], scalar1=-1.0, scalar2=1.0, op0=ALU.mult, op1=ALU.add),
                 r=["fg"], w=["kk"])
            for c in range(8):
                bk = bank_rot[bri % 3]
                bri += 1
                for kc in range(8):
                    k.op("pe", lambda e, c=c, kc=kc, bk=bk: e.matmul(P[bk][:, 0:W + 4], wx[:, kc, c * 128:(c + 1) * 128], aT[:, kc, :],
                                                                     start=(kc == 0), stop=(kc == 7)), r=["wx", "aT"], w=[PK[bk]])
                k.op("act", lambda e, c=c, bk=bk: e.activation(out=u[:, c, :], in_=P[bk][:, 0:W + 4], func=AF.Copy), r=[PK[bk]], w=["u"])
            if getattr(pb, 'stop', 99) == 3:
                k.barrier()
                return
            for j in range(5):
                if j == 0:
                    k.op("dve", lambda e, j=j: e.tensor_tensor(out=acc[:], in0=u[:, :, j:j + W],
                                                               in1=cw[j][:].unsqueeze(2).to_broadcast([128, 8, W]), op=ALU.mult),
                         r=["u", "cw0"], w=["acc"])
                else:
                    k.op("pool", lambda e, j=j: e.tensor_tensor(out=tmp[:], in0=u[:, :, j:j + W],
                                                                in1=cw[j][:].unsqueeze(2).to_broadcast([128, 8, W]), op=ALU.mult),
                         r=["u", "cw%d" % j], w=["tmp"])
                    k.op("dve", lambda e: e.tensor_tensor(out=acc[:], in0=acc[:], in1=tmp[:], op=ALU.add), r=["acc", "tmp"], w=["acc"])
            k.op("dve", lambda e: e.tensor_tensor(out=acc[:], in0=acc[:], in1=cbc[:].unsqueeze(2).to_broadcast([128, 8, W]), op=ALU.add),
                 r=["acc", "cbc"], w=["acc"])
            k.op("act", lambda e: e.activation(out=xbc[:], in_=acc[:], func=AF.Silu), r=["acc"], w=["xbc"])
            if getattr(pb, 'stop', 99) == 4:
                k.barrier()
                return
            for tl in range(2):
                sl0 = 2 + tl * 128
                for kc in range(8):
                    k.op("pe", lambda e, kc=kc, sl0=sl0: e.matmul(P[6][:], aT[:, kc, sl0:sl0 + 128], wi[:, kc, :],
                                                                  start=(kc == 0), stop=(kc == 7)), r=["aT", "wi"], w=[PK[6]])
                k.op("act", lambda e, tl=tl: e.activation(out=vtok[:, tl, :], in_=P[6][:], func=AF.Copy), r=[PK[6]], w=["vtok"])
                for kc in range(8):
                    k.op("pe", lambda e, kc=kc, sl0=sl0: e.matmul(P[7][:, 0:8], aT[:, kc, sl0:sl0 + 128], wdt[:, kc, :],
                                                                  start=(kc == 0), stop=(kc == 7)), r=["aT", "wdt"], w=[PK[7]])
                k.op("dve", lambda e, tl=tl: e.tensor_tensor(out=dtr[:, tl, :], in0=P[7][:, 0:8], in1=dtb[:], op=ALU.add),
                     r=[PK[7], "dtb"], w=["dtr"])
            if getattr(pb, 'stop', 99) == 5:
                k.barrier()
                return
            tls = [0, 1] if dirn == 0 else [1, 0]
            for tl in tls:
                sl = slice(tl * 128, (tl + 1) * 128)
                grow = g0 + tl * 128
                for c in range(4):
                    k.op("dve", lambda e, c=c: e.tensor_tensor_scan(out=b2[:, c, :], data0=C["ones_f"][:, 0:128], data1=la[:, c, sl],
                                                                    initial=0.0, op0=ALU.mult, op1=ALU.add),
                         r=["la", "ones_f"], w=["b2"])
                if dirn == 0:
                    bsrc, bkey, last = b2, "b2", 127
                else:
                    k.op("dve", lambda e: e.tensor_tensor(out=bb[:], in0=la[:, :, sl], in1=b2[:], op=ALU.subtract), r=["la", "b2"], w=["bb"])
                    for c in range(4):
                        k.op("dve", lambda e, c=c: e.tensor_scalar(out=bb[:, c, :], in0=bb[:, c, :], scalar1=b2[:, c, 127:128], scalar2=None,
                                                                   op0=ALU.add), r=["bb", "b2"], w=["bb"])
                    bsrc, bkey, last = bb, "bb", 0
                k.op("dve", lambda e: e.tensor_copy(out=sc[:, 0, :], in_=bsrc[:, :, 64]), r=[bkey], w=["sc"])
                k.op("dve", lambda e: e.tensor_copy(out=sc[:, 2, :], in_=bsrc[:, :, last]), r=[bkey], w=["sc"])
                k.op("dve", lambda e: e.tensor_tensor(out=sc[:, 1, :], in0=sc[:, 2, :], in1=sc[:, 0, :], op=ALU.subtract), r=["sc"], w=["sc"])
                k.op("dve", lambda e: e.tensor_scalar(out=nb[:], in0=sc[:, 0, :], scalar1=-1.0, scalar2=None, op0=ALU.mult), r=["sc"], w=["nb"])
                k.op("act", lambda e: e.activation(out=esc[:], in_=sc[:], func=AF.Exp), r=["sc"], w=["esc"])
                for c in range(4):
                    k.op("act", lambda e, c=c: e.activation(out=Eq[:, c, :], in_=bsrc[:, c, :], func=AF.Exp, bias=nb[:, c:c + 1], scale=1.0),
                         r=[bkey, "nb"], w=["Eq"])
                    k.op("act", lambda e, c=c: e.activation(out=Ek[:, c, :], in_=bsrc[:, c, :], func=AF.Exp, bias=sc[:, 0, c:c + 1], scale=-1.0),
                         r=[bkey, "sc"], w=["Ek"])
                k.op("dve", lambda e: e.tensor_tensor(out=Qt[:], in0=Qsil[:, :, sl], in1=Eq[:], op=ALU.mult), r=["Qsil", "Eq"], w=["Qt"])
                k.op("dve", lambda e: e.tensor_tensor(out=Kt[:], in0=kk[:, :, sl], in1=Ek[:], op=ALU.mult), r=["kk", "Ek"], w=["Kt"])
                k.op("pool", lambda e: e.tensor_copy(out=KtA[0:64, :, :], in_=Kt[0:64, :, :]), r=["Kt"], w=["KtA"])
                k.op("pool", lambda e: e.tensor_copy(out=KtB[64:128, :, :], in_=Kt[64:128, :, :]), r=["Kt"], w=["KtB"])
                k.op("dve", lambda e: e.tensor_tensor(out=ShpA[0:64, :, :], in0=Sh[0:64, :, :],
                                                      in1=esc[0:64, 0, :].unsqueeze(2).to_broadcast([64, 4, 64]), op=ALU.mult),
                     r=["Sh", "esc"], w=["ShpA"])
                k.op("dve", lambda e: e.tensor_tensor(out=ShpB[64:128, :, :], in0=Sh[64:128, :, :],
                                                      in1=esc[64:128, 0, :].unsqueeze(2).to_broadcast([64, 4, 64]), op=ALU.mult),
                     r=["Sh", "esc"], w=["ShpB"])
                if getattr(pb, 'stop', 99) == 51:
                    k.barrier()
                    return
                for c in range(4):
                    k.op("pe", lambda e, c=c: e.transpose(out=P0b[:, c * 128:(c + 1) * 128], in_=Kt[:, c, :], identity=C["ident_b"][:]),
                         r=["Kt", "ident_b"], w=[PK[0]])
                k.op("act", lambda e: e.activation(out=Ktok[:].rearrange("p a t -> p (a t)"), in_=P0b[:, 0:512], func=AF.Copy),
                     r=[PK[0]], w=["Ktok"])
                if getattr(pb, 'stop', 99) == 52:
                    k.barrier()
                    return
                for h in range(8):
                    c, base = h // 2, (h % 2) * 64
                    bk = 1 + h // 4
                    Kz = KtA if h % 2 == 0 else KtB
                    k.op("pe", lambda e, h=h, c=c, bk=bk, Kz=Kz: e.matmul(
                        P[bk][:, (h % 4) * 128:(h % 4 + 1) * 128], Kz[:, c, :], Qt[:, c, :], start=True, stop=True),
                        r=["KtA", "KtB", "Qt"], w=[PK[bk]])
                for hh in range(2):
                    k.op("dve", lambda e, hh=hh: e.tensor_tensor(
                        out=attm[:, hh * 4:(hh + 1) * 4, :], in0=P[1 + hh][:].rearrange("p (a t) -> p a t", a=4),
                        in1=m_att_f[:].unsqueeze(1).to_broadcast([128, 4, 128]), op=ALU.mult), r=[PK[1 + hh], "masks"], w=["attm"])
                if getattr(pb, 'stop', 99) == 53:
                    k.barrier()
                    return
                for h in range(8):
                    c, base = h // 2, (h % 2) * 64
                    k.op("pe", lambda e, h=h: e.matmul(P[3][:, h * 64:(h + 1) * 64], attm[:, h, :], vtok[:, tl, h * 64:(h + 1) * 64],
                                                       start=True, stop=False), r=["attm", "vtok"], w=[PK[3]])
                    Sz = ShpA if h % 2 == 0 else ShpB
                    k.op("pe", lambda e, h=h, c=c, Sz=Sz: e.matmul(P[3][:, h * 64:(h + 1) * 64], Qt[:, c, :],
                                                                   Sz[:, c, :], start=False, stop=True),
                         r=["Qt", "ShpA", "ShpB"], w=[PK[3]])
                k.op("act", lambda e: e.activation(out=osb[:], in_=P[3][:], func=AF.Copy), r=[PK[3]], w=["osb"])
                k.dma("sp", lambda e, grow=grow: e.dma_start(out=D["oh"][dirn, grow:grow + 128, :], in_=osb[:]), r=["osb"], w=["oh"])
                if getattr(pb, 'stop', 99) == 54:
                    k.barrier()
                    return
                for c in range(4):
                    k.op("pe", lambda e, c=c: e.matmul(P[4][:, c * 128:(c + 1) * 128], Ktok[:, c, :], vtok[:, tl, c * 128:(c + 1) * 128],
                                                       start=True, stop=True), r=["Ktok", "vtok"], w=[PK[4]])
                p4v = P[4][:].rearrange("p (c x) -> p c x", c=4)
                k.op("dve", lambda e: e.tensor_tensor(out=tU[0:64, :, :], in0=p4v[0:64, :, 0:64],
                                                      in1=esc[0:64, 1, :].unsqueeze(2).to_broadcast([64, 4, 64]), op=ALU.mult),
                     r=[PK[4], "esc"], w=["tU"])
                k.op("dve", lambda e: e.tensor_tensor(out=tU[64:128, :, :], in0=p4v[64:128, :, 64:128],
                                                      in1=esc[64:128, 1, :].unsqueeze(2).to_broadcast([64, 4, 64]), op=ALU.mult),
                     r=[PK[4], "esc"], w=["tU"])
                k.op("dve", lambda e: e.tensor_tensor(out=Sh[:], in0=Sh[:], in1=esc[:, 2, :].unsqueeze(2).to_broadcast([128, 4, 64]), op=ALU.mult),
                     r=["Sh", "esc"], w=["Sh"])
                k.op("dve", lambda e: e.tensor_tensor(out=Sh[:], in0=Sh[:], in1=tU[:], op=ALU.add), r=["Sh", "tU"], w=["Sh"])
                if getattr(pb, 'stop', 99) == 6:
                    k.barrier()
                    return
                for c in range(4):
                    k.op("pe", lambda e, c=c: e.transpose(out=P0b[:, c * 128:(c + 1) * 128], in_=xbc[:, c, sl], identity=C["ident_b"][:]),
                         r=["xbc", "ident_b"], w=[PK[0]])
                for g in range(2):
                    k.op("pe", lambda e, g=g: e.transpose(out=P0b[:, 512 + g * 128:512 + (g + 1) * 128], in_=xbc[:, 4 + g, sl],
                                                          identity=C["ident_b"][:]), r=["xbc", "ident_b"], w=[PK[0]])
                k.op("act", lambda e: e.activation(out=xmt[:].rearrange("p a v -> p (a v)"), in_=P0b[:, 0:512], func=AF.Copy),
                     r=[PK[0]], w=["xmt"])
                k.op("act", lambda e: e.activation(out=Btok[:].rearrange("p a v -> p (a v)"), in_=P0b[:, 512:768], func=AF.Copy),
                     r=[PK[0]], w=["Btok"])
                k.op("act", lambda e: e.activation(out=dts[:, 3, :], in_=dtr[:, tl, :], func=AF.Exp), r=["dtr"], w=["dts3"])
                k.op("act", lambda e: e.activation(out=dts[:, 0, :], in_=dts[:, 3, :], func=AF.Ln, bias=1.0, scale=1.0), r=["dts3"], w=["dts0"])
                k.op("dve", lambda e: e.tensor_tensor(out=dts[:, 1, :], in0=dts[:, 0, :], in1=negA[:], op=ALU.mult), r=["dts0", "negA"], w=["dts1"])
                if getattr(pb, 'stop', 99) == 7:
                    k.barrier()
                    return
                k.op("pe", lambda e: e.matmul(P[7][:, 0:8], m_cum[:], dts[:, 1, :], start=True, stop=True), r=["dts1", "masks"], w=[PK[7]])
                k.op("pe", lambda e: e.matmul(P[7][:, 8:16], m_wend[:], dts[:, 1, :], start=True, stop=True), r=["dts1", "masks"], w=[PK[7]])
                k.op("pe", lambda e: e.matmul(P[7][:, 16:24], C["ones_f"][:], dts[:, 1, :], start=True, stop=True), r=["dts1", "ones_f"], w=[PK[7]])
                k.op("act", lambda e: e.activation(out=ec[:], in_=P[7][:, 0:24], func=AF.Exp), r=[PK[7]], w=["ec"])
                k.op("dve", lambda e: e.tensor_tensor(out=LH[:], in0=m_lh[:].unsqueeze(1).to_broadcast([128, 8, 128]),
                                                      in1=dts[:, 1, :].unsqueeze(2).to_broadcast([128, 8, 128]), op=ALU.mult),
                     r=["dts1", "masks"], w=["LH"])
                for h in range(8):
                    bk = 1 + h // 4
                    k.op("pe", lambda e, h=h, bk=bk: e.matmul(P[bk][:, (h % 4) * 128:(h % 4 + 1) * 128], LH[:, h, :], m_rhs[:],
                                                              start=True, stop=True), r=["LH", "masks"], w=[PK[bk]])
                for hh in range(2):
                    k.op("act", lambda e, hh=hh: e.activation(out=Em[:, hh * 4:(hh + 1) * 4, :].rearrange("p a t -> p (a t)"), in_=P[1 + hh][:],
                                                              func=AF.Exp), r=[PK[1 + hh]], w=["Em"])
                if getattr(pb, 'stop', 99) == 8:
                    k.barrier()
                    return
                for g in range(2):
                    k.op("pe", lambda e, g=g: e.matmul(P[3][:, g * 128:(g + 1) * 128], xbc[:, 4 + g, sl], xbc[:, 6 + g, sl], start=True, stop=True),
                         r=["xbc"], w=[PK[3]])
                k.op("dve", lambda e: e.tensor_tensor(out=cbm[:], in0=P[3][:, 0:256].rearrange("p (a t) -> p a t", a=2),
                                                      in1=m_att_f[:].unsqueeze(1).to_broadcast([128, 2, 128]), op=ALU.mult),
                     r=[PK[3], "masks"], w=["cbm"])
                for g in range(2):
                    k.op("dve", lambda e, g=g: e.tensor_tensor(out=Mm[:, g * 4:(g + 1) * 4, :], in0=Em[:, g * 4:(g + 1) * 4, :],
                                                               in1=cbm[:, g, :].unsqueeze(1).to_broadcast([128, 4, 128]), op=ALU.mult),
                         r=["Em", "cbm"], w=["Mm"])
                k.op("dve", lambda e: e.tensor_tensor(out=dts[:, 2, :], in0=dts[:, 0, :], in1=ec[:, 8:16], op=ALU.mult), r=["dts0", "ec"], w=["dts2"])
                k.op("pool", lambda e: e.tensor_tensor(out=xdt[:], in0=xmt[:], in1=dts[:, 0, :].unsqueeze(2).to_broadcast([128, 8, 64]), op=ALU.mult),
                     r=["xmt", "dts0"], w=["xdt"])
                k.op("pool", lambda e: e.tensor_tensor(out=xdtw[:], in0=xmt[:], in1=dts[:, 2, :].unsqueeze(2).to_broadcast([128, 8, 64]), op=ALU.mult),
                     r=["xmt", "dts2"], w=["xdtw"])
                if getattr(pb, 'stop', 99) == 9:
                    k.barrier()
                    return
                for h in range(8):
                    k.op("pe", lambda e, h=h: e.matmul(P[4][:, h * 64:(h + 1) * 64], Mm[:, h, :], xdt[:, h, :], start=True, stop=True),
                         r=["Mm", "xdt"], w=[PK[4]])
                for g in range(2):
                    k.op("pe", lambda e, g=g: e.matmul(P[5][:, g * 256:(g + 1) * 256], xbc[:, 6 + g, sl],
                                                       Ssb[:, g * 4:(g + 1) * 4, :].rearrange("p a v -> p (a v)"), start=True, stop=True),
                         r=["xbc", "Ssb"], w=[PK[5]])
                k.op("dve", lambda e: e.tensor_tensor(out=yi[:], in0=P[5][:].rearrange("p (a v) -> p a v", a=8),
                                                      in1=ec[:, 0:8].unsqueeze(2).to_broadcast([128, 8, 64]), op=ALU.mult),
                     r=[PK[5], "ec"], w=["yi"])
                k.op("dve", lambda e: e.tensor_tensor(out=ysb[:], in0=P[4][:].rearrange("p (a v) -> p a v", a=8), in1=yi[:], op=ALU.add),
                     r=[PK[4], "yi"], w=["ysb"])
                if dirn == 0:
                    k.op("pool", lambda e: e.tensor_tensor(out=yi[:], in0=xmt[:], in1=dsk[:].unsqueeze(2).to_broadcast([128, 8, 64]), op=ALU.mult),
                         r=["xmt", "dsk", "ysb"], w=["yi"])
                    k.op("dve", lambda e: e.tensor_tensor(out=ysb[:], in0=ysb[:], in1=yi[:], op=ALU.add), r=["ysb", "yi"], w=["ysb"])
                k.dma("sp", lambda e, grow=grow: e.dma_start(out=D["ys"][dirn, grow:grow + 128, :], in_=ysb[:].rearrange("p a v -> p (a v)")),
                      r=["ysb"], w=["ys"])
                for g in range(2):
                    k.op("pe", lambda e, g=g: e.matmul(P[6][:, g * 256:(g + 1) * 256], Btok[:, g, :],
                                                       xdtw[:, g * 4:(g + 1) * 4, :].rearrange("p a v -> p (a v)"), start=True, stop=True),
                         r=["Btok", "xdtw"], w=[PK[6]])
                k.op("dve", lambda e: e.tensor_tensor(out=Ss[:], in0=Ss[:], in1=ec[:, 16:24].unsqueeze(2).to_broadcast([128, 8, 64]), op=ALU.mult),
                     r=["Ss", "ec"], w=["Ss"])
                k.op("dve", lambda e: e.tensor_tensor(out=Ss[:], in0=Ss[:], in1=P[6][:].rearrange("p (a v) -> p a v", a=8), op=ALU.add),
                     r=["Ss", PK[6]], w=["Ss"])
                k.op("act", lambda e: e.activation(out=Ssb[:], in_=Ss[:], func=AF.Copy), r=["Ss"], w=["Ssb"])
        k.barrier()


def phase_mix_merge(pb, C, ntiles=66):
    k, D, nc = pb.k, pb.D, pb.nc
    with ExitStack() as es:
        P = [pb.ps(es, "P%d" % i, [128, 512], F32) for i in range(8)]
        PK = ["P%d" % i for i in range(8)]
        P0b = P[0][:].bitcast(BF16)
        mv = load_modv(pb, es, 0, (0, 1, 2))
        stg = pb.sb(es, "wstg", [128, 512], F32)
        wg = load_w_bf16(pb, es, "wg", D["w_in"][:, 2048:2560], 8, 512, stg, "wstg")
        wz = load_w_bf16(pb, es, "wz", D["w_in"][:, 2560:3072], 8, 512, stg, "wstg")
        wo = load_w_bf16(pb, es, "wo", D["w_out_rec"], 8, 1024, stg, "wstg")
        hg = pb.sb(es, "hg", [128, 512], F32)
        mn = pb.sb(es, "mn", [128, 512], F32)
        k.dma("sp", lambda e: e.dma_start(out=hg[:], in_=D["hgrn_norm"].to_broadcast([128, 512])), w=["gains"])
        k.dma("sp", lambda e: e.dma_start(out=mn[:], in_=D["mamba_norm"].to_broadcast([128, 512])), w=["gains"])
        xt = [pb.sb(es, "xt%d" % i, [128, 1024], F32) for i in range(2)]
        an = [pb.sb(es, "an%d" % i, [128, 1024], BF16) for i in range(2)]
        junk = pb.sb(es, "junk", [128, 1024], BF16)
        t1 = pb.sb(es, "t1", [128, 1024], F32)
        rstd = [pb.sb(es, "rstd%d" % i, [128, 4], F32) for i in range(2)]
        aT = pb.sb(es, "aT", [128, 8, 128], BF16)
        sg = pb.sb(es, "sg", [128, 512], F32)
        sz = pb.sb(es, "sz", [128, 512], F32)
        o0 = [pb.sb(es, "o0%d" % i, [128, 512], F32) for i in range(2)]
        o1 = [pb.sb(es, "o1%d" % i, [128, 512], F32) for i in range(2)]
        y0 = [pb.sb(es, "y0%d" % i, [128, 512], F32) for i in range(2)]
        y1 = [pb.sb(es, "y1%d" % i, [128, 512], F32) for i in range(2)]
        sq = pb.sb(es, "sq", [128, 512], F32)
        st8 = pb.sb(es, "st8", [128, 3, 8], F32)
        rs2 = pb.sb(es, "rs2", [128, 4], F32)
        cat = pb.sb(es, "cat", [128, 1024], BF16)
        catT = pb.sb(es, "catT", [128, 8, 128], BF16)
        xo = [pb.sb(es, "xo%d" % i, [128, 1024], F32) for i in range(2)]
        for j in range(ntiles):
            b = j % 2
            if j < 2:
                s, src, rr, grow = 1, D["ctx"], j * 128, T_LAT + j * 128
            else:
                s, src, rr, grow = 0, D["x"], (j - 2) * 128, (j - 2) * 128
            k.dma("sp", lambda e: e.dma_start(out=xt[b][:], in_=src[rr:rr + 128, :]), w=["xt%d" % b])
            k.dma("sp", lambda e: e.dma_start(out=o0[b][:], in_=D["oh"][0, grow:grow + 128, :]), r=["oh"], w=["o0%d" % b])
            k.dma("sp", lambda e: e.dma_start(out=o1[b][:], in_=D["oh"][1, grow:grow + 128, :]), r=["oh"], w=["o1%d" % b])
            k.dma("sp", lambda e: e.dma_start(out=y0[b][:], in_=D["ys"][0, grow:grow + 128, :]), r=["ys"], w=["y0%d" % b])
            k.dma("sp", lambda e: e.dma_start(out=y1[b][:], in_=D["ys"][1, grow:grow + 128, :]), r=["ys"], w=["y1%d" % b])
            norm_mod_tile(pb, xt[b], "xt%d" % b, rstd[b], "rstd%d" % b, junk, t1, mv[(s, 1)], mv[(s, 0)], an[b], "an%d" % b)
            for kc in range(8):
                k.op("pe", lambda e, kc=kc: e.transpose(out=P0b[:, kc * 128:(kc + 1) * 128], in_=an[b][:, kc * 128:(kc + 1) * 128],
                                                        identity=C["ident_b"][:]), r=["an%d" % b, "ident_b"], w=[PK[0]])
            k.op("act", lambda e: e.activation(out=aT[:].rearrange("p a t -> p (a t)"), in_=P0b, func=AF.Copy), r=[PK[0]], w=["aT"])
            for kc in range(8):
                k.op("pe", lambda e, kc=kc: e.matmul(P[1][:], aT[:, kc, :], wg[:, kc, :], start=(kc == 0), stop=(kc == 7)),
                     r=["aT", "wg"], w=[PK[1]])
            k.op("act", lambda e: e.activation(out=sg[:], in_=P[1][:], func=AF.Sigmoid), r=[PK[1]], w=["sg"])
            for kc in range(8):
                k.op("pe", lambda e, kc=kc: e.matmul(P[2][:], aT[:, kc, :], wz[:, kc, :], start=(kc == 0), stop=(kc == 7)),
                     r=["aT", "wz"], w=[PK[2]])
            k.op("act", lambda e: e.activation(out=sz[:], in_=P[2][:], func=AF.Silu), r=[PK[2]], w=["sz"])
            ok0, ok1, yk0, yk1 = "o0%d" % b, "o1%d" % b, "y0%d" % b, "y1%d" % b
            k.op("dve", lambda e: e.tensor_tensor(out=o0[b][:], in0=o0[b][:], in1=o1[b][:], op=ALU.add), r=[ok0, ok1], w=[ok0])
            k.op("pool", lambda e: e.tensor_tensor(out=sq[:], in0=o0[b][:], in1=o0[b][:], op=ALU.mult), r=[ok0], w=["sq"])
            k.op("dve", lambda e: e.tensor_reduce(out=st8[:, 0, :], in_=sq[:].rearrange("p (a v) -> p a v", a=8), axis=AX.X, op=ALU.add),
                 r=["sq"], w=["st8"])
            k.op("act", lambda e: e.activation(out=st8[:, 1, :], in_=st8[:, 0, :], func=AF.Sqrt, bias=EPS, scale=1.0 / 64), r=["st8"], w=["st8"])
            k.op("dve", lambda e: e.reciprocal(out=st8[:, 2, :], in_=st8[:, 1, :]), r=["st8"], w=["st8"])
            k.op("dve", lambda e: e.tensor_tensor(out=o0[b][:].rearrange("p (a v) -> p a v", a=8), in0=o0[b][:].rearrange("p (a v) -> p a v", a=8),
                                                  in1=st8[:, 2, :].unsqueeze(2).to_broadcast([128, 8, 64]), op=ALU.mult), r=[ok0, "st8"], w=[ok0])
            k.op("pool", lambda e: e.tensor_tensor(out=o0[b][:], in0=o0[b][:], in1=hg[:], op=ALU.mult), r=[ok0, "gains"], w=[ok0])
            k.op("dve", lambda e: e.tensor_tensor(out=cat[:, 0:512], in0=o0[b][:], in1=sg[:], op=ALU.mult), r=[ok0, "sg"], w=["cat"])
            k.op("pool", lambda e: e.tensor_tensor(out=y0[b][:], in0=y0[b][:], in1=y1[b][:], op=ALU.add), r=[yk0, yk1], w=[yk0])
            k.op("dve", lambda e: e.tensor_tensor(out=y0[b][:], in0=y0[b][:], in1=sz[:], op=ALU.mult), r=[yk0, "sz"], w=[yk0])
            k.op("act", lambda e: e.activation(out=junk[:, 0:512], in_=y0[b][:], func=AF.Square, accum_out=rs2[:, 0:1]), r=[yk0], w=["junk", "rs2"])
            k.op("act", lambda e: e.activation(out=rs2[:, 1:2], in_=rs2[:, 0:1], func=AF.Sqrt, bias=EPS, scale=1.0 / 512), r=["rs2"], w=["rs2"])
            k.op("dve", lambda e: e.reciprocal(out=rs2[:, 2:3], in_=rs2[:, 1:2]), r=["rs2"], w=["rs2"])
            k.op("dve", lambda e: e.scalar_tensor_tensor(out=cat[:, 512:1024], in0=y0[b][:], scalar=rs2[:, 2:3], in1=mn[:],
                                                         op0=ALU.mult, op1=ALU.mult), r=[yk0, "rs2", "gains"], w=["cat"])
            for c in range(8):
                k.op("pe", lambda e, c=c: e.transpose(out=P0b[:, c * 128:(c + 1) * 128], in_=cat[:, c * 128:(c + 1) * 128],
                                                      identity=C["ident_b"][:]), r=["cat", "ident_b"], w=[PK[0]])
            k.op("act", lambda e: e.activation(out=catT[:].rearrange("p a t -> p (a t)"), in_=P0b, func=AF.Copy), r=[PK[0]], w=["catT"])
            for dh in range(2):
                for c in range(8):
                    k.op("pe", lambda e, c=c, dh=dh: e.matmul(P[3 + dh][:], catT[:, c, :], wo[:, c, dh * 512:(dh + 1) * 512],
                                                              start=(c == 0), stop=(c == 7)), r=["catT", "wo"], w=[PK[3 + dh]])
                k.op("dve", lambda e, dh=dh: e.tensor_tensor(out=xo[b][:, dh * 512:(dh + 1) * 512], in0=P[3 + dh][:],
                                                             in1=mv[(s, 2)][:, dh * 512:(dh + 1) * 512], op=ALU.mult),
                     r=[PK[3 + dh], "modv"], w=["xo%d" % b])
            k.op("pool", lambda e: e.tensor_tensor(out=xo[b][:], in0=xo[b][:], in1=xt[b][:], op=ALU.add), r=["xo%d" % b, "xt%d" % b], w=["xo%d" % b])
            k.dma("sp", lambda e: e.dma_start(out=D["xres"][grow:grow + 128, :], in_=xo[b][:]), r=["xo%d" % b], w=["xres"])
        k.barrier()


def declare_all(pb):
    pb.din("x", [T_LAT, DM]); pb.din("ctx", [T_CTX, DM]); pb.din("c", [1, DM]); pb.din("c_ctx", [1, DM])
    pb.din("w_mod", [2, DM, 6 * DM]); pb.din("b_mod", [2, 6 * DM]); pb.din("norm_mix", [2, DM]); pb.din("norm_ffn", [2, DM])
    pb.din("norm_out", [1, DM])
    pb.din("w_in", [DM, 4112]); pb.din("w_out_rec", [DM, DM]); pb.din("conv_w", [5, DM]); pb.din("conv_b", [1, DM])
    pb.din("lb_gamma", [2, 2, 512]); pb.din("dt_bias", [2, 8]); pb.din("a_log", [2, 8]); pb.din("d_skip", [1, 8])
    pb.din("hgrn_norm", [1, 512]); pb.din("mamba_norm", [1, 512])
    pb.din("w_dq", [DM, 384]); pb.din("q_norm", [1, 384]); pb.din("w_uq_r", [384, 2048]); pb.din("w_dkv", [DM, 256])
    pb.din("kv_norm", [1, 256]); pb.din("w_ukv_r", [256, 2048]); pb.din("w_kr_r", [DM, 128]); pb.din("w_o", [DM, DM])
    pb.din("w_router", [2, DM, NE]); pb.din("w_gate", [2, NE, DM, DM]); pb.din("w_up", [2, NE, DM, DM]); pb.din("w_down", [2, NE, DM, DM])
    for nm, v in host_consts().items():
        pb.din("c_" + nm, v.shape)
    for nm, v in attn_host_consts().items():
        pb.din("c_" + nm, v.shape)
    pb.dscr("xres", [NROWS, DM]); pb.dscr("hn", [NROWS, DM], BF16); pb.dscr("aff", [NROWS, 16]); pb.dscr("modrep", [2, 2, 6, 128, DM])
    pb.dscr("oh", [2, NTOK, 512]); pb.dscr("ys", [2, NTOK, 512])
    pb.dscr("KT", [NH, 128, NTOK], BF16); pb.dscr("KRT", [64, NTOK], BF16); pb.dscr("Vd", [NTOK, 1024], BF16)
    pb.dscr("QT", [NH, 128, T_LAT], BF16); pb.dscr("QRT", [NH, 64, T_LAT], BF16); pb.dscr("Od", [T_LAT, 1024], BF16)


def make_inputs(inp, b):
    f = lambda a: np.ascontiguousarray(a, dtype=np.float32)
    w_uq_r, w_ukv_r, w_kr_r = attn_layout_weights(inp["w_uq"][0], inp["w_ukv"][0], inp["w_kr"][0])
    im = {"x": f(inp["x"][b]), "ctx": f(inp["ctx"][b]), "c": f(inp["c"][b:b + 1]), "c_ctx": f(inp["c_ctx"][None, :]),
          "w_mod": f(inp["w_mod"]), "b_mod": f(inp["b_mod"]), "norm_mix": f(inp["norm_mix"]), "norm_ffn": f(inp["norm_ffn"]),
          "norm_out": f(inp["norm_out"][None, :]),
          "w_in": f(inp["w_in"][0]), "w_out_rec": f(inp["w_out_rec"][0]), "conv_w": f(inp["conv_w"][0]), "conv_b": f(inp["conv_b"]),
          "lb_gamma": f(inp["lb_gamma"]), "dt_bias": f(inp["dt_bias"][0]), "a_log": f(inp["a_log"][0]), "d_skip": f(inp["d_skip"]),
          "hgrn_norm": f(inp["hgrn_norm"]), "mamba_norm": f(inp["mamba_norm"]),
          "w_dq": f(inp["w_dq"][0]), "q_norm": f(inp["q_norm"]), "w_uq_r": f(w_uq_r), "w_dkv": f(inp["w_dkv"][0]),
          "kv_norm": f(inp["kv_norm"]), "w_ukv_r": f(w_ukv_r), "w_kr_r": f(w_kr_r), "w_o": f(inp["w_o"][0]),
          "w_router": f(inp["w_router"]), "w_gate": f(inp["w_gate"]), "w_up": f(inp["w_up"]), "w_down": f(inp["w_down"])}
    for nm, v in host_consts().items():
        im["c_" + nm] = v
    for nm, v in attn_host_consts().items():
        im["c_" + nm] = v
    return im


def build_program(phases, dbg=False, opts=None):
    opts = opts or {}
    nc = bass.Bass("TRN2", target_bir_lowering=False)
    pb = PB(nc)
    declare_all(pb)
    pb.dout("out", [T_LAT, DM])
    if dbg:
        pb.dout("dbg", [NTOK, DM])
    with ExitStack() as es:
        C = load_consts(pb, es)
        phase_init(pb, copy_x=("init" in phases))
        phase_mod(pb, C)
        if "mixf" in phases:
            phase_mix_pass(pb, C, 0, **opts.get("mix", {}))
        if "mixb" in phases:
            phase_mix_pass(pb, C, 1, **opts.get("mix", {}))
        if "mixm" in phases:
            phase_mix_merge(pb, C, **opts.get("merge", {}))
        if "moe0" in phases:
            phase_moe(pb, C, 0)
        if "attn_pre" in phases:
            phase_attn_pre(pb, C)
        if "attn_main" in phases:
            phase_attn_main(pb, C, **opts.get("attn", {}))
        if "attn_post" in phases:
            phase_attn_post(pb, C)
        if "moe1" in phases:
            phase_moe(pb, C, 1)
        if "final" in phases:
            phase_final(pb, C)
        k = pb.k
        if dbg:
            for i in range(8):
                k.dma("sp", lambda e, i=i: e.dma_start(out=pb.D["dbg"][i * 1056:(i + 1) * 1056, :], in_=pb.D["xres"][i * 1056:(i + 1) * 1056, :]),
                      r=["xres"], w=["dbg"])
        k.finish()
    return nc, pb


ALL_PHASES = ["mixf", "mixb", "mixm", "moe0", "attn_pre", "attn_main", "attn_post", "moe1", "final"]


def kernel(**inputs):
    from concourse.bass_utils import run_bass_kernel_spmd
    inp = {k: np.asarray(v) for k, v in inputs.items()}
    nb = inp["x"].shape[0]
    nc, pb = build_program(ALL_PHASES)
    in_maps = [make_inputs(inp, b) for b in range(nb)]
    res = run_bass_kernel_spmd(nc, in_maps, core_ids=list(range(nb)))
    out = np.stack([np.asarray(res.results[b]["out"]) for b in range(nb)], axis=0)
    return out.astype(np.float32)
```

```python
import numpy as np
import concourse.bass as bass
import concourse.mybir as mybir

F32 = mybir.dt.float32
BF16 = mybir.dt.bfloat16
I32 = mybir.dt.int32
U32 = mybir.dt.uint32
AF = mybir.ActivationFunctionType
ALU = mybir.AluOpType
AX = mybir.AxisListType


class KF:
    NDS = 8

    def __init__(self, nc):
        self.nc = nc
        self.eng = {"pe": nc.tensor, "dve": nc.vector, "act": nc.scalar, "pool": nc.gpsimd, "sp": nc.sync}
        self.csem = {}
        self.ccnt = {}
        for e in self.eng:
            self.csem[e] = nc.alloc_semaphore("cs_" + e)
            self.ccnt[e] = 0
        self.dsem = {}
        self.dval = {}
        self.drot = {}
        for q in ("sp", "pool", "act"):
            self.dsem[q] = [nc.alloc_semaphore("ds_%s%d" % (q, i)) for i in range(self.NDS)]
            self.dval[q] = [0] * self.NDS
            self.drot[q] = 0
        self.waited = {e: {} for e in self.eng}
        self.lastw = {}
        self.readers = {}
        self.same_eng_sync = {"pe": False, "dve": True, "act": True, "pool": True, "sp": True}
        self.nins = 0

    def _wait(self, e, tok):
        sem, val, src = tok
        if src == e and not self.same_eng_sync[e]:
            return
        key = id(sem)
        if self.waited[e].get(key, 0) >= val:
            return
        self.eng[e].wait_ge(sem, val)
        self.waited[e][key] = val
        self.nins += 1

    def _deps(self, e, r, w):
        for k in r:
            t = self.lastw.get(k)
            if t is not None:
                self._wait(e, t)
        for k in w:
            t = self.lastw.get(k)
            if t is not None:
                self._wait(e, t)
            for t in self.readers.get(k, ()):
                self._wait(e, t)

    def _commit(self, tok, r, w):
        for k in w:
            self.lastw[k] = tok
            self.readers[k] = []
        for k in r:
            self.readers.setdefault(k, []).append(tok)

    def op(self, e, fn, r=(), w=()):
        r = [x for x in r if x is not None]
        w = [x for x in w if x is not None]
        self._deps(e, r, w)
        ins = fn(self.eng[e])
        self.ccnt[e] += 1
        ins.then_inc(self.csem[e], 1)
        tok = (self.csem[e], self.ccnt[e], e)
        self._commit(tok, r, w)
        self.nins += 1
        return tok

    def dma(self, q, fn, r=(), w=()):
        r = [x for x in r if x is not None]
        w = [x for x in w if x is not None]
        i = self.drot[q]
        self.drot[q] = (i + 1) % self.NDS
        sem = self.dsem[q][i]
        self._wait(q, (sem, self.dval[q][i], None))
        self._deps(q, r, w)
        ins = fn(self.eng[q])
        self.dval[q][i] += 16
        ins.then_inc(sem, 16)
        tok = (sem, self.dval[q][i], None)
        self._commit(tok, r, w)
        self.nins += 1
        return tok

    def barrier(self):
        toks = []
        for e in self.eng:
            if self.ccnt[e] > 0:
                toks.append((self.csem[e], self.ccnt[e], None))
        for q in self.dsem:
            for i in range(self.NDS):
                if self.dval[q][i] > 0:
                    toks.append((self.dsem[q][i], self.dval[q][i], None))
        for e in self.eng:
            for t in toks:
                if t[0] is self.csem[e]:
                    continue
                self._wait(e, t)
        self.lastw = {}
        self.readers = {}

    def finish(self):
        for e in self.eng:
            if e != "sp" and self.ccnt[e] > 0:
                self._wait("sp", (self.csem[e], self.ccnt[e], None))
        for q in self.dsem:
            for i in range(self.NDS):
                if self.dval[q][i] > 0:
                    self._wait("sp", (self.dsem[q][i], self.dval[q][i], None))

from contextlib import ExitStack

T_LAT = 8192
T_CTX = 256
NTOK = T_LAT + T_CTX
NROWS = NTOK + 128
DM = 1024
EPS = 1e-6
NE = 16


def host_consts():
    c = {}
    c["ident"] = np.eye(128, dtype=np.float32)
    p = np.arange(128)
    c["triu_incl"] = (p[:, None] <= p[None, :]).astype(np.float32)
    c["triu_strict"] = (p[:, None] < p[None, :]).astype(np.float32)
    c["tril_incl"] = (p[:, None] >= p[None, :]).astype(np.float32)
    c["tril_strict"] = (p[:, None] > p[None, :]).astype(np.float32)
    c["iota_q"] = np.tile(np.arange(1024, dtype=np.float32)[None, :], (128, 1))
    q = np.arange(128, dtype=np.float32)
    c["ctxadd"] = np.tile((T_LAT + np.maximum(q - 32, 0))[None, :], (128, 1)).astype(np.float32)
    return c


class PB:
    def __init__(self, nc):
        self.nc = nc
        self.k = KF(nc)
        self.D = {}
        self.uid = 0

    def din(self, name, shape, dt=F32):
        self.D[name] = self.nc.dram_tensor(name, list(shape), dt, kind="ExternalInput").ap()
        return self.D[name]

    def dout(self, name, shape, dt=F32):
        self.D[name] = self.nc.dram_tensor(name, list(shape), dt, kind="ExternalOutput").ap()
        return self.D[name]

    def dscr(self, name, shape, dt=F32):
        self.D[name] = self.nc.dram_tensor(name, list(shape), dt, kind="Internal").ap()
        return self.D[name]

    def sb(self, es, name, shape, dt):
        self.uid += 1
        return es.enter_context(self.nc.sbuf_tensor("%s_%d" % (name, self.uid), list(shape), dt))

    def ps(self, es, name, shape, dt):
        self.uid += 1
        return es.enter_context(self.nc.psum_tensor("%s_%d" % (name, self.uid), list(shape), dt))


def load_consts(pb, es):
    k, D = pb.k, pb.D
    C = {}
    for nm in ("ident", "triu_incl", "triu_strict", "tril_incl", "tril_strict"):
        f = pb.sb(es, nm + "_f", [128, 128], F32)
        b = pb.sb(es, nm + "_b", [128, 128], BF16)
        k.dma("sp", lambda e, f=f, nm=nm: e.dma_start(out=f[:], in_=D["c_" + nm]), w=[nm + "_f"])
        k.op("dve", lambda e, f=f, b=b: e.tensor_copy(out=b[:], in_=f[:]), r=[nm + "_f"], w=[nm + "_b"])
        C[nm + "_f"] = f
        C[nm + "_b"] = b
    ones_f = pb.sb(es, "ones_f", [128, 128], F32)
    ones_b = pb.sb(es, "ones_b", [128, 128], BF16)
    k.op("dve", lambda e: e.memset(ones_f[:], 1.0), w=["ones_f"])
    k.op("dve", lambda e: e.memset(ones_b[:], 1.0), w=["ones_b"])
    C["ones_f"] = ones_f
    C["ones_b"] = ones_b
    return C


def phase_init(pb, copy_x=True):
    k, D = pb.k, pb.D
    with ExitStack() as es:
        z = pb.sb(es, "zt", [128, 1024], F32)
        zb = pb.sb(es, "zb", [128, 1024], BF16)
        k.op("dve", lambda e: e.memset(z[:], 0.0), w=["zt"])
        k.op("dve", lambda e: e.memset(zb[:], 0.0), w=["zb"])
        if copy_x:
            for i in range(8):
                k.dma("sp", lambda e, i=i: e.dma_start(out=D["xres"][i * 1024:(i + 1) * 1024, :],
                                                       in_=D["x"][i * 1024:(i + 1) * 1024, :]), w=["xres"])
            k.dma("sp", lambda e: e.dma_start(out=D["xres"][T_LAT:NTOK, :], in_=D["ctx"]), w=["xres"])
        k.dma("sp", lambda e: e.dma_start(out=D["xres"][NTOK:NROWS, :], in_=z[:]), r=["zt"], w=["xres"])
        k.dma("sp", lambda e: e.dma_start(out=D["hn"][NTOK:NROWS, :], in_=zb[:]), r=["zb"], w=["hn"])
        k.dma("sp", lambda e: e.dma_start(out=D["aff"][NTOK:NROWS, :], in_=z[:, 0:16]), r=["zt"], w=["aff"])
        k.barrier()


def phase_mod(pb, C):
    k, D, nc = pb.k, pb.D, pb.nc
    with ExitStack() as es:
        s8 = pb.sb(es, "s8", [8, 2, 128], F32)
        scol = pb.sb(es, "scol", [128, 2, 8], F32)
        srep = pb.sb(es, "srep", [128, 2, 8, 128], F32)
        ps_t = pb.ps(es, "ps_t", [128, 512], F32)
        k.dma("sp", lambda e: e.dma_start(out=s8[:, 0, :], in_=D["c"].rearrange("o (a b) -> (o a) b", a=8)), w=["s8"])
        k.dma("sp", lambda e: e.dma_start(out=s8[:, 1, :], in_=D["c_ctx"].rearrange("o (a b) -> (o a) b", a=8)), w=["s8"])
        k.op("act", lambda e: e.activation(out=s8[:], in_=s8[:], func=AF.Silu), r=["s8"], w=["s8"])
        for s in range(2):
            k.op("pe", lambda e, s=s: e.transpose(out=ps_t[:, s * 8:(s + 1) * 8], in_=s8[:, s, :],
                                                  identity=C["ident_f"][0:8, 0:8]), r=["s8", "ident_f"], w=["ps_t"])
        k.op("dve", lambda e: e.tensor_copy(out=scol[:].rearrange("p s c -> p (s c)"), in_=ps_t[:, 0:16]),
             r=["ps_t"], w=["scol"])
        for s in range(2):
            k.op("dve", lambda e, s=s: e.tensor_copy(out=srep[:, s, :, :],
                                                     in_=scol[:, s, :].unsqueeze(2).to_broadcast([128, 8, 128])),
                 r=["scol"], w=["srep"])
        wm = [pb.sb(es, "wm%d" % i, [128, 8, 512], F32) for i in range(2)]
        bm = [pb.sb(es, "bm%d" % i, [128, 512], F32) for i in range(2)]
        gn = [pb.sb(es, "gn%d" % i, [128, 512], F32) for i in range(2)]
        ob = [pb.sb(es, "ob%d" % i, [128, 512], F32) for i in range(4)]
        psm = [pb.ps(es, "psm%d" % i, [128, 512], F32) for i in range(2)]
        it = 0
        oi = 0
        for l in range(2):
            for ncn in range(12):
                b = it % 2
                it += 1
                n0 = ncn * 512
                slot = ncn // 2
                half = ncn % 2
                k.dma("sp", lambda e, b=b, l=l, n0=n0: e.dma_start(
                    out=wm[b][:], in_=D["w_mod"][l, :, n0:n0 + 512].rearrange("(kc p) n -> p kc n", p=128)),
                    w=["wm%d" % b])
                k.dma("sp", lambda e, b=b, l=l, n0=n0: e.dma_start(
                    out=bm[b][:], in_=D["b_mod"][l:l + 1, n0:n0 + 512].to_broadcast([128, 512])), w=["bm%d" % b])
                if slot in (1, 4):
                    gsrc = D["norm_mix"] if slot == 1 else D["norm_ffn"]
                    k.dma("sp", lambda e, b=b, l=l, half=half, gsrc=gsrc: e.dma_start(
                        out=gn[b][:], in_=gsrc[l:l + 1, half * 512:(half + 1) * 512].to_broadcast([128, 512])),
                        w=["gn%d" % b])
                for s in range(2):
                    pm = psm[s]
                    for kc in range(8):
                        k.op("pe", lambda e, s=s, kc=kc, b=b, pm=pm: e.matmul(
                            pm[:], srep[:, s, kc, :], wm[b][:, kc, :], start=(kc == 0), stop=(kc == 7)),
                            r=["srep", "wm%d" % b], w=["psm%d" % s])
                    o = ob[oi % 4]
                    okey = "ob%d" % (oi % 4)
                    oi += 1
                    k.op("dve", lambda e, o=o, pm=pm, b=b: e.tensor_tensor(out=o[:], in0=pm[:], in1=bm[b][:], op=ALU.add),
                         r=["psm%d" % s, "bm%d" % b], w=[okey])
                    if slot in (1, 4):
                        k.op("dve", lambda e, o=o, b=b: e.scalar_tensor_tensor(
                            out=o[:], in0=o[:], scalar=1.0, in1=gn[b][:], op0=ALU.add, op1=ALU.mult),
                            r=[okey, "gn%d" % b], w=[okey])
                    k.dma("sp", lambda e, o=o, l=l, s=s, slot=slot, half=half: e.dma_start(
                        out=D["modrep"][l, s, slot, :, half * 512:(half + 1) * 512], in_=o[:]),
                        r=[okey], w=["modrep"])
        k.barrier()


def norm_mod_tile(pb, xt, xkey, rstd, rkey, junk, t1, G, SH, hn, hnkey):
    k = pb.k
    k.op("act", lambda e: e.activation(out=junk[:], in_=xt[:], func=AF.Square, accum_out=rstd[:, 0:1]),
         r=[xkey], w=["junk", rkey])
    k.op("act", lambda e: e.activation(out=rstd[:, 1:2], in_=rstd[:, 0:1], func=AF.Sqrt, bias=EPS, scale=1.0 / DM),
         r=[rkey], w=[rkey])
    k.op("dve", lambda e: e.reciprocal(out=rstd[:, 2:3], in_=rstd[:, 1:2]), r=[rkey], w=[rkey])
    k.op("dve", lambda e: e.scalar_tensor_tensor(out=t1[:], in0=xt[:], scalar=rstd[:, 2:3], in1=G[:],
                                                 op0=ALU.mult, op1=ALU.mult),
         r=[xkey, rkey, "modv"], w=["t1"])
    k.op("pool", lambda e: e.tensor_tensor(out=hn[:], in0=t1[:], in1=SH[:], op=ALU.add),
         r=["t1", "modv"], w=[hnkey])


def load_modv(pb, es, l, slots):
    k, D = pb.k, pb.D
    out = {}
    for s in range(2):
        for slot in slots:
            t = pb.sb(es, "mv%d%d" % (s, slot), [128, 1024], F32)
            k.dma("sp", lambda e, t=t, s=s, slot=slot: e.dma_start(out=t[:], in_=D["modrep"][l, s, slot, :, :]),
                  r=["modrep"], w=["modv"])
            out[(s, slot)] = t
    return out


def topk_threshold(pb, es, C, aff, J, cap, tag, psum):
    k = pb.k
    lo = pb.sb(es, "lo" + tag, [128, 16], F32)
    hi = pb.sb(es, "hi" + tag, [128, 16], F32)
    mid = pb.sb(es, "mid" + tag, [128, 16], F32)
    cnt = pb.sb(es, "cnt" + tag, [128, 16], F32)
    ge = pb.sb(es, "ge" + tag, [128, 16], U32)
    lt = pb.sb(es, "lt" + tag, [128, 16], U32)
    cmp = pb.sb(es, "cmp" + tag, [128, J, 16], BF16)
    K = "tk" + tag
    k.op("dve", lambda e: e.memset(lo[:], 0.0), w=[K])
    k.op("dve", lambda e: e.memset(hi[:], 1.0), w=[K])
    ncol = J * 16
    for it in range(30):
        k.op("dve", lambda e: e.tensor_tensor(out=mid[:], in0=lo[:], in1=hi[:], op=ALU.add), r=[K], w=[K + "m"])
        k.op("dve", lambda e: e.tensor_scalar(out=mid[:], in0=mid[:], scalar1=0.5, scalar2=None, op0=ALU.mult),
             r=[K + "m"], w=[K + "m"])
        k.op("dve", lambda e: e.tensor_tensor(out=cmp[:], in0=aff, in1=mid[:].unsqueeze(1).to_broadcast([128, J, 16]),
                                              op=ALU.is_ge), r=[K + "m", "aff_all"], w=[K + "c"])
        cf = cmp[:].rearrange("p j e -> p (j e)")
        for c0 in range(0, ncol, 512):
            c1 = min(ncol, c0 + 512)
            k.op("pe", lambda e, c0=c0, c1=c1: e.matmul(psum[:, c0:c1], C["ones_b"][:], cf[:, c0:c1], start=True, stop=True),
                 r=[K + "c", "ones_b"], w=[K + "p"])
        k.op("dve", lambda e: e.tensor_reduce(out=cnt[:], in_=psum[:, 0:ncol].rearrange("p (j e) -> p e j", e=16),
                                              axis=AX.X, op=ALU.add), r=[K + "p"], w=[K + "n"])
        k.op("dve", lambda e: e.tensor_scalar(out=ge[:], in0=cnt[:], scalar1=float(cap), scalar2=None, op0=ALU.is_ge),
             r=[K + "n"], w=[K + "g"])
        k.op("dve", lambda e: e.tensor_scalar(out=lt[:], in0=cnt[:], scalar1=float(cap), scalar2=None, op0=ALU.is_lt),
             r=[K + "n"], w=[K + "g"])
        k.op("dve", lambda e: e.copy_predicated(out=lo[:], mask=ge[:], data=mid[:]), r=[K + "g", K + "m"], w=[K])
        k.op("dve", lambda e: e.copy_predicated(out=hi[:], mask=lt[:], data=mid[:]), r=[K + "g", K + "m"], w=[K])
    return lo, K


def phase_moe(pb, C, l):
    k, D, nc = pb.k, pb.D, pb.nc
    has_ctx = (l == 0)
    NT = 66 if has_ctx else 64
    NPT = 9 if has_ctx else 8
    NP = NPT * 128
    with ExitStack() as es:
        mv = load_modv(pb, es, l, (3, 4, 5))
        iota_q = pb.sb(es, "iota_q", [128, 1024], F32)
        ctxadd = pb.sb(es, "ctxadd", [128, 128], F32)
        k.dma("sp", lambda e: e.dma_start(out=iota_q[:], in_=D["c_iota_q"]), w=["iota_q"])
        k.dma("sp", lambda e: e.dma_start(out=ctxadd[:], in_=D["c_ctxadd"]), w=["ctxadd"])
        aff_all = pb.sb(es, "aff_all", [128, NT, 16], F32)
        wr_f = pb.sb(es, "wr_f", [128, 8, 16], F32)
        wr = pb.sb(es, "wr", [128, 8, 16], BF16)
        k.dma("sp", lambda e: e.dma_start(out=wr_f[:], in_=D["w_router"][l].rearrange("(kc p) n -> p kc n", p=128)),
              w=["wr_f"])
        k.op("dve", lambda e: e.tensor_copy(out=wr[:], in_=wr_f[:]), r=["wr_f"], w=["wr"])
        with ExitStack() as es2:
            xt = [pb.sb(es2, "xt%d" % i, [128, 1024], F32) for i in range(2)]
            hn = [pb.sb(es2, "hn%d" % i, [128, 1024], BF16) for i in range(2)]
            hnT = [pb.sb(es2, "hnT%d" % i, [128, 8, 128], BF16) for i in range(2)]
            junk = pb.sb(es2, "junk", [128, 1024], BF16)
            t1 = pb.sb(es2, "t1", [128, 1024], F32)
            rstd = [pb.sb(es2, "rstd%d" % i, [128, 4], F32) for i in range(2)]
            psT = [pb.ps(es2, "psT%d" % i, [128, 1024], BF16) for i in range(2)]
            psL = [pb.ps(es2, "psL%d" % i, [128, 16], F32) for i in range(2)]
            for j in range(NT):
                b = j % 2
                s = 0 if j < 64 else 1
                r0 = j * 128
                k.dma("sp", lambda e, b=b, r0=r0: e.dma_start(out=xt[b][:], in_=D["xres"][r0:r0 + 128, :]),
                      r=["xres"], w=["xt%d" % b])
                norm_mod_tile(pb, xt[b], "xt%d" % b, rstd[b], "rstd%d" % b, junk, t1, mv[(s, 4)], mv[(s, 3)], hn[b], "hn%d" % b)
                k.dma("sp", lambda e, b=b, r0=r0: e.dma_start(out=D["hn"][r0:r0 + 128, :], in_=hn[b][:]),
                      r=["hn%d" % b], w=["hn"])
                for kc in range(8):
                    k.op("pe", lambda e, b=b, kc=kc: e.transpose(out=psT[b][:, kc * 128:(kc + 1) * 128],
                                                                 in_=hn[b][:, kc * 128:(kc + 1) * 128],
                                                                 identity=C["ident_b"][:]),
                         r=["hn%d" % b, "ident_b"], w=["psT%d" % b])
                k.op("act", lambda e, b=b: e.activation(out=hnT[b][:].rearrange("p a t -> p (a t)"), in_=psT[b][:], func=AF.Copy),
                     r=["psT%d" % b], w=["hnT%d" % b])
                for kc in range(8):
                    k.op("pe", lambda e, b=b, kc=kc: e.matmul(psL[b][:], hnT[b][:, kc, :], wr[:, kc, :],
                                                              start=(kc == 0), stop=(kc == 7)),
                         r=["hnT%d" % b, "wr"], w=["psL%d" % b])
                k.op("dve", lambda e, b=b, j=j: e.tensor_copy(out=aff_all[:, j, :], in_=psL[b][:]),
                     r=["psL%d" % b], w=["aff_all"])
            mx = pb.sb(es2, "mx", [128, NT], F32)
            k.op("dve", lambda e: e.tensor_reduce(out=mx[:], in_=aff_all[:], axis=AX.X, op=ALU.max), r=["aff_all"], w=["mx"])
            k.op("dve", lambda e: e.tensor_tensor(out=aff_all[:], in0=aff_all[:],
                                                  in1=mx[:].unsqueeze(2).to_broadcast([128, NT, 16]), op=ALU.subtract),
                 r=["aff_all", "mx"], w=["aff_all"])
            k.op("act", lambda e: e.activation(out=aff_all[:], in_=aff_all[:], func=AF.Exp), r=["aff_all"], w=["aff_all"])
            k.op("dve", lambda e: e.tensor_reduce(out=mx[:], in_=aff_all[:], axis=AX.X, op=ALU.add), r=["aff_all"], w=["mx"])
            k.op("dve", lambda e: e.reciprocal(out=mx[:], in_=mx[:]), r=["mx"], w=["mx"])
            k.op("dve", lambda e: e.tensor_tensor(out=aff_all[:], in0=aff_all[:],
                                                  in1=mx[:].unsqueeze(2).to_broadcast([128, NT, 16]), op=ALU.mult),
                 r=["aff_all", "mx"], w=["aff_all"])
            k.dma("sp", lambda e: e.dma_start(out=D["aff"][0:NT * 128, :].rearrange("(j p) e -> p j e", p=128), in_=aff_all[:]),
                  r=["aff_all"], w=["aff"])
            k.barrier()
        m_all = pb.sb(es, "m_all", [128, NT, 16], BF16)
        cnt_incl = pb.sb(es, "cnt_incl", [128, NT, 16], F32)
        with ExitStack() as es2:
            pst = [pb.ps(es2, "pst%d" % i, [128, 512], F32) for i in range(3)]
            psbig = pb.ps(es2, "psbig", [128, 1024], F32)
            thr_l, Kl = topk_threshold(pb, es2, C, aff_all[:, 0:64, :], 64, 1024, "L", psbig)
            k.op("dve", lambda e: e.tensor_tensor(out=m_all[:, 0:64, :], in0=aff_all[:, 0:64, :],
                                                  in1=thr_l[:].unsqueeze(1).to_broadcast([128, 64, 16]), op=ALU.is_ge),
                 r=["aff_all", Kl], w=["m_all"])
            if has_ctx:
                thr_c, Kc = topk_threshold(pb, es2, C, aff_all[:, 64:66, :], 2, 32, "C", psbig)
                k.op("dve", lambda e: e.tensor_tensor(out=m_all[:, 64:66, :], in0=aff_all[:, 64:66, :],
                                                      in1=thr_c[:].unsqueeze(1).to_broadcast([128, 2, 16]), op=ALU.is_ge),
                     r=["aff_all", Kc], w=["m_all"])
            tot = pb.sb(es2, "tot", [128, NT, 16], F32)
            binc = pb.sb(es2, "binc", [128, NT, 16], F32)
            mf = m_all[:].rearrange("p j e -> p (j e)")
            tf = tot[:].rearrange("p j e -> p (j e)")
            cf = cnt_incl[:].rearrange("p j e -> p (j e)")
            ncol = NT * 16
            for ci, c0 in enumerate(range(0, ncol, 512)):
                c1 = min(ncol, c0 + 512)
                w_ = c1 - c0
                k.op("pe", lambda e, c0=c0, c1=c1, ci=ci, w_=w_: e.matmul(pst[ci][:, 0:w_], C["ones_b"][:], mf[:, c0:c1],
                                                                          start=True, stop=True),
                     r=["m_all", "ones_b"], w=["pst%d" % ci])
                k.op("dve", lambda e, c0=c0, c1=c1, ci=ci, w_=w_: e.tensor_copy(out=tf[:, c0:c1], in_=pst[ci][:, 0:w_]),
                     r=["pst%d" % ci], w=["tot"])
            for e_ in range(16):
                k.op("dve", lambda e, e_=e_: e.tensor_tensor_scan(out=binc[:, 0:64, e_], data0=C["ones_f"][:, 0:64],
                                                                  data1=tot[:, 0:64, e_], initial=0.0,
                                                                  op0=ALU.mult, op1=ALU.add),
                     r=["tot", "ones_f"], w=["binc"])
            if has_ctx:
                k.op("dve", lambda e: e.tensor_copy(out=binc[:, 64, :], in_=tot[:, 64, :]), r=["tot"], w=["binc"])
                k.op("dve", lambda e: e.tensor_tensor(out=binc[:, 65, :], in0=tot[:, 64, :], in1=tot[:, 65, :], op=ALU.add),
                     r=["tot"], w=["binc"])
            k.op("dve", lambda e: e.tensor_tensor(out=binc[:], in0=binc[:], in1=tot[:], op=ALU.subtract),
                 r=["binc", "tot"], w=["binc"])
            bf_ = binc[:].rearrange("p j e -> p (j e)")
            for ci, c0 in enumerate(range(0, ncol, 512)):
                c1 = min(ncol, c0 + 512)
                w_ = c1 - c0
                k.op("pe", lambda e, c0=c0, c1=c1, ci=ci, w_=w_: e.matmul(pst[ci][:, 0:w_], C["triu_incl_b"][:], mf[:, c0:c1],
                                                                          start=True, stop=True),
                     r=["m_all", "triu_incl_b", "tot"], w=["pst%d" % ci])
                k.op("dve", lambda e, c0=c0, c1=c1, ci=ci, w_=w_: e.tensor_tensor(out=cf[:, c0:c1], in0=pst[ci][:, 0:w_],
                                                                                  in1=bf_[:, c0:c1], op=ALU.add),
                     r=["pst%d" % ci, "binc"], w=["cnt_incl"])
            k.barrier()
        with ExitStack() as es2:
            Bt = [pb.sb(es2, "Bt%d" % i, [128, 1024], BF16) for i in range(2)]
            row_sb = pb.sb(es2, "row_sb", [1, NP], F32)
            idx_all = pb.sb(es2, "idx_all", [128, NE, NPT], I32)
            xg = pb.sb(es2, "xg", [128, NPT, 1024], BF16)
            ga = [pb.sb(es2, "ga%d" % i_, [128, NPT, 16], F32) for i_ in range(2)]
            xgT = pb.sb(es2, "xgT", [128, 8, NP], BF16)
            hidT = pb.sb(es2, "hidT", [128, 8, NP], BF16)
            sg = [pb.sb(es2, "sg%d" % i, [128, 512], F32) for i in range(2)]
            yt = [pb.sb(es2, "yt%d" % i, [128, 1024], F32) for i in range(2)]
            Wb = {nm: pb.sb(es2, "W" + nm, [128, 8, 1024], BF16) for nm in ("g", "u", "d")}
            stg = [pb.sb(es2, "stg%d" % i, [128, 4, 1024], F32) for i in range(2)]
            rowA = pb.ps(es2, "rowA", [1, 512], F32)
            rowB = pb.ps(es2, "rowB", [1, 512], F32)
            rowC = pb.ps(es2, "rowC", [128, 512], F32)
            psX = pb.ps(es2, "psX", [128, 1024], BF16)
            psG = pb.ps(es2, "psG", [128, 512], F32)
            psU = pb.ps(es2, "psU", [128, 512], F32)
            psY = [pb.ps(es2, "psY%d" % i, [128, 512], F32) for i in range(2)]
            sti = 0
            bi = 0
            yi = 0
            pchunks = [(0, 512), (512, 1024)] + ([(1024, 1152)] if has_ctx else [])
            for ex in range(NE):
                for j in range(64):
                    B_ = Bt[bi % 2]
                    bkey = "Bt%d" % (bi % 2)
                    eng = "dve"
                    bi += 1
                    k.op(eng, lambda e, B_=B_, j=j, ex=ex: e.tensor_scalar(
                        out=B_[:], in0=iota_q[:], scalar1=cnt_incl[:, j, ex:ex + 1], scalar2=None, op0=ALU.is_ge),
                        r=["iota_q", "cnt_incl"], w=[bkey])
                    k.op("pe", lambda e, B_=B_, j=j: e.matmul(rowA[:], C["ones_b"][:, 0:1], B_[:, 0:512],
                                                              start=(j == 0), stop=(j == 63)),
                         r=[bkey, "ones_b"], w=["rowA"])
                    k.op("pe", lambda e, B_=B_, j=j: e.matmul(rowB[:], C["ones_b"][:, 0:1], B_[:, 512:1024],
                                                              start=(j == 0), stop=(j == 63)),
                         r=[bkey, "ones_b"], w=["rowB"])
                k.op("dve", lambda e: e.tensor_copy(out=row_sb[0:1, 0:512], in_=rowA[:]), r=["rowA"], w=["row_sb"])
                k.op("dve", lambda e: e.tensor_copy(out=row_sb[0:1, 512:1024], in_=rowB[:]), r=["rowB"], w=["row_sb"])
                if has_ctx:
                    for j in (64, 65):
                        B_ = Bt[bi % 2]
                        bkey = "Bt%d" % (bi % 2)
                        bi += 1
                        k.op("dve", lambda e, B_=B_, j=j, ex=ex: e.tensor_scalar(
                            out=B_[:, 0:128], in0=iota_q[:, 0:128], scalar1=cnt_incl[:, j, ex:ex + 1], scalar2=None,
                            op0=ALU.is_ge), r=["iota_q", "cnt_incl"], w=[bkey])
                        k.op("pe", lambda e, B_=B_, j=j: e.matmul(rowC[0:1, 0:128], C["ones_b"][:, 0:1], B_[:, 0:128],
                                                                  start=(j == 64), stop=(j == 65)),
                             r=[bkey, "ones_b"], w=["rowC"])
                    k.op("dve", lambda e: e.tensor_tensor(out=row_sb[0:1, 1024:1152], in0=rowC[0:1, 0:128],
                                                          in1=ctxadd[0:1, :], op=ALU.add),
                         r=["rowC", "ctxadd"], w=["row_sb"])
                for i in range(NPT):
                    k.op("pe", lambda e, i=i: e.transpose(out=rowC[:, 256 + i:257 + i], in_=row_sb[0:1, i * 128:(i + 1) * 128],
                                                          identity=C["ident_f"][0:1, 0:1]),
                         r=["row_sb", "ident_f"], w=["rowC"])
                k.op("dve", lambda e, ex=ex: e.tensor_copy(out=idx_all[:, ex, :], in_=rowC[:, 256:256 + NPT]), r=["rowC"], w=["idx%d" % ex])
            def gathers(ex, gb):
                for i in range(NPT):
                    k.dma("pool", lambda e, i=i: e.indirect_dma_start(
                        out=xg[:, i, :], out_offset=None, in_=D["hn"],
                        in_offset=bass.IndirectOffsetOnAxis(ap=idx_all[:, ex, i:i + 1], axis=0)),
                        r=["idx%d" % ex, "hn"], w=["xg%d" % i])
                    k.dma("pool", lambda e, i=i: e.indirect_dma_start(
                        out=ga[gb][:, i, :], out_offset=None, in_=D["aff"],
                        in_offset=bass.IndirectOffsetOnAxis(ap=idx_all[:, ex, i:i + 1], axis=0)),
                        r=["idx%d" % ex, "aff"], w=["ga%d_%d" % (gb, i)])

            gathers(0, 0)
            prev_sc, cur_sc = [], []
            for ex in range(NE):
                gb = ex % 2
                for nm, src in (("g", D["w_gate"]), ("u", D["w_up"]), ("d", D["w_down"])):
                    for hf in range(2):
                        sbuf_ = stg[sti % 2]
                        skey = "stg%d" % (sti % 2)
                        sti += 1
                        k.dma("sp", lambda e, sbuf_=sbuf_, src=src, hf=hf, ex=ex: e.dma_start(
                            out=sbuf_[:], in_=src[l, ex, hf * 512:(hf + 1) * 512, :].rearrange("(kc p) n -> p kc n", p=128)),
                            w=[skey])
                        k.op("act", lambda e, sbuf_=sbuf_, nm=nm, hf=hf: e.activation(
                            out=Wb[nm][:, hf * 4:(hf + 1) * 4, :], in_=sbuf_[:], func=AF.Copy),
                            r=[skey], w=["W" + nm])
                for i in range(NPT):
                    for kc in range(8):
                        k.op("pe", lambda e, i=i, kc=kc: e.transpose(out=psX[:, kc * 128:(kc + 1) * 128],
                                                                     in_=xg[:, i, kc * 128:(kc + 1) * 128],
                                                                     identity=C["ident_b"][:]),
                             r=["xg%d" % i, "ident_b"], w=["psX"])
                    k.op("dve", lambda e, i=i: e.tensor_copy(out=xgT[:, :, i * 128:(i + 1) * 128],
                                                             in_=psX[:].rearrange("p (a t) -> p a t", a=8)),
                         r=["psX"], w=["xgT"])
                if ex + 1 < NE:
                    gathers(ex + 1, 1 - gb)
                for fc in range(8):
                    for (p0, p1) in pchunks:
                        n = p1 - p0
                        for kc in range(8):
                            k.op("pe", lambda e, fc=fc, kc=kc, p0=p0, p1=p1, n=n: e.matmul(
                                psG[:, 0:n], Wb["g"][:, kc, fc * 128:(fc + 1) * 128], xgT[:, kc, p0:p1],
                                start=(kc == 0), stop=(kc == 7)), r=["Wg", "xgT"], w=["psG"])
                        for kc in range(8):
                            k.op("pe", lambda e, fc=fc, kc=kc, p0=p0, p1=p1, n=n: e.matmul(
                                psU[:, 0:n], Wb["u"][:, kc, fc * 128:(fc + 1) * 128], xgT[:, kc, p0:p1],
                                start=(kc == 0), stop=(kc == 7)), r=["Wu", "xgT"], w=["psU"])
                        s_ = sg[yi % 2]
                        skey = "sg%d" % (yi % 2)
                        yi += 1
                        k.op("act", lambda e, s_=s_, n=n: e.activation(out=s_[:, 0:n], in_=psG[:, 0:n], func=AF.Silu),
                             r=["psG"], w=[skey])
                        k.op("dve", lambda e, s_=s_, n=n, fc=fc, p0=p0, p1=p1: e.tensor_tensor(
                            out=hidT[:, fc, p0:p1], in0=psU[:, 0:n], in1=s_[:, 0:n], op=ALU.mult),
                            r=["psU", skey], w=["hidT"])
                for i in range(NPT):
                    s = 0 if i < 8 else 1
                    y_ = yt[i % 2]
                    ykey = "yt%d" % (i % 2)
                    for dh in range(2):
                        py = psY[dh]
                        for fc in range(8):
                            k.op("pe", lambda e, i=i, fc=fc, dh=dh, py=py: e.matmul(
                                py[:], hidT[:, fc, i * 128:(i + 1) * 128], Wb["d"][:, fc, dh * 512:(dh + 1) * 512],
                                start=(fc == 0), stop=(fc == 7)), r=["hidT", "Wd"], w=["psY%d" % dh])
                        k.op("dve", lambda e, i=i, dh=dh, py=py, y_=y_, s=s, ex=ex: e.scalar_tensor_tensor(
                            out=y_[:, dh * 512:(dh + 1) * 512], in0=py[:], scalar=ga[gb][:, i, ex:ex + 1],
                            in1=mv[(s, 5)][:, dh * 512:(dh + 1) * 512], op0=ALU.mult, op1=ALU.mult),
                            r=["psY%d" % dh, "ga%d_%d" % (gb, i), "modv"], w=[ykey])
                    for t_ in prev_sc:
                        k._wait("pool", t_)
                    cur_sc.append(k.dma("pool", lambda e, i=i, y_=y_: e.indirect_dma_start(
                        out=D["xres"], out_offset=bass.IndirectOffsetOnAxis(ap=idx_all[:, ex, i:i + 1], axis=0),
                        in_=y_[:], in_offset=None, compute_op=ALU.add),
                        r=[ykey, "idx%d" % ex], w=[]))
                prev_sc, cur_sc = cur_sc, []
            k.barrier()


def phase_final(pb, C):
    k, D = pb.k, pb.D
    with ExitStack() as es:
        g = pb.sb(es, "gfin", [128, 1024], F32)
        k.dma("sp", lambda e: e.dma_start(out=g[:], in_=D["norm_out"].to_broadcast([128, 1024])), w=["gfin"])
        xt = [pb.sb(es, "fx%d" % i, [128, 1024], F32) for i in range(2)]
        ot = [pb.sb(es, "fo%d" % i, [128, 1024], F32) for i in range(2)]
        junk = pb.sb(es, "fjunk", [128, 1024], BF16)
        rstd = [pb.sb(es, "frs%d" % i, [128, 4], F32) for i in range(2)]
        for j in range(64):
            b = j % 2
            r0 = j * 128
            k.dma("sp", lambda e, b=b, r0=r0: e.dma_start(out=xt[b][:], in_=D["xres"][r0:r0 + 128, :]),
                  r=["xres"], w=["fx%d" % b])
            k.op("act", lambda e, b=b: e.activation(out=junk[:], in_=xt[b][:], func=AF.Square, accum_out=rstd[b][:, 0:1]),
                 r=["fx%d" % b], w=["fjunk", "frs%d" % b])
            k.op("act", lambda e, b=b: e.activation(out=rstd[b][:, 1:2], in_=rstd[b][:, 0:1], func=AF.Sqrt, bias=EPS,
                                                    scale=1.0 / DM), r=["frs%d" % b], w=["frs%d" % b])
            k.op("dve", lambda e, b=b: e.reciprocal(out=rstd[b][:, 2:3], in_=rstd[b][:, 1:2]), r=["frs%d" % b], w=["frs%d" % b])
            k.op("dve", lambda e, b=b: e.scalar_tensor_tensor(out=ot[b][:], in0=xt[b][:], scalar=rstd[b][:, 2:3], in1=g[:],
                                                              op0=ALU.mult, op1=ALU.mult),
                 r=["fx%d" % b, "frs%d" % b, "gfin"], w=["fo%d" % b])
            k.dma("sp", lambda e, b=b, r0=r0: e.dma_start(out=D["out"][r0:r0 + 128, :], in_=ot[b][:]),
                  r=["fo%d" % b], w=["out"])
        k.barrier()


NH = 8
ATT_SCALE = 1.0 / float(np.sqrt(192.0))


def attn_host_consts():
    c = {}
    t = np.arange(T_LAT)
    row = (t // 64).astype(np.float32)
    col = (t % 64).astype(np.float32)
    nf = 16
    inv = (10000.0 ** (-np.arange(nf, dtype=np.float32) / nf)).astype(np.float32)
    cosT = np.zeros((64, T_LAT), np.float32)
    sinT = np.zeros((64, T_LAT), np.float32)
    for a, pos in ((0, row), (1, col)):
        ang = (pos[None, :] * inv[:, None]).astype(np.float32)
        for b in range(2):
            d0 = a * 32 + b * 16
            cosT[d0:d0 + 16] = np.cos(ang)
            sinT[d0:d0 + 16] = np.sin(ang) * (-1.0 if b == 0 else 1.0)
    c["cos2"] = np.concatenate([cosT, cosT], 0)
    c["sin2"] = np.concatenate([sinT, sinT], 0)
    return c


def rope_swap_perm():
    d = np.arange(64)
    a, b, f = d // 32, (d // 16) % 2, d % 16
    return a * 32 + (1 - b) * 16 + f


def attn_layout_weights(w_uq, w_ukv, w_kr):
    sp = rope_swap_perm()
    nope = np.concatenate([np.arange(h * 192, h * 192 + 128) for h in range(NH)])
    rope = np.concatenate([np.arange(h * 192 + 128, h * 192 + 192) for h in range(NH)])
    ropes = np.concatenate([h * 192 + 128 + sp for h in range(NH)])
    w_uq_r = np.ascontiguousarray(w_uq[:, np.concatenate([nope, rope, ropes])])
    kn = np.concatenate([np.arange(h * 256, h * 256 + 128) for h in range(NH)])
    vv = np.concatenate([np.arange(h * 256 + 128, h * 256 + 256) for h in range(NH)])
    w_ukv_r = np.ascontiguousarray(w_ukv[:, np.concatenate([kn, vv])])
    w_kr_r = np.ascontiguousarray(np.concatenate([w_kr, w_kr[:, sp]], 1))
    return w_uq_r, w_ukv_r, w_kr_r


def load_w_bf16(pb, es, name, src_ap, kc, n, stg, stgkey):
    k = pb.k
    t = pb.sb(es, name, [128, kc, n], BF16)
    for c0 in range(0, n, 512):
        c1 = min(n, c0 + 512)
        for q in range(kc):
            k.dma("sp", lambda e, q=q, c0=c0, c1=c1: e.dma_start(out=stg[:, 0:c1 - c0], in_=src_ap[q * 128:(q + 1) * 128, c0:c1]),
                  w=[stgkey])
            k.op("dve", lambda e, q=q, c0=c0, c1=c1: e.tensor_copy(out=t[:, q, c0:c1], in_=stg[:, 0:c1 - c0]),
                 r=[stgkey], w=[name])
    return t


def rms_small(pb, ps_ap, n, rstd, rkey, junk, gain_rep, outbf, outkey, pskey):
    k = pb.k
    k.op("act", lambda e: e.activation(out=junk[:, 0:n], in_=ps_ap, func=AF.Square, accum_out=rstd[:, 0:1]),
         r=[pskey], w=["junk", rkey])
    k.op("act", lambda e: e.activation(out=rstd[:, 1:2], in_=rstd[:, 0:1], func=AF.Sqrt, bias=EPS, scale=1.0 / n),
         r=[rkey], w=[rkey])
    k.op("dve", lambda e: e.reciprocal(out=rstd[:, 2:3], in_=rstd[:, 1:2]), r=[rkey], w=[rkey])
    k.op("dve", lambda e: e.scalar_tensor_tensor(out=outbf, in0=ps_ap, scalar=rstd[:, 2:3], in1=gain_rep,
                                                 op0=ALU.mult, op1=ALU.mult), r=[pskey, rkey, "gains"], w=[outkey])


def phase_attn_pre(pb, C):
    k, D, nc = pb.k, pb.D, pb.nc
    l = 1
    with ExitStack() as es:
        mv = load_modv(pb, es, l, (0, 1))
        stg = pb.sb(es, "wstg", [128, 512], F32)
        w_dq = load_w_bf16(pb, es, "w_dq", D["w_dq"], 8, 384, stg, "wstg")
        w_dkv = load_w_bf16(pb, es, "w_dkv", D["w_dkv"], 8, 256, stg, "wstg")
        w_kr = load_w_bf16(pb, es, "w_kr", D["w_kr_r"], 8, 128, stg, "wstg")
        w_uq = load_w_bf16(pb, es, "w_uq", D["w_uq_r"], 3, 2048, stg, "wstg")
        w_ukv = load_w_bf16(pb, es, "w_ukv", D["w_ukv_r"], 2, 2048, stg, "wstg")
        qn_rep = pb.sb(es, "qn_rep", [128, 384], F32)
        kvn_rep = pb.sb(es, "kvn_rep", [128, 256], F32)
        k.dma("sp", lambda e: e.dma_start(out=qn_rep[:], in_=D["q_norm"].to_broadcast([128, 384])), w=["gains"])
        k.dma("sp", lambda e: e.dma_start(out=kvn_rep[:], in_=D["kv_norm"].to_broadcast([128, 256])), w=["gains"])
        xt = [pb.sb(es, "xt%d" % i, [128, 1024], F32) for i in range(2)]
        an = [pb.sb(es, "an%d" % i, [128, 1024], BF16) for i in range(2)]
        junk = pb.sb(es, "junk", [128, 1024], BF16)
        t1 = pb.sb(es, "t1", [128, 1024], F32)
        rstd = [pb.sb(es, "rstd%d" % i, [128, 4], F32) for i in range(2)]
        rs2 = [pb.sb(es, "rs2%d" % i, [128, 4], F32) for i in range(2)]
        aT = pb.sb(es, "aT", [128, 8, 512], BF16)
        cqT = pb.sb(es, "cqT", [128, 3, 512], BF16)
        ckvT = pb.sb(es, "ckvT", [128, 2, 512], BF16)
        cqn = pb.sb(es, "cqn", [128, 384], BF16)
        ckvn = pb.sb(es, "ckvn", [128, 256], BF16)
        cosb = pb.sb(es, "cosb", [128, 512], F32)
        sinb = pb.sb(es, "sinb", [128, 512], F32)
        ostg = [pb.sb(es, "ostg%d" % i, [128, 512], BF16) for i in range(3)]
        vstg = [pb.sb(es, "vstg%d" % i, [128, 1024], BF16) for i in range(2)]
        ra = pb.sb(es, "ra", [128, 512], F32)
        rb = pb.sb(es, "rb", [128, 512], F32)
        psT = pb.ps(es, "psT", [128, 1024], BF16)
        psS = pb.ps(es, "psS", [128, 512], F32)
        psA = pb.ps(es, "psA", [128, 512], F32)
        psB = pb.ps(es, "psB", [128, 512], F32)
        psV = [pb.ps(es, "psV%d" % i, [128, 512], F32) for i in range(2)]
        oi = 0
        vi = 0
        xi = 0
        supers = [(i * 512, 4, 0) for i in range(16)] + [(T_LAT, 2, 1)]
        for (t0, ntile, s) in supers:
            nt = ntile * 128
            is_lat = (s == 0)
            for tl in range(ntile):
                b = xi % 2
                xi += 1
                r0 = t0 + tl * 128
                k.dma("sp", lambda e, b=b, r0=r0: e.dma_start(out=xt[b][:], in_=D["xres"][r0:r0 + 128, :]),
                      r=["xres"], w=["xt%d" % b])
                norm_mod_tile(pb, xt[b], "xt%d" % b, rstd[b], "rstd%d" % b, junk, t1, mv[(s, 1)], mv[(s, 0)], an[b], "an%d" % b)
                for kc in range(8):
                    k.op("pe", lambda e, b=b, kc=kc: e.transpose(out=psT[:, kc * 128:(kc + 1) * 128],
                                                                 in_=an[b][:, kc * 128:(kc + 1) * 128], identity=C["ident_b"][:]),
                         r=["an%d" % b, "ident_b"], w=["psT"])
                k.op("act", lambda e, tl=tl: e.activation(out=aT[:, :, tl * 128:(tl + 1) * 128],
                                                          in_=psT[:].rearrange("p (a t) -> p a t", a=8), func=AF.Copy),
                     r=["psT"], w=["aT"])
                if is_lat:
                    for kc in range(8):
                        k.op("pe", lambda e, kc=kc, tl=tl: e.matmul(psS[:, 0:384], aT[:, kc, tl * 128:(tl + 1) * 128], w_dq[:, kc, :],
                                                                    start=(kc == 0), stop=(kc == 7)), r=["aT", "w_dq"], w=["psS"])
                    rms_small(pb, psS[:, 0:384], 384, rs2[b], "rs2%d" % b, junk, qn_rep[:], cqn[:], "cqn", "psS")
                    for c in range(3):
                        k.op("pe", lambda e, c=c: e.transpose(out=psT[:, c * 128:(c + 1) * 128], in_=cqn[:, c * 128:(c + 1) * 128],
                                                              identity=C["ident_b"][:]), r=["cqn", "ident_b"], w=["psT"])
                    k.op("act", lambda e, tl=tl: e.activation(out=cqT[:, :, tl * 128:(tl + 1) * 128],
                                                              in_=psT[:, 0:384].rearrange("p (a t) -> p a t", a=3), func=AF.Copy),
                         r=["psT"], w=["cqT"])
                for kc in range(8):
                    k.op("pe", lambda e, kc=kc, tl=tl: e.matmul(psS[:, 0:256], aT[:, kc, tl * 128:(tl + 1) * 128], w_dkv[:, kc, :],
                                                                start=(kc == 0), stop=(kc == 7)), r=["aT", "w_dkv"], w=["psS"])
                rms_small(pb, psS[:, 0:256], 256, rs2[b], "rs2%d" % b, junk, kvn_rep[:], ckvn[:], "ckvn", "psS")
                for c in range(2):
                    k.op("pe", lambda e, c=c: e.transpose(out=psT[:, c * 128:(c + 1) * 128], in_=ckvn[:, c * 128:(c + 1) * 128],
                                                          identity=C["ident_b"][:]), r=["ckvn", "ident_b"], w=["psT"])
                k.op("act", lambda e, tl=tl: e.activation(out=ckvT[:, :, tl * 128:(tl + 1) * 128],
                                                          in_=psT[:, 0:256].rearrange("p (a t) -> p a t", a=2), func=AF.Copy),
                     r=["psT"], w=["ckvT"])
                v_ = vstg[vi % 2]
                vkey = "vstg%d" % (vi % 2)
                vi += 1
                for dh in range(2):
                    for c in range(2):
                        k.op("pe", lambda e, c=c, dh=dh, tl=tl: e.matmul(
                            psV[dh][:], ckvT[:, c, tl * 128:(tl + 1) * 128], w_ukv[:, c, 1024 + dh * 512:1024 + (dh + 1) * 512],
                            start=(c == 0), stop=(c == 1)), r=["ckvT", "w_ukv"], w=["psV%d" % dh])
                    k.op("act", lambda e, dh=dh, v_=v_: e.activation(out=v_[:, dh * 512:(dh + 1) * 512], in_=psV[dh][:], func=AF.Copy),
                         r=["psV%d" % dh], w=[vkey])
                k.dma("sp", lambda e, v_=v_, r0=r0: e.dma_start(out=D["Vd"][r0:r0 + 128, :], in_=v_[:]), r=[vkey], w=["Vd"])
            if is_lat:
                k.dma("sp", lambda e, t0=t0: e.dma_start(out=cosb[:], in_=D["c_cos2"][:, t0:t0 + 512]), w=["cosb"])
                k.dma("sp", lambda e, t0=t0: e.dma_start(out=sinb[:], in_=D["c_sin2"][:, t0:t0 + 512]), w=["sinb"])
            for h in range(NH):
                for c in range(2):
                    k.op("pe", lambda e, c=c, h=h: e.matmul(psA[:, 0:nt], w_ukv[:, c, h * 128:(h + 1) * 128], ckvT[:, c, 0:nt],
                                                            start=(c == 0), stop=(c == 1)), r=["ckvT", "w_ukv"], w=["psA"])
                o_ = ostg[oi % 3]
                okey = "ostg%d" % (oi % 3)
                oi += 1
                k.op("act", lambda e, o_=o_: e.activation(out=o_[:, 0:nt], in_=psA[:, 0:nt], func=AF.Copy), r=["psA"], w=[okey])
                k.dma("sp", lambda e, o_=o_, h=h, t0=t0: e.dma_start(out=D["KT"][h, :, t0:t0 + nt], in_=o_[:, 0:nt]),
                      r=[okey], w=["KT"])
            for kc in range(8):
                k.op("pe", lambda e, kc=kc: e.matmul(psA[0:64, 0:nt], w_kr[:, kc, 0:64], aT[:, kc, 0:nt],
                                                     start=(kc == 0), stop=(kc == 7)), r=["aT", "w_kr"], w=["psA"])
            o_ = ostg[oi % 3]
            okey = "ostg%d" % (oi % 3)
            oi += 1
            if is_lat:
                for kc in range(8):
                    k.op("pe", lambda e, kc=kc: e.matmul(psB[0:64, 0:nt], w_kr[:, kc, 64:128], aT[:, kc, 0:nt],
                                                         start=(kc == 0), stop=(kc == 7)), r=["aT", "w_kr"], w=["psB"])
                k.op("dve", lambda e: e.tensor_tensor(out=ra[0:64, :], in0=psA[0:64, :], in1=cosb[0:64, :], op=ALU.mult),
                     r=["psA", "cosb"], w=["ra"])
                k.op("dve", lambda e: e.tensor_tensor(out=rb[0:64, :], in0=psB[0:64, :], in1=sinb[0:64, :], op=ALU.mult),
                     r=["psB", "sinb"], w=["rb"])
                k.op("dve", lambda e, o_=o_: e.tensor_tensor(out=o_[0:64, :], in0=ra[0:64, :], in1=rb[0:64, :], op=ALU.add),
                     r=["ra", "rb"], w=[okey])
            else:
                k.op("act", lambda e, o_=o_: e.activation(out=o_[0:64, 0:nt], in_=psA[0:64, 0:nt], func=AF.Copy), r=["psA"], w=[okey])
            k.dma("sp", lambda e, o_=o_, t0=t0: e.dma_start(out=D["KRT"][:, t0:t0 + nt], in_=o_[0:64, 0:nt]), r=[okey], w=["KRT"])
            if not is_lat:
                continue
            for h in range(NH):
                for c in range(3):
                    k.op("pe", lambda e, c=c, h=h: e.matmul(psA[:], w_uq[:, c, h * 128:(h + 1) * 128], cqT[:, c, :],
                                                            start=(c == 0), stop=(c == 2)), r=["cqT", "w_uq"], w=["psA"])
                o_ = ostg[oi % 3]
                okey = "ostg%d" % (oi % 3)
                oi += 1
                k.op("act", lambda e, o_=o_: e.activation(out=o_[:], in_=psA[:], func=AF.Copy), r=["psA"], w=[okey])
                k.dma("sp", lambda e, o_=o_, h=h, t0=t0: e.dma_start(out=D["QT"][h, :, t0:t0 + 512], in_=o_[:]), r=[okey], w=["QT"])
            for hp in range(4):
                for c in range(3):
                    k.op("pe", lambda e, c=c, hp=hp: e.matmul(psA[:], w_uq[:, c, 1024 + hp * 128:1024 + (hp + 1) * 128], cqT[:, c, :],
                                                              start=(c == 0), stop=(c == 2)), r=["cqT", "w_uq"], w=["psA"])
                for c in range(3):
                    k.op("pe", lambda e, c=c, hp=hp: e.matmul(psB[:], w_uq[:, c, 1536 + hp * 128:1536 + (hp + 1) * 128], cqT[:, c, :],
                                                              start=(c == 0), stop=(c == 2)), r=["cqT", "w_uq"], w=["psB"])
                o_ = ostg[oi % 3]
                okey = "ostg%d" % (oi % 3)
                oi += 1
                k.op("dve", lambda e: e.tensor_tensor(out=ra[:], in0=psA[:], in1=cosb[:], op=ALU.mult), r=["psA", "cosb"], w=["ra"])
                k.op("dve", lambda e: e.tensor_tensor(out=rb[:], in0=psB[:], in1=sinb[:], op=ALU.mult), r=["psB", "sinb"], w=["rb"])
                k.op("dve", lambda e, o_=o_: e.tensor_tensor(out=o_[:], in0=ra[:], in1=rb[:], op=ALU.add), r=["ra", "rb"], w=[okey])
                k.dma("sp", lambda e, o_=o_, hp=hp, t0=t0: e.dma_start(
                    out=D["QRT"][2 * hp:2 * hp + 2, :, t0:t0 + 512].rearrange("h d t -> (h d) t"), in_=o_[:]), r=[okey], w=["QRT"])
        k.barrier()


def phase_attn_main(pb, C, heads=range(NH), qblocks=range(16)):
    k, D, nc = pb.k, pb.D, pb.nc
    NKB = NTOK // 128
    with ExitStack() as es:
        KRT = pb.sb(es, "KRT", [128, NTOK], BF16)
        k.op("pool", lambda e: e.memset(KRT[64:128, :], 0.0), w=["sKRT"])
        k.dma("sp", lambda e: e.dma_start(out=KRT[0:64, :], in_=D["KRT"]), r=["KRT"], w=["sKRT"])
        KT = [pb.sb(es, "KT%d" % i, [128, NTOK], BF16) for i in range(2)]
        Vh = [pb.sb(es, "Vh%d" % i, [128, NKB, 130], BF16) for i in range(2)]
        for i in range(2):
            k.op("dve", lambda e, i=i: e.memset(Vh[i][:, :, 128:130], 1.0), w=["Vh%d" % i])
        Qb = [pb.sb(es, "Qb%d" % i, [128, 512], BF16) for i in range(2)]
        QRb = [pb.sb(es, "QRb%d" % i, [128, 512], BF16) for i in range(2)]
        for i in range(2):
            k.op("pool", lambda e, i=i: e.memset(QRb[i][64:128, :], 0.0), w=["QRb%d" % i])
        PT = [pb.sb(es, "PT%d" % i, [128, 512], BF16) for i in range(4)]
        Ost = [pb.sb(es, "Ost%d" % i, [128, 4, 128], BF16) for i in range(2)]
        rinv = pb.sb(es, "rinv", [128, 8], F32)
        psS = [pb.ps(es, "psS%d" % i, [128, 512], F32) for i in range(3)]
        psO = [pb.ps(es, "psO%d" % i, [128, 512], F32) for i in range(4)]
        qi = 0
        pi = 0
        si = 0
        for hi, h in enumerate(heads):
            hb = hi % 2
            k.dma("sp", lambda e, hb=hb, h=h: e.dma_start(out=KT[hb][:], in_=D["KT"][h]), r=["KT"], w=["KT%d" % hb])
            k.dma("sp", lambda e, hb=hb, h=h: e.dma_start(
                out=Vh[hb][:, :, 0:128], in_=D["Vd"][:, h * 128:(h + 1) * 128].rearrange("(kb p) v -> p kb v", p=128)),
                r=["Vd"], w=["Vh%d" % hb])
            for qb in qblocks:
                b = qi % 2
                qi += 1
                q0 = qb * 512
                k.dma("sp", lambda e, b=b, h=h, q0=q0: e.dma_start(out=Qb[b][:], in_=D["QT"][h, :, q0:q0 + 512]),
                      r=["QT"], w=["Qb%d" % b])
                k.dma("sp", lambda e, b=b, h=h, q0=q0: e.dma_start(out=QRb[b][0:64, :], in_=D["QRT"][h, :, q0:q0 + 512]),
                      r=["QRT"], w=["QRb%d" % b])

                def qk(kb, sb_):
                    k.op("pe", lambda e: e.matmul(psS[sb_][:], KT[hb][:, kb * 128:(kb + 1) * 128], Qb[b][:], start=True, stop=False),
                         r=["KT%d" % hb, "Qb%d" % b], w=["psS%d" % sb_])
                    k.op("pe", lambda e: e.matmul(psS[sb_][:], KRT[:, kb * 128:(kb + 1) * 128], QRb[b][:], start=False, stop=True),
                         r=["sKRT", "QRb%d" % b], w=["psS%d" % sb_])

                sbs = []
                for pre in range(2):
                    sbs.append(si % 3)
                    qk(pre, si % 3)
                    si += 1
                for kb in range(NKB):
                    if kb + 2 < NKB:
                        sbs.append(si % 3)
                        qk(kb + 2, si % 3)
                        si += 1
                    sb_ = sbs[kb]
                    p_ = PT[pi % 4]
                    pkey = "PT%d" % (pi % 4)
                    pi += 1
                    k.op("act", lambda e, sb_=sb_, p_=p_: e.activation(out=p_[:], in_=psS[sb_][:], func=AF.Exp, scale=ATT_SCALE),
                         r=["psS%d" % sb_], w=[pkey])
                    for qt in range(4):
                        k.op("pe", lambda e, qt=qt, p_=p_, kb=kb: e.matmul(
                            psO[qt][:, 0:130], p_[:, qt * 128:(qt + 1) * 128], Vh[hb][:, kb, :],
                            start=(kb == 0), stop=(kb == NKB - 1)), r=[pkey, "Vh%d" % hb], w=["psO%d" % qt])
                o_ = Ost[b]
                okey = "Ost%d" % b
                for qt in range(4):
                    k.op("dve", lambda e, qt=qt: e.reciprocal(out=rinv[:, qt:qt + 1], in_=psO[qt][:, 128:129]),
                         r=["psO%d" % qt], w=["rinv"])
                    k.op("dve", lambda e, qt=qt, o_=o_: e.tensor_scalar(out=o_[:, qt, :], in0=psO[qt][:, 0:128],
                                                                        scalar1=rinv[:, qt:qt + 1], scalar2=None, op0=ALU.mult),
                         r=["psO%d" % qt, "rinv"], w=[okey])
                k.dma("sp", lambda e, o_=o_, h=h, q0=q0: e.dma_start(
                    out=D["Od"][q0:q0 + 512, h * 128:(h + 1) * 128].rearrange("(qt p) v -> p qt v", p=128), in_=o_[:]),
                    r=[okey], w=["Od"])
        k.barrier()


def phase_attn_post(pb, C):
    k, D, nc = pb.k, pb.D, pb.nc
    with ExitStack() as es:
        mv = load_modv(pb, es, 1, (2,))
        stg = pb.sb(es, "wstg", [128, 512], F32)
        w_o = load_w_bf16(pb, es, "w_o", D["w_o"], 8, 1024, stg, "wstg")
        ot = [pb.sb(es, "ot%d" % i, [128, 1024], BF16) for i in range(2)]
        oT = [pb.sb(es, "oT%d" % i, [128, 8, 128], BF16) for i in range(2)]
        xt = [pb.sb(es, "xt%d" % i, [128, 1024], F32) for i in range(2)]
        tm = [pb.sb(es, "tm%d" % i, [128, 1024], F32) for i in range(2)]
        psT = pb.ps(es, "psT", [128, 1024], BF16)
        psY = [pb.ps(es, "psY%d" % i, [128, 512], F32) for i in range(2)]
        for j in range(64):
            b = j % 2
            r0 = j * 128
            k.dma("sp", lambda e, b=b, r0=r0: e.dma_start(out=ot[b][:], in_=D["Od"][r0:r0 + 128, :]), r=["Od"], w=["ot%d" % b])
            k.dma("sp", lambda e, b=b, r0=r0: e.dma_start(out=xt[b][:], in_=D["xres"][r0:r0 + 128, :]), r=["xres"], w=["xt%d" % b])
            for c in range(8):
                k.op("pe", lambda e, b=b, c=c: e.transpose(out=psT[:, c * 128:(c + 1) * 128], in_=ot[b][:, c * 128:(c + 1) * 128],
                                                           identity=C["ident_b"][:]), r=["ot%d" % b, "ident_b"], w=["psT"])
            k.op("act", lambda e, b=b: e.activation(out=oT[b][:].rearrange("p a t -> p (a t)"), in_=psT[:], func=AF.Copy),
                 r=["psT"], w=["oT%d" % b])
            for dh in range(2):
                for c in range(8):
                    k.op("pe", lambda e, b=b, c=c, dh=dh: e.matmul(psY[dh][:], oT[b][:, c, :], w_o[:, c, dh * 512:(dh + 1) * 512],
                                                                   start=(c == 0), stop=(c == 7)), r=["oT%d" % b, "w_o"], w=["psY%d" % dh])
                k.op("dve", lambda e, b=b, dh=dh: e.tensor_tensor(out=tm[b][:, dh * 512:(dh + 1) * 512], in0=psY[dh][:],
                                                                  in1=mv[(0, 2)][:, dh * 512:(dh + 1) * 512], op=ALU.mult),
                     r=["psY%d" % dh, "modv"], w=["tm%d" % b])
            k.op("pool", lambda e, b=b: e.tensor_tensor(out=tm[b][:], in0=tm[b][:], in1=xt[b][:], op=ALU.add),
                 r=["tm%d" % b, "xt%d" % b], w=["tm%d" % b])
            k.dma("sp", lambda e, b=b, r0=r0: e.dma_start(out=D["xres"][r0:r0 + 128, :], in_=tm[b][:]), r=["tm%d" % b], w=["xres"])
        k.barrier()


def load_cols(pb, es, C, name, src_rows_ap, n, psbank, pskey):
    k = pb.k
    rows = pb.sb(es, name + "_r", [n, 128], F32)
    cols = pb.sb(es, name, [128, n], F32)
    k.dma("sp", lambda e: e.dma_start(out=rows[:], in_=src_rows_ap), w=[name + "_r"])
    k.op("pe", lambda e: e.transpose(out=psbank[:, 0:n], in_=rows[:], identity=C["ident_f"][0:n, 0:n]),
         r=[name + "_r", "ident_f"], w=[pskey])
    k.op("dve", lambda e: e.tensor_copy(out=cols[:], in_=psbank[:, 0:n]), r=[pskey], w=[name])
    return cols


def phase_mix_pass(pb, C, dirn, nsup_lat=32, do_ctx=True):
    k, D, nc = pb.k, pb.D, pb.nc
    W = 256
    with ExitStack() as es:
        P = [pb.ps(es, "P%d" % i, [128, 512], F32) for i in range(8)]
        PK = ["P%d" % i for i in range(8)]
        P0b = P[0][:].bitcast(BF16)
        mv = load_modv(pb, es, 0, (0, 1))
        stg = pb.sb(es, "wstg", [128, 512], F32)
        win = D["w_in"]
        wq = load_w_bf16(pb, es, "wq", win[:, 0:512], 8, 512, stg, "wstg")
        wf = load_w_bf16(pb, es, "wf", win[:, 512 + 512 * dirn:1024 + 512 * dirn], 8, 512, stg, "wstg")
        wi = load_w_bf16(pb, es, "wi", win[:, 1536:2048], 8, 512, stg, "wstg")
        wx = load_w_bf16(pb, es, "wx", win[:, 3072:4096], 8, 1024, stg, "wstg")
        wdt = load_w_bf16(pb, es, "wdt", win[:, 4096 + 8 * dirn:4104 + 8 * dirn], 8, 8, stg, "wstg")
        lg0 = load_cols(pb, es, C, "lg0", D["lb_gamma"][0, dirn, :].rearrange("(c p) -> c p", p=128), 4, P[7], PK[7])
        lg1 = load_cols(pb, es, C, "lg1", D["lb_gamma"][1, dirn, :].rearrange("(c p) -> c p", p=128), 4, P[7], PK[7])
        lbc = pb.sb(es, "lbc", [128, 4], F32)
        oml = pb.sb(es, "oml", [128, 4], F32)
        k.op("dve", lambda e: e.tensor_tensor(out=lbc[:], in0=lg0[:], in1=lg1[:], op=ALU.subtract), r=["lg0", "lg1"], w=["lbc"])
        k.op("act", lambda e: e.activation(out=lbc[:], in_=lbc[:], func=AF.Sigmoid), r=["lbc"], w=["lbc"])
        k.op("dve", lambda e: e.tensor_scalar(out=oml[:], in0=lbc[:], scalar1=-1.0, scalar2=1.0, op0=ALU.mult, op1=ALU.add),
             r=["lbc"], w=["oml"])
        cw = [load_cols(pb, es, C, "cw%d" % j, D["conv_w"][j, :].rearrange("(c p) -> c p", p=128), 8, P[7], PK[7]) for j in range(5)]
        cbc = load_cols(pb, es, C, "cbc", D["conv_b"].rearrange("o (c p) -> (o c) p", p=128), 8, P[7], PK[7])
        dtb = pb.sb(es, "dtb", [128, 8], F32)
        negA = pb.sb(es, "negA", [128, 8], F32)
        k.dma("sp", lambda e: e.dma_start(out=dtb[:], in_=D["dt_bias"][dirn:dirn + 1, :].to_broadcast([128, 8])), w=["dtb"])
        k.dma("sp", lambda e: e.dma_start(out=negA[:], in_=D["a_log"][dirn:dirn + 1, :].to_broadcast([128, 8])), w=["negA"])
        k.op("act", lambda e: e.activation(out=negA[:], in_=negA[:], func=AF.Exp), r=["negA"], w=["negA"])
        k.op("dve", lambda e: e.tensor_scalar(out=negA[:], in0=negA[:], scalar1=-1.0, scalar2=None, op0=ALU.mult), r=["negA"], w=["negA"])
        dsk = pb.sb(es, "dsk", [128, 8], F32)
        k.dma("sp", lambda e: e.dma_start(out=dsk[:], in_=D["d_skip"].to_broadcast([128, 8])), w=["dsk"])
        if getattr(pb, 'stop', 99) == 0:
            k.barrier()
            return
        if dirn == 0:
            m_att_b, m_att_f = C["triu_incl_b"], C["triu_incl_f"]
            m_cum, m_wend, m_lh = C["triu_incl_f"], C["tril_strict_f"], C["tril_strict_f"]
            m_rhs = C["triu_incl_f"]
        else:
            m_att_b, m_att_f = C["tril_incl_b"], C["tril_incl_f"]
            m_cum, m_wend, m_lh = C["tril_incl_f"], C["triu_strict_f"], C["triu_strict_f"]
            m_rhs = C["tril_incl_f"]
        xt = [pb.sb(es, "xt%d" % i, [128, 1024], F32) for i in range(2)]
        an = [pb.sb(es, "an%d" % i, [128, 1024], BF16) for i in range(2)]
        hx = pb.sb(es, "hx", [4, 1024], F32)
        ah = pb.sb(es, "ah", [4, 1024], BF16)
        junk = pb.sb(es, "junk", [128, 1024], BF16)
        t1 = pb.sb(es, "t1", [128, 1024], F32)
        rstd = [pb.sb(es, "rstd%d" % i, [128, 4], F32) for i in range(3)]
        aT = pb.sb(es, "aT", [128, 8, W + 4], BF16)
        Qsil = pb.sb(es, "Qsil", [128, 4, W], F32)
        fg = pb.sb(es, "fg", [128, 4, W], F32)
        la = pb.sb(es, "la", [128, 4, W], F32)
        kk = pb.sb(es, "kk", [128, 4, W], F32)
        u = pb.sb(es, "u", [128, 8, W + 4], F32)
        acc = pb.sb(es, "acc", [128, 8, W], F32)
        tmp = pb.sb(es, "tmp", [128, 8, W], F32)
        xbc = pb.sb(es, "xbc", [128, 8, W], BF16)
        vtok = pb.sb(es, "vtok", [128, 2, 512], BF16)
        dtr = pb.sb(es, "dtr", [128, 2, 8], F32)
        bb = pb.sb(es, "bb", [128, 4, 128], F32)
        b2 = pb.sb(es, "b2", [128, 4, 128], F32)
        sc = pb.sb(es, "sc", [128, 3, 4], F32)
        esc = pb.sb(es, "esc", [128, 3, 4], F32)
        nb = pb.sb(es, "nb", [128, 4], F32)
        Eq = pb.sb(es, "Eq", [128, 4, 128], F32)
        Ek = pb.sb(es, "Ek", [128, 4, 128], F32)
        Qt = pb.sb(es, "Qt", [128, 4, 128], BF16)
        Kt = pb.sb(es, "Kt", [128, 4, 128], BF16)
        Ktok = pb.sb(es, "Ktok", [128, 4, 128], BF16)
        attm = pb.sb(es, "attm", [128, 8, 128], BF16)
        Sh = pb.sb(es, "Sh", [128, 4, 64], F32)
        ShpA = pb.sb(es, "ShpA", [128, 4, 64], BF16)
        ShpB = pb.sb(es, "ShpB", [128, 4, 64], BF16)
        KtA = pb.sb(es, "KtA", [128, 4, 128], BF16)
        KtB = pb.sb(es, "KtB", [128, 4, 128], BF16)
        for nm_, t_ in (("ShpA", ShpA), ("ShpB", ShpB), ("KtA", KtA), ("KtB", KtB)):
            k.op("dve", lambda e, t_=t_: e.memset(t_[:], 0.0), w=[nm_])
        tU = pb.sb(es, "tU", [128, 4, 64], F32)
        osb = pb.sb(es, "osb", [128, 512], F32)
        xmt = pb.sb(es, "xmt", [128, 8, 64], BF16)
        Btok = pb.sb(es, "Btok", [128, 2, 128], BF16)
        dts = pb.sb(es, "dts", [128, 4, 8], F32)
        ec = pb.sb(es, "ec", [128, 24], F32)
        LH = pb.sb(es, "LH", [128, 8, 128], F32)
        Em = pb.sb(es, "Em", [128, 8, 128], F32)
        cbm = pb.sb(es, "cbm", [128, 2, 128], F32)
        Mm = pb.sb(es, "Mm", [128, 8, 128], BF16)
        xdt = pb.sb(es, "xdt", [128, 8, 64], BF16)
        xdtw = pb.sb(es, "xdtw", [128, 8, 64], BF16)
        yi = pb.sb(es, "yi", [128, 8, 64], F32)
        ysb = pb.sb(es, "ysb", [128, 8, 64], F32)
        Ss = pb.sb(es, "Ss", [128, 8, 64], F32)
        Ssb = pb.sb(es, "Ssb", [128, 8, 64], BF16)
        k.op("dve", lambda e: e.memset(Sh[:], 0.0), w=["Sh"])
        k.op("dve", lambda e: e.memset(Ss[:], 0.0), w=["Ss"])
        k.op("dve", lambda e: e.memset(Ssb[:], 0.0), w=["Ssb"])

        supers = []
        if do_ctx:
            supers.append((1, 0))
        lat_order = list(range(nsup_lat)) if dirn == 0 else list(range(nsup_lat - 1, -1, -1))
        supers += [(0, i) for i in lat_order]
        bank_rot = [1, 2, 3]
        bri = 0
        xi = 0
        for (s, si) in supers:
            src = D["x"] if s == 0 else D["ctx"]
            Ts = T_LAT if s == 0 else T_CTX
            r0 = si * W
            g0 = r0 if s == 0 else T_LAT + r0
            for tl in range(2):
                b = xi % 2
                xi += 1
                rr = r0 + tl * 128
                k.dma("sp", lambda e, b=b, rr=rr: e.dma_start(out=xt[b][:], in_=src[rr:rr + 128, :]), w=["xt%d" % b])
                norm_mod_tile(pb, xt[b], "xt%d" % b, rstd[b], "rstd%d" % b, junk, t1, mv[(s, 1)], mv[(s, 0)], an[b], "an%d" % b)
                for kc in range(8):
                    k.op("pe", lambda e, b=b, kc=kc: e.transpose(out=P0b[:, kc * 128:(kc + 1) * 128],
                                                                 in_=an[b][:, kc * 128:(kc + 1) * 128], identity=C["ident_b"][:]),
                         r=["an%d" % b, "ident_b"], w=[PK[0]])
                k.op("act", lambda e, tl=tl: e.activation(out=aT[:, :, 2 + tl * 128:2 + (tl + 1) * 128],
                                                          in_=P0b.rearrange("p (a t) -> p a t", a=8), func=AF.Copy),
                     r=[PK[0]], w=["aT"])
            if getattr(pb, 'stop', 99) == 1:
                k.barrier()
                return
            has_l = r0 >= 2
            has_r = r0 + W + 2 <= Ts
            lrow = r0 - 2 if has_l else 0
            rrow = r0 + W if has_r else Ts - 2
            k.dma("sp", lambda e: e.dma_start(out=hx[0:2, :], in_=src[lrow:lrow + 2, :]), w=["hx"])
            k.dma("sp", lambda e: e.dma_start(out=hx[2:4, :], in_=src[rrow:rrow + 2, :]), w=["hx"])
            k.op("act", lambda e: e.activation(out=junk[0:4, :], in_=hx[:], func=AF.Square, accum_out=rstd[2][0:4, 0:1]),
                 r=["hx"], w=["junk", "rstd2"])
            k.op("act", lambda e: e.activation(out=rstd[2][0:4, 1:2], in_=rstd[2][0:4, 0:1], func=AF.Sqrt, bias=EPS, scale=1.0 / DM),
                 r=["rstd2"], w=["rstd2"])
            k.op("dve", lambda e: e.reciprocal(out=rstd[2][0:4, 2:3], in_=rstd[2][0:4, 1:2]), r=["rstd2"], w=["rstd2"])
            k.op("dve", lambda e: e.scalar_tensor_tensor(out=t1[0:4, :], in0=hx[:], scalar=rstd[2][0:4, 2:3], in1=mv[(s, 1)][0:4, :],
                                                         op0=ALU.mult, op1=ALU.mult), r=["hx", "rstd2", "modv"], w=["t1"])
            k.op("pool", lambda e: e.tensor_tensor(out=ah[:], in0=t1[0:4, :], in1=mv[(s, 0)][0:4, :], op=ALU.add),
                 r=["t1", "modv"], w=["ah"])
            for kc in range(8):
                k.op("pe", lambda e, kc=kc: e.transpose(out=P0b[:, kc * 4:(kc + 1) * 4], in_=ah[:, kc * 128:(kc + 1) * 128],
                                                        identity=C["ident_b"][0:4, 0:4]), r=["ah", "ident_b"], w=[PK[0]])
            hv = P0b[:, 0:32].rearrange("p (a t) -> p a t", a=8)
            if has_l:
                k.op("dve", lambda e: e.tensor_copy(out=aT[:, :, 0:2], in_=hv[:, :, 0:2]), r=[PK[0]], w=["aT"])
            else:
                k.op("dve", lambda e: e.memset(aT[:, :, 0:2], 0.0), r=[PK[0]], w=["aT"])
            if has_r:
                k.op("dve", lambda e: e.tensor_copy(out=aT[:, :, W + 2:W + 4], in_=hv[:, :, 2:4]), r=[PK[0]], w=["aT"])
            else:
                k.op("dve", lambda e: e.memset(aT[:, :, W + 2:W + 4], 0.0), r=[PK[0]], w=["aT"])
            if getattr(pb, 'stop', 99) == 2:
                k.barrier()
                return
            for c in range(4):
                bk = bank_rot[bri % 3]
                bri += 1
                for kc in range(8):
                    k.op("pe", lambda e, c=c, kc=kc, bk=bk: e.matmul(P[bk][:, 0:W], wq[:, kc, c * 128:(c + 1) * 128], aT[:, kc, 2:W + 2],
                                                                     start=(kc == 0), stop=(kc == 7)), r=["wq", "aT"], w=[PK[bk]])
                k.op("act", lambda e, c=c, bk=bk: e.activation(out=Qsil[:, c, :], in_=P[bk][:, 0:W], func=AF.Silu), r=[PK[bk]], w=["Qsil"])
            for c in range(4):
                bk = bank_rot[bri % 3]
                bri += 1
                for kc in range(8):
                    k.op("pe", lambda e, c=c, kc=kc, bk=bk: e.matmul(P[bk][:, 0:W], wf[:, kc, c * 128:(c + 1) * 128], aT[:, kc, 2:W + 2],
                                                                     start=(kc == 0), stop=(kc == 7)), r=["wf", "aT"], w=[PK[bk]])
                k.op("act", lambda e, c=c, bk=bk: e.activation(out=fg[:, c, :], in_=P[bk][:, 0:W], func=AF.Sigmoid), r=[PK[bk]], w=["fg"])
                k.op("dve", lambda e, c=c: e.tensor_scalar(out=fg[:, c, :], in0=fg[:, c, :], scalar1=oml[:, c:c + 1], scalar2=lbc[:, c:c + 1],
                                                           op0=ALU.mult, op1=ALU.add), r=["fg", "oml", "lbc"], w=["fg"])
            k.op("act", lambda e: e.activation(out=la[:], in_=fg[:], func=AF.Ln), r=["fg"], w=["la"])
            k.op("dve", lambda e: e.tensor_scalar(out=kk[:], in0=fg[:], scalar1=-1.0, scalar2=1.0, op0=ALU.mult, op1=ALU.add),
                 r=["fg"], w=["kk"])
            for c in range(8):
                bk = bank_rot[bri % 3]
                bri += 1
                for kc in range(8):
                    k.op("pe", lambda e, c=c, kc=kc, bk=bk: e.matmul(P[bk][:, 0:W + 4], wx[:, kc, c * 128:(c + 1) * 128], aT[:, kc, :],
                                                                     start=(kc == 0), stop=(kc == 7)), r=["wx", "aT"], w=[PK[bk]])
                k.op("act", lambda e, c=c, bk=bk: e.activation(out=u[:, c, :], in_=P[bk][:, 0:W + 4], func=AF.Copy), r=[PK[bk]], w=["u"])
            if getattr(pb, 'stop', 99) == 3:
                k.barrier()
                return
            for j in range(5):
                if j == 0:
                    k.op("dve", lambda e, j=j: e.tensor_tensor(out=acc[:], in0=u[:, :, j:j + W],
                                                               in1=cw[j][:].unsqueeze(2).to_broadcast([128, 8, W]), op=ALU.mult),
                         r=["u", "cw0"], w=["acc"])
                else:
                    k.op("pool", lambda e, j=j: e.tensor_tensor(out=tmp[:], in0=u[:, :, j:j + W],
                                                                in1=cw[j][:].unsqueeze(2).to_broadcast([128, 8, W]), op=ALU.mult),
                         r=["u", "cw%d" % j], w=["tmp"])
                    k.op("dve", lambda e: e.tensor_tensor(out=acc[:], in0=acc[:], in1=tmp[:], op=ALU.add), r=["acc", "tmp"], w=["acc"])
            k.op("dve", lambda e: e.tensor_tensor(out=acc[:], in0=acc[:], in1=cbc[:].unsqueeze(2).to_broadcast([128, 8, W]), op=ALU.add),
                 r=["acc", "cbc"], w=["acc"])
            k.op("act", lambda e: e.activation(out=xbc[:], in_=acc[:], func=AF.Silu), r=["acc"], w=["xbc"])
            if getattr(pb, 'stop', 99) == 4:
                k.barrier()
                return
            for tl in range(2):
                sl0 = 2 + tl * 128
                for kc in range(8):
                    k.op("pe", lambda e, kc=kc, sl0=sl0: e.matmul(P[6][:], aT[:, kc, sl0:sl0 + 128], wi[:, kc, :],
                                                                  start=(kc == 0), stop=(kc == 7)), r=["aT", "wi"], w=[PK[6]])
                k.op("act", lambda e, tl=tl: e.activation(out=vtok[:, tl, :], in_=P[6][:], func=AF.Copy), r=[PK[6]], w=["vtok"])
                for kc in range(8):
                    k.op("pe", lambda e, kc=kc, sl0=sl0: e.matmul(P[7][:, 0:8], aT[:, kc, sl0:sl0 + 128], wdt[:, kc, :],
                                                                  start=(kc == 0), stop=(kc == 7)), r=["aT", "wdt"], w=[PK[7]])
                k.op("dve", lambda e, tl=tl: e.tensor_tensor(out=dtr[:, tl, :], in0=P[7][:, 0:8], in1=dtb[:], op=ALU.add),
                     r=[PK[7], "dtb"], w=["dtr"])
            if getattr(pb, 'stop', 99) == 5:
                k.barrier()
                return
            tls = [0, 1] if dirn == 0 else [1, 0]
            for tl in tls:
                sl = slice(tl * 128, (tl + 1) * 128)
                grow = g0 + tl * 128
                for c in range(4):
                    k.op("dve", lambda e, c=c: e.tensor_tensor_scan(out=b2[:, c, :], data0=C["ones_f"][:, 0:128], data1=la[:, c, sl],
                                                                    initial=0.0, op0=ALU.mult, op1=ALU.add),
                         r=["la", "ones_f"], w=["b2"])
                if dirn == 0:
                    bsrc, bkey, last = b2, "b2", 127
                else:
                    k.op("dve", lambda e: e.tensor_tensor(out=bb[:], in0=la[:, :, sl], in1=b2[:], op=ALU.subtract), r=["la", "b2"], w=["bb"])
                    for c in range(4):
                        k.op("dve", lambda e, c=c: e.tensor_scalar(out=bb[:, c, :], in0=bb[:, c, :], scalar1=b2[:, c, 127:128], scalar2=None,
                                                                   op0=ALU.add), r=["bb", "b2"], w=["bb"])
                    bsrc, bkey, last = bb, "bb", 0
                k.op("dve", lambda e: e.tensor_copy(out=sc[:, 0, :], in_=bsrc[:, :, 64]), r=[bkey], w=["sc"])
                k.op("dve", lambda e: e.tensor_copy(out=sc[:, 2, :], in_=bsrc[:, :, last]), r=[bkey], w=["sc"])
                k.op("dve", lambda e: e.tensor_tensor(out=sc[:, 1, :], in0=sc[:, 2, :], in1=sc[:, 0, :], op=ALU.subtract), r=["sc"], w=["sc"])
                k.op("dve", lambda e: e.tensor_scalar(out=nb[:], in0=sc[:, 0, :], scalar1=-1.0, scalar2=None, op0=ALU.mult), r=["sc"], w=["nb"])
                k.op("act", lambda e: e.activation(out=esc[:], in_=sc[:], func=AF.Exp), r=["sc"], w=["esc"])
                for c in range(4):
                    k.op("act", lambda e, c=c: e.activation(out=Eq[:, c, :], in_=bsrc[:, c, :], func=AF.Exp, bias=nb[:, c:c + 1], scale=1.0),
                         r=[bkey, "nb"], w=["Eq"])
                    k.op("act", lambda e, c=c: e.activation(out=Ek[:, c, :], in_=bsrc[:, c, :], func=AF.Exp, bias=sc[:, 0, c:c + 1], scale=-1.0),
                         r=[bkey, "sc"], w=["Ek"])
                k.op("dve", lambda e: e.tensor_tensor(out=Qt[:], in0=Qsil[:, :, sl], in1=Eq[:], op=ALU.mult), r=["Qsil", "Eq"], w=["Qt"])
                k.op("dve", lambda e: e.tensor_tensor(out=Kt[:], in0=kk[:, :, sl], in1=Ek[:], op=ALU.mult), r=["kk", "Ek"], w=["Kt"])
                k.op("pool", lambda e: e.tensor_copy(out=KtA[0:64, :, :], in_=Kt[0:64, :, :]), r=["Kt"], w=["KtA"])
                k.op("pool", lambda e: e.tensor_copy(out=KtB[64:128, :, :], in_=Kt[64:128, :, :]), r=["Kt"], w=["KtB"])
                k.op("dve", lambda e: e.tensor_tensor(out=ShpA[0:64, :, :], in0=Sh[0:64, :, :],
                                                      in1=esc[0:64, 0, :].unsqueeze(2).to_broadcast([64, 4, 64]), op=ALU.mult),
                     r=["Sh", "esc"], w=["ShpA"])
                k.op("dve", lambda e: e.tensor_tensor(out=ShpB[64:128, :, :], in0=Sh[64:128, :, :],
                                                      in1=esc[64:128, 0, :].unsqueeze(2).to_broadcast([64, 4, 64]), op=ALU.mult),
                     r=["Sh", "esc"], w=["ShpB"])
                if getattr(pb, 'stop', 99) == 51:
                    k.barrier()
                    return
                for c in range(4):
                    k.op("pe", lambda e, c=c: e.transpose(out=P0b[:, c * 128:(c + 1) * 128], in_=Kt[:, c, :], identity=C["ident_b"][:]),
                         r=["Kt", "ident_b"], w=[PK[0]])
                k.op("act", lambda e: e.activation(out=Ktok[:].rearrange("p a t -> p (a t)"), in_=P0b[:, 0:512], func=AF.Copy),
                     r=[PK[0]], w=["Ktok"])
                if getattr(pb, 'stop', 99) == 52:
                    k.barrier()
                    return
                for h in range(8):
                    c, base = h // 2, (h % 2) * 64
                    bk = 1 + h // 4
                    Kz = KtA if h % 2 == 0 else KtB
                    k.op("pe", lambda e, h=h, c=c, bk=bk, Kz=Kz: e.matmul(
                        P[bk][:, (h % 4) * 128:(h % 4 + 1) * 128], Kz[:, c, :], Qt[:, c, :], start=True, stop=True),
                        r=["KtA", "KtB", "Qt"], w=[PK[bk]])
                for hh in range(2):
                    k.op("dve", lambda e, hh=hh: e.tensor_tensor(
                        out=attm[:, hh * 4:(hh + 1) * 4, :], in0=P[1 + hh][:].rearrange("p (a t) -> p a t", a=4),
                        in1=m_att_f[:].unsqueeze(1).to_broadcast([128, 4, 128]), op=ALU.mult), r=[PK[1 + hh], "masks"], w=["attm"])
                if getattr(pb, 'stop', 99) == 53:
                    k.barrier()
                    return
                for h in range(8):
                    c, base = h // 2, (h % 2) * 64
                    k.op("pe", lambda e, h=h: e.matmul(P[3][:, h * 64:(h + 1) * 64], attm[:, h, :], vtok[:, tl, h * 64:(h + 1) * 64],
                                                       start=True, stop=False), r=["attm", "vtok"], w=[PK[3]])
                    Sz = ShpA if h % 2 == 0 else ShpB
                    k.op("pe", lambda e, h=h, c=c, Sz=Sz: e.matmul(P[3][:, h * 64:(h + 1) * 64], Qt[:, c, :],
                                                                   Sz[:, c, :], start=False, stop=True),
                         r=["Qt", "ShpA", "ShpB"], w=[PK[3]])
                k.op("act", lambda e: e.activation(out=osb[:], in_=P[3][:], func=AF.Copy), r=[PK[3]], w=["osb"])
                k.dma("sp", lambda e, grow=grow: e.dma_start(out=D["oh"][dirn, grow:grow + 128, :], in_=osb[:]), r=["osb"], w=["oh"])
                if getattr(pb, 'stop', 99) == 54:
                    k.barrier()
                    return
                for c in range(4):
                    k.op("pe", lambda e, c=c: e.matmul(P[4][:, c * 128:(c + 1) * 128], Ktok[:, c, :], vtok[:, tl, c * 128:(c + 1) * 128],
                                                       start=True, stop=True), r=["Ktok", "vtok"], w=[PK[4]])
                p4v = P[4][:].rearrange("p (c x) -> p c x", c=4)
                k.op("dve", lambda e: e.tensor_tensor(out=tU[0:64, :, :], in0=p4v[0:64, :, 0:64],
                                                      in1=esc[0:64, 1, :].unsqueeze(2).to_broadcast([64, 4, 64]), op=ALU.mult),
                     r=[PK[4], "esc"], w=["tU"])
                k.op("dve", lambda e: e.tensor_tensor(out=tU[64:128, :, :], in0=p4v[64:128, :, 64:128],
                                                      in1=esc[64:128, 1, :].unsqueeze(2).to_broadcast([64, 4, 64]), op=ALU.mult),
                     r=[PK[4], "esc"], w=["tU"])
                k.op("dve", lambda e: e.tensor_tensor(out=Sh[:], in0=Sh[:], in1=esc[:, 2, :].unsqueeze(2).to_broadcast([128, 4, 64]), op=ALU.mult),
                     r=["Sh", "esc"], w=["Sh"])
                k.op("dve", lambda e: e.tensor_tensor(out=Sh[:], in0=Sh[:], in1=tU[:], op=ALU.add), r=["Sh", "tU"], w=["Sh"])
                if getattr(pb, 'stop', 99) == 6:
                    k.barrier()
                    return
                for c in range(4):
                    k.op("pe", lambda e, c=c: e.transpose(out=P0b[:, c * 128:(c + 1) * 128], in_=xbc[:, c, sl], identity=C["ident_b"][:]),
                         r=["xbc", "ident_b"], w=[PK[0]])
                for g in range(2):
                    k.op("pe", lambda e, g=g: e.transpose(out=P0b[:, 512 + g * 128:512 + (g + 1) * 128], in_=xbc[:, 4 + g, sl],
                                                          identity=C["ident_b"][:]), r=["xbc", "ident_b"], w=[PK[0]])
                k.op("act", lambda e: e.activation(out=xmt[:].rearrange("p a v -> p (a v)"), in_=P0b[:, 0:512], func=AF.Copy),
                     r=[PK[0]], w=["xmt"])
                k.op("act", lambda e: e.activation(out=Btok[:].rearrange("p a v -> p (a v)"), in_=P0b[:, 512:768], func=AF.Copy),
                     r=[PK[0]], w=["Btok"])
                k.op("act", lambda e: e.activation(out=dts[:, 3, :], in_=dtr[:, tl, :], func=AF.Exp), r=["dtr"], w=["dts3"])
                k.op("act", lambda e: e.activation(out=dts[:, 0, :], in_=dts[:, 3, :], func=AF.Ln, bias=1.0, scale=1.0), r=["dts3"], w=["dts0"])
                k.op("dve", lambda e: e.tensor_tensor(out=dts[:, 1, :], in0=dts[:, 0, :], in1=negA[:], op=ALU.mult), r=["dts0", "negA"], w=["dts1"])
                if getattr(pb, 'stop', 99) == 7:
                    k.barrier()
                    return
                k.op("pe", lambda e: e.matmul(P[7][:, 0:8], m_cum[:], dts[:, 1, :], start=True, stop=True), r=["dts1", "masks"], w=[PK[7]])
                k.op("pe", lambda e: e.matmul(P[7][:, 8:16], m_wend[:], dts[:, 1, :], start=True, stop=True), r=["dts1", "masks"], w=[PK[7]])
                k.op("pe", lambda e: e.matmul(P[7][:, 16:24], C["ones_f"][:], dts[:, 1, :], start=True, stop=True), r=["dts1", "ones_f"], w=[PK[7]])
                k.op("act", lambda e: e.activation(out=ec[:], in_=P[7][:, 0:24], func=AF.Exp), r=[PK[7]], w=["ec"])
                k.op("dve", lambda e: e.tensor_tensor(out=LH[:], in0=m_lh[:].unsqueeze(1).to_broadcast([128, 8, 128]),
                                                      in1=dts[:, 1, :].unsqueeze(2).to_broadcast([128, 8, 128]), op=ALU.mult),
                     r=["dts1", "masks"], w=["LH"])
                for h in range(8):
                    bk = 1 + h // 4
                    k.op("pe", lambda e, h=h, bk=bk: e.matmul(P[bk][:, (h % 4) * 128:(h % 4 + 1) * 128], LH[:, h, :], m_rhs[:],
                                                              start=True, stop=True), r=["LH", "masks"], w=[PK[bk]])
                for hh in range(2):
                    k.op("act", lambda e, hh=hh: e.activation(out=Em[:, hh * 4:(hh + 1) * 4, :].rearrange("p a t -> p (a t)"), in_=P[1 + hh][:],
                                                              func=AF.Exp), r=[PK[1 + hh]], w=["Em"])
                if getattr(pb, 'stop', 99) == 8:
                    k.barrier()
                    return
                for g in range(2):
                    k.op("pe", lambda e, g=g: e.matmul(P[3][:, g * 128:(g + 1) * 128], xbc[:, 4 + g, sl], xbc[:, 6 + g, sl], start=True, stop=True),
                         r=["xbc"], w=[PK[3]])
                k.op("dve", lambda e: e.tensor_tensor(out=cbm[:], in0=P[3][:, 0:256].rearrange("p (a t) -> p a t", a=2),
                                                      in1=m_att_f[:].unsqueeze(1).to_broadcast([128, 2, 128]), op=ALU.mult),
                     r=[PK[3], "masks"], w=["cbm"])
                for g in range(2):
                    k.op("dve", lambda e, g=g: e.tensor_tensor(out=Mm[:, g * 4:(g + 1) * 4, :], in0=Em[:, g * 4:(g + 1) * 4, :],
                                                               in1=cbm[:, g, :].unsqueeze(1).to_broadcast([128, 4, 128]), op=ALU.mult),
                         r=["Em", "cbm"], w=["Mm"])
                k.op("dve", lambda e: e.tensor_tensor(out=dts[:, 2, :], in0=dts[:, 0, :], in1=ec[:, 8:16], op=ALU.mult), r=["dts0", "ec"], w=["dts2"])
                k.op("pool", lambda e: e.tensor_tensor(out=xdt[:], in0=xmt[:], in1=dts[:, 0, :].unsqueeze(2).to_broadcast([128, 8, 64]), op=ALU.mult),
                     r=["xmt", "dts0"], w=["xdt"])
                k.op("pool", lambda e: e.tensor_tensor(out=xdtw[:], in0=xmt[:], in1=dts[:, 2, :].unsqueeze(2).to_broadcast([128, 8, 64]), op=ALU.mult),
                     r=["xmt", "dts2"], w=["xdtw"])
                if getattr(pb, 'stop', 99) == 9:
                    k.barrier()
                    return
                for h in range(8):
                    k.op("pe", lambda e, h=h: e.matmul(P[4][:, h * 64:(h + 1) * 64], Mm[:, h, :], xdt[:, h, :], start=True, stop=True),
                         r=["Mm", "xdt"], w=[PK[4]])
                for g in range(2):
                    k.op("pe", lambda e, g=g: e.matmul(P[5][:, g * 256:(g + 1) * 256], xbc[:, 6 + g, sl],
                                                       Ssb[:, g * 4:(g + 1) * 4, :].rearrange("p a v -> p (a v)"), start=True, stop=True),
                         r=["xbc", "Ssb"], w=[PK[5]])
                k.op("dve", lambda e: e.tensor_tensor(out=yi[:], in0=P[5][:].rearrange("p (a v) -> p a v", a=8),
                                                      in1=ec[:, 0:8].unsqueeze(2).to_broadcast([128, 8, 64]), op=ALU.mult),
                     r=[PK[5], "ec"], w=["yi"])
                k.op("dve", lambda e: e.tensor_tensor(out=ysb[:], in0=P[4][:].rearrange("p (a v) -> p a v", a=8), in1=yi[:], op=ALU.add),
                     r=[PK[4], "yi"], w=["ysb"])
                if dirn == 0:
                    k.op("pool", lambda e: e.tensor_tensor(out=yi[:], in0=xmt[:], in1=dsk[:].unsqueeze(2).to_broadcast([128, 8, 64]), op=ALU.mult),
                         r=["xmt", "dsk", "ysb"], w=["yi"])
                    k.op("dve", lambda e: e.tensor_tensor(out=ysb[:], in0=ysb[:], in1=yi[:], op=ALU.add), r=["ysb", "yi"], w=["ysb"])
                k.dma("sp", lambda e, grow=grow: e.dma_start(out=D["ys"][dirn, grow:grow + 128, :], in_=ysb[:].rearrange("p a v -> p (a v)")),
                      r=["ysb"], w=["ys"])
                for g in range(2):
                    k.op("pe", lambda e, g=g: e.matmul(P[6][:, g * 256:(g + 1) * 256], Btok[:, g, :],
                                                       xdtw[:, g * 4:(g + 1) * 4, :].rearrange("p a v -> p (a v)"), start=True, stop=True),
                         r=["Btok", "xdtw"], w=[PK[6]])
                k.op("dve", lambda e: e.tensor_tensor(out=Ss[:], in0=Ss[:], in1=ec[:, 16:24].unsqueeze(2).to_broadcast([128, 8, 64]), op=ALU.mult),
                     r=["Ss", "ec"], w=["Ss"])
                k.op("dve", lambda e: e.tensor_tensor(out=Ss[:], in0=Ss[:], in1=P[6][:].rearrange("p (a v) -> p a v", a=8), op=ALU.add),
                     r=["Ss", PK[6]], w=["Ss"])
                k.op("act", lambda e: e.activation(out=Ssb[:], in_=Ss[:], func=AF.Copy), r=["Ss"], w=["Ssb"])
        k.barrier()


def phase_mix_merge(pb, C, ntiles=66):
    k, D, nc = pb.k, pb.D, pb.nc
    with ExitStack() as es:
        P = [pb.ps(es, "P%d" % i, [128, 512], F32) for i in range(8)]
        PK = ["P%d" % i for i in range(8)]
        P0b = P[0][:].bitcast(BF16)
        mv = load_modv(pb, es, 0, (0, 1, 2))
        stg = pb.sb(es, "wstg", [128, 512], F32)
        wg = load_w_bf16(pb, es, "wg", D["w_in"][:, 2048:2560], 8, 512, stg, "wstg")
        wz = load_w_bf16(pb, es, "wz", D["w_in"][:, 2560:3072], 8, 512, stg, "wstg")
        wo = load_w_bf16(pb, es, "wo", D["w_out_rec"], 8, 1024, stg, "wstg")
        hg = pb.sb(es, "hg", [128, 512], F32)
        mn = pb.sb(es, "mn", [128, 512], F32)
        k.dma("sp", lambda e: e.dma_start(out=hg[:], in_=D["hgrn_norm"].to_broadcast([128, 512])), w=["gains"])
        k.dma("sp", lambda e: e.dma_start(out=mn[:], in_=D["mamba_norm"].to_broadcast([128, 512])), w=["gains"])
        xt = [pb.sb(es, "xt%d" % i, [128, 1024], F32) for i in range(2)]
        an = [pb.sb(es, "an%d" % i, [128, 1024], BF16) for i in range(2)]
        junk = pb.sb(es, "junk", [128, 1024], BF16)
        t1 = pb.sb(es, "t1", [128, 1024], F32)
        rstd = [pb.sb(es, "rstd%d" % i, [128, 4], F32) for i in range(2)]
        aT = pb.sb(es, "aT", [128, 8, 128], BF16)
        sg = pb.sb(es, "sg", [128, 512], F32)
        sz = pb.sb(es, "sz", [128, 512], F32)
        o0 = [pb.sb(es, "o0%d" % i, [128, 512], F32) for i in range(2)]
        o1 = [pb.sb(es, "o1%d" % i, [128, 512], F32) for i in range(2)]
        y0 = [pb.sb(es, "y0%d" % i, [128, 512], F32) for i in range(2)]
        y1 = [pb.sb(es, "y1%d" % i, [128, 512], F32) for i in range(2)]
        sq = pb.sb(es, "sq", [128, 512], F32)
        st8 = pb.sb(es, "st8", [128, 3, 8], F32)
        rs2 = pb.sb(es, "rs2", [128, 4], F32)
        cat = pb.sb(es, "cat", [128, 1024], BF16)
        catT = pb.sb(es, "catT", [128, 8, 128], BF16)
        xo = [pb.sb(es, "xo%d" % i, [128, 1024], F32) for i in range(2)]
        for j in range(ntiles):
            b = j % 2
            if j < 2:
                s, src, rr, grow = 1, D["ctx"], j * 128, T_LAT + j * 128
            else:
                s, src, rr, grow = 0, D["x"], (j - 2) * 128, (j - 2) * 128
            k.dma("sp", lambda e: e.dma_start(out=xt[b][:], in_=src[rr:rr + 128, :]), w=["xt%d" % b])
            k.dma("sp", lambda e: e.dma_start(out=o0[b][:], in_=D["oh"][0, grow:grow + 128, :]), r=["oh"], w=["o0%d" % b])
            k.dma("sp", lambda e: e.dma_start(out=o1[b][:], in_=D["oh"][1, grow:grow + 128, :]), r=["oh"], w=["o1%d" % b])
            k.dma("sp", lambda e: e.dma_start(out=y0[b][:], in_=D["ys"][0, grow:grow + 128, :]), r=["ys"], w=["y0%d" % b])
            k.dma("sp", lambda e: e.dma_start(out=y1[b][:], in_=D["ys"][1, grow:grow + 128, :]), r=["ys"], w=["y1%d" % b])
            norm_mod_tile(pb, xt[b], "xt%d" % b, rstd[b], "rstd%d" % b, junk, t1, mv[(s, 1)], mv[(s, 0)], an[b], "an%d" % b)
            for kc in range(8):
                k.op("pe", lambda e, kc=kc: e.transpose(out=P0b[:, kc * 128:(kc + 1) * 128], in_=an[b][:, kc * 128:(kc + 1) * 128],
                                                        identity=C["ident_b"][:]), r=["an%d" % b, "ident_b"], w=[PK[0]])
            k.op("act", lambda e: e.activation(out=aT[:].rearrange("p a t -> p (a t)"), in_=P0b, func=AF.Copy), r=[PK[0]], w=["aT"])
            for kc in range(8):
                k.op("pe", lambda e, kc=kc: e.matmul(P[1][:], aT[:, kc, :], wg[:, kc, :], start=(kc == 0), stop=(kc == 7)),
                     r=["aT", "wg"], w=[PK[1]])
            k.op("act", lambda e: e.activation(out=sg[:], in_=P[1][:], func=AF.Sigmoid), r=[PK[1]], w=["sg"])
            for kc in range(8):
                k.op("pe", lambda e, kc=kc: e.matmul(P[2][:], aT[:, kc, :], wz[:, kc, :], start=(kc == 0), stop=(kc == 7)),
                     r=["aT", "wz"], w=[PK[2]])
            k.op("act", lambda e: e.activation(out=sz[:], in_=P[2][:], func=AF.Silu), r=[PK[2]], w=["sz"])
            ok0, ok1, yk0, yk1 = "o0%d" % b, "o1%d" % b, "y0%d" % b, "y1%d" % b
            k.op("dve", lambda e: e.tensor_tensor(out=o0[b][:], in0=o0[b][:], in1=o1[b][:], op=ALU.add), r=[ok0, ok1], w=[ok0])
            k.op("pool", lambda e: e.tensor_tensor(out=sq[:], in0=o0[b][:], in1=o0[b][:], op=ALU.mult), r=[ok0], w=["sq"])
            k.op("dve", lambda e: e.tensor_reduce(out=st8[:, 0, :], in_=sq[:].rearrange("p (a v) -> p a v", a=8), axis=AX.X, op=ALU.add),
                 r=["sq"], w=["st8"])
            k.op("act", lambda e: e.activation(out=st8[:, 1, :], in_=st8[:, 0, :], func=AF.Sqrt, bias=EPS, scale=1.0 / 64), r=["st8"], w=["st8"])
            k.op("dve", lambda e: e.reciprocal(out=st8[:, 2, :], in_=st8[:, 1, :]), r=["st8"], w=["st8"])
            k.op("dve", lambda e: e.tensor_tensor(out=o0[b][:].rearrange("p (a v) -> p a v", a=8), in0=o0[b][:].rearrange("p (a v) -> p a v", a=8),
                                                  in1=st8[:, 2, :].unsqueeze(2).to_broadcast([128, 8, 64]), op=ALU.mult), r=[ok0, "st8"], w=[ok0])
            k.op("pool", lambda e: e.tensor_tensor(out=o0[b][:], in0=o0[b][:], in1=hg[:], op=ALU.mult), r=[ok0, "gains"], w=[ok0])
            k.op("dve", lambda e: e.tensor_tensor(out=cat[:, 0:512], in0=o0[b][:], in1=sg[:], op=ALU.mult), r=[ok0, "sg"], w=["cat"])
            k.op("pool", lambda e: e.tensor_tensor(out=y0[b][:], in0=y0[b][:], in1=y1[b][:], op=ALU.add), r=[yk0, yk1], w=[yk0])
            k.op("dve", lambda e: e.tensor_tensor(out=y0[b][:], in0=y0[b][:], in1=sz[:], op=ALU.mult), r=[yk0, "sz"], w=[yk0])
            k.op("act", lambda e: e.activation(out=junk[:, 0:512], in_=y0[b][:], func=AF.Square, accum_out=rs2[:, 0:1]), r=[yk0], w=["junk", "rs2"])
            k.op("act", lambda e: e.activation(out=rs2[:, 1:2], in_=rs2[:, 0:1], func=AF.Sqrt, bias=EPS, scale=1.0 / 512), r=["rs2"], w=["rs2"])
            k.op("dve", lambda e: e.reciprocal(out=rs2[:, 2:3], in_=rs2[:, 1:2]), r=["rs2"], w=["rs2"])
            k.op("dve", lambda e: e.scalar_tensor_tensor(out=cat[:, 512:1024], in0=y0[b][:], scalar=rs2[:, 2:3], in1=mn[:],
                                                         op0=ALU.mult, op1=ALU.mult), r=[yk0, "rs2", "gains"], w=["cat"])
            for c in range(8):
                k.op("pe", lambda e, c=c: e.transpose(out=P0b[:, c * 128:(c + 1) * 128], in_=cat[:, c * 128:(c + 1) * 128],
                                                      identity=C["ident_b"][:]), r=["cat", "ident_b"], w=[PK[0]])
            k.op("act", lambda e: e.activation(out=catT[:].rearrange("p a t -> p (a t)"), in_=P0b, func=AF.Copy), r=[PK[0]], w=["catT"])
            for dh in range(2):
                for c in range(8):
                    k.op("pe", lambda e, c=c, dh=dh: e.matmul(P[3 + dh][:], catT[:, c, :], wo[:, c, dh * 512:(dh + 1) * 512],
                                                              start=(c == 0), stop=(c == 7)), r=["catT", "wo"], w=[PK[3 + dh]])
                k.op("dve", lambda e, dh=dh: e.tensor_tensor(out=xo[b][:, dh * 512:(dh + 1) * 512], in0=P[3 + dh][:],
                                                             in1=mv[(s, 2)][:, dh * 512:(dh + 1) * 512], op=ALU.mult),
                     r=[PK[3 + dh], "modv"], w=["xo%d" % b])
            k.op("pool", lambda e: e.tensor_tensor(out=xo[b][:], in0=xo[b][:], in1=xt[b][:], op=ALU.add), r=["xo%d" % b, "xt%d" % b], w=["xo%d" % b])
            k.dma("sp", lambda e: e.dma_start(out=D["xres"][grow:grow + 128, :], in_=xo[b][:]), r=["xo%d" % b], w=["xres"])
        k.barrier()


def declare_all(pb):
    pb.din("x", [T_LAT, DM]); pb.din("ctx", [T_CTX, DM]); pb.din("c", [1, DM]); pb.din("c_ctx", [1, DM])
    pb.din("w_mod", [2, DM, 6 * DM]); pb.din("b_mod", [2, 6 * DM]); pb.din("norm_mix", [2, DM]); pb.din("norm_ffn", [2, DM])
    pb.din("norm_out", [1, DM])
    pb.din("w_in", [DM, 4112]); pb.din("w_out_rec", [DM, DM]); pb.din("conv_w", [5, DM]); pb.din("conv_b", [1, DM])
    pb.din("lb_gamma", [2, 2, 512]); pb.din("dt_bias", [2, 8]); pb.din("a_log", [2, 8]); pb.din("d_skip", [1, 8])
    pb.din("hgrn_norm", [1, 512]); pb.din("mamba_norm", [1, 512])
    pb.din("w_dq", [DM, 384]); pb.din("q_norm", [1, 384]); pb.din("w_uq_r", [384, 2048]); pb.din("w_dkv", [DM, 256])
    pb.din("kv_norm", [1, 256]); pb.din("w_ukv_r", [256, 2048]); pb.din("w_kr_r", [DM, 128]); pb.din("w_o", [DM, DM])
    pb.din("w_router", [2, DM, NE]); pb.din("w_gate", [2, NE, DM, DM]); pb.din("w_up", [2, NE, DM, DM]); pb.din("w_down", [2, NE, DM, DM])
    for nm, v in host_consts().items():
        pb.din("c_" + nm, v.shape)
    for nm, v in attn_host_consts().items():
        pb.din("c_" + nm, v.shape)
    pb.dscr("xres", [NROWS, DM]); pb.dscr("hn", [NROWS, DM], BF16); pb.dscr("aff", [NROWS, 16]); pb.dscr("modrep", [2, 2, 6, 128, DM])
    pb.dscr("oh", [2, NTOK, 512]); pb.dscr("ys", [2, NTOK, 512])
    pb.dscr("KT", [NH, 128, NTOK], BF16); pb.dscr("KRT", [64, NTOK], BF16); pb.dscr("Vd", [NTOK, 1024], BF16)
    pb.dscr("QT", [NH, 128, T_LAT], BF16); pb.dscr("QRT", [NH, 64, T_LAT], BF16); pb.dscr("Od", [T_LAT, 1024], BF16)


def make_inputs(inp, b):
    f = lambda a: np.ascontiguousarray(a, dtype=np.float32)
    w_uq_r, w_ukv_r, w_kr_r = attn_layout_weights(inp["w_uq"][0], inp["w_ukv"][0], inp["w_kr"][0])
    im = {"x": f(inp["x"][b]), "ctx": f(inp["ctx"][b]), "c": f(inp["c"][b:b + 1]), "c_ctx": f(inp["c_ctx"][None, :]),
          "w_mod": f(inp["w_mod"]), "b_mod": f(inp["b_mod"]), "norm_mix": f(inp["norm_mix"]), "norm_ffn": f(inp["norm_ffn"]),
          "norm_out": f(inp["norm_out"][None, :]),
          "w_in": f(inp["w_in"][0]), "w_out_rec": f(inp["w_out_rec"][0]), "conv_w": f(inp["conv_w"][0]), "conv_b": f(inp["conv_b"]),
          "lb_gamma": f(inp["lb_gamma"]), "dt_bias": f(inp["dt_bias"][0]), "a_log": f(inp["a_log"][0]), "d_skip": f(inp["d_skip"]),
          "hgrn_norm": f(inp["hgrn_norm"]), "mamba_norm": f(inp["mamba_norm"]),
          "w_dq": f(inp["w_dq"][0]), "q_norm": f(inp["q_norm"]), "w_uq_r": f(w_uq_r), "w_dkv": f(inp["w_dkv"][0]),
          "kv_norm": f(inp["kv_norm"]), "w_ukv_r": f(w_ukv_r), "w_kr_r": f(w_kr_r), "w_o": f(inp["w_o"][0]),
          "w_router": f(inp["w_router"]), "w_gate": f(inp["w_gate"]), "w_up": f(inp["w_up"]), "w_down": f(inp["w_down"])}
    for nm, v in host_consts().items():
        im["c_" + nm] = v
    for nm, v in attn_host_consts().items():
        im["c_" + nm] = v
    return im


def build_program(phases, dbg=False, opts=None):
    opts = opts or {}
    nc = bass.Bass("TRN2", target_bir_lowering=False)
    pb = PB(nc)
    declare_all(pb)
    pb.dout("out", [T_LAT, DM])
    if dbg:
        pb.dout("dbg", [NTOK, DM])
    with ExitStack() as es:
        C = load_consts(pb, es)
        phase_init(pb, copy_x=("init" in phases))
        phase_mod(pb, C)
        if "mixf" in phases:
            phase_mix_pass(pb, C, 0, **opts.get("mix", {}))
        if "mixb" in phases:
            phase_mix_pass(pb, C, 1, **opts.get("mix", {}))
        if "mixm" in phases:
            phase_mix_merge(pb, C, **opts.get("merge", {}))
        if "moe0" in phases:
            phase_moe(pb, C, 0)
        if "attn_pre" in phases:
            phase_attn_pre(pb, C)
        if "attn_main" in phases:
            phase_attn_main(pb, C, **opts.get("attn", {}))
        if "attn_post" in phases:
            phase_attn_post(pb, C)
        if "moe1" in phases:
            phase_moe(pb, C, 1)
        if "final" in phases:
            phase_final(pb, C)
        k = pb.k
        if dbg:
            for i in range(8):
                k.dma("sp", lambda e, i=i: e.dma_start(out=pb.D["dbg"][i * 1056:(i + 1) * 1056, :], in_=pb.D["xres"][i * 1056:(i + 1) * 1056, :]),
                      r=["xres"], w=["dbg"])
        k.finish()
    return nc, pb


ALL_PHASES = ["mixf", "mixb", "mixm", "moe0", "attn_pre", "attn_main", "attn_post", "moe1", "final"]


def kernel(**inputs):
    from concourse.bass_utils import run_bass_kernel_spmd
    inp = {k: np.asarray(v) for k, v in inputs.items()}
    nb = inp["x"].shape[0]
    nc, pb = build_program(ALL_PHASES)
    in_maps = [make_inputs(inp, b) for b in range(nb)]
    res = run_bass_kernel_spmd(nc, in_maps, core_ids=list(range(nb)))
    out = np.stack([np.asarray(res.results[b]["out"]) for b in range(nb)], axis=0)
    return out.astype(np.float32)
```

```python
import numpy as np
import concourse.bass as bass
import concourse.mybir as mybir

F32 = mybir.dt.float32
BF16 = mybir.dt.bfloat16
I32 = mybir.dt.int32
U32 = mybir.dt.uint32
AF = mybir.ActivationFunctionType
ALU = mybir.AluOpType
AX = mybir.AxisListType


class KF:
    NDS = 8

    def __init__(self, nc):
        self.nc = nc
        self.eng = {"pe": nc.tensor, "dve": nc.vector, "act": nc.scalar, "pool": nc.gpsimd, "sp": nc.sync}
        self.csem = {}
        self.ccnt = {}
        for e in self.eng:
            self.csem[e] = nc.alloc_semaphore("cs_" + e)
            self.ccnt[e] = 0
        self.dsem = {}
        self.dval = {}
        self.drot = {}
        for q in ("sp", "pool", "act"):
            self.dsem[q] = [nc.alloc_semaphore("ds_%s%d" % (q, i)) for i in range(self.NDS)]
            self.dval[q] = [0] * self.NDS
            self.drot[q] = 0
        self.waited = {e: {} for e in self.eng}
        self.lastw = {}
        self.readers = {}
        self.same_eng_sync = {"pe": False, "dve": True, "act": True, "pool": True, "sp": True}
        self.nins = 0

    def _wait(self, e, tok):
        sem, val, src = tok
        if src == e and not self.same_eng_sync[e]:
            return
        key = id(sem)
        if self.waited[e].get(key, 0) >= val:
            return
        self.eng[e].wait_ge(sem, val)
        self.waited[e][key] = val
        self.nins += 1

    def _deps(self, e, r, w):
        for k in r:
            t = self.lastw.get(k)
            if t is not None:
                self._wait(e, t)
        for k in w:
            t = self.lastw.get(k)
            if t is not None:
                self._wait(e, t)
            for t in self.readers.get(k, ()):
                self._wait(e, t)

    def _commit(self, tok, r, w):
        for k in w:
            self.lastw[k] = tok
            self.readers[k] = []
        for k in r:
            self.readers.setdefault(k, []).append(tok)

    def op(self, e, fn, r=(), w=()):
        r = [x for x in r if x is not None]
        w = [x for x in w if x is not None]
        self._deps(e, r, w)
        ins = fn(self.eng[e])
        self.ccnt[e] += 1
        ins.then_inc(self.csem[e], 1)
        tok = (self.csem[e], self.ccnt[e], e)
        self._commit(tok, r, w)
        self.nins += 1
        return tok

    def dma(self, q, fn, r=(), w=()):
        r = [x for x in r if x is not None]
        w = [x for x in w if x is not None]
        i = self.drot[q]
        self.drot[q] = (i + 1) % self.NDS
        sem = self.dsem[q][i]
        self._wait(q, (sem, self.dval[q][i], None))
        self._deps(q, r, w)
        ins = fn(self.eng[q])
        self.dval[q][i] += 16
        ins.then_inc(sem, 16)
        tok = (sem, self.dval[q][i], None)
        self._commit(tok, r, w)
        self.nins += 1
        return tok

    def barrier(self):
        toks = []
        for e in self.eng:
            if self.ccnt[e] > 0:
                toks.append((self.csem[e], self.ccnt[e], None))
        for q in self.dsem:
            for i in range(self.NDS):
                if self.dval[q][i] > 0:
                    toks.append((self.dsem[q][i], self.dval[q][i], None))
        for e in self.eng:
            for t in toks:
                if t[0] is self.csem[e]:
                    continue
                self._wait(e, t)
        self.lastw = {}
        self.readers = {}

    def finish(self):
        for e in self.eng:
            if e != "sp" and self.ccnt[e] > 0:
                self._wait("sp", (self.csem[e], self.ccnt[e], None))
        for q in self.dsem:
            for i in range(self.NDS):
                if self.dval[q][i] > 0:
                    self._wait("sp", (self.dsem[q][i], self.dval[q][i], None))

from contextlib import ExitStack

T_LAT = 8192
T_CTX = 256
NTOK = T_LAT + T_CTX
NROWS = NTOK + 128
DM = 1024
EPS = 1e-6
NE = 16


def host_consts():
    c = {}
    c["ident"] = np.eye(128, dtype=np.float32)
    p = np.arange(128)
    c["triu_incl"] = (p[:, None] <= p[None, :]).astype(np.float32)
    c["triu_strict"] = (p[:, None] < p[None, :]).astype(np.float32)
    c["tril_incl"] = (p[:, None] >= p[None, :]).astype(np.float32)
    c["tril_strict"] = (p[:, None] > p[None, :]).astype(np.float32)
    c["iota_q"] = np.tile(np.arange(1024, dtype=np.float32)[None, :], (128, 1))
    q = np.arange(128, dtype=np.float32)
    c["ctxadd"] = np.tile((T_LAT + np.maximum(q - 32, 0))[None, :], (128, 1)).astype(np.float32)
    return c


class PB:
    def __init__(self, nc):
        self.nc = nc
        self.k = KF(nc)
        self.D = {}
        self.uid = 0

    def din(self, name, shape, dt=F32):
        self.D[name] = self.nc.dram_tensor(name, list(shape), dt, kind="ExternalInput").ap()
        return self.D[name]

    def dout(self, name, shape, dt=F32):
        self.D[name] = self.nc.dram_tensor(name, list(shape), dt, kind="ExternalOutput").ap()
        return self.D[name]

    def dscr(self, name, shape, dt=F32):
        self.D[name] = self.nc.dram_tensor(name, list(shape), dt, kind="Internal").ap()
        return self.D[name]

    def sb(self, es, name, shape, dt):
        self.uid += 1
        return es.enter_context(self.nc.sbuf_tensor("%s_%d" % (name, self.uid), list(shape), dt))

    def ps(self, es, name, shape, dt):
        self.uid += 1
        return es.enter_context(self.nc.psum_tensor("%s_%d" % (name, self.uid), list(shape), dt))


def load_consts(pb, es):
    k, D = pb.k, pb.D
    C = {}
    for nm in ("ident", "triu_incl", "triu_strict", "tril_incl", "tril_strict"):
        f = pb.sb(es, nm + "_f", [128, 128], F32)
        b = pb.sb(es, nm + "_b", [128, 128], BF16)
        k.dma("sp", lambda e, f=f, nm=nm: e.dma_start(out=f[:], in_=D["c_" + nm]), w=[nm + "_f"])
        k.op("dve", lambda e, f=f, b=b: e.tensor_copy(out=b[:], in_=f[:]), r=[nm + "_f"], w=[nm + "_b"])
        C[nm + "_f"] = f
        C[nm + "_b"] = b
    ones_f = pb.sb(es, "ones_f", [128, 128], F32)
    ones_b = pb.sb(es, "ones_b", [128, 128], BF16)
    k.op("dve", lambda e: e.memset(ones_f[:], 1.0), w=["ones_f"])
    k.op("dve", lambda e: e.memset(ones_b[:], 1.0), w=["ones_b"])
    C["ones_f"] = ones_f
    C["ones_b"] = ones_b
    return C


def phase_init(pb, copy_x=True):
    k, D = pb.k, pb.D
    with ExitStack() as es:
        z = pb.sb(es, "zt", [128, 1024], F32)
        zb = pb.sb(es, "zb", [128, 1024], BF16)
        k.op("dve", lambda e: e.memset(z[:], 0.0), w=["zt"])
        k.op("dve", lambda e: e.memset(zb[:], 0.0), w=["zb"])
        if copy_x:
            for i in range(8):
                k.dma("sp", lambda e, i=i: e.dma_start(out=D["xres"][i * 1024:(i + 1) * 1024, :],
                                                       in_=D["x"][i * 1024:(i + 1) * 1024, :]), w=["xres"])
            k.dma("sp", lambda e: e.dma_start(out=D["xres"][T_LAT:NTOK, :], in_=D["ctx"]), w=["xres"])
        k.dma("sp", lambda e: e.dma_start(out=D["xres"][NTOK:NROWS, :], in_=z[:]), r=["zt"], w=["xres"])
        k.dma("sp", lambda e: e.dma_start(out=D["hn"][NTOK:NROWS, :], in_=zb[:]), r=["zb"], w=["hn"])
        k.dma("sp", lambda e: e.dma_start(out=D["aff"][NTOK:NROWS, :], in_=z[:, 0:16]), r=["zt"], w=["aff"])
        k.barrier()


def phase_mod(pb, C):
    k, D, nc = pb.k, pb.D, pb.nc
    with ExitStack() as es:
        s8 = pb.sb(es, "s8", [8, 2, 128], F32)
        scol = pb.sb(es, "scol", [128, 2, 8], F32)
        srep = pb.sb(es, "srep", [128, 2, 8, 128], F32)
        ps_t = pb.ps(es, "ps_t", [128, 512], F32)
        k.dma("sp", lambda e: e.dma_start(out=s8[:, 0, :], in_=D["c"].rearrange("o (a b) -> (o a) b", a=8)), w=["s8"])
        k.dma("sp", lambda e: e.dma_start(out=s8[:, 1, :], in_=D["c_ctx"].rearrange("o (a b) -> (o a) b", a=8)), w=["s8"])
        k.op("act", lambda e: e.activation(out=s8[:], in_=s8[:], func=AF.Silu), r=["s8"], w=["s8"])
        for s in range(2):
            k.op("pe", lambda e, s=s: e.transpose(out=ps_t[:, s * 8:(s + 1) * 8], in_=s8[:, s, :],
                                                  identity=C["ident_f"][0:8, 0:8]), r=["s8", "ident_f"], w=["ps_t"])
        k.op("dve", lambda e: e.tensor_copy(out=scol[:].rearrange("p s c -> p (s c)"), in_=ps_t[:, 0:16]),
             r=["ps_t"], w=["scol"])
        for s in range(2):
            k.op("dve", lambda e, s=s: e.tensor_copy(out=srep[:, s, :, :],
                                                     in_=scol[:, s, :].unsqueeze(2).to_broadcast([128, 8, 128])),
                 r=["scol"], w=["srep"])
        wm = [pb.sb(es, "wm%d" % i, [128, 8, 512], F32) for i in range(2)]
        bm = [pb.sb(es, "bm%d" % i, [128, 512], F32) for i in range(2)]
        gn = [pb.sb(es, "gn%d" % i, [128, 512], F32) for i in range(2)]
        ob = [pb.sb(es, "ob%d" % i, [128, 512], F32) for i in range(4)]
        psm = [pb.ps(es, "psm%d" % i, [128, 512], F32) for i in range(2)]
        it = 0
        oi = 0
        for l in range(2):
            for ncn in range(12):
                b = it % 2
                it += 1
                n0 = ncn * 512
                slot = ncn // 2
                half = ncn % 2
                k.dma("sp", lambda e, b=b, l=l, n0=n0: e.dma_start(
                    out=wm[b][:], in_=D["w_mod"][l, :, n0:n0 + 512].rearrange("(kc p) n -> p kc n", p=128)),
                    w=["wm%d" % b])
                k.dma("sp", lambda e, b=b, l=l, n0=n0: e.dma_start(
                    out=bm[b][:], in_=D["b_mod"][l:l + 1, n0:n0 + 512].to_broadcast([128, 512])), w=["bm%d" % b])
                if slot in (1, 4):
                    gsrc = D["norm_mix"] if slot == 1 else D["norm_ffn"]
                    k.dma("sp", lambda e, b=b, l=l, half=half, gsrc=gsrc: e.dma_start(
                        out=gn[b][:], in_=gsrc[l:l + 1, half * 512:(half + 1) * 512].to_broadcast([128, 512])),
                        w=["gn%d" % b])
                for s in range(2):
                    pm = psm[s]
                    for kc in range(8):
                        k.op("pe", lambda e, s=s, kc=kc, b=b, pm=pm: e.matmul(
                            pm[:], srep[:, s, kc, :], wm[b][:, kc, :], start=(kc == 0), stop=(kc == 7)),
                            r=["srep", "wm%d" % b], w=["psm%d" % s])
                    o = ob[oi % 4]
                    okey = "ob%d" % (oi % 4)
                    oi += 1
                    k.op("dve", lambda e, o=o, pm=pm, b=b: e.tensor_tensor(out=o[:], in0=pm[:], in1=bm[b][:], op=ALU.add),
                         r=["psm%d" % s, "bm%d" % b], w=[okey])
                    if slot in (1, 4):
                        k.op("dve", lambda e, o=o, b=b: e.scalar_tensor_tensor(
                            out=o[:], in0=o[:], scalar=1.0, in1=gn[b][:], op0=ALU.add, op1=ALU.mult),
                            r=[okey, "gn%d" % b], w=[okey])
                    k.dma("sp", lambda e, o=o, l=l, s=s, slot=slot, half=half: e.dma_start(
                        out=D["modrep"][l, s, slot, :, half * 512:(half + 1) * 512], in_=o[:]),
                        r=[okey], w=["modrep"])
        k.barrier()


def norm_mod_tile(pb, xt, xkey, rstd, rkey, junk, t1, G, SH, hn, hnkey):
    k = pb.k
    k.op("act", lambda e: e.activation(out=junk[:], in_=xt[:], func=AF.Square, accum_out=rstd[:, 0:1]),
         r=[xkey], w=["junk", rkey])
    k.op("act", lambda e: e.activation(out=rstd[:, 1:2], in_=rstd[:, 0:1], func=AF.Sqrt, bias=EPS, scale=1.0 / DM),
         r=[rkey], w=[rkey])
    k.op("dve", lambda e: e.reciprocal(out=rstd[:, 2:3], in_=rstd[:, 1:2]), r=[rkey], w=[rkey])
    k.op("dve", lambda e: e.scalar_tensor_tensor(out=t1[:], in0=xt[:], scalar=rstd[:, 2:3], in1=G[:],
                                                 op0=ALU.mult, op1=ALU.mult),
         r=[xkey, rkey, "modv"], w=["t1"])
    k.op("pool", lambda e: e.tensor_tensor(out=hn[:], in0=t1[:], in1=SH[:], op=ALU.add),
         r=["t1", "modv"], w=[hnkey])


def load_modv(pb, es, l, slots):
    k, D = pb.k, pb.D
    out = {}
    for s in range(2):
        for slot in slots:
            t = pb.sb(es, "mv%d%d" % (s, slot), [128, 1024], F32)
            k.dma("sp", lambda e, t=t, s=s, slot=slot: e.dma_start(out=t[:], in_=D["modrep"][l, s, slot, :, :]),
                  r=["modrep"], w=["modv"])
            out[(s, slot)] = t
    return out


def topk_threshold(pb, es, C, aff, J, cap, tag, psum):
    k = pb.k
    lo = pb.sb(es, "lo" + tag, [128, 16], F32)
    hi = pb.sb(es, "hi" + tag, [128, 16], F32)
    mid = pb.sb(es, "mid" + tag, [128, 16], F32)
    cnt = pb.sb(es, "cnt" + tag, [128, 16], F32)
    ge = pb.sb(es, "ge" + tag, [128, 16], U32)
    lt = pb.sb(es, "lt" + tag, [128, 16], U32)
    cmp = pb.sb(es, "cmp" + tag, [128, J, 16], BF16)
    K = "tk" + tag
    k.op("dve", lambda e: e.memset(lo[:], 0.0), w=[K])
    k.op("dve", lambda e: e.memset(hi[:], 1.0), w=[K])
    ncol = J * 16
    for it in range(30):
        k.op("dve", lambda e: e.tensor_tensor(out=mid[:], in0=lo[:], in1=hi[:], op=ALU.add), r=[K], w=[K + "m"])
        k.op("dve", lambda e: e.tensor_scalar(out=mid[:], in0=mid[:], scalar1=0.5, scalar2=None, op0=ALU.mult),
             r=[K + "m"], w=[K + "m"])
        k.op("dve", lambda e: e.tensor_tensor(out=cmp[:], in0=aff, in1=mid[:].unsqueeze(1).to_broadcast([128, J, 16]),
                                              op=ALU.is_ge), r=[K + "m", "aff_all"], w=[K + "c"])
        cf = cmp[:].rearrange("p j e -> p (j e)")
        for c0 in range(0, ncol, 512):
            c1 = min(ncol, c0 + 512)
            k.op("pe", lambda e, c0=c0, c1=c1: e.matmul(psum[:, c0:c1], C["ones_b"][:], cf[:, c0:c1], start=True, stop=True),
                 r=[K + "c", "ones_b"], w=[K + "p"])
        k.op("dve", lambda e: e.tensor_reduce(out=cnt[:], in_=psum[:, 0:ncol].rearrange("p (j e) -> p e j", e=16),
                                              axis=AX.X, op=ALU.add), r=[K + "p"], w=[K + "n"])
        k.op("dve", lambda e: e.tensor_scalar(out=ge[:], in0=cnt[:], scalar1=float(cap), scalar2=None, op0=ALU.is_ge),
             r=[K + "n"], w=[K + "g"])
        k.op("dve", lambda e: e.tensor_scalar(out=lt[:], in0=cnt[:], scalar1=float(cap), scalar2=None, op0=ALU.is_lt),
             r=[K + "n"], w=[K + "g"])
        k.op("dve", lambda e: e.copy_predicated(out=lo[:], mask=ge[:], data=mid[:]), r=[K + "g", K + "m"], w=[K])
        k.op("dve", lambda e: e.copy_predicated(out=hi[:], mask=lt[:], data=mid[:]), r=[K + "g", K + "m"], w=[K])
    return lo, K


def phase_moe(pb, C, l):
    k, D, nc = pb.k, pb.D, pb.nc
    has_ctx = (l == 0)
    NT = 66 if has_ctx else 64
    NPT = 9 if has_ctx else 8
    NP = NPT * 128
    with ExitStack() as es:
        mv = load_modv(pb, es, l, (3, 4, 5))
        iota_q = pb.sb(es, "iota_q", [128, 1024], F32)
        ctxadd = pb.sb(es, "ctxadd", [128, 128], F32)
        k.dma("sp", lambda e: e.dma_start(out=iota_q[:], in_=D["c_iota_q"]), w=["iota_q"])
        k.dma("sp", lambda e: e.dma_start(out=ctxadd[:], in_=D["c_ctxadd"]), w=["ctxadd"])
        aff_all = pb.sb(es, "aff_all", [128, NT, 16], F32)
        wr_f = pb.sb(es, "wr_f", [128, 8, 16], F32)
        wr = pb.sb(es, "wr", [128, 8, 16], BF16)
        k.dma("sp", lambda e: e.dma_start(out=wr_f[:], in_=D["w_router"][l].rearrange("(kc p) n -> p kc n", p=128)),
              w=["wr_f"])
        k.op("dve", lambda e: e.tensor_copy(out=wr[:], in_=wr_f[:]), r=["wr_f"], w=["wr"])
        with ExitStack() as es2:
            xt = [pb.sb(es2, "xt%d" % i, [128, 1024], F32) for i in range(2)]
            hn = [pb.sb(es2, "hn%d" % i, [128, 1024], BF16) for i in range(2)]
            hnT = [pb.sb(es2, "hnT%d" % i, [128, 8, 128], BF16) for i in range(2)]
            junk = pb.sb(es2, "junk", [128, 1024], BF16)
            t1 = pb.sb(es2, "t1", [128, 1024], F32)
            rstd = [pb.sb(es2, "rstd%d" % i, [128, 4], F32) for i in range(2)]
            psT = [pb.ps(es2, "psT%d" % i, [128, 1024], BF16) for i in range(2)]
            psL = [pb.ps(es2, "psL%d" % i, [128, 16], F32) for i in range(2)]
            for j in range(NT):
                b = j % 2
                s = 0 if j < 64 else 1
                r0 = j * 128
                k.dma("sp", lambda e, b=b, r0=r0: e.dma_start(out=xt[b][:], in_=D["xres"][r0:r0 + 128, :]),
                      r=["xres"], w=["xt%d" % b])
                norm_mod_tile(pb, xt[b], "xt%d" % b, rstd[b], "rstd%d" % b, junk, t1, mv[(s, 4)], mv[(s, 3)], hn[b], "hn%d" % b)
                k.dma("sp", lambda e, b=b, r0=r0: e.dma_start(out=D["hn"][r0:r0 + 128, :], in_=hn[b][:]),
                      r=["hn%d" % b], w=["hn"])
                for kc in range(8):
                    k.op("pe", lambda e, b=b, kc=kc: e.transpose(out=psT[b][:, kc * 128:(kc + 1) * 128],
                                                                 in_=hn[b][:, kc * 128:(kc + 1) * 128],
                                                                 identity=C["ident_b"][:]),
                         r=["hn%d" % b, "ident_b"], w=["psT%d" % b])
                k.op("act", lambda e, b=b: e.activation(out=hnT[b][:].rearrange("p a t -> p (a t)"), in_=psT[b][:], func=AF.Copy),
                     r=["psT%d" % b], w=["hnT%d" % b])
                for kc in range(8):
                    k.op("pe", lambda e, b=b, kc=kc: e.matmul(psL[b][:], hnT[b][:, kc, :], wr[:, kc, :],
                                                              start=(kc == 0), stop=(kc == 7)),
                         r=["hnT%d" % b, "wr"], w=["psL%d" % b])
                k.op("dve", lambda e, b=b, j=j: e.tensor_copy(out=aff_all[:, j, :], in_=psL[b][:]),
                     r=["psL%d" % b], w=["aff_all"])
            mx = pb.sb(es2, "mx", [128, NT], F32)
            k.op("dve", lambda e: e.tensor_reduce(out=mx[:], in_=aff_all[:], axis=AX.X, op=ALU.max), r=["aff_all"], w=["mx"])
            k.op("dve", lambda e: e.tensor_tensor(out=aff_all[:], in0=aff_all[:],
                                                  in1=mx[:].unsqueeze(2).to_broadcast([128, NT, 16]), op=ALU.subtract),
                 r=["aff_all", "mx"], w=["aff_all"])
            k.op("act", lambda e: e.activation(out=aff_all[:], in_=aff_all[:], func=AF.Exp), r=["aff_all"], w=["aff_all"])
            k.op("dve", lambda e: e.tensor_reduce(out=mx[:], in_=aff_all[:], axis=AX.X, op=ALU.add), r=["aff_all"], w=["mx"])
            k.op("dve", lambda e: e.reciprocal(out=mx[:], in_=mx[:]), r=["mx"], w=["mx"])
            k.op("dve", lambda e: e.tensor_tensor(out=aff_all[:], in0=aff_all[:],
                                                  in1=mx[:].unsqueeze(2).to_broadcast([128, NT, 16]), op=ALU.mult),
                 r=["aff_all", "mx"], w=["aff_all"])
            k.dma("sp", lambda e: e.dma_start(out=D["aff"][0:NT * 128, :].rearrange("(j p) e -> p j e", p=128), in_=aff_all[:]),
                  r=["aff_all"], w=["aff"])
            k.barrier()
        m_all = pb.sb(es, "m_all", [128, NT, 16], BF16)
        cnt_incl = pb.sb(es, "cnt_incl", [128, NT, 16], F32)
        with ExitStack() as es2:
            pst = [pb.ps(es2, "pst%d" % i, [128, 512], F32) for i in range(3)]
            psbig = pb.ps(es2, "psbig", [128, 1024], F32)
            thr_l, Kl = topk_threshold(pb, es2, C, aff_all[:, 0:64, :], 64, 1024, "L", psbig)
            k.op("dve", lambda e: e.tensor_tensor(out=m_all[:, 0:64, :], in0=aff_all[:, 0:64, :],
                                                  in1=thr_l[:].unsqueeze(1).to_broadcast([128, 64, 16]), op=ALU.is_ge),
                 r=["aff_all", Kl], w=["m_all"])
            if has_ctx:
                thr_c, Kc = topk_threshold(pb, es2, C, aff_all[:, 64:66, :], 2, 32, "C", psbig)
                k.op("dve", lambda e: e.tensor_tensor(out=m_all[:, 64:66, :], in0=aff_all[:, 64:66, :],
                                                      in1=thr_c[:].unsqueeze(1).to_broadcast([128, 2, 16]), op=ALU.is_ge),
                     r=["aff_all", Kc], w=["m_all"])
            tot = pb.sb(es2, "tot", [128, NT, 16], F32)
            binc = pb.sb(es2, "binc", [128, NT, 16], F32)
            mf = m_all[:].rearrange("p j e -> p (j e)")
            tf = tot[:].rearrange("p j e -> p (j e)")
            cf = cnt_incl[:].rearrange("p j e -> p (j e)")
            ncol = NT * 16
            for ci, c0 in enumerate(range(0, ncol, 512)):
                c1 = min(ncol, c0 + 512)
                w_ = c1 - c0
                k.op("pe", lambda e, c0=c0, c1=c1, ci=ci, w_=w_: e.matmul(pst[ci][:, 0:w_], C["ones_b"][:], mf[:, c0:c1],
                                                                          start=True, stop=True),
                     r=["m_all", "ones_b"], w=["pst%d" % ci])
                k.op("dve", lambda e, c0=c0, c1=c1, ci=ci, w_=w_: e.tensor_copy(out=tf[:, c0:c1], in_=pst[ci][:, 0:w_]),
                     r=["pst%d" % ci], w=["tot"])
            for e_ in range(16):
                k.op("dve", lambda e, e_=e_: e.tensor_tensor_scan(out=binc[:, 0:64, e_], data0=C["ones_f"][:, 0:64],
                                                                  data1=tot[:, 0:64, e_], initial=0.0,
                                                                  op0=ALU.mult, op1=ALU.add),
                     r=["tot", "ones_f"], w=["binc"])
            if has_ctx:
                k.op("dve", lambda e: e.tensor_copy(out=binc[:, 64, :], in_=tot[:, 64, :]), r=["tot"], w=["binc"])
                k.op("dve", lambda e: e.tensor_tensor(out=binc[:, 65, :], in0=tot[:, 64, :], in1=tot[:, 65, :], op=ALU.add),
                     r=["tot"], w=["binc"])
            k.op("dve", lambda e: e.tensor_tensor(out=binc[:], in0=binc[:], in1=tot[:], op=ALU.subtract),
                 r=["binc", "tot"], w=["binc"])
            bf_ = binc[:].rearrange("p j e -> p (j e)")
            for ci, c0 in enumerate(range(0, ncol, 512)):
                c1 = min(ncol, c0 + 512)
                w_ = c1 - c0
                k.op("pe", lambda e, c0=c0, c1=c1, ci=ci, w_=w_: e.matmul(pst[ci][:, 0:w_], C["triu_incl_b"][:], mf[:, c0:c1],
                                                                          start=True, stop=True),
                     r=["m_all", "triu_incl_b", "tot"], w=["pst%d" % ci])
                k.op("dve", lambda e, c0=c0, c1=c1, ci=ci, w_=w_: e.tensor_tensor(out=cf[:, c0:c1], in0=pst[ci][:, 0:w_],
                                                                                  in1=bf_[:, c0:c1], op=ALU.add),
                     r=["pst%d" % ci, "binc"], w=["cnt_incl"])
            k.barrier()
        with ExitStack() as es2:
            Bt = [pb.sb(es2, "Bt%d" % i, [128, 1024], BF16) for i in range(2)]
            row_sb = pb.sb(es2, "row_sb", [1, NP], F32)
            idx_all = pb.sb(es2, "idx_all", [128, NE, NPT], I32)
            xg = pb.sb(es2, "xg", [128, NPT, 1024], BF16)
            ga = [pb.sb(es2, "ga%d" % i_, [128, NPT, 16], F32) for i_ in range(2)]
            xgT = pb.sb(es2, "xgT", [128, 8, NP], BF16)
            hidT = pb.sb(es2, "hidT", [128, 8, NP], BF16)
            sg = [pb.sb(es2, "sg%d" % i, [128, 512], F32) for i in range(2)]
            yt = [pb.sb(es2, "yt%d" % i, [128, 1024], F32) for i in range(2)]
            Wb = {nm: pb.sb(es2, "W" + nm, [128, 8, 1024], BF16) for nm in ("g", "u", "d")}
            stg = [pb.sb(es2, "stg%d" % i, [128, 4, 1024], F32) for i in range(2)]
            rowA = pb.ps(es2, "rowA", [128, 512], F32)
            rowB = pb.ps(es2, "rowB", [128, 512], F32)
            rowC = pb.ps(es2, "rowC", [128, 512], F32)
            psX = pb.ps(es2, "psX", [128, 1024], BF16)
            psG = pb.ps(es2, "psG", [128, 512], F32)
            psU = pb.ps(es2, "psU", [128, 512], F32)
            psY = [pb.ps(es2, "psY%d" % i, [128, 512], F32) for i in range(2)]
            sti = 0
            bi = 0
            yi = 0
            pchunks = [(0, 512), (512, 1024)] + ([(1024, 1152)] if has_ctx else [])
            for ex in range(NE):
                for j in range(64):
                    B_ = Bt[bi % 2]
                    bkey = "Bt%d" % (bi % 2)
                    eng = "dve"
                    bi += 1
                    k.op(eng, lambda e, B_=B_, j=j, ex=ex: e.tensor_scalar(
                        out=B_[:], in0=iota_q[:], scalar1=cnt_incl[:, j, ex:ex + 1], scalar2=None, op0=ALU.is_ge),
                        r=["iota_q", "cnt_incl"], w=[bkey])
                    k.op("pe", lambda e, B_=B_, j=j: e.matmul(rowA[0:1, :], C["ones_b"][:, 0:1], B_[:, 0:512],
                                                              start=(j == 0), stop=(j == 63)),
                         r=[bkey, "ones_b"], w=["rowA"])
                    k.op("pe", lambda e, B_=B_, j=j: e.matmul(rowB[0:1, :], C["ones_b"][:, 0:1], B_[:, 512:1024],
                                                              start=(j == 0), stop=(j == 63)),
                         r=[bkey, "ones_b"], w=["rowB"])
                k.op("dve", lambda e: e.tensor_copy(out=row_sb[0:1, 0:512], in_=rowA[0:1, :]), r=["rowA"], w=["row_sb"])
                k.op("dve", lambda e: e.tensor_copy(out=row_sb[0:1, 512:1024], in_=rowB[0:1, :]), r=["rowB"], w=["row_sb"])
                if has_ctx:
                    for j in (64, 65):
                        B_ = Bt[bi % 2]
                        bkey = "Bt%d" % (bi % 2)
                        bi += 1
                        k.op("dve", lambda e, B_=B_, j=j, ex=ex: e.tensor_scalar(
                            out=B_[:, 0:128], in0=iota_q[:, 0:128], scalar1=cnt_incl[:, j, ex:ex + 1], scalar2=None,
                            op0=ALU.is_ge), r=["iota_q", "cnt_incl"], w=[bkey])
                        k.op("pe", lambda e, B_=B_, j=j: e.matmul(rowC[0:1, 0:128], C["ones_b"][:, 0:1], B_[:, 0:128],
                                                                  start=(j == 64), stop=(j == 65)),
                             r=[bkey, "ones_b"], w=["rowC"])
                    k.op("dve", lambda e: e.tensor_tensor(out=row_sb[0:1, 1024:1152], in0=rowC[0:1, 0:128],
                                                          in1=ctxadd[0:1, :], op=ALU.add),
                         r=["rowC", "ctxadd"], w=["row_sb"])
                for i in range(NPT):
                    k.op("pe", lambda e, i=i: e.transpose(out=rowC[:, 256 + i:257 + i], in_=row_sb[0:1, i * 128:(i + 1) * 128],
                                                          identity=C["ident_f"][0:1, 0:1]),
                         r=["row_sb", "ident_f"], w=["rowC"])
                k.op("dve", lambda e, ex=ex: e.tensor_copy(out=idx_all[:, ex, :], in_=rowC[:, 256:256 + NPT]), r=["rowC"], w=["idx%d" % ex])
            def gathers(ex, gb):
                for i in range(NPT):
                    k.dma("pool", lambda e, i=i: e.indirect_dma_start(
                        out=xg[:, i, :], out_offset=None, in_=D["hn"],
                        in_offset=bass.IndirectOffsetOnAxis(ap=idx_all[:, ex, i:i + 1], axis=0)),
                        r=["idx%d" % ex, "hn"], w=["xg%d" % i])
                    k.dma("pool", lambda e, i=i: e.indirect_dma_start(
                        out=ga[gb][:, i, :], out_offset=None, in_=D["aff"],
                        in_offset=bass.IndirectOffsetOnAxis(ap=idx_all[:, ex, i:i + 1], axis=0)),
                        r=["idx%d" % ex, "aff"], w=["ga%d_%d" % (gb, i)])

            gathers(0, 0)
            prev_sc, cur_sc = [], []
            for ex in range(NE):
                gb = ex % 2
                for nm, src in (("g", D["w_gate"]), ("u", D["w_up"]), ("d", D["w_down"])):
                    for hf in range(2):
                        sbuf_ = stg[sti % 2]
                        skey = "stg%d" % (sti % 2)
                        sti += 1
                        k.dma("sp", lambda e, sbuf_=sbuf_, src=src, hf=hf, ex=ex: e.dma_start(
                            out=sbuf_[:], in_=src[l, ex, hf * 512:(hf + 1) * 512, :].rearrange("(kc p) n -> p kc n", p=128)),
                            w=[skey])
                        k.op("act", lambda e, sbuf_=sbuf_, nm=nm, hf=hf: e.activation(
                            out=Wb[nm][:, hf * 4:(hf + 1) * 4, :], in_=sbuf_[:], func=AF.Copy),
                            r=[skey], w=["W" + nm])
                for i in range(NPT):
                    for kc in range(8):
                        k.op("pe", lambda e, i=i, kc=kc: e.transpose(out=psX[:, kc * 128:(kc + 1) * 128],
                                                                     in_=xg[:, i, kc * 128:(kc + 1) * 128],
                                                                     identity=C["ident_b"][:]),
                             r=["xg%d" % i, "ident_b"], w=["psX"])
                    k.op("dve", lambda e, i=i: e.tensor_copy(out=xgT[:, :, i * 128:(i + 1) * 128],
                                                             in_=psX[:].rearrange("p (a t) -> p a t", a=8)),
                         r=["psX"], w=["xgT"])
                if ex + 1 < NE:
                    gathers(ex + 1, 1 - gb)
                for fc in range(8):
                    for (p0, p1) in pchunks:
                        n = p1 - p0
                        if yi % 2 == 0:
                            pG, pU, gkey, ukey = psG, psU, "psG", "psU"
                        else:
                            pG, pU, gkey, ukey = rowA, rowB, "rowA", "rowB"
                        for kc in range(8):
                            k.op("pe", lambda e, fc=fc, kc=kc, p0=p0, p1=p1, n=n, pG=pG: e.matmul(
                                pG[:, 0:n], Wb["g"][:, kc, fc * 128:(fc + 1) * 128], xgT[:, kc, p0:p1],
                                start=(kc == 0), stop=(kc == 7)), r=["Wg", "xgT"], w=[gkey])
                        for kc in range(8):
                            k.op("pe", lambda e, fc=fc, kc=kc, p0=p0, p1=p1, n=n, pU=pU: e.matmul(
                                pU[:, 0:n], Wb["u"][:, kc, fc * 128:(fc + 1) * 128], xgT[:, kc, p0:p1],
                                start=(kc == 0), stop=(kc == 7)), r=["Wu", "xgT"], w=[ukey])
                        s_ = sg[yi % 2]
                        skey = "sg%d" % (yi % 2)
                        yi += 1
                        k.op("act", lambda e, s_=s_, n=n, pG=pG: e.activation(out=s_[:, 0:n], in_=pG[:, 0:n], func=AF.Silu),
                             r=[gkey], w=[skey])
                        k.op("dve", lambda e, s_=s_, n=n, fc=fc, p0=p0, p1=p1, pU=pU: e.tensor_tensor(
                            out=hidT[:, fc, p0:p1], in0=pU[:, 0:n], in1=s_[:, 0:n], op=ALU.mult),
                            r=[ukey, skey], w=["hidT"])
                for i in range(NPT):
                    s = 0 if i < 8 else 1
                    y_ = yt[i % 2]
                    ykey = "yt%d" % (i % 2)
                    for dh in range(2):
                        py = psY[dh]
                        for fc in range(8):
                            k.op("pe", lambda e, i=i, fc=fc, dh=dh, py=py: e.matmul(
                                py[:], hidT[:, fc, i * 128:(i + 1) * 128], Wb["d"][:, fc, dh * 512:(dh + 1) * 512],
                                start=(fc == 0), stop=(fc == 7)), r=["hidT", "Wd"], w=["psY%d" % dh])
                        k.op("dve", lambda e, i=i, dh=dh, py=py, y_=y_, s=s, ex=ex: e.scalar_tensor_tensor(
                            out=y_[:, dh * 512:(dh + 1) * 512], in0=py[:], scalar=ga[gb][:, i, ex:ex + 1],
                            in1=mv[(s, 5)][:, dh * 512:(dh + 1) * 512], op0=ALU.mult, op1=ALU.mult),
                            r=["psY%d" % dh, "ga%d_%d" % (gb, i), "modv"], w=[ykey])
                    for t_ in prev_sc:
                        k._wait("pool", t_)
                    cur_sc.append(k.dma("pool", lambda e, i=i, y_=y_: e.indirect_dma_start(
                        out=D["xres"], out_offset=bass.IndirectOffsetOnAxis(ap=idx_all[:, ex, i:i + 1], axis=0),
                        in_=y_[:], in_offset=None, compute_op=ALU.add),
                        r=[ykey, "idx%d" % ex], w=[]))
                prev_sc, cur_sc = cur_sc, []
            k.barrier()


def phase_final(pb, C):
    k, D = pb.k, pb.D
    with ExitStack() as es:
        g = pb.sb(es, "gfin", [128, 1024], F32)
        k.dma("sp", lambda e: e.dma_start(out=g[:], in_=D["norm_out"].to_broadcast([128, 1024])), w=["gfin"])
        xt = [pb.sb(es, "fx%d" % i, [128, 1024], F32) for i in range(2)]
        ot = [pb.sb(es, "fo%d" % i, [128, 1024], F32) for i in range(2)]
        junk = pb.sb(es, "fjunk", [128, 1024], BF16)
        rstd = [pb.sb(es, "frs%d" % i, [128, 4], F32) for i in range(2)]
        for j in range(64):
            b = j % 2
            r0 = j * 128
            k.dma("sp", lambda e, b=b, r0=r0: e.dma_start(out=xt[b][:], in_=D["xres"][r0:r0 + 128, :]),
                  r=["xres"], w=["fx%d" % b])
            k.op("act", lambda e, b=b: e.activation(out=junk[:], in_=xt[b][:], func=AF.Square, accum_out=rstd[b][:, 0:1]),
                 r=["fx%d" % b], w=["fjunk", "frs%d" % b])
            k.op("act", lambda e, b=b: e.activation(out=rstd[b][:, 1:2], in_=rstd[b][:, 0:1], func=AF.Sqrt, bias=EPS,
                                                    scale=1.0 / DM), r=["frs%d" % b], w=["frs%d" % b])
            k.op("dve", lambda e, b=b: e.reciprocal(out=rstd[b][:, 2:3], in_=rstd[b][:, 1:2]), r=["frs%d" % b], w=["frs%d" % b])
            k.op("dve", lambda e, b=b: e.scalar_tensor_tensor(out=ot[b][:], in0=xt[b][:], scalar=rstd[b][:, 2:3], in1=g[:],
                                                              op0=ALU.mult, op1=ALU.mult),
                 r=["fx%d" % b, "frs%d" % b, "gfin"], w=["fo%d" % b])
            k.dma("sp", lambda e, b=b, r0=r0: e.dma_start(out=D["out"][r0:r0 + 128, :], in_=ot[b][:]),
                  r=["fo%d" % b], w=["out"])
        k.barrier()


NH = 8
ATT_SCALE = 1.0 / float(np.sqrt(192.0))


def attn_host_consts():
    c = {}
    t = np.arange(T_LAT)
    row = (t // 64).astype(np.float32)
    col = (t % 64).astype(np.float32)
    nf = 16
    inv = (10000.0 ** (-np.arange(nf, dtype=np.float32) / nf)).astype(np.float32)
    cosT = np.zeros((64, T_LAT), np.float32)
    sinT = np.zeros((64, T_LAT), np.float32)
    for a, pos in ((0, row), (1, col)):
        ang = (pos[None, :] * inv[:, None]).astype(np.float32)
        for b in range(2):
            d0 = a * 32 + b * 16
            cosT[d0:d0 + 16] = np.cos(ang)
            sinT[d0:d0 + 16] = np.sin(ang) * (-1.0 if b == 0 else 1.0)
    c["cos2"] = np.concatenate([cosT, cosT], 0)
    c["sin2"] = np.concatenate([sinT, sinT], 0)
    return c


def rope_swap_perm():
    d = np.arange(64)
    a, b, f = d // 32, (d // 16) % 2, d % 16
    return a * 32 + (1 - b) * 16 + f


def attn_layout_weights(w_uq, w_ukv, w_kr):
    sp = rope_swap_perm()
    nope = np.concatenate([np.arange(h * 192, h * 192 + 128) for h in range(NH)])
    rope = np.concatenate([np.arange(h * 192 + 128, h * 192 + 192) for h in range(NH)])
    ropes = np.concatenate([h * 192 + 128 + sp for h in range(NH)])
    w_uq_r = np.ascontiguousarray(w_uq[:, np.concatenate([nope, rope, ropes])])
    kn = np.concatenate([np.arange(h * 256, h * 256 + 128) for h in range(NH)])
    vv = np.concatenate([np.arange(h * 256 + 128, h * 256 + 256) for h in range(NH)])
    w_ukv_r = np.ascontiguousarray(w_ukv[:, np.concatenate([kn, vv])])
    w_kr_r = np.ascontiguousarray(np.concatenate([w_kr, w_kr[:, sp]], 1))
    return w_uq_r, w_ukv_r, w_kr_r


def load_w_bf16(pb, es, name, src_ap, kc, n, stg, stgkey):
    k = pb.k
    t = pb.sb(es, name, [128, kc, n], BF16)
    for c0 in range(0, n, 512):
        c1 = min(n, c0 + 512)
        for q in range(kc):
            k.dma("sp", lambda e, q=q, c0=c0, c1=c1: e.dma_start(out=stg[:, 0:c1 - c0], in_=src_ap[q * 128:(q + 1) * 128, c0:c1]),
                  w=[stgkey])
            k.op("dve", lambda e, q=q, c0=c0, c1=c1: e.tensor_copy(out=t[:, q, c0:c1], in_=stg[:, 0:c1 - c0]),
                 r=[stgkey], w=[name])
    return t


def rms_small(pb, ps_ap, n, rstd, rkey, junk, gain_rep, outbf, outkey, pskey):
    k = pb.k
    k.op("act", lambda e: e.activation(out=junk[:, 0:n], in_=ps_ap, func=AF.Square, accum_out=rstd[:, 0:1]),
         r=[pskey], w=["junk", rkey])
    k.op("act", lambda e: e.activation(out=rstd[:, 1:2], in_=rstd[:, 0:1], func=AF.Sqrt, bias=EPS, scale=1.0 / n),
         r=[rkey], w=[rkey])
    k.op("dve", lambda e: e.reciprocal(out=rstd[:, 2:3], in_=rstd[:, 1:2]), r=[rkey], w=[rkey])
    k.op("dve", lambda e: e.scalar_tensor_tensor(out=outbf, in0=ps_ap, scalar=rstd[:, 2:3], in1=gain_rep,
                                                 op0=ALU.mult, op1=ALU.mult), r=[pskey, rkey, "gains"], w=[outkey])


def phase_attn_pre(pb, C):
    k, D, nc = pb.k, pb.D, pb.nc
    l = 1
    with ExitStack() as es:
        mv = load_modv(pb, es, l, (0, 1))
        stg = pb.sb(es, "wstg", [128, 512], F32)
        w_dq = load_w_bf16(pb, es, "w_dq", D["w_dq"], 8, 384, stg, "wstg")
        w_dkv = load_w_bf16(pb, es, "w_dkv", D["w_dkv"], 8, 256, stg, "wstg")
        w_kr = load_w_bf16(pb, es, "w_kr", D["w_kr_r"], 8, 128, stg, "wstg")
        w_uq = load_w_bf16(pb, es, "w_uq", D["w_uq_r"], 3, 2048, stg, "wstg")
        w_ukv = load_w_bf16(pb, es, "w_ukv", D["w_ukv_r"], 2, 2048, stg, "wstg")
        qn_rep = pb.sb(es, "qn_rep", [128, 384], F32)
        kvn_rep = pb.sb(es, "kvn_rep", [128, 256], F32)
        k.dma("sp", lambda e: e.dma_start(out=qn_rep[:], in_=D["q_norm"].to_broadcast([128, 384])), w=["gains"])
        k.dma("sp", lambda e: e.dma_start(out=kvn_rep[:], in_=D["kv_norm"].to_broadcast([128, 256])), w=["gains"])
        xt = [pb.sb(es, "xt%d" % i, [128, 1024], F32) for i in range(2)]
        an = [pb.sb(es, "an%d" % i, [128, 1024], BF16) for i in range(2)]
        junk = pb.sb(es, "junk", [128, 1024], BF16)
        t1 = pb.sb(es, "t1", [128, 1024], F32)
        rstd = [pb.sb(es, "rstd%d" % i, [128, 4], F32) for i in range(2)]
        rs2 = [pb.sb(es, "rs2%d" % i, [128, 4], F32) for i in range(2)]
        aT = pb.sb(es, "aT", [128, 8, 512], BF16)
        cqT = pb.sb(es, "cqT", [128, 3, 512], BF16)
        ckvT = pb.sb(es, "ckvT", [128, 2, 512], BF16)
        cqn = pb.sb(es, "cqn", [128, 384], BF16)
        ckvn = pb.sb(es, "ckvn", [128, 256], BF16)
        cosb = pb.sb(es, "cosb", [128, 512], F32)
        sinb = pb.sb(es, "sinb", [128, 512], F32)
        ostg = [pb.sb(es, "ostg%d" % i, [128, 512], BF16) for i in range(3)]
        vstg = [pb.sb(es, "vstg%d" % i, [128, 1024], BF16) for i in range(2)]
        ra = pb.sb(es, "ra", [128, 512], F32)
        rb = pb.sb(es, "rb", [128, 512], F32)
        psT = pb.ps(es, "psT", [128, 1024], BF16)
        psS = pb.ps(es, "psS", [128, 512], F32)
        psA = pb.ps(es, "psA", [128, 512], F32)
        psB = pb.ps(es, "psB", [128, 512], F32)
        psV = [pb.ps(es, "psV%d" % i, [128, 512], F32) for i in range(2)]
        oi = 0
        vi = 0
        xi = 0
        supers = [(i * 512, 4, 0) for i in range(16)] + [(T_LAT, 2, 1)]
        for (t0, ntile, s) in supers:
            nt = ntile * 128
            is_lat = (s == 0)
            for tl in range(ntile):
                b = xi % 2
                xi += 1
                r0 = t0 + tl * 128
                k.dma("sp", lambda e, b=b, r0=r0: e.dma_start(out=xt[b][:], in_=D["xres"][r0:r0 + 128, :]),
                      r=["xres"], w=["xt%d" % b])
                norm_mod_tile(pb, xt[b], "xt%d" % b, rstd[b], "rstd%d" % b, junk, t1, mv[(s, 1)], mv[(s, 0)], an[b], "an%d" % b)
                for kc in range(8):
                    k.op("pe", lambda e, b=b, kc=kc: e.transpose(out=psT[:, kc * 128:(kc + 1) * 128],
                                                                 in_=an[b][:, kc * 128:(kc + 1) * 128], identity=C["ident_b"][:]),
                         r=["an%d" % b, "ident_b"], w=["psT"])
                k.op("act", lambda e, tl=tl: e.activation(out=aT[:, :, tl * 128:(tl + 1) * 128],
                                                          in_=psT[:].rearrange("p (a t) -> p a t", a=8), func=AF.Copy),
                     r=["psT"], w=["aT"])
                if is_lat:
                    for kc in range(8):
                        k.op("pe", lambda e, kc=kc, tl=tl: e.matmul(psS[:, 0:384], aT[:, kc, tl * 128:(tl + 1) * 128], w_dq[:, kc, :],
                                                                    start=(kc == 0), stop=(kc == 7)), r=["aT", "w_dq"], w=["psS"])
                    rms_small(pb, psS[:, 0:384], 384, rs2[b], "rs2%d" % b, junk, qn_rep[:], cqn[:], "cqn", "psS")
                    for c in range(3):
                        k.op("pe", lambda e, c=c: e.transpose(out=psT[:, c * 128:(c + 1) * 128], in_=cqn[:, c * 128:(c + 1) * 128],
                                                              identity=C["ident_b"][:]), r=["cqn", "ident_b"], w=["psT"])
                    k.op("act", lambda e, tl=tl: e.activation(out=cqT[:, :, tl * 128:(tl + 1) * 128],
                                                              in_=psT[:, 0:384].rearrange("p (a t) -> p a t", a=3), func=AF.Copy),
                         r=["psT"], w=["cqT"])
                for kc in range(8):
                    k.op("pe", lambda e, kc=kc, tl=tl: e.matmul(psS[:, 0:256], aT[:, kc, tl * 128:(tl + 1) * 128], w_dkv[:, kc, :],
                                                                start=(kc == 0), stop=(kc == 7)), r=["aT", "w_dkv"], w=["psS"])
                rms_small(pb, psS[:, 0:256], 256, rs2[b], "rs2%d" % b, junk, kvn_rep[:], ckvn[:], "ckvn", "psS")
                for c in range(2):
                    k.op("pe", lambda e, c=c: e.transpose(out=psT[:, c * 128:(c + 1) * 128], in_=ckvn[:, c * 128:(c + 1) * 128],
                                                          identity=C["ident_b"][:]), r=["ckvn", "ident_b"], w=["psT"])
                k.op("act", lambda e, tl=tl: e.activation(out=ckvT[:, :, tl * 128:(tl + 1) * 128],
                                                          in_=psT[:, 0:256].rearrange("p (a t) -> p a t", a=2), func=AF.Copy),
                     r=["psT"], w=["ckvT"])
                v_ = vstg[vi % 2]
                vkey = "vstg%d" % (vi % 2)
                vi += 1
                for dh in range(2):
                    for c in range(2):
                        k.op("pe", lambda e, c=c, dh=dh, tl=tl: e.matmul(
                            psV[dh][:], ckvT[:, c, tl * 128:(tl + 1) * 128], w_ukv[:, c, 1024 + dh * 512:1024 + (dh + 1) * 512],
                            start=(c == 0), stop=(c == 1)), r=["ckvT", "w_ukv"], w=["psV%d" % dh])
                    k.op("act", lambda e, dh=dh, v_=v_: e.activation(out=v_[:, dh * 512:(dh + 1) * 512], in_=psV[dh][:], func=AF.Copy),
                         r=["psV%d" % dh], w=[vkey])
                k.dma("sp", lambda e, v_=v_, r0=r0: e.dma_start(out=D["Vd"][r0:r0 + 128, :], in_=v_[:]), r=[vkey], w=["Vd"])
            if is_lat:
                k.dma("sp", lambda e, t0=t0: e.dma_start(out=cosb[:], in_=D["c_cos2"][:, t0:t0 + 512]), w=["cosb"])
                k.dma("sp", lambda e, t0=t0: e.dma_start(out=sinb[:], in_=D["c_sin2"][:, t0:t0 + 512]), w=["sinb"])
            for h in range(NH):
                for c in range(2):
                    k.op("pe", lambda e, c=c, h=h: e.matmul(psA[:, 0:nt], w_ukv[:, c, h * 128:(h + 1) * 128], ckvT[:, c, 0:nt],
                                                            start=(c == 0), stop=(c == 1)), r=["ckvT", "w_ukv"], w=["psA"])
                o_ = ostg[oi % 3]
                okey = "ostg%d" % (oi % 3)
                oi += 1
                k.op("act", lambda e, o_=o_: e.activation(out=o_[:, 0:nt], in_=psA[:, 0:nt], func=AF.Copy), r=["psA"], w=[okey])
                k.dma("sp", lambda e, o_=o_, h=h, t0=t0: e.dma_start(out=D["KT"][h, :, t0:t0 + nt], in_=o_[:, 0:nt]),
                      r=[okey], w=["KT"])
            for kc in range(8):
                k.op("pe", lambda e, kc=kc: e.matmul(psA[0:64, 0:nt], w_kr[:, kc, 0:64], aT[:, kc, 0:nt],
                                                     start=(kc == 0), stop=(kc == 7)), r=["aT", "w_kr"], w=["psA"])
            o_ = ostg[oi % 3]
            okey = "ostg%d" % (oi % 3)
            oi += 1
            if is_lat:
                for kc in range(8):
                    k.op("pe", lambda e, kc=kc: e.matmul(psB[0:64, 0:nt], w_kr[:, kc, 64:128], aT[:, kc, 0:nt],
                                                         start=(kc == 0), stop=(kc == 7)), r=["aT", "w_kr"], w=["psB"])
                k.op("dve", lambda e: e.tensor_tensor(out=ra[0:64, :], in0=psA[0:64, :], in1=cosb[0:64, :], op=ALU.mult),
                     r=["psA", "cosb"], w=["ra"])
                k.op("dve", lambda e: e.tensor_tensor(out=rb[0:64, :], in0=psB[0:64, :], in1=sinb[0:64, :], op=ALU.mult),
                     r=["psB", "sinb"], w=["rb"])
                k.op("dve", lambda e, o_=o_: e.tensor_tensor(out=o_[0:64, :], in0=ra[0:64, :], in1=rb[0:64, :], op=ALU.add),
                     r=["ra", "rb"], w=[okey])
            else:
                k.op("act", lambda e, o_=o_: e.activation(out=o_[0:64, 0:nt], in_=psA[0:64, 0:nt], func=AF.Copy), r=["psA"], w=[okey])
            k.dma("sp", lambda e, o_=o_, t0=t0: e.dma_start(out=D["KRT"][:, t0:t0 + nt], in_=o_[0:64, 0:nt]), r=[okey], w=["KRT"])
            if not is_lat:
                continue
            for h in range(NH):
                for c in range(3):
                    k.op("pe", lambda e, c=c, h=h: e.matmul(psA[:], w_uq[:, c, h * 128:(h + 1) * 128], cqT[:, c, :],
                                                            start=(c == 0), stop=(c == 2)), r=["cqT", "w_uq"], w=["psA"])
                o_ = ostg[oi % 3]
                okey = "ostg%d" % (oi % 3)
                oi += 1
                k.op("act", lambda e, o_=o_: e.activation(out=o_[:], in_=psA[:], func=AF.Copy), r=["psA"], w=[okey])
                k.dma("sp", lambda e, o_=o_, h=h, t0=t0: e.dma_start(out=D["QT"][h, :, t0:t0 + 512], in_=o_[:]), r=[okey], w=["QT"])
            for hp in range(4):
                for c in range(3):
                    k.op("pe", lambda e, c=c, hp=hp: e.matmul(psA[:], w_uq[:, c, 1024 + hp * 128:1024 + (hp + 1) * 128], cqT[:, c, :],
                                                              start=(c == 0), stop=(c == 2)), r=["cqT", "w_uq"], w=["psA"])
                for c in range(3):
                    k.op("pe", lambda e, c=c, hp=hp: e.matmul(psB[:], w_uq[:, c, 1536 + hp * 128:1536 + (hp + 1) * 128], cqT[:, c, :],
                                                              start=(c == 0), stop=(c == 2)), r=["cqT", "w_uq"], w=["psB"])
                o_ = ostg[oi % 3]
                okey = "ostg%d" % (oi % 3)
                oi += 1
                k.op("dve", lambda e: e.tensor_tensor(out=ra[:], in0=psA[:], in1=cosb[:], op=ALU.mult), r=["psA", "cosb"], w=["ra"])
                k.op("dve", lambda e: e.tensor_tensor(out=rb[:], in0=psB[:], in1=sinb[:], op=ALU.mult), r=["psB", "sinb"], w=["rb"])
                k.op("dve", lambda e, o_=o_: e.tensor_tensor(out=o_[:], in0=ra[:], in1=rb[:], op=ALU.add), r=["ra", "rb"], w=[okey])
                k.dma("sp", lambda e, o_=o_, hp=hp, t0=t0: e.dma_start(
                    out=D["QRT"][2 * hp:2 * hp + 2, :, t0:t0 + 512].rearrange("h d t -> (h d) t"), in_=o_[:]), r=[okey], w=["QRT"])
        k.barrier()


def phase_attn_main(pb, C, heads=range(NH), qblocks=range(16)):
    k, D, nc = pb.k, pb.D, pb.nc
    NKB = NTOK // 128
    with ExitStack() as es:
        KRT = pb.sb(es, "KRT", [128, NTOK], BF16)
        k.op("pool", lambda e: e.memset(KRT[64:128, :], 0.0), w=["sKRT"])
        k.dma("sp", lambda e: e.dma_start(out=KRT[0:64, :], in_=D["KRT"]), r=["KRT"], w=["sKRT"])
        KT = [pb.sb(es, "KT%d" % i, [128, NTOK], BF16) for i in range(2)]
        Vh = [pb.sb(es, "Vh%d" % i, [128, NKB, 130], BF16) for i in range(2)]
        for i in range(2):
            k.op("dve", lambda e, i=i: e.memset(Vh[i][:, :, 128:130], 1.0), w=["Vh%d" % i])
        Qb = [pb.sb(es, "Qb%d" % i, [128, 512], BF16) for i in range(2)]
        QRb = [pb.sb(es, "QRb%d" % i, [128, 512], BF16) for i in range(2)]
        for i in range(2):
            k.op("pool", lambda e, i=i: e.memset(QRb[i][64:128, :], 0.0), w=["QRb%d" % i])
        PT = [pb.sb(es, "PT%d" % i, [128, 512], BF16) for i in range(4)]
        Ost = [pb.sb(es, "Ost%d" % i, [128, 4, 128], BF16) for i in range(2)]
        rinv = pb.sb(es, "rinv", [128, 8], F32)
        psS = [pb.ps(es, "psS%d" % i, [128, 512], F32) for i in range(3)]
        psO = [pb.ps(es, "psO%d" % i, [128, 512], F32) for i in range(4)]
        qi = 0
        pi = 0
        si = 0
        for hi, h in enumerate(heads):
            hb = hi % 2
            k.dma("sp", lambda e, hb=hb, h=h: e.dma_start(out=KT[hb][:], in_=D["KT"][h]), r=["KT"], w=["KT%d" % hb])
            k.dma("sp", lambda e, hb=hb, h=h: e.dma_start(
                out=Vh[hb][:, :, 0:128], in_=D["Vd"][:, h * 128:(h + 1) * 128].rearrange("(kb p) v -> p kb v", p=128)),
                r=["Vd"], w=["Vh%d" % hb])
            for qb in qblocks:
                b = qi % 2
                qi += 1
                q0 = qb * 512
                k.dma("sp", lambda e, b=b, h=h, q0=q0: e.dma_start(out=Qb[b][:], in_=D["QT"][h, :, q0:q0 + 512]),
                      r=["QT"], w=["Qb%d" % b])
                k.dma("sp", lambda e, b=b, h=h, q0=q0: e.dma_start(out=QRb[b][0:64, :], in_=D["QRT"][h, :, q0:q0 + 512]),
                      r=["QRT"], w=["QRb%d" % b])

                def qk(kb, sb_):
                    k.op("pe", lambda e: e.matmul(psS[sb_][:], KT[hb][:, kb * 128:(kb + 1) * 128], Qb[b][:], start=True, stop=False),
                         r=["KT%d" % hb, "Qb%d" % b], w=["psS%d" % sb_])
                    k.op("pe", lambda e: e.matmul(psS[sb_][:], KRT[:, kb * 128:(kb + 1) * 128], QRb[b][:], start=False, stop=True),
                         r=["sKRT", "QRb%d" % b], w=["psS%d" % sb_])

                sbs = []
                for pre in range(2):
                    sbs.append(si % 3)
                    qk(pre, si % 3)
                    si += 1
                for kb in range(NKB):
                    if kb + 2 < NKB:
                        sbs.append(si % 3)
                        qk(kb + 2, si % 3)
                        si += 1
                    sb_ = sbs[kb]
                    p_ = PT[pi % 4]
                    pkey = "PT%d" % (pi % 4)
                    pi += 1
                    k.op("act", lambda e, sb_=sb_, p_=p_: e.activation(out=p_[:], in_=psS[sb_][:], func=AF.Exp, scale=ATT_SCALE),
                         r=["psS%d" % sb_], w=[pkey])
                    for qt in range(4):
                        k.op("pe", lambda e, qt=qt, p_=p_, kb=kb: e.matmul(
                            psO[qt][:, 0:130], p_[:, qt * 128:(qt + 1) * 128], Vh[hb][:, kb, :],
                            start=(kb == 0), stop=(kb == NKB - 1)), r=[pkey, "Vh%d" % hb], w=["psO%d" % qt])
                o_ = Ost[b]
                okey = "Ost%d" % b
                for qt in range(4):
                    k.op("dve", lambda e, qt=qt: e.reciprocal(out=rinv[:, qt:qt + 1], in_=psO[qt][:, 128:129]),
                         r=["psO%d" % qt], w=["rinv"])
                    k.op("dve", lambda e, qt=qt, o_=o_: e.tensor_scalar(out=o_[:, qt, :], in0=psO[qt][:, 0:128],
                                                                        scalar1=rinv[:, qt:qt + 1], scalar2=None, op0=ALU.mult),
                         r=["psO%d" % qt, "rinv"], w=[okey])
                k.dma("sp", lambda e, o_=o_, h=h, q0=q0: e.dma_start(
                    out=D["Od"][q0:q0 + 512, h * 128:(h + 1) * 128].rearrange("(qt p) v -> p qt v", p=128), in_=o_[:]),
                    r=[okey], w=["Od"])
        k.barrier()


def phase_attn_post(pb, C):
    k, D, nc = pb.k, pb.D, pb.nc
    with ExitStack() as es:
        mv = load_modv(pb, es, 1, (2,))
        stg = pb.sb(es, "wstg", [128, 512], F32)
        w_o = load_w_bf16(pb, es, "w_o", D["w_o"], 8, 1024, stg, "wstg")
        ot = [pb.sb(es, "ot%d" % i, [128, 1024], BF16) for i in range(2)]
        oT = [pb.sb(es, "oT%d" % i, [128, 8, 128], BF16) for i in range(2)]
        xt = [pb.sb(es, "xt%d" % i, [128, 1024], F32) for i in range(2)]
        tm = [pb.sb(es, "tm%d" % i, [128, 1024], F32) for i in range(2)]
        psT = pb.ps(es, "psT", [128, 1024], BF16)
        psY = [pb.ps(es, "psY%d" % i, [128, 512], F32) for i in range(2)]
        for j in range(64):
            b = j % 2
            r0 = j * 128
            k.dma("sp", lambda e, b=b, r0=r0: e.dma_start(out=ot[b][:], in_=D["Od"][r0:r0 + 128, :]), r=["Od"], w=["ot%d" % b])
            k.dma("sp", lambda e, b=b, r0=r0: e.dma_start(out=xt[b][:], in_=D["xres"][r0:r0 + 128, :]), r=["xres"], w=["xt%d" % b])
            for c in range(8):
                k.op("pe", lambda e, b=b, c=c: e.transpose(out=psT[:, c * 128:(c + 1) * 128], in_=ot[b][:, c * 128:(c + 1) * 128],
                                                           identity=C["ident_b"][:]), r=["ot%d" % b, "ident_b"], w=["psT"])
            k.op("act", lambda e, b=b: e.activation(out=oT[b][:].rearrange("p a t -> p (a t)"), in_=psT[:], func=AF.Copy),
                 r=["psT"], w=["oT%d" % b])
            for dh in range(2):
                for c in range(8):
                    k.op("pe", lambda e, b=b, c=c, dh=dh: e.matmul(psY[dh][:], oT[b][:, c, :], w_o[:, c, dh * 512:(dh + 1) * 512],
                                                                   start=(c == 0), stop=(c == 7)), r=["oT%d" % b, "w_o"], w=["psY%d" % dh])
                k.op("dve", lambda e, b=b, dh=dh: e.tensor_tensor(out=tm[b][:, dh * 512:(dh + 1) * 512], in0=psY[dh][:],
                                                                  in1=mv[(0, 2)][:, dh * 512:(dh + 1) * 512], op=ALU.mult),
                     r=["psY%d" % dh, "modv"], w=["tm%d" % b])
            k.op("pool", lambda e, b=b: e.tensor_tensor(out=tm[b][:], in0=tm[b][:], in1=xt[b][:], op=ALU.add),
                 r=["tm%d" % b, "xt%d" % b], w=["tm%d" % b])
            k.dma("sp", lambda e, b=b, r0=r0: e.dma_start(out=D["xres"][r0:r0 + 128, :], in_=tm[b][:]), r=["tm%d" % b], w=["xres"])
        k.barrier()


def load_cols(pb, es, C, name, src_rows_ap, n, psbank, pskey):
    k = pb.k
    rows = pb.sb(es, name + "_r", [n, 128], F32)
    cols = pb.sb(es, name, [128, n], F32)
    k.dma("sp", lambda e: e.dma_start(out=rows[:], in_=src_rows_ap), w=[name + "_r"])
    k.op("pe", lambda e: e.transpose(out=psbank[:, 0:n], in_=rows[:], identity=C["ident_f"][0:n, 0:n]),
         r=[name + "_r", "ident_f"], w=[pskey])
    k.op("dve", lambda e: e.tensor_copy(out=cols[:], in_=psbank[:, 0:n]), r=[pskey], w=[name])
    return cols


def phase_mix_pass(pb, C, dirn, nsup_lat=32, do_ctx=True):
    k, D, nc = pb.k, pb.D, pb.nc
    W = 256
    with ExitStack() as es:
        P = [pb.ps(es, "P%d" % i, [128, 512], F32) for i in range(8)]
        PK = ["P%d" % i for i in range(8)]
        P0b = P[0][:].bitcast(BF16)
        mv = load_modv(pb, es, 0, (0, 1))
        stg = pb.sb(es, "wstg", [128, 512], F32)
        win = D["w_in"]
        wq = load_w_bf16(pb, es, "wq", win[:, 0:512], 8, 512, stg, "wstg")
        wf = load_w_bf16(pb, es, "wf", win[:, 512 + 512 * dirn:1024 + 512 * dirn], 8, 512, stg, "wstg")
        wi = load_w_bf16(pb, es, "wi", win[:, 1536:2048], 8, 512, stg, "wstg")
        wx = load_w_bf16(pb, es, "wx", win[:, 3072:4096], 8, 1024, stg, "wstg")
        wdt = load_w_bf16(pb, es, "wdt", win[:, 4096 + 8 * dirn:4104 + 8 * dirn], 8, 8, stg, "wstg")
        lg0 = load_cols(pb, es, C, "lg0", D["lb_gamma"][0, dirn, :].rearrange("(c p) -> c p", p=128), 4, P[7], PK[7])
        lg1 = load_cols(pb, es, C, "lg1", D["lb_gamma"][1, dirn, :].rearrange("(c p) -> c p", p=128), 4, P[7], PK[7])
        lbc = pb.sb(es, "lbc", [128, 4], F32)
        oml = pb.sb(es, "oml", [128, 4], F32)
        k.op("dve", lambda e: e.tensor_tensor(out=lbc[:], in0=lg0[:], in1=lg1[:], op=ALU.subtract), r=["lg0", "lg1"], w=["lbc"])
        k.op("act", lambda e: e.activation(out=lbc[:], in_=lbc[:], func=AF.Sigmoid), r=["lbc"], w=["lbc"])
        k.op("dve", lambda e: e.tensor_scalar(out=oml[:], in0=lbc[:], scalar1=-1.0, scalar2=1.0, op0=ALU.mult, op1=ALU.add),
             r=["lbc"], w=["oml"])
        cw = [load_cols(pb, es, C, "cw%d" % j, D["conv_w"][j, :].rearrange("(c p) -> c p", p=128), 8, P[7], PK[7]) for j in range(5)]
        cbc = load_cols(pb, es, C, "cbc", D["conv_b"].rearrange("o (c p) -> (o c) p", p=128), 8, P[7], PK[7])
        dtb = pb.sb(es, "dtb", [128, 8], F32)
        negA = pb.sb(es, "negA", [128, 8], F32)
        k.dma("sp", lambda e: e.dma_start(out=dtb[:], in_=D["dt_bias"][dirn:dirn + 1, :].to_broadcast([128, 8])), w=["dtb"])
        k.dma("sp", lambda e: e.dma_start(out=negA[:], in_=D["a_log"][dirn:dirn + 1, :].to_broadcast([128, 8])), w=["negA"])
        k.op("act", lambda e: e.activation(out=negA[:], in_=negA[:], func=AF.Exp), r=["negA"], w=["negA"])
        k.op("dve", lambda e: e.tensor_scalar(out=negA[:], in0=negA[:], scalar1=-1.0, scalar2=None, op0=ALU.mult), r=["negA"], w=["negA"])
        dsk = pb.sb(es, "dsk", [128, 8], F32)
        k.dma("sp", lambda e: e.dma_start(out=dsk[:], in_=D["d_skip"].to_broadcast([128, 8])), w=["dsk"])
        if getattr(pb, 'stop', 99) == 0:
            k.barrier()
            return
        if dirn == 0:
            m_att_b, m_att_f = C["triu_incl_b"], C["triu_incl_f"]
            m_cum, m_wend, m_lh = C["triu_incl_f"], C["tril_strict_f"], C["tril_strict_f"]
            m_rhs = C["triu_incl_f"]
        else:
            m_att_b, m_att_f = C["tril_incl_b"], C["tril_incl_f"]
            m_cum, m_wend, m_lh = C["tril_incl_f"], C["triu_strict_f"], C["triu_strict_f"]
            m_rhs = C["tril_incl_f"]
        xt = [pb.sb(es, "xt%d" % i, [128, 1024], F32) for i in range(2)]
        an = [pb.sb(es, "an%d" % i, [128, 1024], BF16) for i in range(2)]
        hx = pb.sb(es, "hx", [4, 1024], F32)
        ah = pb.sb(es, "ah", [4, 1024], BF16)
        junk = pb.sb(es, "junk", [128, 1024], BF16)
        t1 = pb.sb(es, "t1", [128, 1024], F32)
        rstd = [pb.sb(es, "rstd%d" % i, [128, 4], F32) for i in range(3)]
        aT = pb.sb(es, "aT", [128, 8, W + 4], BF16)
        Qsil = pb.sb(es, "Qsil", [128, 4, W], F32)
        fg = pb.sb(es, "fg", [128, 4, W], F32)
        la = pb.sb(es, "la", [128, 4, W], F32)
        kk = pb.sb(es, "kk", [128, 4, W], F32)
        u = pb.sb(es, "u", [128, 8, W + 4], F32)
        acc = pb.sb(es, "acc", [128, 8, W], F32)
        tmp = pb.sb(es, "tmp", [128, 8, W], F32)
        xbc = pb.sb(es, "xbc", [128, 8, W], BF16)
        vtok = pb.sb(es, "vtok", [128, 2, 512], BF16)
        dtr = pb.sb(es, "dtr", [128, 2, 8], F32)
        bb = pb.sb(es, "bb", [128, 4, 128], F32)
        b2 = pb.sb(es, "b2", [128, 4, 128], F32)
        sc = pb.sb(es, "sc", [128, 3, 4], F32)
        esc = pb.sb(es, "esc", [128, 3, 4], F32)
        nb = pb.sb(es, "nb", [128, 4], F32)
        Eq = pb.sb(es, "Eq", [128, 4, 128], F32)
        Ek = pb.sb(es, "Ek", [128, 4, 128], F32)
        Qt = pb.sb(es, "Qt", [128, 4, 128], BF16)
        Kt = pb.sb(es, "Kt", [128, 4, 128], BF16)
        Ktok = pb.sb(es, "Ktok", [128, 4, 128], BF16)
        attm = pb.sb(es, "attm", [128, 8, 128], BF16)
        Sh = pb.sb(es, "Sh", [128, 4, 64], F32)
        ShpA = pb.sb(es, "ShpA", [128, 4, 64], BF16)
        ShpB = pb.sb(es, "ShpB", [128, 4, 64], BF16)
        KtA = pb.sb(es, "KtA", [128, 4, 128], BF16)
        KtB = pb.sb(es, "KtB", [128, 4, 128], BF16)
        for nm_, t_ in (("ShpA", ShpA), ("ShpB", ShpB), ("KtA", KtA), ("KtB", KtB)):
            k.op("dve", lambda e, t_=t_: e.memset(t_[:], 0.0), w=[nm_])
        tU = pb.sb(es, "tU", [128, 4, 64], F32)
        osb = pb.sb(es, "osb", [128, 512], F32)
        xmt = pb.sb(es, "xmt", [128, 8, 64], BF16)
        Btok = pb.sb(es, "Btok", [128, 2, 128], BF16)
        dts = pb.sb(es, "dts", [128, 4, 8], F32)
        ec = pb.sb(es, "ec", [128, 24], F32)
        LH = pb.sb(es, "LH", [128, 8, 128], F32)
        Em = pb.sb(es, "Em", [128, 8, 128], F32)
        cbm = pb.sb(es, "cbm", [128, 2, 128], F32)
        Mm = pb.sb(es, "Mm", [128, 8, 128], BF16)
        xdt = pb.sb(es, "xdt", [128, 8, 64], BF16)
        xdtw = pb.sb(es, "xdtw", [128, 8, 64], BF16)
        yi = pb.sb(es, "yi", [128, 8, 64], F32)
        ysb = pb.sb(es, "ysb", [128, 8, 64], F32)
        Ss = pb.sb(es, "Ss", [128, 8, 64], F32)
        Ssb = pb.sb(es, "Ssb", [128, 8, 64], BF16)
        k.op("dve", lambda e: e.memset(Sh[:], 0.0), w=["Sh"])
        k.op("dve", lambda e: e.memset(Ss[:], 0.0), w=["Ss"])
        k.op("dve", lambda e: e.memset(Ssb[:], 0.0), w=["Ssb"])

        supers = []
        if do_ctx:
            supers.append((1, 0))
        lat_order = list(range(nsup_lat)) if dirn == 0 else list(range(nsup_lat - 1, -1, -1))
        supers += [(0, i) for i in lat_order]
        bank_rot = [1, 2, 3]
        bri = 0
        xi = 0
        for (s, si) in supers:
            src = D["x"] if s == 0 else D["ctx"]
            Ts = T_LAT if s == 0 else T_CTX
            r0 = si * W
            g0 = r0 if s == 0 else T_LAT + r0
            for tl in range(2):
                b = xi % 2
                xi += 1
                rr = r0 + tl * 128
                k.dma("sp", lambda e, b=b, rr=rr: e.dma_start(out=xt[b][:], in_=src[rr:rr + 128, :]), w=["xt%d" % b])
                norm_mod_tile(pb, xt[b], "xt%d" % b, rstd[b], "rstd%d" % b, junk, t1, mv[(s, 1)], mv[(s, 0)], an[b], "an%d" % b)
                for kc in range(8):
                    k.op("pe", lambda e, b=b, kc=kc: e.transpose(out=P0b[:, kc * 128:(kc + 1) * 128],
                                                                 in_=an[b][:, kc * 128:(kc + 1) * 128], identity=C["ident_b"][:]),
                         r=["an%d" % b, "ident_b"], w=[PK[0]])
                k.op("act", lambda e, tl=tl: e.activation(out=aT[:, :, 2 + tl * 128:2 + (tl + 1) * 128],
                                                          in_=P0b.rearrange("p (a t) -> p a t", a=8), func=AF.Copy),
                     r=[PK[0]], w=["aT"])
            if getattr(pb, 'stop', 99) == 1:
                k.barrier()
                return
            has_l = r0 >= 2
            has_r = r0 + W + 2 <= Ts
            lrow = r0 - 2 if has_l else 0
            rrow = r0 + W if has_r else Ts - 2
            k.dma("sp", lambda e: e.dma_start(out=hx[0:2, :], in_=src[lrow:lrow + 2, :]), w=["hx"])
            k.dma("sp", lambda e: e.dma_start(out=hx[2:4, :], in_=src[rrow:rrow + 2, :]), w=["hx"])
            k.op("act", lambda e: e.activation(out=junk[0:4, :], in_=hx[:], func=AF.Square, accum_out=rstd[2][0:4, 0:1]),
                 r=["hx"], w=["junk", "rstd2"])
            k.op("act", lambda e: e.activation(out=rstd[2][0:4, 1:2], in_=rstd[2][0:4, 0:1], func=AF.Sqrt, bias=EPS, scale=1.0 / DM),
                 r=["rstd2"], w=["rstd2"])
            k.op("dve", lambda e: e.reciprocal(out=rstd[2][0:4, 2:3], in_=rstd[2][0:4, 1:2]), r=["rstd2"], w=["rstd2"])
            k.op("dve", lambda e: e.scalar_tensor_tensor(out=t1[0:4, :], in0=hx[:], scalar=rstd[2][0:4, 2:3], in1=mv[(s, 1)][0:4, :],
                                                         op0=ALU.mult, op1=ALU.mult), r=["hx", "rstd2", "modv"], w=["t1"])
            k.op("pool", lambda e: e.tensor_tensor(out=ah[:], in0=t1[0:4, :], in1=mv[(s, 0)][0:4, :], op=ALU.add),
                 r=["t1", "modv"], w=["ah"])
            for kc in range(8):
                k.op("pe", lambda e, kc=kc: e.transpose(out=P0b[:, kc * 4:(kc + 1) * 4], in_=ah[:, kc * 128:(kc + 1) * 128],
                                                        identity=C["ident_b"][0:4, 0:4]), r=["ah", "ident_b"], w=[PK[0]])
            hv = P0b[:, 0:32].rearrange("p (a t) -> p a t", a=8)
            if has_l:
                k.op("dve", lambda e: e.tensor_copy(out=aT[:, :, 0:2], in_=hv[:, :, 0:2]), r=[PK[0]], w=["aT"])
            else:
                k.op("dve", lambda e: e.memset(aT[:, :, 0:2], 0.0), r=[PK[0]], w=["aT"])
            if has_r:
                k.op("dve", lambda e: e.tensor_copy(out=aT[:, :, W + 2:W + 4], in_=hv[:, :, 2:4]), r=[PK[0]], w=["aT"])
            else:
                k.op("dve", lambda e: e.memset(aT[:, :, W + 2:W + 4], 0.0), r=[PK[0]], w=["aT"])
            if getattr(pb, 'stop', 99) == 2:
                k.barrier()
                return
            for c in range(4):
                bk = bank_rot[bri % 3]
                bri += 1
                for kc in range(8):
                    k.op("pe", lambda e, c=c, kc=kc, bk=bk: e.matmul(P[bk][:, 0:W], wq[:, kc, c * 128:(c + 1) * 128], aT[:, kc, 2:W + 2],
                                                                     start=(kc == 0), stop=(kc == 7)), r=["wq", "aT"], w=[PK[bk]])
                k.op("act", lambda e, c=c, bk=bk: e.activation(out=Qsil[:, c, :], in_=P[bk][:, 0:W], func=AF.Silu), r=[PK[bk]], w=["Qsil"])
            for c in range(4):
                bk = bank_rot[bri % 3]
                bri += 1
                for kc in range(8):
                    k.op("pe", lambda e, c=c, kc=kc, bk=bk: e.matmul(P[bk][:, 0:W], wf[:, kc, c * 128:(c + 1) * 128], aT[:, kc, 2:W + 2],
                                                                     start=(kc == 0), stop=(kc == 7)), r=["wf", "aT"], w=[PK[bk]])
                k.op("act", lambda e, c=c, bk=bk: e.activation(out=fg[:, c, :], in_=P[bk][:, 0:W], func=AF.Sigmoid), r=[PK[bk]], w=["fg"])
                k.op("dve", lambda e, c=c: e.tensor_scalar(out=fg[:, c, :], in0=fg[:, c, :], scalar1=oml[:, c:c + 1], scalar2=lbc[:, c:c + 1],
                                                           op0=ALU.mult, op1=ALU.add), r=["fg", "oml", "lbc"], w=["fg"])
            k.op("act", lambda e: e.activation(out=la[:], in_=fg[:], func=AF.Ln), r=["fg"], w=["la"])
            k.op("dve", lambda e: e.tensor_scalar(out=kk[:], in0=fg[:], scalar1=-1.0, scalar2=1.0, op0=ALU.mult, op1=ALU.add),
                 r=["fg"], w=["kk"])
            for c in range(8):
                bk = bank_rot[bri % 3]
                bri += 1
                for kc in range(8):
                    k.op("pe", lambda e, c=c, kc=kc, bk=bk: e.matmul(P[bk][:, 0:W + 4], wx[:, kc, c * 128:(c + 1) * 128], aT[:, kc, :],
                                                                     start=(kc == 0), stop=(kc == 7)), r=["wx", "aT"], w=[PK[bk]])
                k.op("act", lambda e, c=c, bk=bk: e.activation(out=u[:, c, :], in_=P[bk][:, 0:W + 4], func=AF.Copy), r=[PK[bk]], w=["u"])
            if getattr(pb, 'stop', 99) == 3:
                k.barrier()
                return
            for j in range(5):
                if j == 0:
                    k.op("dve", lambda e, j=j: e.tensor_tensor(out=acc[:], in0=u[:, :, j:j + W],
                                                               in1=cw[j][:].unsqueeze(2).to_broadcast([128, 8, W]), op=ALU.mult),
                         r=["u", "cw0"], w=["acc"])
                else:
                    k.op("pool", lambda e, j=j: e.tensor_tensor(out=tmp[:], in0=u[:, :, j:j + W],
                                                                in1=cw[j][:].unsqueeze(2).to_broadcast([128, 8, W]), op=ALU.mult),
                         r=["u", "cw%d" % j], w=["tmp"])
                    k.op("dve", lambda e: e.tensor_tensor(out=acc[:], in0=acc[:], in1=tmp[:], op=ALU.add), r=["acc", "tmp"], w=["acc"])
            k.op("dve", lambda e: e.tensor_tensor(out=acc[:], in0=acc[:], in1=cbc[:].unsqueeze(2).to_broadcast([128, 8, W]), op=ALU.add),
                 r=["acc", "cbc"], w=["acc"])
            k.op("act", lambda e: e.activation(out=xbc[:], in_=acc[:], func=AF.Silu), r=["acc"], w=["xbc"])
            if getattr(pb, 'stop', 99) == 4:
                k.barrier()
                return
            for tl in range(2):
                sl0 = 2 + tl * 128
                for kc in range(8):
                    k.op("pe", lambda e, kc=kc, sl0=sl0: e.matmul(P[6][:], aT[:, kc, sl0:sl0 + 128], wi[:, kc, :],
                                                                  start=(kc == 0), stop=(kc == 7)), r=["aT", "wi"], w=[PK[6]])
                k.op("act", lambda e, tl=tl: e.activation(out=vtok[:, tl, :], in_=P[6][:], func=AF.Copy), r=[PK[6]], w=["vtok"])
                for kc in range(8):
                    k.op("pe", lambda e, kc=kc, sl0=sl0: e.matmul(P[7][:, 0:8], aT[:, kc, sl0:sl0 + 128], wdt[:, kc, :],
                                                                  start=(kc == 0), stop=(kc == 7)), r=["aT", "wdt"], w=[PK[7]])
                k.op("dve", lambda e, tl=tl: e.tensor_tensor(out=dtr[:, tl, :], in0=P[7][:, 0:8], in1=dtb[:], op=ALU.add),
                     r=[PK[7], "dtb"], w=["dtr"])
            if getattr(pb, 'stop', 99) == 5:
                k.barrier()
                return
            tls = [0, 1] if dirn == 0 else [1, 0]
            for tl in tls:
                sl = slice(tl * 128, (tl + 1) * 128)
                grow = g0 + tl * 128
                for c in range(4):
                    k.op("dve", lambda e, c=c: e.tensor_tensor_scan(out=b2[:, c, :], data0=C["ones_f"][:, 0:128], data1=la[:, c, sl],
                                                                    initial=0.0, op0=ALU.mult, op1=ALU.add),
                         r=["la", "ones_f"], w=["b2"])
                if dirn == 0:
                    bsrc, bkey, last = b2, "b2", 127
                else:
                    k.op("dve", lambda e: e.tensor_tensor(out=bb[:], in0=la[:, :, sl], in1=b2[:], op=ALU.subtract), r=["la", "b2"], w=["bb"])
                    for c in range(4):
                        k.op("dve", lambda e, c=c: e.tensor_scalar(out=bb[:, c, :], in0=bb[:, c, :], scalar1=b2[:, c, 127:128], scalar2=None,
                                                                   op0=ALU.add), r=["bb", "b2"], w=["bb"])
                    bsrc, bkey, last = bb, "bb", 0
                k.op("dve", lambda e: e.tensor_copy(out=sc[:, 0, :], in_=bsrc[:, :, 64]), r=[bkey], w=["sc"])
                k.op("dve", lambda e: e.tensor_copy(out=sc[:, 2, :], in_=bsrc[:, :, last]), r=[bkey], w=["sc"])
                k.op("dve", lambda e: e.tensor_tensor(out=sc[:, 1, :], in0=sc[:, 2, :], in1=sc[:, 0, :], op=ALU.subtract), r=["sc"], w=["sc"])
                k.op("dve", lambda e: e.tensor_scalar(out=nb[:], in0=sc[:, 0, :], scalar1=-1.0, scalar2=None, op0=ALU.mult), r=["sc"], w=["nb"])
                k.op("act", lambda e: e.activation(out=esc[:], in_=sc[:], func=AF.Exp), r=["sc"], w=["esc"])
                for c in range(4):
                    k.op("act", lambda e, c=c: e.activation(out=Eq[:, c, :], in_=bsrc[:, c, :], func=AF.Exp, bias=nb[:, c:c + 1], scale=1.0),
                         r=[bkey, "nb"], w=["Eq"])
                    k.op("act", lambda e, c=c: e.activation(out=Ek[:, c, :], in_=bsrc[:, c, :], func=AF.Exp, bias=sc[:, 0, c:c + 1], scale=-1.0),
                         r=[bkey, "sc"], w=["Ek"])
                k.op("dve", lambda e: e.tensor_tensor(out=Qt[:], in0=Qsil[:, :, sl], in1=Eq[:], op=ALU.mult), r=["Qsil", "Eq"], w=["Qt"])
                k.op("dve", lambda e: e.tensor_tensor(out=Kt[:], in0=kk[:, :, sl], in1=Ek[:], op=ALU.mult), r=["kk", "Ek"], w=["Kt"])
                k.op("pool", lambda e: e.tensor_copy(out=KtA[0:64, :, :], in_=Kt[0:64, :, :]), r=["Kt"], w=["KtA"])
                k.op("pool", lambda e: e.tensor_copy(out=KtB[64:128, :, :], in_=Kt[64:128, :, :]), r=["Kt"], w=["KtB"])
                k.op("dve", lambda e: e.tensor_tensor(out=ShpA[0:64, :, :], in0=Sh[0:64, :, :],
                                                      in1=esc[0:64, 0, :].unsqueeze(2).to_broadcast([64, 4, 64]), op=ALU.mult),
                     r=["Sh", "esc"], w=["ShpA"])
                k.op("dve", lambda e: e.tensor_tensor(out=ShpB[64:128, :, :], in0=Sh[64:128, :, :],
                                                      in1=esc[64:128, 0, :].unsqueeze(2).to_broadcast([64, 4, 64]), op=ALU.mult),
                     r=["Sh", "esc"], w=["ShpB"])
                if getattr(pb, 'stop', 99) == 51:
                    k.barrier()
                    return
                for c in range(4):
                    k.op("pe", lambda e, c=c: e.transpose(out=P0b[:, c * 128:(c + 1) * 128], in_=Kt[:, c, :], identity=C["ident_b"][:]),
                         r=["Kt", "ident_b"], w=[PK[0]])
                k.op("act", lambda e: e.activation(out=Ktok[:].rearrange("p a t -> p (a t)"), in_=P0b[:, 0:512], func=AF.Copy),
                     r=[PK[0]], w=["Ktok"])
                if getattr(pb, 'stop', 99) == 52:
                    k.barrier()
                    return
                for h in range(8):
                    c, base = h // 2, (h % 2) * 64
                    bk = 1 + h // 4
                    Kz = KtA if h % 2 == 0 else KtB
                    k.op("pe", lambda e, h=h, c=c, bk=bk, Kz=Kz: e.matmul(
                        P[bk][:, (h % 4) * 128:(h % 4 + 1) * 128], Kz[:, c, :], Qt[:, c, :], start=True, stop=True),
                        r=["KtA", "KtB", "Qt"], w=[PK[bk]])
                for hh in range(2):
                    k.op("dve", lambda e, hh=hh: e.tensor_tensor(
                        out=attm[:, hh * 4:(hh + 1) * 4, :], in0=P[1 + hh][:].rearrange("p (a t) -> p a t", a=4),
                        in1=m_att_f[:].unsqueeze(1).to_broadcast([128, 4, 128]), op=ALU.mult), r=[PK[1 + hh], "masks"], w=["attm"])
                if getattr(pb, 'stop', 99) == 53:
                    k.barrier()
                    return
                for h in range(8):
                    c, base = h // 2, (h % 2) * 64
                    k.op("pe", lambda e, h=h: e.matmul(P[3][:, h * 64:(h + 1) * 64], attm[:, h, :], vtok[:, tl, h * 64:(h + 1) * 64],
                                                       start=True, stop=False), r=["attm", "vtok"], w=[PK[3]])
                    Sz = ShpA if h % 2 == 0 else ShpB
                    k.op("pe", lambda e, h=h, c=c, Sz=Sz: e.matmul(P[3][:, h * 64:(h + 1) * 64], Qt[:, c, :],
                                                                   Sz[:, c, :], start=False, stop=True),
                         r=["Qt", "ShpA", "ShpB"], w=[PK[3]])
                k.op("act", lambda e: e.activation(out=osb[:], in_=P[3][:], func=AF.Copy), r=[PK[3]], w=["osb"])
                k.dma("sp", lambda e, grow=grow: e.dma_start(out=D["oh"][dirn, grow:grow + 128, :], in_=osb[:]), r=["osb"], w=["oh"])
                if getattr(pb, 'stop', 99) == 54:
                    k.barrier()
                    return
                for c in range(4):
                    k.op("pe", lambda e, c=c: e.matmul(P[4][:, c * 128:(c + 1) * 128], Ktok[:, c, :], vtok[:, tl, c * 128:(c + 1) * 128],
                                                       start=True, stop=True), r=["Ktok", "vtok"], w=[PK[4]])
                p4v = P[4][:].rearrange("p (c x) -> p c x", c=4)
                k.op("dve", lambda e: e.tensor_tensor(out=tU[0:64, :, :], in0=p4v[0:64, :, 0:64],
                                                      in1=esc[0:64, 1, :].unsqueeze(2).to_broadcast([64, 4, 64]), op=ALU.mult),
                     r=[PK[4], "esc"], w=["tU"])
                k.op("dve", lambda e: e.tensor_tensor(out=tU[64:128, :, :], in0=p4v[64:128, :, 64:128],
                                                      in1=esc[64:128, 1, :].unsqueeze(2).to_broadcast([64, 4, 64]), op=ALU.mult),
                     r=[PK[4], "esc"], w=["tU"])
                k.op("dve", lambda e: e.tensor_tensor(out=Sh[:], in0=Sh[:], in1=esc[:, 2, :].unsqueeze(2).to_broadcast([128, 4, 64]), op=ALU.mult),
                     r=["Sh", "esc"], w=["Sh"])
                k.op("dve", lambda e: e.tensor_tensor(out=Sh[:], in0=Sh[:], in1=tU[:], op=ALU.add), r=["Sh", "tU"], w=["Sh"])
                if getattr(pb, 'stop', 99) == 6:
                    k.barrier()
                    return
                for c in range(4):
                    k.op("pe", lambda e, c=c: e.transpose(out=P0b[:, c * 128:(c + 1) * 128], in_=xbc[:, c, sl], identity=C["ident_b"][:]),
                         r=["xbc", "ident_b"], w=[PK[0]])
                for g in range(2):
                    k.op("pe", lambda e, g=g: e.transpose(out=P0b[:, 512 + g * 128:512 + (g + 1) * 128], in_=xbc[:, 4 + g, sl],
                                                          identity=C["ident_b"][:]), r=["xbc", "ident_b"], w=[PK[0]])
                k.op("act", lambda e: e.activation(out=xmt[:].rearrange("p a v -> p (a v)"), in_=P0b[:, 0:512], func=AF.Copy),
                     r=[PK[0]], w=["xmt"])
                k.op("act", lambda e: e.activation(out=Btok[:].rearrange("p a v -> p (a v)"), in_=P0b[:, 512:768], func=AF.Copy),
                     r=[PK[0]], w=["Btok"])
                k.op("act", lambda e: e.activation(out=dts[:, 3, :], in_=dtr[:, tl, :], func=AF.Exp), r=["dtr"], w=["dts3"])
                k.op("act", lambda e: e.activation(out=dts[:, 0, :], in_=dts[:, 3, :], func=AF.Ln, bias=1.0, scale=1.0), r=["dts3"], w=["dts0"])
                k.op("dve", lambda e: e.tensor_tensor(out=dts[:, 1, :], in0=dts[:, 0, :], in1=negA[:], op=ALU.mult), r=["dts0", "negA"], w=["dts1"])
                if getattr(pb, 'stop', 99) == 7:
                    k.barrier()
                    return
                k.op("pe", lambda e: e.matmul(P[7][:, 0:8], m_cum[:], dts[:, 1, :], start=True, stop=True), r=["dts1", "masks"], w=[PK[7]])
                k.op("pe", lambda e: e.matmul(P[7][:, 8:16], m_wend[:], dts[:, 1, :], start=True, stop=True), r=["dts1", "masks"], w=[PK[7]])
                k.op("pe", lambda e: e.matmul(P[7][:, 16:24], C["ones_f"][:], dts[:, 1, :], start=True, stop=True), r=["dts1", "ones_f"], w=[PK[7]])
                k.op("act", lambda e: e.activation(out=ec[:], in_=P[7][:, 0:24], func=AF.Exp), r=[PK[7]], w=["ec"])
                k.op("dve", lambda e: e.tensor_tensor(out=LH[:], in0=m_lh[:].unsqueeze(1).to_broadcast([128, 8, 128]),
                                                      in1=dts[:, 1, :].unsqueeze(2).to_broadcast([128, 8, 128]), op=ALU.mult),
                     r=["dts1", "masks"], w=["LH"])
                for h in range(8):
                    bk = 1 + h // 4
                    k.op("pe", lambda e, h=h, bk=bk: e.matmul(P[bk][:, (h % 4) * 128:(h % 4 + 1) * 128], LH[:, h, :], m_rhs[:],
                                                              start=True, stop=True), r=["LH", "masks"], w=[PK[bk]])
                for hh in range(2):
                    k.op("act", lambda e, hh=hh: e.activation(out=Em[:, hh * 4:(hh + 1) * 4, :].rearrange("p a t -> p (a t)"), in_=P[1 + hh][:],
                                                              func=AF.Exp), r=[PK[1 + hh]], w=["Em"])
                if getattr(pb, 'stop', 99) == 8:
                    k.barrier()
                    return
                for g in range(2):
                    k.op("pe", lambda e, g=g: e.matmul(P[3][:, g * 128:(g + 1) * 128], xbc[:, 4 + g, sl], xbc[:, 6 + g, sl], start=True, stop=True),
                         r=["xbc"], w=[PK[3]])
                k.op("dve", lambda e: e.tensor_tensor(out=cbm[:], in0=P[3][:, 0:256].rearrange("p (a t) -> p a t", a=2),
                                                      in1=m_att_f[:].unsqueeze(1).to_broadcast([128, 2, 128]), op=ALU.mult),
                     r=[PK[3], "masks"], w=["cbm"])
                for g in range(2):
                    k.op("dve", lambda e, g=g: e.tensor_tensor(out=Mm[:, g * 4:(g + 1) * 4, :], in0=Em[:, g * 4:(g + 1) * 4, :],
                                                               in1=cbm[:, g, :].unsqueeze(1).to_broadcast([128, 4, 128]), op=ALU.mult),
                         r=["Em", "cbm"], w=["Mm"])
                k.op("dve", lambda e: e.tensor_tensor(out=dts[:, 2, :], in0=dts[:, 0, :], in1=ec[:, 8:16], op=ALU.mult), r=["dts0", "ec"], w=["dts2"])
                k.op("pool", lambda e: e.tensor_tensor(out=xdt[:], in0=xmt[:], in1=dts[:, 0, :].unsqueeze(2).to_broadcast([128, 8, 64]), op=ALU.mult),
                     r=["xmt", "dts0"], w=["xdt"])
                k.op("pool", lambda e: e.tensor_tensor(out=xdtw[:], in0=xmt[:], in1=dts[:, 2, :].unsqueeze(2).to_broadcast([128, 8, 64]), op=ALU.mult),
                     r=["xmt", "dts2"], w=["xdtw"])
                if getattr(pb, 'stop', 99) == 9:
                    k.barrier()
                    return
                for h in range(8):
                    k.op("pe", lambda e, h=h: e.matmul(P[4][:, h * 64:(h + 1) * 64], Mm[:, h, :], xdt[:, h, :], start=True, stop=True),
                         r=["Mm", "xdt"], w=[PK[4]])
                for g in range(2):
                    k.op("pe", lambda e, g=g: e.matmul(P[5][:, g * 256:(g + 1) * 256], xbc[:, 6 + g, sl],
                                                       Ssb[:, g * 4:(g + 1) * 4, :].rearrange("p a v -> p (a v)"), start=True, stop=True),
                         r=["xbc", "Ssb"], w=[PK[5]])
                k.op("dve", lambda e: e.tensor_tensor(out=yi[:], in0=P[5][:].rearrange("p (a v) -> p a v", a=8),
                                                      in1=ec[:, 0:8].unsqueeze(2).to_broadcast([128, 8, 64]), op=ALU.mult),
                     r=[PK[5], "ec"], w=["yi"])
                k.op("dve", lambda e: e.tensor_tensor(out=ysb[:], in0=P[4][:].rearrange("p (a v) -> p a v", a=8), in1=yi[:], op=ALU.add),
                     r=[PK[4], "yi"], w=["ysb"])
                if dirn == 0:
                    k.op("pool", lambda e: e.tensor_tensor(out=yi[:], in0=xmt[:], in1=dsk[:].unsqueeze(2).to_broadcast([128, 8, 64]), op=ALU.mult),
                         r=["xmt", "dsk", "ysb"], w=["yi"])
                    k.op("dve", lambda e: e.tensor_tensor(out=ysb[:], in0=ysb[:], in1=yi[:], op=ALU.add), r=["ysb", "yi"], w=["ysb"])
                k.dma("sp", lambda e, grow=grow: e.dma_start(out=D["ys"][dirn, grow:grow + 128, :], in_=ysb[:].rearrange("p a v -> p (a v)")),
                      r=["ysb"], w=["ys"])
                for g in range(2):
                    k.op("pe", lambda e, g=g: e.matmul(P[6][:, g * 256:(g + 1) * 256], Btok[:, g, :],
                                                       xdtw[:, g * 4:(g + 1) * 4, :].rearrange("p a v -> p (a v)"), start=True, stop=True),
                         r=["Btok", "xdtw"], w=[PK[6]])
                k.op("dve", lambda e: e.tensor_tensor(out=Ss[:], in0=Ss[:], in1=ec[:, 16:24].unsqueeze(2).to_broadcast([128, 8, 64]), op=ALU.mult),
                     r=["Ss", "ec"], w=["Ss"])
                k.op("dve", lambda e: e.tensor_tensor(out=Ss[:], in0=Ss[:], in1=P[6][:].rearrange("p (a v) -> p a v", a=8), op=ALU.add),
                     r=["Ss", PK[6]], w=["Ss"])
                k.op("act", lambda e: e.activation(out=Ssb[:], in_=Ss[:], func=AF.Copy), r=["Ss"], w=["Ssb"])
        k.barrier()


def phase_mix_merge(pb, C, ntiles=66):
    k, D, nc = pb.k, pb.D, pb.nc
    with ExitStack() as es:
        P = [pb.ps(es, "P%d" % i, [128, 512], F32) for i in range(8)]
        PK = ["P%d" % i for i in range(8)]
        P0b = P[0][:].bitcast(BF16)
        mv = load_modv(pb, es, 0, (0, 1, 2))
        stg = pb.sb(es, "wstg", [128, 512], F32)
        wg = load_w_bf16(pb, es, "wg", D["w_in"][:, 2048:2560], 8, 512, stg, "wstg")
        wz = load_w_bf16(pb, es, "wz", D["w_in"][:, 2560:3072], 8, 512, stg, "wstg")
        wo = load_w_bf16(pb, es, "wo", D["w_out_rec"], 8, 1024, stg, "wstg")
        hg = pb.sb(es, "hg", [128, 512], F32)
        mn = pb.sb(es, "mn", [128, 512], F32)
        k.dma("sp", lambda e: e.dma_start(out=hg[:], in_=D["hgrn_norm"].to_broadcast([128, 512])), w=["gains"])
        k.dma("sp", lambda e: e.dma_start(out=mn[:], in_=D["mamba_norm"].to_broadcast([128, 512])), w=["gains"])
        xt = [pb.sb(es, "xt%d" % i, [128, 1024], F32) for i in range(2)]
        an = [pb.sb(es, "an%d" % i, [128, 1024], BF16) for i in range(2)]
        junk = pb.sb(es, "junk", [128, 1024], BF16)
        t1 = pb.sb(es, "t1", [128, 1024], F32)
        rstd = [pb.sb(es, "rstd%d" % i, [128, 4], F32) for i in range(2)]
        aT = pb.sb(es, "aT", [128, 8, 128], BF16)
        sg = pb.sb(es, "sg", [128, 512], F32)
        sz = pb.sb(es, "sz", [128, 512], F32)
        o0 = [pb.sb(es, "o0%d" % i, [128, 512], F32) for i in range(2)]
        o1 = [pb.sb(es, "o1%d" % i, [128, 512], F32) for i in range(2)]
        y0 = [pb.sb(es, "y0%d" % i, [128, 512], F32) for i in range(2)]
        y1 = [pb.sb(es, "y1%d" % i, [128, 512], F32) for i in range(2)]
        sq = pb.sb(es, "sq", [128, 512], F32)
        st8 = pb.sb(es, "st8", [128, 3, 8], F32)
        rs2 = pb.sb(es, "rs2", [128, 4], F32)
        cat = pb.sb(es, "cat", [128, 1024], BF16)
        catT = pb.sb(es, "catT", [128, 8, 128], BF16)
        xo = [pb.sb(es, "xo%d" % i, [128, 1024], F32) for i in range(2)]
        for j in range(ntiles):
            b = j % 2
            if j < 2:
                s, src, rr, grow = 1, D["ctx"], j * 128, T_LAT + j * 128
            else:
                s, src, rr, grow = 0, D["x"], (j - 2) * 128, (j - 2) * 128
            k.dma("sp", lambda e: e.dma_start(out=xt[b][:], in_=src[rr:rr + 128, :]), w=["xt%d" % b])
            k.dma("sp", lambda e: e.dma_start(out=o0[b][:], in_=D["oh"][0, grow:grow + 128, :]), r=["oh"], w=["o0%d" % b])
            k.dma("sp", lambda e: e.dma_start(out=o1[b][:], in_=D["oh"][1, grow:grow + 128, :]), r=["oh"], w=["o1%d" % b])
            k.dma("sp", lambda e: e.dma_start(out=y0[b][:], in_=D["ys"][0, grow:grow + 128, :]), r=["ys"], w=["y0%d" % b])
            k.dma("sp", lambda e: e.dma_start(out=y1[b][:], in_=D["ys"][1, grow:grow + 128, :]), r=["ys"], w=["y1%d" % b])
            norm_mod_tile(pb, xt[b], "xt%d" % b, rstd[b], "rstd%d" % b, junk, t1, mv[(s, 1)], mv[(s, 0)], an[b], "an%d" % b)
            for kc in range(8):
                k.op("pe", lambda e, kc=kc: e.transpose(out=P0b[:, kc * 128:(kc + 1) * 128], in_=an[b][:, kc * 128:(kc + 1) * 128],
                                                        identity=C["ident_b"][:]), r=["an%d" % b, "ident_b"], w=[PK[0]])
            k.op("act", lambda e: e.activation(out=aT[:].rearrange("p a t -> p (a t)"), in_=P0b, func=AF.Copy), r=[PK[0]], w=["aT"])
            for kc in range(8):
                k.op("pe", lambda e, kc=kc: e.matmul(P[1][:], aT[:, kc, :], wg[:, kc, :], start=(kc == 0), stop=(kc == 7)),
                     r=["aT", "wg"], w=[PK[1]])
            k.op("act", lambda e: e.activation(out=sg[:], in_=P[1][:], func=AF.Sigmoid), r=[PK[1]], w=["sg"])
            for kc in range(8):
                k.op("pe", lambda e, kc=kc: e.matmul(P[2][:], aT[:, kc, :], wz[:, kc, :], start=(kc == 0), stop=(kc == 7)),
                     r=["aT", "wz"], w=[PK[2]])
            k.op("act", lambda e: e.activation(out=sz[:], in_=P[2][:], func=AF.Silu), r=[PK[2]], w=["sz"])
            ok0, ok1, yk0, yk1 = "o0%d" % b, "o1%d" % b, "y0%d" % b, "y1%d" % b
            k.op("dve", lambda e: e.tensor_tensor(out=o0[b][:], in0=o0[b][:], in1=o1[b][:], op=ALU.add), r=[ok0, ok1], w=[ok0])
            k.op("pool", lambda e: e.tensor_tensor(out=sq[:], in0=o0[b][:], in1=o0[b][:], op=ALU.mult), r=[ok0], w=["sq"])
            k.op("dve", lambda e: e.tensor_reduce(out=st8[:, 0, :], in_=sq[:].rearrange("p (a v) -> p a v", a=8), axis=AX.X, op=ALU.add),
                 r=["sq"], w=["st8"])
            k.op("act", lambda e: e.activation(out=st8[:, 1, :], in_=st8[:, 0, :], func=AF.Sqrt, bias=EPS, scale=1.0 / 64), r=["st8"], w=["st8"])
            k.op("dve", lambda e: e.reciprocal(out=st8[:, 2, :], in_=st8[:, 1, :]), r=["st8"], w=["st8"])
            k.op("dve", lambda e: e.tensor_tensor(out=o0[b][:].rearrange("p (a v) -> p a v", a=8), in0=o0[b][:].rearrange("p (a v) -> p a v", a=8),
                                                  in1=st8[:, 2, :].unsqueeze(2).to_broadcast([128, 8, 64]), op=ALU.mult), r=[ok0, "st8"], w=[ok0])
            k.op("pool", lambda e: e.tensor_tensor(out=o0[b][:], in0=o0[b][:], in1=hg[:], op=ALU.mult), r=[ok0, "gains"], w=[ok0])
            k.op("dve", lambda e: e.tensor_tensor(out=cat[:, 0:512], in0=o0[b][:], in1=sg[:], op=ALU.mult), r=[ok0, "sg"], w=["cat"])
            k.op("pool", lambda e: e.tensor_tensor(out=y0[b][:], in0=y0[b][:], in1=y1[b][:], op=ALU.add), r=[yk0, yk1], w=[yk0])
            k.op("dve", lambda e: e.tensor_tensor(out=y0[b][:], in0=y0[b][:], in1=sz[:], op=ALU.mult), r=[yk0, "sz"], w=[yk0])
            k.op("act", lambda e: e.activation(out=junk[:, 0:512], in_=y0[b][:], func=AF.Square, accum_out=rs2[:, 0:1]), r=[yk0], w=["junk", "rs2"])
            k.op("act", lambda e: e.activation(out=rs2[:, 1:2], in_=rs2[:, 0:1], func=AF.Sqrt, bias=EPS, scale=1.0 / 512), r=["rs2"], w=["rs2"])
            k.op("dve", lambda e: e.reciprocal(out=rs2[:, 2:3], in_=rs2[:, 1:2]), r=["rs2"], w=["rs2"])
            k.op("dve", lambda e: e.scalar_tensor_tensor(out=cat[:, 512:1024], in0=y0[b][:], scalar=rs2[:, 2:3], in1=mn[:],
                                                         op0=ALU.mult, op1=ALU.mult), r=[yk0, "rs2", "gains"], w=["cat"])
            for c in range(8):
                k.op("pe", lambda e, c=c: e.transpose(out=P0b[:, c * 128:(c + 1) * 128], in_=cat[:, c * 128:(c + 1) * 128],
                                                      identity=C["ident_b"][:]), r=["cat", "ident_b"], w=[PK[0]])
            k.op("act", lambda e: e.activation(out=catT[:].rearrange("p a t -> p (a t)"), in_=P0b, func=AF.Copy), r=[PK[0]], w=["catT"])
            for dh in range(2):
                for c in range(8):
                    k.op("pe", lambda e, c=c, dh=dh: e.matmul(P[3 + dh][:], catT[:, c, :], wo[:, c, dh * 512:(dh + 1) * 512],
                                                              start=(c == 0), stop=(c == 7)), r=["catT", "wo"], w=[PK[3 + dh]])
                k.op("dve", lambda e, dh=dh: e.tensor_tensor(out=xo[b][:, dh * 512:(dh + 1) * 512], in0=P[3 + dh][:],
                                                             in1=mv[(s, 2)][:, dh * 512:(dh + 1) * 512], op=ALU.mult),
                     r=[PK[3 + dh], "modv"], w=["xo%d" % b])
            k.op("pool", lambda e: e.tensor_tensor(out=xo[b][:], in0=xo[b][:], in1=xt[b][:], op=ALU.add), r=["xo%d" % b, "xt%d" % b], w=["xo%d" % b])
            k.dma("sp", lambda e: e.dma_start(out=D["xres"][grow:grow + 128, :], in_=xo[b][:]), r=["xo%d" % b], w=["xres"])
        k.barrier()


def declare_all(pb):
    pb.din("x", [T_LAT, DM]); pb.din("ctx", [T_CTX, DM]); pb.din("c", [1, DM]); pb.din("c_ctx", [1, DM])
    pb.din("w_mod", [2, DM, 6 * DM]); pb.din("b_mod", [2, 6 * DM]); pb.din("norm_mix", [2, DM]); pb.din("norm_ffn", [2, DM])
    pb.din("norm_out", [1, DM])
    pb.din("w_in", [DM, 4112]); pb.din("w_out_rec", [DM, DM]); pb.din("conv_w", [5, DM]); pb.din("conv_b", [1, DM])
    pb.din("lb_gamma", [2, 2, 512]); pb.din("dt_bias", [2, 8]); pb.din("a_log", [2, 8]); pb.din("d_skip", [1, 8])
    pb.din("hgrn_norm", [1, 512]); pb.din("mamba_norm", [1, 512])
    pb.din("w_dq", [DM, 384]); pb.din("q_norm", [1, 384]); pb.din("w_uq_r", [384, 2048]); pb.din("w_dkv", [DM, 256])
    pb.din("kv_norm", [1, 256]); pb.din("w_ukv_r", [256, 2048]); pb.din("w_kr_r", [DM, 128]); pb.din("w_o", [DM, DM])
    pb.din("w_router", [2, DM, NE]); pb.din("w_gate", [2, NE, DM, DM]); pb.din("w_up", [2, NE, DM, DM]); pb.din("w_down", [2, NE, DM, DM])
    for nm, v in host_consts().items():
        pb.din("c_" + nm, v.shape)
    for nm, v in attn_host_consts().items():
        pb.din("c_" + nm, v.shape)
    pb.dscr("xres", [NROWS, DM]); pb.dscr("hn", [NROWS, DM], BF16); pb.dscr("aff", [NROWS, 16]); pb.dscr("modrep", [2, 2, 6, 128, DM])
    pb.dscr("oh", [2, NTOK, 512]); pb.dscr("ys", [2, NTOK, 512])
    pb.dscr("KT", [NH, 128, NTOK], BF16); pb.dscr("KRT", [64, NTOK], BF16); pb.dscr("Vd", [NTOK, 1024], BF16)
    pb.dscr("QT", [NH, 128, T_LAT], BF16); pb.dscr("QRT", [NH, 64, T_LAT], BF16); pb.dscr("Od", [T_LAT, 1024], BF16)


def make_inputs(inp, b):
    f = lambda a: np.ascontiguousarray(a, dtype=np.float32)
    w_uq_r, w_ukv_r, w_kr_r = attn_layout_weights(inp["w_uq"][0], inp["w_ukv"][0], inp["w_kr"][0])
    im = {"x": f(inp["x"][b]), "ctx": f(inp["ctx"][b]), "c": f(inp["c"][b:b + 1]), "c_ctx": f(inp["c_ctx"][None, :]),
          "w_mod": f(inp["w_mod"]), "b_mod": f(inp["b_mod"]), "norm_mix": f(inp["norm_mix"]), "norm_ffn": f(inp["norm_ffn"]),
          "norm_out": f(inp["norm_out"][None, :]),
          "w_in": f(inp["w_in"][0]), "w_out_rec": f(inp["w_out_rec"][0]), "conv_w": f(inp["conv_w"][0]), "conv_b": f(inp["conv_b"]),
          "lb_gamma": f(inp["lb_gamma"]), "dt_bias": f(inp["dt_bias"][0]), "a_log": f(inp["a_log"][0]), "d_skip": f(inp["d_skip"]),
          "hgrn_norm": f(inp["hgrn_norm"]), "mamba_norm": f(inp["mamba_norm"]),
          "w_dq": f(inp["w_dq"][0]), "q_norm": f(inp["q_norm"]), "w_uq_r": f(w_uq_r), "w_dkv": f(inp["w_dkv"][0]),
          "kv_norm": f(inp["kv_norm"]), "w_ukv_r": f(w_ukv_r), "w_kr_r": f(w_kr_r), "w_o": f(inp["w_o"][0]),
          "w_router": f(inp["w_router"]), "w_gate": f(inp["w_gate"]), "w_up": f(inp["w_up"]), "w_down": f(inp["w_down"])}
    for nm, v in host_consts().items():
        im["c_" + nm] = v
    for nm, v in attn_host_consts().items():
        im["c_" + nm] = v
    return im


def build_program(phases, dbg=False, opts=None):
    opts = opts or {}
    nc = bass.Bass("TRN2", target_bir_lowering=False)
    pb = PB(nc)
    declare_all(pb)
    pb.dout("out", [T_LAT, DM])
    if dbg:
        pb.dout("dbg", [NTOK, DM])
    with ExitStack() as es:
        C = load_consts(pb, es)
        phase_init(pb, copy_x=("init" in phases))
        phase_mod(pb, C)
        if "mixf" in phases:
            phase_mix_pass(pb, C, 0, **opts.get("mix", {}))
        if "mixb" in phases:
            phase_mix_pass(pb, C, 1, **opts.get("mix", {}))
        if "mixm" in phases:
            phase_mix_merge(pb, C, **opts.get("merge", {}))
        if "moe0" in phases:
            phase_moe(pb, C, 0)
        if "attn_pre" in phases:
            phase_attn_pre(pb, C)
        if "attn_main" in phases:
            phase_attn_main(pb, C, **opts.get("attn", {}))
        if "attn_post" in phases:
            phase_attn_post(pb, C)
        if "moe1" in phases:
            phase_moe(pb, C, 1)
        if "final" in phases:
            phase_final(pb, C)
        k = pb.k
        if dbg:
            for i in range(8):
                k.dma("sp", lambda e, i=i: e.dma_start(out=pb.D["dbg"][i * 1056:(i + 1) * 1056, :], in_=pb.D["xres"][i * 1056:(i + 1) * 1056, :]),
                      r=["xres"], w=["dbg"])
        k.finish()
    return nc, pb


ALL_PHASES = ["mixf", "mixb", "mixm", "moe0", "attn_pre", "attn_main", "attn_post", "moe1", "final"]


def kernel(**inputs):
    from concourse.bass_utils import run_bass_kernel_spmd
    inp = {k: np.asarray(v) for k, v in inputs.items()}
    nb = inp["x"].shape[0]
    nc, pb = build_program(ALL_PHASES)
    in_maps = [make_inputs(inp, b) for b in range(nb)]
    res = run_bass_kernel_spmd(nc, in_maps, core_ids=list(range(nb)))
    out = np.stack([np.asarray(res.results[b]["out"]) for b in range(nb)], axis=0)
    return out.astype(np.float32)
```
